# Optimizing a Trainium2 kernel written in Bass

```python
import jax, jax.numpy as jnp
from jax import lax
import numpy as np

D_MODEL = 1024
BATCH = 16
SEQ = 2048
DEPTH = 1

GRID_W = 64
CTX_LEN = 256
N_HEADS_M = 4
HEAD_DIM_M = 128
W_M = N_HEADS_M * HEAD_DIM_M
N_HEADS_H = 4
HEAD_DIM_H = 128
W_H = N_HEADS_H * HEAD_DIM_H
CONV_K = 3
CHUNK_M = 64
CHUNK_H = 32
N_EXPERTS = 32
TOP_K = 4
D_EXPERT = D_MODEL
SWIGLU_LIMIT = 7.0
SWIGLU_ALPHA = 1.702
MOE_BLOCK = 256
EPS = 1e-6
SPLIT_SIZES = (2 * W_M, W_M, W_M, W_H, W_H, W_H, 2 * W_H, D_MODEL, D_MODEL, 4 * N_HEADS_M)
SPLIT_POINTS = tuple(np.cumsum(SPLIT_SIZES)[:-1].tolist())
N_IN = int(sum(SPLIT_SIZES))

kernel_name = 'hybrid_mlstm_hgrn2_moe_dit'


def rmsnorm(x, w):
    xf = x.astype(jnp.float32)
    y = xf * lax.rsqrt(jnp.mean(xf * xf, axis=-1, keepdims=True) + EPS)
    return (y * w.astype(jnp.float32)).astype(x.dtype)


def head_norm(o, w, dtype):
    B, H, T, d = o.shape
    y = o * lax.rsqrt(jnp.mean(o * o, axis=-1, keepdims=True) + EPS) * w.astype(jnp.float32).reshape(H, 1, d)
    return y.transpose(0, 2, 1, 3).reshape(B, T, H * d).astype(dtype)


def ada_mod(cvec, w, b):
    return jnp.split(jax.nn.silu(cvec) @ w + b, 6, axis=-1)


def modulate(x, shift, scale):
    return x * (1 + scale[:, None, :]) + shift[:, None, :]


def split_heads(a, H):
    B, T, W = a.shape
    return a.reshape(B, T, H, W // H).transpose(0, 2, 1, 3)


def short_conv(a, w, rows, cols):
    B, T, C = a.shape
    y = lax.conv_general_dilated(a.reshape(B, rows, cols, C), w[:, :, None, :].astype(a.dtype), (1, 1), 'SAME',
                                 dimension_numbers=('NHWC', 'HWIO', 'NHWC'), feature_group_count=C)
    return y.reshape(B, T, C)


def to_chunks(a, L):
    B, H, T = a.shape[:3]
    return jnp.moveaxis(a.reshape(B, H, T // L, L, *a.shape[3:]), 2, 0)


def from_chunks(a):
    NC, B, H, L = a.shape[:4]
    return jnp.moveaxis(a, 0, 2).reshape(B, H, NC * L, *a.shape[4:])


def mlstm_scan(q, k, v, ig, lf, state0, with_output):
    L = CHUNK_M
    mask = jnp.tril(jnp.ones((L, L), bool))
    xs = tuple(to_chunks(a, L) for a in (q, k, v, ig, lf))

    def step(carry, inp):
        C, n, m = carry
        qc, kc, vc, ic, fc = inp
        b = jnp.cumsum(fc, axis=-1)
        bL = b[..., -1]
        logw = bL[..., None] - b + ic
        m_new = jnp.maximum(bL + m, jnp.max(logw, axis=-1))
        w = jnp.exp(logw - m_new[..., None])
        decay = jnp.exp(bL + m - m_new)
        C_new = decay[..., None, None] * C + jnp.einsum('bhs,bhsv,bhsd->bhvd', w, vc, kc)
        n_new = decay[..., None] * n + jnp.einsum('bhs,bhsd->bhd', w, kc)
        if not with_output:
            return (C_new, n_new, m_new), None
        logD = jnp.where(mask, b[..., :, None] - b[..., None, :] + ic[..., None, :], -jnp.inf)
        m_inter = b + m[..., None]
        m_t = jnp.maximum(jnp.max(logD, axis=-1), m_inter)
        s = jnp.einsum('bhtd,bhsd->bhts', qc, kc) * jnp.exp(logD - m_t[..., None])
        inter = jnp.exp(m_inter - m_t)
        num = jnp.einsum('bhts,bhsv->bhtv', s, vc) + inter[..., None] * jnp.einsum('bhtd,bhvd->bhtv', qc, C)
        den = jnp.sum(s, axis=-1) + inter * jnp.einsum('bhtd,bhd->bht', qc, n)
        h = num / jnp.maximum(jnp.abs(den), jnp.exp(-m_t))[..., None]
        return (C_new, n_new, m_new), h

    state, h = lax.scan(step, state0, xs)
    return (from_chunks(h) if with_output else None), state


def hgrn_scan(q, k, i, lf, S0, with_output):
    L = CHUNK_H
    mask = jnp.tril(jnp.ones((L, L), bool))
    xs = tuple(to_chunks(a, L) for a in (q, k, i, lf))

    def step(S, inp):
        qc, kc, ic, fc = inp
        A = jnp.cumsum(fc, axis=2)
        AL = A[:, :, -1]
        S_new = jnp.exp(AL)[..., None] * S + jnp.einsum('bhsd,bhsv->bhdv', kc * jnp.exp(AL[:, :, None] - A), ic)
        if not with_output:
            return S_new, None
        diff = jnp.where(mask[:, :, None], A[:, :, :, None, :] - A[:, :, None, :, :], -jnp.inf)
        s = jnp.einsum('bhtsd,bhsd->bhts', qc[:, :, :, None, :] * jnp.exp(diff), kc)
        o = jnp.einsum('bhts,bhsv->bhtv', s, ic) + jnp.einsum('bhtd,bhdv->bhtv', qc * jnp.exp(A), S)
        return S_new, o

    S, o = lax.scan(step, S0, xs)
    return (from_chunks(o) if with_output else None), S


def flip_t(a, d):
    return jnp.flip(a, axis=2) if d else a


def bidir(scan_fn, lat_dirs, ctx_dirs, state0, ctx_out):
    outs_l, outs_c = [], []
    for d in range(2):
        hc, st = scan_fn(*[flip_t(a, d) for a in ctx_dirs[d]], state0, ctx_out)
        hl, _ = scan_fn(*[flip_t(a, d) for a in lat_dirs[d]], st, True)
        outs_l.append(flip_t(hl, d))
        if ctx_out:
            outs_c.append(flip_t(hc, d))
    return outs_l[0] + outs_l[1], (outs_c[0] + outs_c[1] if ctx_out else None)


def mixer_features(h, w_in, b_in, conv_w, lb, rows, cols):
    B, T, _ = h.shape
    f32 = jnp.float32
    p = h @ w_in + b_in
    mqk, mv, mo, hq, hi, hg, hf, ga, gb, mg = jnp.split(p, SPLIT_POINTS, axis=-1)
    mq, mk = jnp.split(jax.nn.silu(short_conv(mqk, conv_w, rows, cols)), 2, axis=-1)
    mq = split_heads(mq, N_HEADS_M).astype(f32) * HEAD_DIM_M ** -0.5
    mk = split_heads(mk, N_HEADS_M).astype(f32)
    mv = split_heads(mv, N_HEADS_M).astype(f32)
    mg = mg.astype(f32).reshape(B, T, 4, N_HEADS_M).transpose(2, 0, 3, 1)
    m_dirs = [(mq, mk, mv, mg[d], jax.nn.log_sigmoid(mg[2 + d])) for d in range(2)]
    hq = split_heads(jax.nn.silu(hq), N_HEADS_H).astype(f32)
    hi = split_heads(hi, N_HEADS_H).astype(f32)
    z = hf.astype(f32).reshape(B, T, 2, W_H)
    h_dirs = []
    for d in range(2):
        lb_d, zd = lb[d], z[:, :, d]
        logf = jnp.logaddexp(jnp.log(lb_d), jnp.log1p(-lb_d) + jax.nn.log_sigmoid(zd))
        kk = (1 - lb_d) * jax.nn.sigmoid(-zd)
        h_dirs.append((hq, split_heads(kk, N_HEADS_H), hi, split_heads(logf, N_HEADS_H)))
    return m_dirs, h_dirs, (mo, hg, ga, gb)


def merge_branches(mo_h, hg_h, gates, m_norm, h_norm, w_pa, w_pb, w_out, dtype):
    mo, hg, ga, gb = gates
    a = head_norm(mo_h, m_norm, dtype) * jax.nn.sigmoid(mo)
    b = head_norm(hg_h, h_norm, dtype) * jax.nn.silu(hg)
    y = jax.nn.sigmoid(ga) * (a @ w_pa) + jax.nn.sigmoid(gb) * (b @ w_pb)
    return y @ w_out


def token_mixer(hl, hc, w_in, b_in, conv_w, lb, m_norm, h_norm, w_pa, w_pb, w_out, ctx_out):
    B, S, _ = hl.shape
    rows = S // GRID_W
    lm, lh, lg = mixer_features(hl, w_in, b_in, conv_w, lb, rows, GRID_W)
    cm, ch, cg = mixer_features(hc, w_in, b_in, conv_w, lb, 1, hc.shape[1])
    f32 = jnp.float32
    m_state0 = (jnp.zeros((B, N_HEADS_M, HEAD_DIM_M, HEAD_DIM_M), f32),
                jnp.zeros((B, N_HEADS_M, HEAD_DIM_M), f32), jnp.zeros((B, N_HEADS_M), f32))
    h_state0 = jnp.zeros((B, N_HEADS_H, HEAD_DIM_H, HEAD_DIM_H), f32)
    ml, mc = bidir(mlstm_scan, lm, cm, m_state0, ctx_out)
    gl, gc = bidir(hgrn_scan, lh, ch, h_state0, ctx_out)
    yl = merge_branches(ml, gl, lg, m_norm, h_norm, w_pa, w_pb, w_out, hl.dtype)
    yc = merge_branches(mc, gc, cg, m_norm, h_norm, w_pa, w_pb, w_out, hc.dtype) if ctx_out else None
    return yl, yc


def moe_ffn(h, w_router, b_router, w_gu, b_gu, w_dn, b_dn):
    B, T, D = h.shape
    f32 = jnp.float32
    xf = h.reshape(B * T, D)
    N = B * T
    logits = xf.astype(f32) @ w_router.astype(f32) + b_router.astype(f32)
    top_v, top_e = lax.top_k(logits, TOP_K)
    wts = jax.nn.softmax(top_v, axis=-1)
    e_flat = top_e.reshape(-1)
    tok_flat = jnp.repeat(jnp.arange(N, dtype=jnp.int32), TOP_K)
    order = jnp.argsort(e_flat)
    se, stok, sw = e_flat[order], tok_flat[order], wts.reshape(-1)[order]
    counts = jnp.bincount(e_flat, length=N_EXPERTS)
    pcounts = (counts + MOE_BLOCK - 1) // MOE_BLOCK * MOE_BLOCK
    start = jnp.cumsum(counts) - counts
    pend = jnp.cumsum(pcounts)
    pstart = pend - pcounts
    dest = pstart[se] + jnp.arange(N * TOP_K) - start[se]
    P = -(-N * TOP_K // MOE_BLOCK) * MOE_BLOCK + N_EXPERTS * MOE_BLOCK
    n_blocks = P // MOE_BLOCK
    ptok = jnp.full((P,), N, jnp.int32).at[dest].set(stok)
    pw = jnp.zeros((P,), f32).at[dest].set(sw)
    block_e = jnp.minimum(jnp.searchsorted(pend, jnp.arange(n_blocks) * MOE_BLOCK, side='right'), N_EXPERTS - 1)
    xb = jnp.concatenate([xf, jnp.zeros((1, D), xf.dtype)])[ptok].reshape(n_blocks, MOE_BLOCK, D)

    def expert_block(args):
        xblk, e = args
        gate, up = jnp.split(xblk @ w_gu[e] + b_gu[e], 2, axis=-1)
        gate = jnp.minimum(gate, SWIGLU_LIMIT)
        up = jnp.clip(up, -SWIGLU_LIMIT, SWIGLU_LIMIT)
        act = (up + 1) * gate * jax.nn.sigmoid(SWIGLU_ALPHA * gate)
        return act @ w_dn[e] + b_dn[e]

    yb = lax.map(expert_block, (xb, block_e)).reshape(P, D)
    y = jax.ops.segment_sum(yb * pw[:, None].astype(yb.dtype), ptok, num_segments=N + 1)[:N]
    return y.reshape(B, T, D)


def setup_inputs(seed: int = 0) -> dict:
    key = jax.random.key(seed)
    ks = jax.random.split(key, 32)
    nrm = jax.random.normal
    D, F, E, H = D_MODEL, D_EXPERT, N_EXPERTS, N_HEADS_M
    i_bias = 0.1 * nrm(ks[10], (DEPTH, 2, H))
    f_bias = 3.0 + 3.0 * jax.random.uniform(ks[11], (DEPTH, 2, H))
    b_in = jnp.concatenate([0.02 * nrm(ks[12], (DEPTH, N_IN - 4 * H)),
                            jnp.concatenate([i_bias, f_bias], axis=1).reshape(DEPTH, 4 * H)], axis=-1)
    return {
        'x': nrm(ks[0], (BATCH, SEQ, D)),
        'c': nrm(ks[1], (BATCH, D)),
        'ctx': nrm(ks[2], (BATCH, CTX_LEN, D)),
        'c_ctx': nrm(ks[3], (D,)),
        'w_ada': 0.5 * D ** -0.5 * nrm(ks[4], (DEPTH, D, 6 * D)),
        'b_ada': 0.02 * nrm(ks[5], (DEPTH, 6 * D)),
        'norm_mix_pre': 1.0 + 0.05 * nrm(ks[6], (DEPTH, D)),
        'norm_mix_post': 1.0 + 0.05 * nrm(ks[7], (DEPTH, D)),
        'norm_ffn_pre': 1.0 + 0.05 * nrm(ks[8], (DEPTH, D)),
        'norm_ffn_post': 1.0 + 0.05 * nrm(ks[9], (DEPTH, D)),
        'w_in': D ** -0.5 * nrm(ks[13], (DEPTH, D, N_IN)),
        'b_in': b_in,
        'conv_w': (CONV_K * CONV_K) ** -0.5 * nrm(ks[14], (DEPTH, CONV_K, CONV_K, 2 * W_M)),
        'lb_raw': 0.1 * nrm(ks[15], (DEPTH + 1, 2, W_H)),
        'm_norm': 1.0 + 0.05 * nrm(ks[16], (DEPTH, W_M)),
        'h_norm': 1.0 + 0.05 * nrm(ks[17], (DEPTH, W_H)),
        'w_pa': W_M ** -0.5 * nrm(ks[18], (DEPTH, W_M, D)),
        'w_pb': W_H ** -0.5 * nrm(ks[19], (DEPTH, W_H, D)),
        'w_out': D ** -0.5 * nrm(ks[20], (DEPTH, D, D)),
        'w_router': D ** -0.5 * nrm(ks[21], (DEPTH, D, E)),
        'b_router': 0.01 * nrm(ks[22], (DEPTH, E)),
        'w_gu': D ** -0.5 * nrm(ks[23], (DEPTH, E, D, 2 * F)),
        'b_gu': 0.02 * nrm(ks[24], (DEPTH, E, 2 * F)),
        'w_dn': F ** -0.5 * nrm(ks[25], (DEPTH, E, F, D)),
        'b_dn': 0.02 * nrm(ks[26], (DEPTH, E, D)),
    }


def reference(x, c, ctx, c_ctx, w_ada, b_ada, norm_mix_pre, norm_mix_post, norm_ffn_pre, norm_ffn_post,
              w_in, b_in, conv_w, lb_raw, m_norm, h_norm, w_pa, w_pb, w_out,
              w_router, b_router, w_gu, b_gu, w_dn, b_dn):
    lb_all = jnp.cumsum(jax.nn.softmax(lb_raw.astype(jnp.float32), axis=0), axis=0)
    for l in range(DEPTH):
        last = l == DEPTH - 1
        sh1, sc1, g1, sh2, sc2, g2 = ada_mod(c, w_ada[l], b_ada[l])
        csh1, csc1, cg1, csh2, csc2, cg2 = ada_mod(c_ctx[None, :], w_ada[l], b_ada[l])
        hl = modulate(rmsnorm(x, norm_mix_pre[l]), sh1, sc1)
        hc = modulate(rmsnorm(ctx, norm_mix_pre[l]), csh1, csc1)
        yl, yc = token_mixer(hl, hc, w_in[l], b_in[l], conv_w[l], lb_all[l], m_norm[l], h_norm[l],
                             w_pa[l], w_pb[l], w_out[l], not last)
        x = x + g1[:, None, :] * rmsnorm(yl, norm_mix_post[l])
        hl = modulate(rmsnorm(x, norm_ffn_pre[l]), sh2, sc2)
        x = x + g2[:, None, :] * rmsnorm(moe_ffn(hl, w_router[l], b_router[l], w_gu[l], b_gu[l], w_dn[l], b_dn[l]),
                                         norm_ffn_post[l])
        if not last:
            ctx = ctx + cg1[:, None, :] * rmsnorm(yc, norm_mix_post[l])
            hc = modulate(rmsnorm(ctx, norm_ffn_pre[l]), csh2, csc2)
            ctx = ctx + cg2[:, None, :] * rmsnorm(
                moe_ffn(hc, w_router[l], b_router[l], w_gu[l], b_gu[l], w_dn[l], b_dn[l]), norm_ffn_post[l])
    return x
```

```python
import contextlib
import numpy as np
import concourse.bass as bass
import concourse.mybir as mybir
from concourse.bass_utils import run_bass_kernel_spmd

F32 = mybir.dt.float32
BF16 = mybir.dt.bfloat16
AF = mybir.ActivationFunctionType
ALU = mybir.AluOpType
AX = mybir.AxisListType

PE, DVE, ACT, POOL, SP = "pe", "dve", "act", "pool", "sp"
COMPUTE = (PE, DVE, ACT, POOL)
EPOCH = 12000
N_DMA_SEM = 24
N_EPOCH_SEM = 12
EPS = 1e-6
NT = 18
TOK = 2304
N_CORES = 8


class Prog:
    def __init__(self, nc, same_engine_sync=True):
        self.nc = nc
        self.same = same_engine_sync
        self.streams = {e: [] for e in (PE, DVE, ACT, POOL, SP)}
        self.count = {e: 0 for e in COMPUTE}
        self.waited = {}
        self.state = {}
        self.dma_tot = [0] * N_DMA_SEM
        self.dma_rr = 0
        self.barrier_toks = set()

    def barrier(self):
        toks = set()
        for e in COMPUTE:
            c = self.count[e]
            if c > 0:
                toks.add(((e, (c - 1) // EPOCH), ((c - 1) % EPOCH) + 1))
        for s_ in range(N_DMA_SEM):
            if self.dma_tot[s_] > 0:
                toks.add((("dma", s_), self.dma_tot[s_]))
        self.barrier_toks = toks

    def _deps(self, reads, writes):
        deps = set()
        for k in reads:
            st = self.state.get(k)
            if st and st[0]:
                deps.add(st[0])
        for k in writes:
            st = self.state.get(k)
            if st:
                if st[0]:
                    deps.add(st[0])
                deps.update(st[1])
        return deps

    def _commit(self, reads, writes, tok):
        for k in writes:
            self.state[k] = [tok, []]
        for k in reads:
            if k in writes:
                continue
            st = self.state.setdefault(k, [None, []])
            st[1].append(tok)
            if len(st[1]) > 64:
                best = {}
                for (key, val) in st[1]:
                    if best.get(key, 0) < val:
                        best[key] = val
                st[1] = list(best.items())

    def _waits(self, eng, deps, own_key=None):
        best = {}
        for (key, val) in deps:
            if key == own_key and not self.same:
                continue
            if self.waited.get((eng, key), 0) >= val:
                continue
            if best.get(key, 0) < val:
                best[key] = val
        out = []
        for key, val in best.items():
            self.waited[(eng, key)] = val
            out.append((key, val))
        return out

    def op(self, eng, fn, reads=(), writes=()):
        reads, writes = tuple(reads), tuple(writes)
        c = self.count[eng]
        own_key = (eng, c // EPOCH)
        deps = self._deps(reads, writes) | self.barrier_toks
        if eng == PE:
            deps = {d for d in deps if d[0][0] != PE}
        waits = self._waits(eng, deps, own_key)
        self.count[eng] = c + 1
        tok = (own_key, (c % EPOCH) + 1)
        self.streams[eng].append(("op", waits, fn, own_key))
        self._commit(reads, writes, tok)

    def dma(self, q, fn, reads=(), writes=()):
        reads, writes = tuple(reads), tuple(writes)
        s = self.dma_rr
        self.dma_rr = (s + 1) % N_DMA_SEM
        key = ("dma", s)
        deps = self._deps(reads, writes) | self.barrier_toks
        if self.dma_tot[s] > 0:
            deps.add((key, self.dma_tot[s]))
        waits = self._waits(q, deps, None)
        self.dma_tot[s] += 16
        tok = (key, self.dma_tot[s])
        self.streams[q].append(("dma", waits, fn, key))
        self._commit(reads, writes, tok)
        return tok

    def final_wait(self, eng, toks):
        waits = self._waits(eng, set(toks), None)
        self.streams[eng].append(("wait", waits, None, None))

    def emit(self):
        nc = self.nc
        with contextlib.ExitStack() as es:
            sems = {}
            for e in COMPUTE:
                nep = self.count[e] // EPOCH + 1
                assert nep <= N_EPOCH_SEM, (e, self.count[e])
                for i in range(nep):
                    sems[(e, i)] = es.enter_context(nc.semaphore(f"s_{e}_{i}"))
            for s in range(N_DMA_SEM):
                sems[("dma", s)] = es.enter_context(nc.semaphore(f"s_dma_{s}"))
            block = es.enter_context(nc.Block())

            def run(eng_name):
                def body(engine):
                    for kind, waits, fn, key in self.streams[eng_name]:
                        for (k, v) in waits:
                            engine.wait_ge(sems[k], v)
                        if kind == "op":
                            fn(engine).then_inc(sems[key], 1)
                        elif kind == "dma":
                            fn(engine).then_inc(sems[key], 16)
                return body

            block.sync(run(SP))
            block.tensor(run(PE))
            block.vector(run(DVE))
            block.scalar(run(ACT))
            block.gpsimd(run(POOL))


def kk(name, idxs):
    return [(name, i) for i in idxs]


def build_nc(nb=2, dumps=(), stop=None, n_exp=32):
    nc = bass.Bass("TRN2", target_bir_lowering=False)
    P = Prog(nc)
    D = {}

    def din(name, shape, dt=F32):
        D[name] = nc.dram_tensor(name, list(shape), dt, kind="ExternalInput").ap()
        return D[name]

    x_d = din("x", [2, 2048, 1024])
    ctx_d = din("ctx", [2, 256, 1024])
    cvT_d = din("cvT", [128, 24])
    wada_d = din("w_ada", [12, 128, 8, 512])
    bada_d = din("b_ada", [1, 6144])
    nrm_d = din("nrm", [1, 4096])
    win_d = din("w_in", [52, 128, 8, 128])
    wmg_d = din("w_mg", [128, 8, 16])
    bcol_d = din("b_in_col", [128, 52])
    brow_d = din("b_in_row", [1, 6672])
    conv_d = din("conv_col", [128, 8, 9])
    lb_d = din("lb_raw", [1, 2048])
    mn_d = din("m_norm", [1, 512])
    hn_d = din("h_norm", [1, 512])
    wpa_d = din("w_pa", [128, 4, 1024])
    wpb_d = din("w_pb", [128, 4, 1024])
    wout_d = din("w_out", [128, 8, 1024])
    wr_d = din("w_router", [128, 8, 32])
    br_d = din("b_router", [1, 32])
    wgu_d = din("w_gu", [32, 128, 8, 2048])
    bgu_d = din("b_gu_col", [128, 32, 16])
    wdn_d = din("w_dn", [32, 128, 8, 1024])
    bdn_d = din("b_dn", [32, 1024])
    ident_d = din("ident", [128, 128])
    triF_d = din("triF", [128, 128])
    triB_d = din("triB", [128, 128])
    ones_d = din("ones", [128, 128])
    sel_d = din("sel", [32, 4096])
    out_d = nc.dram_tensor("out", [2, 2048, 1024], F32, kind="ExternalOutput").ap()
    bc_d = nc.dram_tensor("bc_scr", [14, 128, 1024], F32).ap()
    x1_d = nc.dram_tensor("x1_scr", [2, 2048, 1024], F32).ap()
    dump_d = {}
    for (nm, shape) in dumps:
        dump_d[nm] = nc.dram_tensor("dbg_" + nm, list(shape), F32, kind="ExternalOutput").ap()
    out_toks = []

    def MM(out, lhsT, rhs, start, stop, reads, writes):
        P.op(PE, lambda e: e.matmul(out, lhsT=lhsT, rhs=rhs, start=start, stop=stop), reads, writes)

    def TR(out, in_, ident, reads, writes):
        P.op(PE, lambda e: e.transpose(out, in_, ident), reads, writes)

    def ACTV(out, in_, func, reads, writes, bias=None, scale=None):
        kw = {}
        if bias is not None:
            kw["bias"] = bias
        if scale is not None:
            kw["scale"] = scale
        P.op(ACT, lambda e: e.activation(out=out, in_=in_, func=func, **kw), reads, writes)

    def TS(out, in0, s1, s2, op0, op1, reads, writes, eng=DVE):
        if s2 is None:
            P.op(eng, lambda e: e.tensor_scalar(out=out, in0=in0, scalar1=s1, scalar2=None, op0=op0), reads, writes)
        else:
            P.op(eng, lambda e: e.tensor_scalar(out=out, in0=in0, scalar1=s1, scalar2=s2, op0=op0, op1=op1),
                 reads, writes)

    def TT(out, in0, in1, op, reads, writes, eng=DVE):
        P.op(eng, lambda e: e.tensor_tensor(out=out, in0=in0, in1=in1, op=op), reads, writes)

    def STT(out, in0, scalar, in1, op0, op1, reads, writes):
        P.op(DVE, lambda e: e.scalar_tensor_tensor(out=out, in0=in0, scalar=scalar, in1=in1, op0=op0, op1=op1),
             reads, writes)

    def CP(out, in_, reads, writes, eng=DVE):
        if eng == ACT:
            P.op(ACT, lambda e: e.activation(out=out, in_=in_, func=AF.Copy), reads, writes)
        else:
            P.op(eng, lambda e: e.tensor_copy(out=out, in_=in_), reads, writes)

    def RED(out, in_, reads, writes):
        P.op(DVE, lambda e: e.tensor_reduce(out=out, in_=in_, axis=AX.X, op=ALU.add), reads, writes)

    def RECIP(out, in_, reads, writes):
        P.op(DVE, lambda e: e.reciprocal(out=out, in_=in_), reads, writes)

    def MSET(ap, val, writes, eng=DVE):
        P.op(eng, lambda e: e.memset(ap, val), (), writes)

    def DMA(q, out, in_, reads, writes):
        return P.dma(q, lambda e: e.dma_start(out=out, in_=in_), reads, writes)

    def bcast(ap1n, n):
        return ap1n.partition_broadcast(128)

    def rstd_from_ss(rs, ss, inv_n, key):
        TS(rs, ss, inv_n, EPS, ALU.mult, ALU.add, [key], [key])
        ACTV(rs, rs, AF.Sqrt, [key], [key])
        RECIP(rs, rs, [key], [key])

    with contextlib.ExitStack() as top:
        uniq = [0]

        def sb(name, shape, dt=F32, stack=top):
            uniq[0] += 1
            return stack.enter_context(nc.sbuf_tensor(f"sb_{name}_{uniq[0]}", list(shape), dt))

        psf = [top.enter_context(nc.psum_tensor(f"psf{i}", [128, 512], F32)) for i in range(6)]
        psb = [top.enter_context(nc.psum_tensor(f"psb{i}", [128, 1024], BF16)) for i in range(2)]
        rr = {"f": 0, "b": 0, "t": 0}

        def nps():
            i = rr["f"]
            rr["f"] = (i + 1) % 6
            return psf[i], ("psf", i)

        def npsb():
            i = rr["b"]
            rr["b"] = (i + 1) % 2
            return psb[i], ("psb", i)

        ident_f = sb("ident_f", [128, 128])
        ident_b = sb("ident_b", [128, 128], BF16)
        triF = sb("triF", [128, 128])
        triB = sb("triB", [128, 128])
        ones_f = sb("ones_f", [128, 128])
        bcol = sb("bcol", [128, 52])
        convc = sb("convc", [128, 8, 9])
        lb_bc = sb("lb_bc", [128, 2, 512])
        oml_bc = sb("oml_bc", [128, 2, 512])
        mn_bc = sb("mn_bc", [128, 512])
        hn_bc = sb("hn_bc", [128, 512])
        br_bc = sb("br_bc", [128, 32])
        wr_sb = sb("wr_sb", [128, 8, 32], BF16)
        wmg_sb = sb("wmg_sb", [128, 8, 16], BF16)
        bmg_bc = sb("bmg_bc", [128, 16])
        DMA(SP, ident_f[:], ident_d, [], ["ident_f"])
        DMA(POOL, ident_b[:], ident_d, [], ["ident_b"])
        DMA(SP, triF[:], triF_d, [], ["triF"])
        DMA(SP, triB[:], triB_d, [], ["triB"])
        DMA(SP, ones_f[:], ones_d, [], ["ones_f"])
        DMA(SP, bcol[:], bcol_d, [], ["bcol"])
        DMA(SP, convc[:], conv_d, [], ["convc"])
        DMA(SP, mn_bc[:], bcast(mn_d, 512), [], ["mn_bc"])
        DMA(SP, hn_bc[:], bcast(hn_d, 512), [], ["hn_bc"])
        DMA(SP, br_bc[:], bcast(br_d, 32), [], ["br_bc"])
        DMA(POOL, wr_sb[:], wr_d, [], ["wr_sb"])
        DMA(POOL, wmg_sb[:], wmg_d, [], ["wmg_sb"])
        DMA(SP, bmg_bc[:], bcast(brow_d[:, 6656:6672], 16), [], ["bmg_bc"])
        lbf = lb_bc[:].rearrange("p a b -> p (a b)")
        omlf = oml_bc[:].rearrange("p a b -> p (a b)")
        with contextlib.ExitStack() as sl:
            lbr = sb("lbr", [128, 2048], stack=sl)
            DMA(SP, lbr[:], bcast(lb_d, 2048), [], ["lbr"])
            TT(lbf, lbr[:, 0:1024], lbr[:, 1024:2048], ALU.subtract, ["lbr"], ["lb_bc"])
            ACTV(lbf, lbf, AF.Sigmoid, ["lb_bc"], ["lb_bc"])
            TS(omlf, lbf, -1.0, 1.0, ALU.mult, ALU.add, ["lb_bc"], ["oml_bc"])
        P.barrier()

        with contextlib.ExitStack() as s0:
            cT = sb("cT", [128, 24], stack=s0)
            sg0 = sb("sg0", [128, 24], stack=s0)
            cb = sb("cb", [128, 24, 128], stack=s0)
            nrm_bc = sb("nrm_bc", [128, 4, 1024], stack=s0)
            wa = [sb(f"wa{i}", [128, 8, 512], stack=s0) for i in range(2)]
            ba = [sb(f"ba{i}", [128, 512], stack=s0) for i in range(2)]
            tA = [sb(f"tA{i}", [128, 512], stack=s0) for i in range(3)]
            tB = [sb(f"tB{i}", [128, 512], stack=s0) for i in range(3)]
            DMA(SP, cT[:], cvT_d, [], ["cT"])
            DMA(SP, nrm_bc[:].rearrange("p a b -> p (a b)"), bcast(nrm_d, 4096), [], ["nrm_bc"])
            ACTV(sg0[:], cT[:], AF.Sigmoid, ["cT"], ["sg0"])
            TT(cT[:], cT[:], sg0[:], ALU.mult, ["cT", "sg0"], ["cT"])
            for i in range(24):
                TS(cb[:, i, :], ones_f[:], cT[:, i:i + 1], None, ALU.mult, None, ["ones_f", "cT"], [("cb", i)])
            ti_rot = 0
            for cbi in range(12):
                w, half = cbi // 2, cbi % 2
                cs = slice(half * 512, (half + 1) * 512)
                wt, bt = wa[cbi % 2], ba[cbi % 2]
                DMA(SP, wt[:], wada_d[cbi], [], [("wa", cbi % 2)])
                DMA(SP, bt[:], bcast(bada_d[:, cbi * 512:(cbi + 1) * 512], 512), [], [("ba", cbi % 2)])
                for r in range(3):
                    if r == 2 and w > 1:
                        continue
                    pt, pk = nps()
                    for k in range(8):
                        MM(pt[:, :], cb[:, r * 8 + k, :], wt[:, k, :], k == 0, k == 7,
                           [("cb", r * 8 + k), ("wa", cbi % 2)], [pk])
                    a_, b_ = tA[ti_rot % 3], tB[ti_rot % 3]
                    ka, kb = ("tA", ti_rot % 3), ("tB", ti_rot % 3)
                    ti_rot += 1
                    TT(a_[:], pt[:, :], bt[:], ALU.add, [pk, ("ba", cbi % 2)], [ka])
                    if w in (1, 4):
                        STT(b_[:], a_[:], 1.0, nrm_bc[:, 0 if w == 1 else 2, cs], ALU.add, ALU.mult,
                            [ka, "nrm_bc"], [kb])
                        src, ksrc = b_, kb
                    elif w in (2, 5):
                        TT(b_[:], a_[:], nrm_bc[:, 1 if w == 2 else 3, cs], ALU.mult, [ka, "nrm_bc"], [kb])
                        src, ksrc = b_, kb
                    else:
                        src, ksrc = a_, ka
                    tidx = r * 6 + w if r < 2 else 12 + w
                    DMA(SP, bc_d[tidx, :, cs], src[:], [ksrc], [("bc", tidx, half)])

        P.barrier()

        def load_bc(dst, tidx, key):
            DMA(SP, dst[:], bc_d[tidx], [("bc", tidx, 0), ("bc", tidx, 1)], [key])

        h2T = sb("h2T", [128, 8, 2048], BF16)
        gT_sb = sb("gT_sb", [32, 2048], BF16)
        G2 = sb("G2", [128, 1024])
        small = sb("small", [128, 64])

        def hTk(t0, t1):
            return kk("hT", range(t0 // 128, (t1 + 127) // 128))

        def dump(nm, ap_sb, dst, reads):
            if nm in dump_d:
                out_toks.append(DMA(POOL, dst(dump_d[nm]), ap_sb, reads, []))

        mx = None
        for b in range(nb):
            mx = contextlib.ExitStack()
            hT = sb("hT", [128, 8, TOK], BF16, stack=mx)
            aT = sb("aT", [128, 4, 2048], BF16, stack=mx)
            bT = sb("bT", [128, 4, 2048], BF16, stack=mx)
            with contextlib.ExitStack() as s1:
                A1 = sb("A1", [128, 1024], stack=s1)
                SH1 = sb("SH1", [128, 1024], stack=s1)
                A1c = sb("A1c", [128, 1024], stack=s1)
                SH1c = sb("SH1c", [128, 1024], stack=s1)
                xin = [sb(f"xin{i}", [128, 1024], stack=s1) for i in range(2)]
                sq = sb("sq", [128, 1024], stack=s1)
                tmp = sb("tmp1", [128, 1024], stack=s1)
                hb = [sb(f"hb{i}", [128, 1024], BF16, stack=s1) for i in range(2)]
                load_bc(A1, b * 6 + 1, "A1")
                load_bc(SH1, b * 6 + 0, "SH1")
                load_bc(A1c, 13, "A1c")
                load_bc(SH1c, 12, "SH1c")
                for ti in range(NT):
                    xi, xk = xin[ti % 2], ("xin", ti % 2)
                    src = ctx_d[b, ti * 128:(ti + 1) * 128, :] if ti < 2 else x_d[b, (ti - 2) * 128:(ti - 1) * 128, :]
                    DMA(SP, xi[:], src, [], [xk])
                    ACTV(sq[:], xi[:], AF.Square, [xk], ["sq"])
                    sc, sk = small[:, ti:ti + 1], ("small", ti)
                    RED(sc, sq[:], ["sq"], [sk])
                    rstd_from_ss(sc, sc, 1.0 / 1024, sk)
                    Am, Ak, Sm, Sk = (A1c, "A1c", SH1c, "SH1c") if ti < 2 else (A1, "A1", SH1, "SH1")
                    STT(tmp[:], xi[:], sc, Am[:], ALU.mult, ALU.mult, [xk, sk, Ak], ["tmp1"])
                    hbt, hbk = hb[ti % 2], ("hb", ti % 2)
                    TT(hbt[:], tmp[:], Sm[:], ALU.add, ["tmp1", Sk], [hbk])
                    pb_, pbk = npsb()
                    for k in range(8):
                        TR(pb_[:, k * 128:(k + 1) * 128], hbt[:, k * 128:(k + 1) * 128], ident_b[:],
                           [hbk, "ident_b"], [pbk])
                    CP(hT[:, :, ti * 128:(ti + 1) * 128], pb_[:].rearrange("p (k t) -> p k t", k=8),
                       [pbk], [("hT", ti)], eng=ACT)
            P.barrier()
            dump("hT", hT[:, 0, :], lambda d: d, kk("hT", range(NT)))
            if stop == "s1":
                break

            with contextlib.ExitStack() as sm:
                wfm = [sb(f"wfm{i}", [128, 8, 128], BF16, stack=sm) for i in range(2)]
                srcf = sb("srcf", [128, TOK], stack=sm)
                accf = sb("accf", [128, TOK], stack=sm)
                stm = [sb(f"stm{i}", [128, 128], BF16, stack=sm) for i in range(2)]
                Pst = [sb(f"Pst{i}", [128, 129], stack=sm) for i in range(2)]
                Sbf = [sb(f"Sbf{i}", [128, 129], BF16, stack=sm) for i in range(2)]
                ssh = sb("ssh", [128, 16], stack=sm)
                tmpf = sb("tmpf", [128, 16, 128], stack=sm)
                a_tm = sb("a_tm", [128, 16, 128], BF16, stack=sm)

                def proj_fm(chunk, wt, wk, dst, dk):
                    DMA(POOL, wt[:], win_d[chunk], [], [wk])
                    for g0 in range(0, TOK, 512):
                        n = min(512, TOK - g0)
                        pt, pk = nps()
                        for k in range(8):
                            MM(pt[:, 0:n], wt[:, k, :], hT[:, k, g0:g0 + n], k == 0, k == 7,
                               [wk] + hTk(g0, g0 + n), [pk])
                        ACTV(dst[:, g0:g0 + n], pt[:, 0:n], AF.Identity, [pk, "bcol"], [dk],
                             bias=bcol[:, chunk:chunk + 1])

                def scan(d, QT, qk, q_lat_only, KT, ktk, Ktm, ktmk, V, vk, nv, dec_of, deck, post):
                    order = [0, 1] + list(range(2, NT)) if d == 0 else [1, 0] + list(range(NT - 1, 1, -1))
                    mask, mkey = (triF, "triF") if d == 0 else (triB, "triB")
                    Pt, Pk, St, Sk = Pst[d], ("Pst", d), Sbf[d], ("Sbf", d)
                    prev = None
                    for idx, c in enumerate(order):
                        cs = slice(c * 128, (c + 1) * 128)
                        qs = slice((c - 2) * 128, (c - 1) * 128) if q_lat_only else cs
                        if c >= 2:
                            p1, p1k = nps()
                            MM(p1[:, 0:128], KT[:, cs], QT[:, qs], True, True, [ktk, qk], [p1k])
                            sm_, smk = stm[rr["t"] % 2], ("stm", rr["t"] % 2)
                            rr["t"] += 1
                            TT(sm_[:], p1[:, 0:128], mask[:], ALU.mult, [p1k, mkey], [smk])
                            po, pok = nps()
                            MM(po[:, 0:nv], sm_[:], V[:, c, 0:nv], True, prev is None, [smk, vk], [pok])
                            if prev is not None:
                                MM(po[:, 0:nv], QT[:, qs], St[:, 0:nv], False, True, [qk, Sk], [pok])
                            post(po, pok, c)
                        if idx < len(order) - 1:
                            pu, puk = nps()
                            MM(pu[:, 0:nv], Ktm[:, c, :], V[:, c, 0:nv], True, True, [ktmk, vk], [puk])
                            if prev is None:
                                CP(Pt[:, 0:nv], pu[:, 0:nv], [puk], [Pk])
                            else:
                                STT(Pt[:, 0:nv], Pt[:, 0:nv], dec_of(prev), pu[:, 0:nv], ALU.mult, ALU.add,
                                    [Pk, puk, deck], [Pk])
                            ACTV(St[:, 0:nv], Pt[:, 0:nv], AF.Copy, [Pk, deck], [Sk], scale=dec_of(c))
                            prev = c

                def head_norm_to_T(hs, hsk, nbc, nbk, h, gate, gk, dstT, dstk, tmpf, a_tm):
                    ACTV(tmpf[:], hs[:], AF.Square, [hsk], ["hn_tmp"])
                    RED(ssh[:], tmpf[:], ["hn_tmp"], ["ssh"])
                    rstd_from_ss(ssh[:], ssh[:], 1.0 / 128, "ssh")
                    for ti in range(16):
                        STT(tmpf[:, ti, :], hs[:, ti, :], ssh[:, ti:ti + 1], nbc[:, h * 128:(h + 1) * 128],
                            ALU.mult, ALU.mult, [hsk, "ssh", nbk, "hn_tmp"], ["hn_tmp"])
                    TT(a_tm[:], tmpf[:], gate[:], ALU.mult, ["hn_tmp", gk], ["a_tm"])
                    for t0 in (0, 8):
                        pb_, pbk = npsb()
                        for j in range(8):
                            TR(pb_[:, j * 128:(j + 1) * 128], a_tm[:, t0 + j, :], ident_b[:], ["a_tm", "ident_b"], [pbk])
                        CP(dstT[:, h, t0 * 128:(t0 + 8) * 128], pb_[:, :], [pbk], [dstk], eng=ACT)

                s2x = contextlib.ExitStack()
                gates = sb("gates", [128, 16, NT], stack=s2x)
                lf = sb("lf", [128, 8, NT], stack=s2x)
                bcum = sb("bcum", [128, 8, NT], stack=s2x)
                dec_m = sb("dec_m", [128, 8, NT], stack=s2x)
                w_m = sb("w_m", [128, 8, NT], stack=s2x)
                e_m = sb("e_m", [128, 8, NT], stack=s2x)
                for ti in range(NT):
                    pt, pk = nps()
                    for k in range(8):
                        MM(pt[:, 0:16], hT[:, k, ti * 128:(ti + 1) * 128], wmg_sb[:, k, :], k == 0, k == 7,
                           [("hT", ti), "wmg_sb"], [pk])
                    TT(gates[:, :, ti], pt[:, 0:16], bmg_bc[:], ALU.add, [pk, "bmg_bc"], ["gates"])
                ACTV(lf[:], gates[:, 8:16, :], AF.Sigmoid, ["gates"], ["lf"])
                ACTV(lf[:], lf[:], AF.Ln, ["lf"], ["lf"])
                pt, pk = nps()
                MM(pt[:, 0:72], triF[:], lf[:, 0:4, :].rearrange("p a b -> p (a b)"), True, True, ["triF", "lf"], [pk])
                MM(pt[:, 72:144], triB[:], lf[:, 4:8, :].rearrange("p a b -> p (a b)"), True, True,
                   ["triB", "lf"], [pk])
                CP(bcum[:].rearrange("p a b -> p (a b)"), pt[:, 0:144], [pk], ["bcum"])
                pt2, pk2 = nps()
                MM(pt2[:, 0:144], ones_f[:], lf[:].rearrange("p a b -> p (a b)"), True, True, ["ones_f", "lf"], [pk2])
                ACTV(dec_m[:].rearrange("p a b -> p (a b)"), pt2[:, 0:144], AF.Exp, [pk2], ["dec_m"])
                TT(w_m[:], gates[:, 0:8, :], bcum[:], ALU.subtract, ["gates", "bcum"], ["w_m"])
                ACTV(w_m[:], w_m[:], AF.Exp, ["w_m"], ["w_m"])
                ACTV(e_m[:], bcum[:], AF.Exp, ["bcum"], ["e_m"])
                dump("bcum", bcum[:].rearrange("p a b -> p (a b)"), lambda d: d, ["bcum"])

                with contextlib.ExitStack() as s3:
                    qT = sb("qT", [128, TOK], BF16, stack=s3)
                    kT = sb("kT", [128, TOK], BF16, stack=s3)
                    k_tm = sb("k_tm", [128, NT, 128], BF16, stack=s3)
                    wvo = sb("wvo", [128, 8, 256], BF16, stack=s3)
                    bvo = sb("bvo", [128, 256], stack=s3)
                    vp = sb("vp", [128, NT, 129], BF16, stack=s3)
                    v2_ = sb("v2_0", [128, NT, 129], BF16, stack=s3)
                    v2 = [v2_, v2_]
                    og = sb("og", [128, 16, 128], BF16, stack=s3)
                    hsum = sb("hsum", [128, 16, 128], stack=s3)
                    dsc = sb("dsc", [128, 4], stack=s3)
                    MSET(vp[:, :, 128:129], 1.0, ["vp1"])
                    for h in range(4):
                        for which, dst, dkey, scale in ((0, qT, "qT", 128.0 ** -0.5), (1, kT, "kT", 1.0)):
                            chunk = which * 4 + h
                            proj_fm(chunk, wfm[which], ("wfm", which), srcf, "srcf")
                            wc = convc[:, chunk, :]
                            ls = srcf[:, 256:TOK].rearrange("p (r c) -> p r c", c=64)
                            la = accf[:, 256:TOK].rearrange("p (r c) -> p r c", c=64)
                            TS(accf[:], srcf[:], wc[:, 4:5], None, ALU.mult, None, ["srcf", "convc"], ["accf"])
                            for di in (-1, 0, 1):
                                for dj in (-1, 0, 1):
                                    if di == 0 and dj == 0:
                                        continue
                                    tap = (di + 1) * 3 + (dj + 1)
                                    r0, r1 = max(0, -di), 32 - max(0, di)
                                    c0, c1 = max(0, -dj), 64 - max(0, dj)
                                    STT(la[:, r0:r1, c0:c1], ls[:, r0 + di:r1 + di, c0 + dj:c1 + dj], wc[:, tap:tap + 1],
                                        la[:, r0:r1, c0:c1], ALU.mult, ALU.add, ["srcf", "convc", "accf"], ["accf"])
                            STT(accf[:, 1:256], srcf[:, 0:255], wc[:, 3:4], accf[:, 1:256], ALU.mult, ALU.add,
                                ["srcf", "convc", "accf"], ["accf"])
                            STT(accf[:, 0:255], srcf[:, 1:256], wc[:, 5:6], accf[:, 0:255], ALU.mult, ALU.add,
                                ["srcf", "convc", "accf"], ["accf"])
                            ACTV(srcf[:], accf[:], AF.Sigmoid, ["accf"], ["srcf"])
                            STT(dst[:], accf[:], scale, srcf[:], ALU.mult, ALU.mult, ["accf", "srcf"], [dkey])
                        if h == 0 and b == 0:
                            dump("qT", qT[:], lambda d: d, ["qT"])
                            dump("kT", kT[:], lambda d: d, ["kT"])
                        for t0 in range(0, NT, 8):
                            n = min(8, NT - t0)
                            pb_, pbk = npsb()
                            for j in range(n):
                                TR(pb_[:, j * 128:(j + 1) * 128], kT[:, (t0 + j) * 128:(t0 + j + 1) * 128], ident_b[:],
                                   ["kT", "ident_b"], [pbk])
                            CP(k_tm[:, t0:t0 + n, :].rearrange("p a b -> p (a b)"), pb_[:, 0:n * 128], [pbk], ["k_tm"],
                               eng=ACT)
                        DMA(POOL, wvo[:, :, 0:128], win_d[8 + h], [], ["wvo"])
                        DMA(POOL, wvo[:, :, 128:256], win_d[12 + h], [], ["wvo"])
                        DMA(SP, bvo[:, 0:128], bcast(brow_d[:, 1024 + h * 128:1024 + (h + 1) * 128], 128), [], ["bvo"])
                        DMA(SP, bvo[:, 128:256], bcast(brow_d[:, 1536 + h * 128:1536 + (h + 1) * 128], 128), [], ["bvo"])
                        for ti in range(NT):
                            pt, pk = nps()
                            for k in range(8):
                                MM(pt[:, 0:256], hT[:, k, ti * 128:(ti + 1) * 128], wvo[:, k, :], k == 0, k == 7,
                                   [("hT", ti), "wvo"], [pk])
                            TT(vp[:, ti, 0:128], pt[:, 0:128], bvo[:, 0:128], ALU.add, [pk, "bvo"], ["vp"])
                            if ti >= 2:
                                TT(og[:, ti - 2, :], pt[:, 128:256], bvo[:, 128:256], ALU.add, [pk, "bvo"], ["og"])
                        ACTV(og[:], og[:], AF.Sigmoid, ["og"], ["og"])
                        for d in range(2):
                            col = d * 4 + h
                            for ti in range(NT):
                                TS(v2[d][:, ti, :], vp[:, ti, :], w_m[:, col, ti:ti + 1], None, ALU.mult, None,
                                   ["vp", "vp1", "w_m"], ["v2"])

                            def post(po, pok, c, d=d, col=col):
                                ecol = e_m[:, col, c:c + 1]
                                d1, d2 = dsc[:, 2 * d:2 * d + 1], dsc[:, 2 * d + 1:2 * d + 2]
                                dk_ = ("dsc", d)
                                ACTV(d1, po[:, 128:129], AF.Abs, [pok, "e_m"], [dk_], scale=ecol)
                                TS(d1, d1, 1.0, None, ALU.max, None, [dk_], [dk_])
                                RECIP(d1, d1, [dk_], [dk_])
                                TT(d2, d1, ecol, ALU.mult, [dk_, "e_m"], [dk_])
                                if d == 0:
                                    TS(hsum[:, c - 2, :], po[:, 0:128], d2, None, ALU.mult, None, [pok, dk_], ["hsum"])
                                else:
                                    STT(hsum[:, c - 2, :], po[:, 0:128], d2, hsum[:, c - 2, :], ALU.mult, ALU.add,
                                        [pok, dk_, "hsum"], ["hsum"])

                            scan(d, qT, "qT", False, kT, "kT", k_tm, "k_tm", v2[d], "v2", 129,
                                 lambda c, col=col: dec_m[:, col, c:c + 1], "dec_m", post)
                        if h == 0 and b == 0:
                            dump("hsum", hsum[:].rearrange("p a b -> p (a b)"), lambda d: d, ["hsum"])
                        head_norm_to_T(hsum, "hsum", mn_bc, "mn_bc", h, og, "og", aT, "aT", tmpf, a_tm)
                s2x.close()
                P.barrier()
                if stop == "s3":
                    break

                with contextlib.ExitStack() as s4:
                    hqT = sb("hqT", [128, TOK], BF16, stack=s4)
                    w4 = sb("w4", [128, 8, 384], BF16, stack=s4)
                    w4b = sb("w4b", [128, 8, 128], BF16, stack=s4)
                    b4 = sb("b4", [128, 512], stack=s4)
                    hi_tm = sb("hi_tm", [128, NT, 128], BF16, stack=s4)
                    g_tm = sb("g_tm", [128, 16, 128], BF16, stack=s4)
                    zf = srcf[:].rearrange("p (a b) -> p a b", b=128)
                    kkf = accf[:].rearrange("p (a b) -> p a b", b=128)
                    ktl = sb("ktl", [128, NT, 128], BF16, stack=s4)
                    ktlT = sb("ktlT", [128, TOK], BF16, stack=s4)
                    qtl = sb("qtl", [128, 2048], BF16, stack=s4)
                    dec_h = sb("dec_h", [128, 2, NT], stack=s4)
                    ex_ = sb("ex0", [128, 512], stack=s4)
                    ex = [ex_, ex_]
                    osum = sb("osum", [128, 16, 128], stack=s4)
                    exr = 0
                    for h in range(4):
                        proj_fm(16 + h, wfm[0], ("wfm", 0), srcf, "srcf")
                        ACTV(accf[:], srcf[:], AF.Sigmoid, ["srcf"], ["accf"])
                        TT(hqT[:], srcf[:], accf[:], ALU.mult, ["srcf", "accf"], ["hqT"])
                        for j, ch in enumerate((20 + h, 24 + h, 28 + h)):
                            DMA(POOL, w4[:, :, j * 128:(j + 1) * 128], win_d[ch], [], ["w4"])
                        DMA(POOL, w4b[:], win_d[32 + h], [], ["w4b"])
                        for j, ch in enumerate((20 + h, 24 + h, 28 + h, 32 + h)):
                            DMA(SP, b4[:, j * 128:(j + 1) * 128], bcast(brow_d[:, ch * 128:(ch + 1) * 128], 128), [], ["b4"])
                        for d in range(2):
                            hs = slice(h * 128, (h + 1) * 128)
                            for ti in range(NT):
                                pt, pk = nps()
                                if d == 0:
                                    for k in range(8):
                                        MM(pt[:, 0:384], hT[:, k, ti * 128:(ti + 1) * 128], w4[:, k, :], k == 0, k == 7,
                                           [("hT", ti), "w4"], [pk])
                                    TT(hi_tm[:, ti, :], pt[:, 0:128], b4[:, 0:128], ALU.add, [pk, "b4"], ["hi_tm"])
                                    if ti >= 2:
                                        TT(tmpf[:, ti - 2, :], pt[:, 128:256], b4[:, 128:256], ALU.add, [pk, "b4"], ["hn_tmp"])
                                    TT(zf[:, ti, :], pt[:, 256:384], b4[:, 256:384], ALU.add, [pk, "b4"], ["srcf"])
                                else:
                                    for k in range(8):
                                        MM(pt[:, 0:128], hT[:, k, ti * 128:(ti + 1) * 128], w4b[:, k, :], k == 0, k == 7,
                                           [("hT", ti), "w4b"], [pk])
                                    TT(zf[:, ti, :], pt[:, 0:128], b4[:, 384:512], ALU.add, [pk, "b4"], ["srcf"])
                            if d == 0:
                                ACTV(g_tm[:], tmpf[:], AF.Sigmoid, ["hn_tmp"], ["g_tm"])
                                TT(g_tm[:], g_tm[:], tmpf[:], ALU.mult, ["g_tm", "hn_tmp"], ["g_tm"])
                            ACTV(accf[:], srcf[:], AF.Sigmoid, ["srcf"], ["accf"])
                            for ti in range(NT):
                                TT(zf[:, ti, :], kkf[:, ti, :], oml_bc[:, d, hs], ALU.mult, ["accf", "oml_bc", "srcf"], ["srcf"])
                                TT(zf[:, ti, :], zf[:, ti, :], lb_bc[:, d, hs], ALU.add, ["srcf", "lb_bc"], ["srcf"])
                            TS(accf[:], srcf[:], -1.0, 1.0, ALU.mult, ALU.add, ["srcf"], ["accf"])
                            ACTV(srcf[:], srcf[:], AF.Ln, ["srcf"], ["srcf"])
                            tri, trik = (triF, "triF") if d == 0 else (triB, "triB")
                            for t0 in range(0, NT, 4):
                                n = min(4, NT - t0)
                                fs = slice(t0 * 128, (t0 + n) * 128)
                                pt, pk = nps()
                                MM(pt[:, 0:n * 128], tri[:], srcf[:, fs], True, True, [trik, "srcf"], [pk])
                                e_, ek = ex[0], ("ex", 0)
                                exr += 1
                                ACTV(e_[:, 0:n * 128], pt[:, 0:n * 128], AF.Exp, [pk], [ek], scale=-1.0)
                                TT(ktl[:, t0:t0 + n, :].rearrange("p a b -> p (a b)"), accf[:, fs], e_[:, 0:n * 128], ALU.mult,
                                   ["accf", ek], ["ktl"])
                                pt, pk = nps()
                                for j in range(n):
                                    MM(pt[:, j * 128:(j + 1) * 128], zf[:, t0 + j, :], tri[:], True, True, ["srcf", trik], [pk])
                                e_, ek = ex[0], ("ex", 0)
                                exr += 1
                                ACTV(e_[:, 0:n * 128], pt[:, 0:n * 128], AF.Exp, [pk], [ek])
                                lastc = 127 if d == 0 else 0
                                CP(dec_h[:, d, t0:t0 + n],
                                   e_[:, 0:n * 128].rearrange("p (a b) -> p a b", b=128)[:, :, lastc], [ek], ["dec_h"])
                                for j in range(n):
                                    ti = t0 + j
                                    if ti >= 2:
                                        TT(qtl[:, (ti - 2) * 128:(ti - 1) * 128], hqT[:, ti * 128:(ti + 1) * 128],
                                           e_[:, j * 128:(j + 1) * 128], ALU.mult, ["hqT", ek], ["qtl"])
                            for t0 in range(0, NT, 8):
                                n = min(8, NT - t0)
                                pb_, pbk = npsb()
                                for j in range(n):
                                    TR(pb_[:, j * 128:(j + 1) * 128], ktl[:, t0 + j, :], ident_b[:], ["ktl", "ident_b"], [pbk])
                                CP(ktlT[:, t0 * 128:(t0 + n) * 128], pb_[:, 0:n * 128], [pbk], ["ktlT"], eng=ACT)

                            if h == 0 and b == 0 and d == 0:
                                dump("ktl", ktl[:].rearrange("p a b -> p (a b)"), lambda dd: dd, ["ktl"])
                                dump("qtl", qtl[:], lambda dd: dd, ["qtl"])
                                dump("ktlT", ktlT[:], lambda dd: dd, ["ktlT"])
                                dump("dech", dec_h[:].rearrange("p a b -> p (a b)"), lambda dd: dd, ["dec_h"])
                                dump("hqT", hqT[:], lambda dd: dd, ["hqT"])
                                dump("hitm", hi_tm[:].rearrange("p a b -> p (a b)"), lambda dd: dd, ["hi_tm"])

                            def post(po, pok, c, d=d):
                                if d == 0:
                                    CP(osum[:, c - 2, :], po[:, 0:128], [pok], ["osum"])
                                else:
                                    TT(osum[:, c - 2, :], po[:, 0:128], osum[:, c - 2, :], ALU.add, [pok, "osum"], ["osum"])

                            scan(d, qtl, "qtl", True, ktlT, "ktlT", ktl, "ktl", hi_tm, "hi_tm",
                                 128, lambda c, d=d: dec_h[:, d, c:c + 1], "dec_h", post)
                        if h == 0 and b == 0:
                            dump("osum", osum[:].rearrange("p a b -> p (a b)"), lambda d: d, ["osum"])
                        head_norm_to_T(osum, "osum", hn_bc, "hn_bc", h, g_tm, "g_tm", bT, "bT", tmpf, a_tm)
                P.barrier()
                if stop == "s4":
                    break

            P.barrier()
            with contextlib.ExitStack() as s5:
                G1 = sb("G1", [128, 1024], stack=s5)
                A2 = sb("A2", [128, 1024], stack=s5)
                SH2 = sb("SH2", [128, 1024], stack=s5)
                wpa = sb("wpa", [128, 4, 1024], BF16, stack=s5)
                wpb = sb("wpb", [128, 4, 1024], BF16, stack=s5)
                wout = sb("wout", [128, 8, 1024], BF16, stack=s5)
                wg = [sb(f"wg{i}", [128, 8, 128], BF16, stack=s5) for i in range(2)]
                yT = sb("yT", [128, 8, 512], BF16, stack=s5)
                sga = sb("sga", [128, 512], stack=s5)
                y1 = sb("y1", [128, 512], stack=s5)
                y2 = sb("y2", [128, 512], stack=s5)
                sgb = sga
                yl = sb("yl", [128, 1024], stack=s5)
                sq = sb("sq5", [128, 1024], stack=s5)
                t1 = sq
                xin = [sb(f"xin5{i}", [128, 1024], stack=s5) for i in range(1)]
                x1t = [sb(f"x1t{i}", [128, 1024], stack=s5) for i in range(1)]
                h2b = sb("h2b", [128, 1024], BF16, stack=s5)
                load_bc(G1, b * 6 + 2, "G1")
                load_bc(A2, b * 6 + 4, "A2")
                load_bc(SH2, b * 6 + 3, "SH2")
                load_bc(G2, b * 6 + 5, "G2")
                DMA(POOL, wpa[:], wpa_d, [], ["wpa"])
                DMA(POOL, wpb[:], wpb_d, [], ["wpb"])
                DMA(POOL, wout[:], wout_d, [], ["wout"])
                wgr = 0
                for tg in range(4):
                    tok0 = tg * 512
                    for j in range(8):
                        wga, wgak = wg[0], ("wg", 0)
                        wgb, wgbk = wg[1], ("wg", 1)
                        wgr += 2
                        DMA(POOL, wga[:], win_d[36 + j], [], [wgak])
                        DMA(POOL, wgb[:], win_d[44 + j], [], [wgbk])
                        pa, pak = nps()
                        for k in range(4):
                            MM(pa[:, :], wpa[:, k, j * 128:(j + 1) * 128], aT[:, k, tok0:tok0 + 512], k == 0, k == 3,
                               ["wpa", "aT"], [pak])
                        pga, pgak = nps()
                        for k in range(8):
                            MM(pga[:, :], wga[:, k, :], hT[:, k, 256 + tok0:256 + tok0 + 512], k == 0, k == 7,
                               [wgak] + hTk(256 + tok0, 256 + tok0 + 512), [pgak])
                        ACTV(sga[:], pga[:, :], AF.Sigmoid, [pgak, "bcol"], ["sga"], bias=bcol[:, 36 + j:37 + j])
                        TT(y1[:], pa[:, :], sga[:], ALU.mult, [pak, "sga"], ["y1"])
                        pb2, pb2k = nps()
                        for k in range(4):
                            MM(pb2[:, :], wpb[:, k, j * 128:(j + 1) * 128], bT[:, k, tok0:tok0 + 512], k == 0, k == 3,
                               ["wpb", "bT"], [pb2k])
                        pgb, pgbk = nps()
                        for k in range(8):
                            MM(pgb[:, :], wgb[:, k, :], hT[:, k, 256 + tok0:256 + tok0 + 512], k == 0, k == 7,
                               [wgbk] + hTk(256 + tok0, 256 + tok0 + 512), [pgbk])
                        ACTV(sgb[:], pgb[:, :], AF.Sigmoid, [pgbk, "bcol"], ["sga"], bias=bcol[:, 44 + j:45 + j])
                        TT(y2[:], pb2[:, :], sgb[:], ALU.mult, [pb2k, "sga"], ["y2"])
                        TT(yT[:, j, :], y1[:], y2[:], ALU.add, ["y1", "y2"], [("yT", j)])
                    for tl in range(4):
                        lt = tg * 4 + tl
                        for half in range(2):
                            pt, pk = nps()
                            for k in range(8):
                                MM(pt[:, :], yT[:, k, tl * 128:(tl + 1) * 128], wout[:, k, half * 512:(half + 1) * 512],
                                   k == 0, k == 7, [("yT", k), "wout"], [pk])
                            CP(yl[:, half * 512:(half + 1) * 512], pt[:, :], [pk], ["yl"], eng=ACT)
                        ACTV(sq[:], yl[:], AF.Square, ["yl"], ["sq5"])
                        sc, sk = small[:, 20 + lt:21 + lt], ("small", 20 + lt)
                        RED(sc, sq[:], ["sq5"], [sk])
                        rstd_from_ss(sc, sc, 1.0 / 1024, sk)
                        xi, xk = xin[0], ("xin5", 0)
                        DMA(SP, xi[:], x_d[b, lt * 128:(lt + 1) * 128, :], [], [xk])
                        STT(t1[:], yl[:], sc, G1[:], ALU.mult, ALU.mult, ["yl", sk, "G1"], ["sq5"])
                        xo, xok = x1t[0], ("x1t", 0)
                        TT(xo[:], t1[:], xi[:], ALU.add, ["sq5", xk], [xok])
                        DMA(SP, x1_d[b, lt * 128:(lt + 1) * 128, :], xo[:], [xok], [("x1d", b, lt)])
                        ACTV(sq[:], xo[:], AF.Square, [xok], ["sq5"])
                        sc2, sk2 = small[:, 40 + lt:41 + lt], ("small", 40 + lt)
                        RED(sc2, sq[:], ["sq5"], [sk2])
                        rstd_from_ss(sc2, sc2, 1.0 / 1024, sk2)
                        STT(t1[:], xo[:], sc2, A2[:], ALU.mult, ALU.mult, [xok, sk2, "A2"], ["sq5"])
                        TT(h2b[:], t1[:], SH2[:], ALU.add, ["sq5", "SH2"], ["h2b"])
                        pb_, pbk = npsb()
                        for k in range(8):
                            TR(pb_[:, k * 128:(k + 1) * 128], h2b[:, k * 128:(k + 1) * 128], ident_b[:],
                               ["h2b", "ident_b"], [pbk])
                        CP(h2T[:, :, lt * 128:(lt + 1) * 128], pb_[:].rearrange("p (k t) -> p k t", k=8),
                           [pbk], [("h2T", lt)], eng=ACT)
            mx.close()
            mx = None
            P.barrier()
            if b == 0:
                dump("h2T", h2T[:, 0, :], lambda d: d, kk("h2T", range(16)))
            if stop == "s5":
                break

            with contextlib.ExitStack() as s6:
                g_tm = sb("gr_tm", [128, 16, 32], stack=s6)
                Lt = sb("Lt", [128, 32], stack=s6)
                m8 = sb("m8", [128, 8], stack=s6)
                mk = sb("mk", [128, 32], stack=s6)
                exr_ = sb("exr", [128, 32], stack=s6)
                sc4 = sb("sc4", [128, 4], stack=s6)
                for lt in range(16):
                    pt, pk = nps()
                    for k in range(8):
                        MM(pt[:, 0:32], h2T[:, k, lt * 128:(lt + 1) * 128], wr_sb[:, k, :], k == 0, k == 7,
                           [("h2T", lt), "wr_sb"], [pk])
                    TT(Lt[:], pt[:, 0:32], br_bc[:], ALU.add, [pk, "br_bc"], ["Lt"])
                    P.op(DVE, lambda e: e.max(out=m8[:], in_=Lt[:]), ["Lt"], ["m8"])
                    TS(mk[:], Lt[:], m8[:, 3:4], None, ALU.is_ge, None, ["Lt", "m8"], ["mk"])
                    TS(sc4[:, 0:1], m8[:, 0:1], -1.0, None, ALU.mult, None, ["m8"], ["sc4"])
                    ACTV(exr_[:], Lt[:], AF.Exp, ["Lt", "sc4"], ["exr"], bias=sc4[:, 0:1])
                    TT(exr_[:], exr_[:], mk[:], ALU.mult, ["exr", "mk"], ["exr"])
                    RED(sc4[:, 1:2], exr_[:], ["exr"], ["sc4"])
                    RECIP(sc4[:, 1:2], sc4[:, 1:2], ["sc4"], ["sc4"])
                    TS(g_tm[:, lt, :], exr_[:], sc4[:, 1:2], None, ALU.mult, None, ["exr", "sc4"], ["gr_tm"])
                for t0 in range(0, 16, 4):
                    pt, pk = nps()
                    for j in range(4):
                        MM(pt[0:32, j * 128:(j + 1) * 128], g_tm[:, t0 + j, :], ident_f[:], True, True,
                           ["gr_tm", "ident_f"], [pk])
                    CP(gT_sb[:, t0 * 128:(t0 + 4) * 128], pt[0:32, :], [pk], ["gT_sb"])
            P.barrier()
            if b == 0:
                dump("gT", gT_sb[:], lambda d: d, ["gT_sb"])
            if stop == "s6":
                break

            with contextlib.ExitStack() as s7:
                sel_b = sb("sel_b", [32, 4096], BF16, stack=s7)
                bgu = sb("bgu", [128, 32, 16], stack=s7)
                bdn_b = sb("bdn_b", [32, 1024], BF16, stack=s7)
                DMA(POOL, sel_b[:], sel_d, [], ["sel_b"])
                DMA(SP, bgu[:], bgu_d, [], ["bgu"])
                DMA(POOL, bdn_b[:], bdn_d, [], ["bdn_b"])
                yacc = sb("yacc", [128, 8, 1024], stack=s7)
                wgu = sb("wgu", [128, 8, 2048], BF16, stack=s7)
                wdn = sb("wdn", [128, 8, 1024], BF16, stack=s7)
                act = sb("act", [128, 8, 1024], BF16, stack=s7)
                Gbc = [sb(f"Gbc{i}", [128, 512], stack=s7) for i in range(2)]
                g1 = [sb(f"g1_{i}", [128, 512], stack=s7) for i in range(2)]
                sg = [sb(f"sg_{i}", [128, 512], stack=s7) for i in range(2)]
                u1 = [sb(f"u1_{i}", [128, 512], stack=s7) for i in range(2)]
                ym = sb("ym", [128, 1024], stack=s7)
                sq = sb("sq7", [128, 1024], stack=s7)
                x1r = [sb(f"x1r{i}", [128, 1024], stack=s7) for i in range(2)]
                ot = [sb(f"ot{i}", [128, 1024], stack=s7) for i in range(2)]
                for hp in range(2):
                    tb = hp * 1024
                    for dch in range(8):
                        for tg in range(2):
                            pt, pk = nps()
                            MM(pt[:, :], bdn_b[:, dch * 128:(dch + 1) * 128], gT_sb[:, tb + tg * 512:tb + (tg + 1) * 512],
                               True, True, ["bdn_b", "gT_sb"], [pk])
                            CP(yacc[:, dch, tg * 512:(tg + 1) * 512], pt[:, :], [pk], [("yacc", dch, tg)], eng=ACT)
                    DMA(POOL, wgu[:], wgu_d[0], [], ["wgu"])
                    DMA(POOL, wdn[:], wdn_d[0], [], ["wdn"])
                    rot = 0
                    for e in range(n_exp):
                        for tg in range(2):
                            ts_ = slice(tb + tg * 512, tb + (tg + 1) * 512)
                            h2k = kk("h2T", range((tb + tg * 512) // 128, (tb + (tg + 1) * 512) // 128))
                            pgb_, pgbk = nps()
                            MM(pgb_[:, :], sel_b[:, e * 128:(e + 1) * 128], gT_sb[:, ts_], True, True,
                               ["sel_b", "gT_sb"], [pgbk])
                            CP(Gbc[tg][:], pgb_[:, :], [pgbk], [("Gbc", tg)], eng=ACT)
                            for j in range(8):
                                r_ = rot % 2
                                rot += 1
                                pg, pgk = nps()
                                for k in range(8):
                                    MM(pg[:, :], wgu[:, k, j * 128:(j + 1) * 128], h2T[:, k, ts_], k == 0, k == 7,
                                       ["wgu"] + h2k, [pgk])
                                pu, puk = nps()
                                for k in range(8):
                                    MM(pu[:, :], wgu[:, k, 1024 + j * 128:1024 + (j + 1) * 128], h2T[:, k, ts_],
                                       k == 0, k == 7, ["wgu"] + h2k, [puk])
                                TS(g1[r_][:], pg[:, :], bgu[:, e, j:j + 1], 7.0, ALU.add, ALU.min, [pgk, "bgu"], [("g1", r_)])
                                ACTV(sg[r_][:], g1[r_][:], AF.Sigmoid, [("g1", r_)], [("sg", r_)], scale=1.702)
                                TS(u1[r_][:], pu[:, :], bgu[:, e, 8 + j:9 + j], 7.0, ALU.add, ALU.min, [puk, "bgu"], [("u1", r_)])
                                TS(u1[r_][:], u1[r_][:], -7.0, 1.0, ALU.max, ALU.add, [("u1", r_)], [("u1", r_)])
                                TT(g1[r_][:], g1[r_][:], sg[r_][:], ALU.mult, [("g1", r_), ("sg", r_)], [("g1", r_)])
                                TT(u1[r_][:], u1[r_][:], Gbc[tg][:], ALU.mult, [("u1", r_), ("Gbc", tg)], [("u1", r_)])
                                TT(act[:, j, tg * 512:(tg + 1) * 512], g1[r_][:], u1[r_][:], ALU.mult,
                                   [("g1", r_), ("u1", r_)], [("act", tg)])
                        if e + 1 < n_exp:
                            DMA(POOL, wgu[:], wgu_d[e + 1], [], ["wgu"])
                        for tg in range(2):
                            for dch in range(8):
                                py, pyk = nps()
                                for j in range(8):
                                    MM(py[:, :], wdn[:, j, dch * 128:(dch + 1) * 128], act[:, j, tg * 512:(tg + 1) * 512],
                                       j == 0, j == 7, ["wdn", ("act", tg)], [pyk])
                                TT(yacc[:, dch, tg * 512:(tg + 1) * 512], yacc[:, dch, tg * 512:(tg + 1) * 512], py[:, :],
                                   ALU.add, [pyk, ("yacc", dch, tg)], [("yacc", dch, tg)])
                        if e + 1 < n_exp:
                            DMA(POOL, wdn[:], wdn_d[e + 1], [], ["wdn"])
                    for tl in range(8):
                        lt = hp * 8 + tl
                        tg = tl // 4
                        for half in range(2):
                            pt, pk = nps()
                            for kq in range(4):
                                k = half * 4 + kq
                                MM(pt[:, kq * 128:(kq + 1) * 128], yacc[:, k, tl * 128:(tl + 1) * 128], ident_f[:], True, True,
                                   [("yacc", k, tg), "ident_f"], [pk])
                            CP(ym[:, half * 512:(half + 1) * 512], pt[:, :], [pk], ["ym"], eng=ACT)
                        ACTV(sq[:], ym[:], AF.Square, ["ym"], ["sq7"])
                        sc, sk = small[:, 20 + lt:21 + lt], ("small", 20 + lt)
                        RED(sc, sq[:], ["sq7"], [sk])
                        rstd_from_ss(sc, sc, 1.0 / 1024, sk)
                        xr, xrk = x1r[lt % 2], ("x1r", lt % 2)
                        DMA(SP, xr[:], x1_d[b, lt * 128:(lt + 1) * 128, :], [("x1d", b, lt)], [xrk])
                        STT(sq[:], ym[:], sc, G2[:], ALU.mult, ALU.mult, ["ym", sk, "G2", "sq7"], ["sq7"])
                        o_, ok_ = ot[lt % 2], ("ot", lt % 2)
                        TT(o_[:], sq[:], xr[:], ALU.add, ["sq7", xrk], [ok_])
                        out_toks.append(DMA(SP, out_d[b, lt * 128:(lt + 1) * 128, :], o_[:], [ok_], []))
            P.barrier()

        if mx is not None:
            mx.close()
        P.final_wait(SP, out_toks)
        P.emit()
    return nc


def _c(a):
    return np.ascontiguousarray(a, dtype=np.float32)


def prep_shared(inp):
    w_in = inp["w_in"][0]
    sh = {}
    sh["w_ada"] = _c(inp["w_ada"][0].reshape(8, 128, 12, 512).transpose(2, 1, 0, 3))
    sh["b_ada"] = _c(inp["b_ada"].reshape(1, 6144))
    sh["nrm"] = _c(np.concatenate([inp["norm_mix_pre"][0], inp["norm_mix_post"][0],
                                   inp["norm_ffn_pre"][0], inp["norm_ffn_post"][0]]).reshape(1, 4096))
    sh["w_in"] = _c(w_in[:, :6656].reshape(8, 128, 52, 128).transpose(2, 1, 0, 3))
    sh["w_mg"] = _c(w_in[:, 6656:].reshape(8, 128, 16).transpose(1, 0, 2))
    sh["b_in_col"] = _c(inp["b_in"][0, :6656].reshape(52, 128).T)
    sh["b_in_row"] = _c(inp["b_in"].reshape(1, 6672))
    sh["conv_col"] = _c(inp["conv_w"][0].reshape(9, 8, 128).transpose(2, 1, 0))
    sh["lb_raw"] = _c(inp["lb_raw"].reshape(1, 2048))
    sh["m_norm"] = _c(inp["m_norm"].reshape(1, 512))
    sh["h_norm"] = _c(inp["h_norm"].reshape(1, 512))
    sh["w_pa"] = _c(inp["w_pa"][0].reshape(4, 128, 1024).transpose(1, 0, 2))
    sh["w_pb"] = _c(inp["w_pb"][0].reshape(4, 128, 1024).transpose(1, 0, 2))
    sh["w_out"] = _c(inp["w_out"][0].reshape(8, 128, 1024).transpose(1, 0, 2))
    sh["w_router"] = _c(inp["w_router"][0].reshape(8, 128, 32).transpose(1, 0, 2))
    sh["b_router"] = _c(inp["b_router"].reshape(1, 32))
    sh["w_gu"] = _c(inp["w_gu"][0].reshape(32, 8, 128, 2048).transpose(0, 2, 1, 3))
    sh["b_gu_col"] = _c(inp["b_gu"][0].reshape(32, 16, 128).transpose(2, 0, 1))
    sh["w_dn"] = _c(inp["w_dn"][0].reshape(32, 8, 128, 1024).transpose(0, 2, 1, 3))
    sh["b_dn"] = _c(inp["b_dn"][0])
    sh["ident"] = np.eye(128, dtype=np.float32)
    sh["triF"] = np.triu(np.ones((128, 128), np.float32))
    sh["triB"] = np.tril(np.ones((128, 128), np.float32))
    sh["ones"] = np.ones((128, 128), np.float32)
    sel = np.zeros((32, 32, 128), np.float32)
    for e in range(32):
        sel[e, e, :] = 1.0
    sh["sel"] = sel.reshape(32, 4096)
    return sh


def core_inputs(inp, sh, i):
    m = dict(sh)
    m["x"] = _c(inp["x"][2 * i:2 * i + 2])
    m["ctx"] = _c(inp["ctx"][2 * i:2 * i + 2])
    cv = np.stack([inp["c"][2 * i], inp["c"][2 * i + 1], inp["c_ctx"]])
    m["cvT"] = _c(cv.reshape(3, 8, 128).transpose(2, 0, 1).reshape(128, 24))
    return m


def kernel(**inputs):
    inp = {k: np.asarray(v) for k, v in inputs.items()}
    sh = prep_shared(inp)
    nc = build_nc()
    in_maps = [core_inputs(inp, sh, i) for i in range(N_CORES)]
    res = run_bass_kernel_spmd(nc, in_maps, core_ids=list(range(N_CORES)))
    out = np.concatenate([np.asarray(r["out"], dtype=np.float32) for r in res.results], axis=0)
    return out
```

```python
import contextlib
import numpy as np
import concourse.bass as bass
import concourse.mybir as mybir
from concourse.bass_utils import run_bass_kernel_spmd

F32 = mybir.dt.float32
BF16 = mybir.dt.bfloat16
I32 = mybir.dt.int32
AF = mybir.ActivationFunctionType
ALU = mybir.AluOpType
AX = mybir.AxisListType

PE, DVE, ACT, POOL, SP = "pe", "dve", "act", "pool", "sp"
COMPUTE = (PE, DVE, ACT, POOL)
EPOCH = 12000
N_DMA_SEM = 24
N_EPOCH_SEM = 12
EPS = 1e-6
NT = 18
BLK = 512
NB = 4096 * 4 // BLK + 32
MAXB = 4096 // BLK
TOK = 2304
N_CORES = 8


class Prog:
    def __init__(self, nc, same_engine_sync=True):
        self.nc = nc
        self.same = same_engine_sync
        self.streams = {e: [] for e in (PE, DVE, ACT, POOL, SP)}
        self.count = {e: 0 for e in COMPUTE}
        self.waited = {}
        self.state = {}
        self.dma_tot = [0] * N_DMA_SEM
        self.dma_rr = 0
        self.barrier_toks = set()

    def barrier(self):
        toks = set()
        for e in COMPUTE:
            c = self.count[e]
            if c > 0:
                toks.add(((e, (c - 1) // EPOCH), ((c - 1) % EPOCH) + 1))
        for s_ in range(N_DMA_SEM):
            if self.dma_tot[s_] > 0:
                toks.add((("dma", s_), self.dma_tot[s_]))
        self.barrier_toks = toks

    def _deps(self, reads, writes):
        deps = set()
        for k in reads:
            st = self.state.get(k)
            if st and st[0]:
                deps.add(st[0])
        for k in writes:
            st = self.state.get(k)
            if st:
                if st[0]:
                    deps.add(st[0])
                deps.update(st[1])
        return deps

    def _commit(self, reads, writes, tok):
        for k in writes:
            self.state[k] = [tok, []]
        for k in reads:
            if k in writes:
                continue
            st = self.state.setdefault(k, [None, []])
            st[1].append(tok)
            if len(st[1]) > 64:
                best = {}
                for (key, val) in st[1]:
                    if best.get(key, 0) < val:
                        best[key] = val
                st[1] = list(best.items())

    def _waits(self, eng, deps, own_key=None):
        best = {}
        for (key, val) in deps:
            if key == own_key and not self.same:
                continue
            if self.waited.get((eng, key), 0) >= val:
                continue
            if best.get(key, 0) < val:
                best[key] = val
        out = []
        for key, val in best.items():
            self.waited[(eng, key)] = val
            out.append((key, val))
        return out

    def op(self, eng, fn, reads=(), writes=()):
        reads, writes = tuple(reads), tuple(writes)
        c = self.count[eng]
        own_key = (eng, c // EPOCH)
        deps = self._deps(reads, writes) | self.barrier_toks
        if eng == PE:
            deps = {d for d in deps if d[0][0] != PE}
        waits = self._waits(eng, deps, own_key)
        self.count[eng] = c + 1
        tok = (own_key, (c % EPOCH) + 1)
        self.streams[eng].append(("op", waits, fn, own_key))
        self._commit(reads, writes, tok)

    def dma(self, q, fn, reads=(), writes=()):
        reads, writes = tuple(reads), tuple(writes)
        s = self.dma_rr
        self.dma_rr = (s + 1) % N_DMA_SEM
        key = ("dma", s)
        deps = self._deps(reads, writes) | self.barrier_toks
        if self.dma_tot[s] > 0:
            deps.add((key, self.dma_tot[s]))
        waits = self._waits(q, deps, None)
        self.dma_tot[s] += 16
        tok = (key, self.dma_tot[s])
        self.streams[q].append(("dma", waits, fn, key))
        self._commit(reads, writes, tok)
        return tok

    def final_wait(self, eng, toks):
        waits = self._waits(eng, set(toks), None)
        self.streams[eng].append(("wait", waits, None, None))

    def emit(self):
        nc = self.nc
        with contextlib.ExitStack() as es:
            sems = {}
            for e in COMPUTE:
                nep = self.count[e] // EPOCH + 1
                assert nep <= N_EPOCH_SEM, (e, self.count[e])
                for i in range(nep):
                    sems[(e, i)] = es.enter_context(nc.semaphore(f"s_{e}_{i}"))
            for s in range(N_DMA_SEM):
                sems[("dma", s)] = es.enter_context(nc.semaphore(f"s_dma_{s}"))
            block = es.enter_context(nc.Block())

            def run(eng_name):
                def body(engine):
                    for kind, waits, fn, key in self.streams[eng_name]:
                        for (k, v) in waits:
                            engine.wait_ge(sems[k], v)
                        if kind == "op":
                            fn(engine).then_inc(sems[key], 1)
                        elif kind == "dma":
                            fn(engine).then_inc(sems[key], 16)
                return body

            block.sync(run(SP))
            block.tensor(run(PE))
            block.vector(run(DVE))
            block.scalar(run(ACT))
            block.gpsimd(run(POOL))


def kk(name, idxs):
    return [(name, i) for i in idxs]


def build_nc(nb=2, dumps=(), stop=None, n_exp=32):
    nc = bass.Bass("TRN2", target_bir_lowering=False)
    P = Prog(nc)
    D = {}

    def din(name, shape, dt=F32):
        D[name] = nc.dram_tensor(name, list(shape), dt, kind="ExternalInput").ap()
        return D[name]

    x_d = din("x", [2, 2048, 1024])
    ctx_d = din("ctx", [2, 256, 1024])
    cvT_d = din("cvT", [128, 24])
    wada_d = din("w_ada", [12, 128, 8, 512])
    bada_d = din("b_ada", [1, 6144])
    nrm_d = din("nrm", [1, 4096])
    win_d = din("w_in", [52, 128, 8, 128])
    wmg_d = din("w_mg", [128, 8, 16])
    bcol_d = din("b_in_col", [128, 52])
    brow_d = din("b_in_row", [1, 6672])
    conv_d = din("conv_col", [128, 8, 9])
    lb_d = din("lb_raw", [1, 2048])
    mn_d = din("m_norm", [1, 512])
    hn_d = din("h_norm", [1, 512])
    wpa_d = din("w_pa", [128, 4, 1024])
    wpb_d = din("w_pb", [128, 4, 1024])
    wout_d = din("w_out", [128, 8, 1024])
    wr_d = din("w_router", [128, 8, 32])
    br_d = din("b_router", [1, 32])
    wgu_d = din("w_gu", [32 * 128 * 8, 2048])
    bgu_d = din("b_gu_col", [32 * 128, 16])
    wdn_d = din("w_dn", [32 * 128 * 8, 1024])
    bdn_d = din("b_dn", [32, 1024])
    ident_d = din("ident", [128, 128])
    triF_d = din("triF", [128, 128])
    triB_d = din("triB", [128, 128])
    ones_d = din("ones", [128, 128])
    pidx_d = din("pidx", [128, 1])
    triS_d = din("triS", [128, 128])
    out_d = nc.dram_tensor("out", [2, 2048, 1024], F32, kind="ExternalOutput").ap()
    bc_d = nc.dram_tensor("bc_scr", [14, 128, 1024], F32).ap()
    x1_d = nc.dram_tensor("x1_scr", [2, 2048, 1024], F32).ap()
    h2_d = nc.dram_tensor("h2_scr", [4096, 1024], BF16).ap()
    xs_d = nc.dram_tensor("xs_scr", [NB * BLK, 1024], BF16).ap()
    yb_d = nc.dram_tensor("yb_scr", [NB * BLK, 1024], F32).ap()
    dump_d = {}
    for (nm, shape) in dumps:
        dump_d[nm] = nc.dram_tensor("dbg_" + nm, list(shape), F32, kind="ExternalOutput").ap()
    out_toks = []

    def MM(out, lhsT, rhs, start, stop, reads, writes):
        P.op(PE, lambda e: e.matmul(out, lhsT=lhsT, rhs=rhs, start=start, stop=stop), reads, writes)

    def TR(out, in_, ident, reads, writes):
        P.op(PE, lambda e: e.transpose(out, in_, ident), reads, writes)

    def ACTV(out, in_, func, reads, writes, bias=None, scale=None):
        kw = {}
        if bias is not None:
            kw["bias"] = bias
        if scale is not None:
            kw["scale"] = scale
        P.op(ACT, lambda e: e.activation(out=out, in_=in_, func=func, **kw), reads, writes)

    def TS(out, in0, s1, s2, op0, op1, reads, writes, eng=DVE):
        if s2 is None:
            P.op(eng, lambda e: e.tensor_scalar(out=out, in0=in0, scalar1=s1, scalar2=None, op0=op0), reads, writes)
        else:
            P.op(eng, lambda e: e.tensor_scalar(out=out, in0=in0, scalar1=s1, scalar2=s2, op0=op0, op1=op1),
                 reads, writes)

    def TT(out, in0, in1, op, reads, writes, eng=DVE):
        P.op(eng, lambda e: e.tensor_tensor(out=out, in0=in0, in1=in1, op=op), reads, writes)

    def STT(out, in0, scalar, in1, op0, op1, reads, writes):
        P.op(DVE, lambda e: e.scalar_tensor_tensor(out=out, in0=in0, scalar=scalar, in1=in1, op0=op0, op1=op1),
             reads, writes)

    def CP(out, in_, reads, writes, eng=DVE):
        if eng == ACT:
            P.op(ACT, lambda e: e.activation(out=out, in_=in_, func=AF.Copy), reads, writes)
        else:
            P.op(eng, lambda e: e.tensor_copy(out=out, in_=in_), reads, writes)

    def RED(out, in_, reads, writes):
        P.op(DVE, lambda e: e.tensor_reduce(out=out, in_=in_, axis=AX.X, op=ALU.add), reads, writes)

    def RECIP(out, in_, reads, writes):
        P.op(DVE, lambda e: e.reciprocal(out=out, in_=in_), reads, writes)

    def MSET(ap, val, writes, eng=DVE):
        P.op(eng, lambda e: e.memset(ap, val), (), writes)

    def DMA(q, out, in_, reads, writes):
        return P.dma(q, lambda e: e.dma_start(out=out, in_=in_), reads, writes)

    def bcast(ap1n, n):
        return ap1n.partition_broadcast(128)

    def rstd_from_ss(rs, ss, inv_n, key):
        TS(rs, ss, inv_n, EPS, ALU.mult, ALU.add, [key], [key])
        ACTV(rs, rs, AF.Sqrt, [key], [key])
        RECIP(rs, rs, [key], [key])

    with contextlib.ExitStack() as top:
        uniq = [0]

        def sb(name, shape, dt=F32, stack=top):
            uniq[0] += 1
            return stack.enter_context(nc.sbuf_tensor(f"sb_{name}_{uniq[0]}", list(shape), dt))

        psf = [top.enter_context(nc.psum_tensor(f"psf{i}", [128, 512], F32)) for i in range(6)]
        psb = [top.enter_context(nc.psum_tensor(f"psb{i}", [128, 1024], BF16)) for i in range(2)]
        rr = {"f": 0, "b": 0, "t": 0}

        def nps():
            i = rr["f"]
            rr["f"] = (i + 1) % 6
            return psf[i], ("psf", i)

        def npsb():
            i = rr["b"]
            rr["b"] = (i + 1) % 2
            return psb[i], ("psb", i)

        ident_f = sb("ident_f", [128, 128])
        ident_b = sb("ident_b", [128, 128], BF16)
        triF = sb("triF", [128, 128])
        triB = sb("triB", [128, 128])
        ones_f = sb("ones_f", [128, 128])
        bcol = sb("bcol", [128, 52])
        convc = sb("convc", [128, 8, 9])
        lb_bc = sb("lb_bc", [128, 2, 512])
        oml_bc = sb("oml_bc", [128, 2, 512])
        mn_bc = sb("mn_bc", [128, 512])
        hn_bc = sb("hn_bc", [128, 512])
        br_bc = sb("br_bc", [128, 32])
        wr_sb = sb("wr_sb", [128, 8, 32], BF16)
        wmg_sb = sb("wmg_sb", [128, 8, 16], BF16)
        bmg_bc = sb("bmg_bc", [128, 16])
        DMA(SP, ident_f[:], ident_d, [], ["ident_f"])
        DMA(POOL, ident_b[:], ident_d, [], ["ident_b"])
        DMA(SP, triF[:], triF_d, [], ["triF"])
        DMA(SP, triB[:], triB_d, [], ["triB"])
        DMA(SP, ones_f[:], ones_d, [], ["ones_f"])
        DMA(SP, bcol[:], bcol_d, [], ["bcol"])
        DMA(SP, convc[:], conv_d, [], ["convc"])
        DMA(SP, mn_bc[:], bcast(mn_d, 512), [], ["mn_bc"])
        DMA(SP, hn_bc[:], bcast(hn_d, 512), [], ["hn_bc"])
        DMA(SP, br_bc[:], bcast(br_d, 32), [], ["br_bc"])
        DMA(POOL, wr_sb[:], wr_d, [], ["wr_sb"])
        DMA(POOL, wmg_sb[:], wmg_d, [], ["wmg_sb"])
        DMA(SP, bmg_bc[:], bcast(brow_d[:, 6656:6672], 16), [], ["bmg_bc"])
        lbf = lb_bc[:].rearrange("p a b -> p (a b)")
        omlf = oml_bc[:].rearrange("p a b -> p (a b)")
        with contextlib.ExitStack() as sl:
            lbr = sb("lbr", [128, 2048], stack=sl)
            DMA(SP, lbr[:], bcast(lb_d, 2048), [], ["lbr"])
            TT(lbf, lbr[:, 0:1024], lbr[:, 1024:2048], ALU.subtract, ["lbr"], ["lb_bc"])
            ACTV(lbf, lbf, AF.Sigmoid, ["lb_bc"], ["lb_bc"])
            TS(omlf, lbf, -1.0, 1.0, ALU.mult, ALU.add, ["lb_bc"], ["oml_bc"])
        P.barrier()

        with contextlib.ExitStack() as s0:
            cT = sb("cT", [128, 24], stack=s0)
            sg0 = sb("sg0", [128, 24], stack=s0)
            cb = sb("cb", [128, 24, 128], stack=s0)
            nrm_bc = sb("nrm_bc", [128, 4, 1024], stack=s0)
            wa = [sb(f"wa{i}", [128, 8, 512], stack=s0) for i in range(2)]
            ba = [sb(f"ba{i}", [128, 512], stack=s0) for i in range(2)]
            tA = [sb(f"tA{i}", [128, 512], stack=s0) for i in range(3)]
            tB = [sb(f"tB{i}", [128, 512], stack=s0) for i in range(3)]
            DMA(SP, cT[:], cvT_d, [], ["cT"])
            DMA(SP, nrm_bc[:].rearrange("p a b -> p (a b)"), bcast(nrm_d, 4096), [], ["nrm_bc"])
            ACTV(sg0[:], cT[:], AF.Sigmoid, ["cT"], ["sg0"])
            TT(cT[:], cT[:], sg0[:], ALU.mult, ["cT", "sg0"], ["cT"])
            for i in range(24):
                TS(cb[:, i, :], ones_f[:], cT[:, i:i + 1], None, ALU.mult, None, ["ones_f", "cT"], [("cb", i)])
            ti_rot = 0
            for cbi in range(12):
                w, half = cbi // 2, cbi % 2
                cs = slice(half * 512, (half + 1) * 512)
                wt, bt = wa[cbi % 2], ba[cbi % 2]
                DMA(SP, wt[:], wada_d[cbi], [], [("wa", cbi % 2)])
                DMA(SP, bt[:], bcast(bada_d[:, cbi * 512:(cbi + 1) * 512], 512), [], [("ba", cbi % 2)])
                for r in range(3):
                    if r == 2 and w > 1:
                        continue
                    pt, pk = nps()
                    for k in range(8):
                        MM(pt[:, :], cb[:, r * 8 + k, :], wt[:, k, :], k == 0, k == 7,
                           [("cb", r * 8 + k), ("wa", cbi % 2)], [pk])
                    a_, b_ = tA[ti_rot % 3], tB[ti_rot % 3]
                    ka, kb = ("tA", ti_rot % 3), ("tB", ti_rot % 3)
                    ti_rot += 1
                    TT(a_[:], pt[:, :], bt[:], ALU.add, [pk, ("ba", cbi % 2)], [ka])
                    if w in (1, 4):
                        STT(b_[:], a_[:], 1.0, nrm_bc[:, 0 if w == 1 else 2, cs], ALU.add, ALU.mult,
                            [ka, "nrm_bc"], [kb])
                        src, ksrc = b_, kb
                    elif w in (2, 5):
                        TT(b_[:], a_[:], nrm_bc[:, 1 if w == 2 else 3, cs], ALU.mult, [ka, "nrm_bc"], [kb])
                        src, ksrc = b_, kb
                    else:
                        src, ksrc = a_, ka
                    tidx = r * 6 + w if r < 2 else 12 + w
                    DMA(SP, bc_d[tidx, :, cs], src[:], [ksrc], [("bc", tidx, half)])

        P.barrier()

        def load_bc(dst, tidx, key):
            DMA(SP, dst[:], bc_d[tidx], [("bc", tidx, 0), ("bc", tidx, 1)], [key])

        Ls = sb("Ls", [128, 32, 32])
        m8s = sb("m8s", [128, 32, 8])
        pidx = sb("pidx", [128, 1])
        DMA(SP, pidx[:], pidx_d, [], ["pidx"])
        small = sb("small", [128, 64])

        def hTk(t0, t1):
            return kk("hT", range(t0 // 128, (t1 + 127) // 128))

        def dump(nm, ap_sb, dst, reads):
            if nm in dump_d:
                out_toks.append(DMA(POOL, dst(dump_d[nm]), ap_sb, reads, []))

        mx = None
        for b in range(nb):
            mx = contextlib.ExitStack()
            hT = sb("hT", [128, 8, TOK], BF16, stack=mx)
            aT = sb("aT", [128, 4, 2048], BF16, stack=mx)
            bT = sb("bT", [128, 4, 2048], BF16, stack=mx)
            with contextlib.ExitStack() as s1:
                A1 = sb("A1", [128, 1024], stack=s1)
                SH1 = sb("SH1", [128, 1024], stack=s1)
                A1c = sb("A1c", [128, 1024], stack=s1)
                SH1c = sb("SH1c", [128, 1024], stack=s1)
                xin = [sb(f"xin{i}", [128, 1024], stack=s1) for i in range(2)]
                sq = sb("sq", [128, 1024], stack=s1)
                tmp = sb("tmp1", [128, 1024], stack=s1)
                hb = [sb(f"hb{i}", [128, 1024], BF16, stack=s1) for i in range(2)]
                load_bc(A1, b * 6 + 1, "A1")
                load_bc(SH1, b * 6 + 0, "SH1")
                load_bc(A1c, 13, "A1c")
                load_bc(SH1c, 12, "SH1c")
                for ti in range(NT):
                    xi, xk = xin[ti % 2], ("xin", ti % 2)
                    src = ctx_d[b, ti * 128:(ti + 1) * 128, :] if ti < 2 else x_d[b, (ti - 2) * 128:(ti - 1) * 128, :]
                    DMA(SP, xi[:], src, [], [xk])
                    ACTV(sq[:], xi[:], AF.Square, [xk], ["sq"])
                    sc, sk = small[:, ti:ti + 1], ("small", ti)
                    RED(sc, sq[:], ["sq"], [sk])
                    rstd_from_ss(sc, sc, 1.0 / 1024, sk)
                    Am, Ak, Sm, Sk = (A1c, "A1c", SH1c, "SH1c") if ti < 2 else (A1, "A1", SH1, "SH1")
                    STT(tmp[:], xi[:], sc, Am[:], ALU.mult, ALU.mult, [xk, sk, Ak], ["tmp1"])
                    hbt, hbk = hb[ti % 2], ("hb", ti % 2)
                    TT(hbt[:], tmp[:], Sm[:], ALU.add, ["tmp1", Sk], [hbk])
                    pb_, pbk = npsb()
                    for k in range(8):
                        TR(pb_[:, k * 128:(k + 1) * 128], hbt[:, k * 128:(k + 1) * 128], ident_b[:],
                           [hbk, "ident_b"], [pbk])
                    CP(hT[:, :, ti * 128:(ti + 1) * 128], pb_[:].rearrange("p (k t) -> p k t", k=8),
                       [pbk], [("hT", ti)], eng=ACT)
            P.barrier()
            dump("hT", hT[:, 0, :], lambda d: d, kk("hT", range(NT)))
            if stop == "s1":
                break

            with contextlib.ExitStack() as sm:
                wfm = [sb(f"wfm{i}", [128, 8, 128], BF16, stack=sm) for i in range(2)]
                srcf = sb("srcf", [128, TOK], stack=sm)
                accf = sb("accf", [128, TOK], stack=sm)
                stm = [sb(f"stm{i}", [128, 128], BF16, stack=sm) for i in range(2)]
                Pst = [sb(f"Pst{i}", [128, 129], stack=sm) for i in range(2)]
                Sbf = [sb(f"Sbf{i}", [128, 129], BF16, stack=sm) for i in range(2)]
                ssh = sb("ssh", [128, 16], stack=sm)
                tmpf = sb("tmpf", [128, 16, 128], stack=sm)
                a_tm = sb("a_tm", [128, 16, 128], BF16, stack=sm)

                def proj_fm(chunk, wt, wk, dst, dk):
                    DMA(POOL, wt[:], win_d[chunk], [], [wk])
                    for g0 in range(0, TOK, 512):
                        n = min(512, TOK - g0)
                        pt, pk = nps()
                        for k in range(8):
                            MM(pt[:, 0:n], wt[:, k, :], hT[:, k, g0:g0 + n], k == 0, k == 7,
                               [wk] + hTk(g0, g0 + n), [pk])
                        ACTV(dst[:, g0:g0 + n], pt[:, 0:n], AF.Identity, [pk, "bcol"], [dk],
                             bias=bcol[:, chunk:chunk + 1])

                def scan(d, QT, qk, q_lat_only, KT, ktk, Ktm, ktmk, V, vk, nv, dec_of, deck, post):
                    order = [0, 1] + list(range(2, NT)) if d == 0 else [1, 0] + list(range(NT - 1, 1, -1))
                    mask, mkey = (triF, "triF") if d == 0 else (triB, "triB")
                    Pt, Pk, St, Sk = Pst[d], ("Pst", d), Sbf[d], ("Sbf", d)
                    prev = None
                    for idx, c in enumerate(order):
                        cs = slice(c * 128, (c + 1) * 128)
                        qs = slice((c - 2) * 128, (c - 1) * 128) if q_lat_only else cs
                        if c >= 2:
                            p1, p1k = nps()
                            MM(p1[:, 0:128], KT[:, cs], QT[:, qs], True, True, [ktk, qk], [p1k])
                            sm_, smk = stm[rr["t"] % 2], ("stm", rr["t"] % 2)
                            rr["t"] += 1
                            TT(sm_[:], p1[:, 0:128], mask[:], ALU.mult, [p1k, mkey], [smk])
                            po, pok = nps()
                            MM(po[:, 0:nv], sm_[:], V[:, c, 0:nv], True, prev is None, [smk, vk], [pok])
                            if prev is not None:
                                MM(po[:, 0:nv], QT[:, qs], St[:, 0:nv], False, True, [qk, Sk], [pok])
                            post(po, pok, c)
                        if idx < len(order) - 1:
                            pu, puk = nps()
                            MM(pu[:, 0:nv], Ktm[:, c, :], V[:, c, 0:nv], True, True, [ktmk, vk], [puk])
                            if prev is None:
                                CP(Pt[:, 0:nv], pu[:, 0:nv], [puk], [Pk])
                            else:
                                STT(Pt[:, 0:nv], Pt[:, 0:nv], dec_of(prev), pu[:, 0:nv], ALU.mult, ALU.add,
                                    [Pk, puk, deck], [Pk])
                            ACTV(St[:, 0:nv], Pt[:, 0:nv], AF.Copy, [Pk, deck], [Sk], scale=dec_of(c))
                            prev = c

                def head_norm_to_T(hs, hsk, nbc, nbk, h, gate, gk, dstT, dstk, tmpf, a_tm):
                    ACTV(tmpf[:], hs[:], AF.Square, [hsk], ["hn_tmp"])
                    RED(ssh[:], tmpf[:], ["hn_tmp"], ["ssh"])
                    rstd_from_ss(ssh[:], ssh[:], 1.0 / 128, "ssh")
                    for ti in range(16):
                        STT(tmpf[:, ti, :], hs[:, ti, :], ssh[:, ti:ti + 1], nbc[:, h * 128:(h + 1) * 128],
                            ALU.mult, ALU.mult, [hsk, "ssh", nbk, "hn_tmp"], ["hn_tmp"])
                    TT(a_tm[:], tmpf[:], gate[:], ALU.mult, ["hn_tmp", gk], ["a_tm"])
                    for t0 in (0, 8):
                        pb_, pbk = npsb()
                        for j in range(8):
                            TR(pb_[:, j * 128:(j + 1) * 128], a_tm[:, t0 + j, :], ident_b[:], ["a_tm", "ident_b"], [pbk])
                        CP(dstT[:, h, t0 * 128:(t0 + 8) * 128], pb_[:, :], [pbk], [dstk], eng=ACT)

                s2x = contextlib.ExitStack()
                gates = sb("gates", [128, 16, NT], stack=s2x)
                lf = sb("lf", [128, 8, NT], stack=s2x)
                bcum = sb("bcum", [128, 8, NT], stack=s2x)
                dec_m = sb("dec_m", [128, 8, NT], stack=s2x)
                w_m = sb("w_m", [128, 8, NT], stack=s2x)
                e_m = sb("e_m", [128, 8, NT], stack=s2x)
                for ti in range(NT):
                    pt, pk = nps()
                    for k in range(8):
                        MM(pt[:, 0:16], hT[:, k, ti * 128:(ti + 1) * 128], wmg_sb[:, k, :], k == 0, k == 7,
                           [("hT", ti), "wmg_sb"], [pk])
                    TT(gates[:, :, ti], pt[:, 0:16], bmg_bc[:], ALU.add, [pk, "bmg_bc"], ["gates"])
                ACTV(lf[:], gates[:, 8:16, :], AF.Sigmoid, ["gates"], ["lf"])
                ACTV(lf[:], lf[:], AF.Ln, ["lf"], ["lf"])
                pt, pk = nps()
                MM(pt[:, 0:72], triF[:], lf[:, 0:4, :].rearrange("p a b -> p (a b)"), True, True, ["triF", "lf"], [pk])
                MM(pt[:, 72:144], triB[:], lf[:, 4:8, :].rearrange("p a b -> p (a b)"), True, True,
                   ["triB", "lf"], [pk])
                CP(bcum[:].rearrange("p a b -> p (a b)"), pt[:, 0:144], [pk], ["bcum"])
                pt2, pk2 = nps()
                MM(pt2[:, 0:144], ones_f[:], lf[:].rearrange("p a b -> p (a b)"), True, True, ["ones_f", "lf"], [pk2])
                ACTV(dec_m[:].rearrange("p a b -> p (a b)"), pt2[:, 0:144], AF.Exp, [pk2], ["dec_m"])
                TT(w_m[:], gates[:, 0:8, :], bcum[:], ALU.subtract, ["gates", "bcum"], ["w_m"])
                ACTV(w_m[:], w_m[:], AF.Exp, ["w_m"], ["w_m"])
                ACTV(e_m[:], bcum[:], AF.Exp, ["bcum"], ["e_m"])
                dump("bcum", bcum[:].rearrange("p a b -> p (a b)"), lambda d: d, ["bcum"])

                with contextlib.ExitStack() as s3:
                    qT = sb("qT", [128, TOK], BF16, stack=s3)
                    kT = sb("kT", [128, TOK], BF16, stack=s3)
                    k_tm = sb("k_tm", [128, NT, 128], BF16, stack=s3)
                    wvo = sb("wvo", [128, 8, 256], BF16, stack=s3)
                    bvo = sb("bvo", [128, 256], stack=s3)
                    vp = sb("vp", [128, NT, 129], BF16, stack=s3)
                    v2_ = sb("v2_0", [128, NT, 129], BF16, stack=s3)
                    v2 = [v2_, v2_]
                    og = sb("og", [128, 16, 128], BF16, stack=s3)
                    hsum = sb("hsum", [128, 16, 128], stack=s3)
                    dsc = sb("dsc", [128, 4], stack=s3)
                    MSET(vp[:, :, 128:129], 1.0, ["vp1"])
                    for h in range(4):
                        for which, dst, dkey, scale in ((0, qT, "qT", 128.0 ** -0.5), (1, kT, "kT", 1.0)):
                            chunk = which * 4 + h
                            proj_fm(chunk, wfm[which], ("wfm", which), srcf, "srcf")
                            wc = convc[:, chunk, :]
                            ls = srcf[:, 256:TOK].rearrange("p (r c) -> p r c", c=64)
                            la = accf[:, 256:TOK].rearrange("p (r c) -> p r c", c=64)
                            TS(accf[:], srcf[:], wc[:, 4:5], None, ALU.mult, None, ["srcf", "convc"], ["accf"])
                            for di in (-1, 0, 1):
                                for dj in (-1, 0, 1):
                                    if di == 0 and dj == 0:
                                        continue
                                    tap = (di + 1) * 3 + (dj + 1)
                                    r0, r1 = max(0, -di), 32 - max(0, di)
                                    c0, c1 = max(0, -dj), 64 - max(0, dj)
                                    STT(la[:, r0:r1, c0:c1], ls[:, r0 + di:r1 + di, c0 + dj:c1 + dj], wc[:, tap:tap + 1],
                                        la[:, r0:r1, c0:c1], ALU.mult, ALU.add, ["srcf", "convc", "accf"], ["accf"])
                            STT(accf[:, 1:256], srcf[:, 0:255], wc[:, 3:4], accf[:, 1:256], ALU.mult, ALU.add,
                                ["srcf", "convc", "accf"], ["accf"])
                            STT(accf[:, 0:255], srcf[:, 1:256], wc[:, 5:6], accf[:, 0:255], ALU.mult, ALU.add,
                                ["srcf", "convc", "accf"], ["accf"])
                            ACTV(srcf[:], accf[:], AF.Sigmoid, ["accf"], ["srcf"])
                            STT(dst[:], accf[:], scale, srcf[:], ALU.mult, ALU.mult, ["accf", "srcf"], [dkey])
                        if h == 0 and b == 0:
                            dump("qT", qT[:], lambda d: d, ["qT"])
                            dump("kT", kT[:], lambda d: d, ["kT"])
                        for t0 in range(0, NT, 8):
                            n = min(8, NT - t0)
                            pb_, pbk = npsb()
                            for j in range(n):
                                TR(pb_[:, j * 128:(j + 1) * 128], kT[:, (t0 + j) * 128:(t0 + j + 1) * 128], ident_b[:],
                                   ["kT", "ident_b"], [pbk])
                            CP(k_tm[:, t0:t0 + n, :].rearrange("p a b -> p (a b)"), pb_[:, 0:n * 128], [pbk], ["k_tm"],
                               eng=ACT)
                        DMA(POOL, wvo[:, :, 0:128], win_d[8 + h], [], ["wvo"])
                        DMA(POOL, wvo[:, :, 128:256], win_d[12 + h], [], ["wvo"])
                        DMA(SP, bvo[:, 0:128], bcast(brow_d[:, 1024 + h * 128:1024 + (h + 1) * 128], 128), [], ["bvo"])
                        DMA(SP, bvo[:, 128:256], bcast(brow_d[:, 1536 + h * 128:1536 + (h + 1) * 128], 128), [], ["bvo"])
                        for ti in range(NT):
                            pt, pk = nps()
                            for k in range(8):
                                MM(pt[:, 0:256], hT[:, k, ti * 128:(ti + 1) * 128], wvo[:, k, :], k == 0, k == 7,
                                   [("hT", ti), "wvo"], [pk])
                            TT(vp[:, ti, 0:128], pt[:, 0:128], bvo[:, 0:128], ALU.add, [pk, "bvo"], ["vp"])
                            if ti >= 2:
                                TT(og[:, ti - 2, :], pt[:, 128:256], bvo[:, 128:256], ALU.add, [pk, "bvo"], ["og"])
                        ACTV(og[:], og[:], AF.Sigmoid, ["og"], ["og"])
                        for d in range(2):
                            col = d * 4 + h
                            for ti in range(NT):
                                TS(v2[d][:, ti, :], vp[:, ti, :], w_m[:, col, ti:ti + 1], None, ALU.mult, None,
                                   ["vp", "vp1", "w_m"], ["v2"])

                            def post(po, pok, c, d=d, col=col):
                                ecol = e_m[:, col, c:c + 1]
                                d1, d2 = dsc[:, 2 * d:2 * d + 1], dsc[:, 2 * d + 1:2 * d + 2]
                                dk_ = ("dsc", d)
                                ACTV(d1, po[:, 128:129], AF.Abs, [pok, "e_m"], [dk_], scale=ecol)
                                TS(d1, d1, 1.0, None, ALU.max, None, [dk_], [dk_])
                                RECIP(d1, d1, [dk_], [dk_])
                                TT(d2, d1, ecol, ALU.mult, [dk_, "e_m"], [dk_])
                                if d == 0:
                                    TS(hsum[:, c - 2, :], po[:, 0:128], d2, None, ALU.mult, None, [pok, dk_], ["hsum"])
                                else:
                                    STT(hsum[:, c - 2, :], po[:, 0:128], d2, hsum[:, c - 2, :], ALU.mult, ALU.add,
                                        [pok, dk_, "hsum"], ["hsum"])

                            scan(d, qT, "qT", False, kT, "kT", k_tm, "k_tm", v2[d], "v2", 129,
                                 lambda c, col=col: dec_m[:, col, c:c + 1], "dec_m", post)
                        if h == 0 and b == 0:
                            dump("hsum", hsum[:].rearrange("p a b -> p (a b)"), lambda d: d, ["hsum"])
                        head_norm_to_T(hsum, "hsum", mn_bc, "mn_bc", h, og, "og", aT, "aT", tmpf, a_tm)
                s2x.close()
                P.barrier()
                if stop == "s3":
                    break

                with contextlib.ExitStack() as s4:
                    hqT = sb("hqT", [128, TOK], BF16, stack=s4)
                    w4 = sb("w4", [128, 8, 384], BF16, stack=s4)
                    w4b = sb("w4b", [128, 8, 128], BF16, stack=s4)
                    b4 = sb("b4", [128, 512], stack=s4)
                    hi_tm = sb("hi_tm", [128, NT, 128], BF16, stack=s4)
                    g_tm = sb("g_tm", [128, 16, 128], BF16, stack=s4)
                    zf = srcf[:].rearrange("p (a b) -> p a b", b=128)
                    kkf = accf[:].rearrange("p (a b) -> p a b", b=128)
                    ktl = sb("ktl", [128, NT, 128], BF16, stack=s4)
                    ktlT = sb("ktlT", [128, TOK], BF16, stack=s4)
                    qtl = sb("qtl", [128, 2048], BF16, stack=s4)
                    dec_h = sb("dec_h", [128, 2, NT], stack=s4)
                    ex_ = sb("ex0", [128, 512], stack=s4)
                    ex = [ex_, ex_]
                    osum = sb("osum", [128, 16, 128], stack=s4)
                    exr = 0
                    for h in range(4):
                        proj_fm(16 + h, wfm[0], ("wfm", 0), srcf, "srcf")
                        ACTV(accf[:], srcf[:], AF.Sigmoid, ["srcf"], ["accf"])
                        TT(hqT[:], srcf[:], accf[:], ALU.mult, ["srcf", "accf"], ["hqT"])
                        for j, ch in enumerate((20 + h, 24 + h, 28 + h)):
                            DMA(POOL, w4[:, :, j * 128:(j + 1) * 128], win_d[ch], [], ["w4"])
                        DMA(POOL, w4b[:], win_d[32 + h], [], ["w4b"])
                        for j, ch in enumerate((20 + h, 24 + h, 28 + h, 32 + h)):
                            DMA(SP, b4[:, j * 128:(j + 1) * 128], bcast(brow_d[:, ch * 128:(ch + 1) * 128], 128), [], ["b4"])
                        for d in range(2):
                            hs = slice(h * 128, (h + 1) * 128)
                            for ti in range(NT):
                                pt, pk = nps()
                                if d == 0:
                                    for k in range(8):
                                        MM(pt[:, 0:384], hT[:, k, ti * 128:(ti + 1) * 128], w4[:, k, :], k == 0, k == 7,
                                           [("hT", ti), "w4"], [pk])
                                    TT(hi_tm[:, ti, :], pt[:, 0:128], b4[:, 0:128], ALU.add, [pk, "b4"], ["hi_tm"])
                                    if ti >= 2:
                                        TT(tmpf[:, ti - 2, :], pt[:, 128:256], b4[:, 128:256], ALU.add, [pk, "b4"], ["hn_tmp"])
                                    TT(zf[:, ti, :], pt[:, 256:384], b4[:, 256:384], ALU.add, [pk, "b4"], ["srcf"])
                                else:
                                    for k in range(8):
                                        MM(pt[:, 0:128], hT[:, k, ti * 128:(ti + 1) * 128], w4b[:, k, :], k == 0, k == 7,
                                           [("hT", ti), "w4b"], [pk])
                                    TT(zf[:, ti, :], pt[:, 0:128], b4[:, 384:512], ALU.add, [pk, "b4"], ["srcf"])
                            if d == 0:
                                ACTV(g_tm[:], tmpf[:], AF.Sigmoid, ["hn_tmp"], ["g_tm"])
                                TT(g_tm[:], g_tm[:], tmpf[:], ALU.mult, ["g_tm", "hn_tmp"], ["g_tm"])
                            ACTV(accf[:], srcf[:], AF.Sigmoid, ["srcf"], ["accf"])
                            for ti in range(NT):
                                TT(zf[:, ti, :], kkf[:, ti, :], oml_bc[:, d, hs], ALU.mult, ["accf", "oml_bc", "srcf"], ["srcf"])
                                TT(zf[:, ti, :], zf[:, ti, :], lb_bc[:, d, hs], ALU.add, ["srcf", "lb_bc"], ["srcf"])
                            TS(accf[:], srcf[:], -1.0, 1.0, ALU.mult, ALU.add, ["srcf"], ["accf"])
                            ACTV(srcf[:], srcf[:], AF.Ln, ["srcf"], ["srcf"])
                            tri, trik = (triF, "triF") if d == 0 else (triB, "triB")
                            for t0 in range(0, NT, 4):
                                n = min(4, NT - t0)
                                fs = slice(t0 * 128, (t0 + n) * 128)
                                pt, pk = nps()
                                MM(pt[:, 0:n * 128], tri[:], srcf[:, fs], True, True, [trik, "srcf"], [pk])
                                e_, ek = ex[0], ("ex", 0)
                                exr += 1
                                ACTV(e_[:, 0:n * 128], pt[:, 0:n * 128], AF.Exp, [pk], [ek], scale=-1.0)
                                TT(ktl[:, t0:t0 + n, :].rearrange("p a b -> p (a b)"), accf[:, fs], e_[:, 0:n * 128], ALU.mult,
                                   ["accf", ek], ["ktl"])
                                pt, pk = nps()
                                for j in range(n):
                                    MM(pt[:, j * 128:(j + 1) * 128], zf[:, t0 + j, :], tri[:], True, True, ["srcf", trik], [pk])
                                e_, ek = ex[0], ("ex", 0)
                                exr += 1
                                ACTV(e_[:, 0:n * 128], pt[:, 0:n * 128], AF.Exp, [pk], [ek])
                                lastc = 127 if d == 0 else 0
                                CP(dec_h[:, d, t0:t0 + n],
                                   e_[:, 0:n * 128].rearrange("p (a b) -> p a b", b=128)[:, :, lastc], [ek], ["dec_h"])
                                for j in range(n):
                                    ti = t0 + j
                                    if ti >= 2:
                                        TT(qtl[:, (ti - 2) * 128:(ti - 1) * 128], hqT[:, ti * 128:(ti + 1) * 128],
                                           e_[:, j * 128:(j + 1) * 128], ALU.mult, ["hqT", ek], ["qtl"])
                            for t0 in range(0, NT, 8):
                                n = min(8, NT - t0)
                                pb_, pbk = npsb()
                                for j in range(n):
                                    TR(pb_[:, j * 128:(j + 1) * 128], ktl[:, t0 + j, :], ident_b[:], ["ktl", "ident_b"], [pbk])
                                CP(ktlT[:, t0 * 128:(t0 + n) * 128], pb_[:, 0:n * 128], [pbk], ["ktlT"], eng=ACT)

                            if h == 0 and b == 0 and d == 0:
                                dump("ktl", ktl[:].rearrange("p a b -> p (a b)"), lambda dd: dd, ["ktl"])
                                dump("qtl", qtl[:], lambda dd: dd, ["qtl"])
                                dump("ktlT", ktlT[:], lambda dd: dd, ["ktlT"])
                                dump("dech", dec_h[:].rearrange("p a b -> p (a b)"), lambda dd: dd, ["dec_h"])
                                dump("hqT", hqT[:], lambda dd: dd, ["hqT"])
                                dump("hitm", hi_tm[:].rearrange("p a b -> p (a b)"), lambda dd: dd, ["hi_tm"])

                            def post(po, pok, c, d=d):
                                if d == 0:
                                    CP(osum[:, c - 2, :], po[:, 0:128], [pok], ["osum"])
                                else:
                                    TT(osum[:, c - 2, :], po[:, 0:128], osum[:, c - 2, :], ALU.add, [pok, "osum"], ["osum"])

                            scan(d, qtl, "qtl", True, ktlT, "ktlT", ktl, "ktl", hi_tm, "hi_tm",
                                 128, lambda c, d=d: dec_h[:, d, c:c + 1], "dec_h", post)
                        if h == 0 and b == 0:
                            dump("osum", osum[:].rearrange("p a b -> p (a b)"), lambda d: d, ["osum"])
                        head_norm_to_T(osum, "osum", hn_bc, "hn_bc", h, g_tm, "g_tm", bT, "bT", tmpf, a_tm)
                P.barrier()
                if stop == "s4":
                    break

            P.barrier()
            with contextlib.ExitStack() as s5:
                G1 = sb("G1", [128, 1024], stack=s5)
                A2 = sb("A2", [128, 1024], stack=s5)
                SH2 = sb("SH2", [128, 1024], stack=s5)
                wpa = sb("wpa", [128, 4, 1024], BF16, stack=s5)
                wpb = sb("wpb", [128, 4, 1024], BF16, stack=s5)
                wout = sb("wout", [128, 8, 1024], BF16, stack=s5)
                wg = [sb(f"wg{i}", [128, 8, 128], BF16, stack=s5) for i in range(2)]
                yT = sb("yT", [128, 8, 512], BF16, stack=s5)
                sga = sb("sga", [128, 512], stack=s5)
                y1 = sb("y1", [128, 512], stack=s5)
                y2 = sb("y2", [128, 512], stack=s5)
                sgb = sga
                yl = sb("yl", [128, 1024], stack=s5)
                sq = sb("sq5", [128, 1024], stack=s5)
                t1 = sq
                xin = [sb(f"xin5{i}", [128, 1024], stack=s5) for i in range(1)]
                x1t = [sb(f"x1t{i}", [128, 1024], stack=s5) for i in range(1)]
                h2b = sb("h2b", [128, 1024], BF16, stack=s5)
                h2Tt = sb("h2Tt", [128, 8, 128], BF16, stack=s5)
                load_bc(G1, b * 6 + 2, "G1")
                load_bc(A2, b * 6 + 4, "A2")
                load_bc(SH2, b * 6 + 3, "SH2")
                DMA(POOL, wpa[:], wpa_d, [], ["wpa"])
                DMA(POOL, wpb[:], wpb_d, [], ["wpb"])
                DMA(POOL, wout[:], wout_d, [], ["wout"])
                wgr = 0
                for tg in range(4):
                    tok0 = tg * 512
                    for j in range(8):
                        wga, wgak = wg[0], ("wg", 0)
                        wgb, wgbk = wg[1], ("wg", 1)
                        wgr += 2
                        DMA(POOL, wga[:], win_d[36 + j], [], [wgak])
                        DMA(POOL, wgb[:], win_d[44 + j], [], [wgbk])
                        pa, pak = nps()
                        for k in range(4):
                            MM(pa[:, :], wpa[:, k, j * 128:(j + 1) * 128], aT[:, k, tok0:tok0 + 512], k == 0, k == 3,
                               ["wpa", "aT"], [pak])
                        pga, pgak = nps()
                        for k in range(8):
                            MM(pga[:, :], wga[:, k, :], hT[:, k, 256 + tok0:256 + tok0 + 512], k == 0, k == 7,
                               [wgak] + hTk(256 + tok0, 256 + tok0 + 512), [pgak])
                        ACTV(sga[:], pga[:, :], AF.Sigmoid, [pgak, "bcol"], ["sga"], bias=bcol[:, 36 + j:37 + j])
                        TT(y1[:], pa[:, :], sga[:], ALU.mult, [pak, "sga"], ["y1"])
                        pb2, pb2k = nps()
                        for k in range(4):
                            MM(pb2[:, :], wpb[:, k, j * 128:(j + 1) * 128], bT[:, k, tok0:tok0 + 512], k == 0, k == 3,
                               ["wpb", "bT"], [pb2k])
                        pgb, pgbk = nps()
                        for k in range(8):
                            MM(pgb[:, :], wgb[:, k, :], hT[:, k, 256 + tok0:256 + tok0 + 512], k == 0, k == 7,
                               [wgbk] + hTk(256 + tok0, 256 + tok0 + 512), [pgbk])
                        ACTV(sgb[:], pgb[:, :], AF.Sigmoid, [pgbk, "bcol"], ["sga"], bias=bcol[:, 44 + j:45 + j])
                        TT(y2[:], pb2[:, :], sgb[:], ALU.mult, [pb2k, "sga"], ["y2"])
                        TT(yT[:, j, :], y1[:], y2[:], ALU.add, ["y1", "y2"], [("yT", j)])
                    for tl in range(4):
                        lt = tg * 4 + tl
                        for half in range(2):
                            pt, pk = nps()
                            for k in range(8):
                                MM(pt[:, :], yT[:, k, tl * 128:(tl + 1) * 128], wout[:, k, half * 512:(half + 1) * 512],
                                   k == 0, k == 7, [("yT", k), "wout"], [pk])
                            CP(yl[:, half * 512:(half + 1) * 512], pt[:, :], [pk], ["yl"], eng=ACT)
                        ACTV(sq[:], yl[:], AF.Square, ["yl"], ["sq5"])
                        sc, sk = small[:, 20 + lt:21 + lt], ("small", 20 + lt)
                        RED(sc, sq[:], ["sq5"], [sk])
                        rstd_from_ss(sc, sc, 1.0 / 1024, sk)
                        xi, xk = xin[0], ("xin5", 0)
                        DMA(SP, xi[:], x_d[b, lt * 128:(lt + 1) * 128, :], [], [xk])
                        STT(t1[:], yl[:], sc, G1[:], ALU.mult, ALU.mult, ["yl", sk, "G1"], ["sq5"])
                        xo, xok = x1t[0], ("x1t", 0)
                        TT(xo[:], t1[:], xi[:], ALU.add, ["sq5", xk], [xok])
                        DMA(SP, x1_d[b, lt * 128:(lt + 1) * 128, :], xo[:], [xok], [("x1d", b, lt)])
                        ACTV(sq[:], xo[:], AF.Square, [xok], ["sq5"])
                        sc2, sk2 = small[:, 40 + lt:41 + lt], ("small", 40 + lt)
                        RED(sc2, sq[:], ["sq5"], [sk2])
                        rstd_from_ss(sc2, sc2, 1.0 / 1024, sk2)
                        STT(t1[:], xo[:], sc2, A2[:], ALU.mult, ALU.mult, [xok, sk2, "A2"], ["sq5"])
                        TT(h2b[:], t1[:], SH2[:], ALU.add, ["sq5", "SH2"], ["h2b"])
                        pb_, pbk = npsb()
                        for k in range(8):
                            TR(pb_[:, k * 128:(k + 1) * 128], h2b[:, k * 128:(k + 1) * 128], ident_b[:],
                               ["h2b", "ident_b"], [pbk])
                        gt = b * 16 + lt
                        DMA(SP, h2_d[gt * 128:(gt + 1) * 128, :], h2b[:], ["h2b"], [("h2d", gt)])
                        CP(h2Tt[:], pb_[:].rearrange("p (k t) -> p k t", k=8), [pbk], ["h2Tt"], eng=ACT)
                        pt, pk = nps()
                        for k in range(8):
                            MM(pt[:, 0:32], h2Tt[:, k, :], wr_sb[:, k, :], k == 0, k == 7, ["h2Tt", "wr_sb"], [pk])
                        TT(Ls[:, gt, :], pt[:, 0:32], br_bc[:], ALU.add, [pk, "br_bc"], [("Ls", gt)])
                        P.op(DVE, lambda e, gt=gt: e.max(out=m8s[:, gt, :], in_=Ls[:, gt, :]), [("Ls", gt)], [("m8s", gt)])
            mx.close()
            mx = None
            P.barrier()
            if stop == "s5":
                break

        LsK = kk("Ls", range(32))
        m8K = kk("m8s", range(32))
        desti = sb("desti", [128, 128], I32)
        wk = sb("wk", [128, 32, 4])
        widx = sb("widx", [128, NB], I32)
        widx8 = sb("widx8", [128, 8, NB], I32)
        bidx = sb("bidx", [128, NB], I32)
        if stop in (None, "m1"):
            with contextlib.ExitStack() as m1:
                triS = sb("triS", [128, 128], stack=m1)
                DMA(SP, triS[:], triS_d, [], ["triS"])
                mask = sb("mask", [128, 1024], stack=m1)
                R1 = sb("R1", [128, 1024], stack=m1)
                cA = sb("cA", [128, 1024], stack=m1)
                cB = sb("cB", [128, 1024], stack=m1)
                cnt = sb("cnt", [128, 1024], stack=m1)
                dest = sb("dest", [128, 1024], stack=m1)
                tot = sb("tot", [128, 32], stack=m1)
                nblk = sb("nblk", [128, 32], stack=m1)
                pA = sb("pA", [128, 32], stack=m1)
                pB = sb("pB", [128, 32], stack=m1)
                pstart = sb("pstart", [128, 32], stack=m1)
                junk = sb("junk", [128, 32], stack=m1)
                destk = sb("destk", [128, 128], stack=m1)
                ejf = sb("ejf", [128, NB], stack=m1)
                wif = sb("wif", [128, NB], stack=m1)
                nm = sb("nm", [128, 32], stack=m1)
                exs = sb("exs", [128, 32, 4], stack=m1)
                ssum = sb("ssum", [128, 32], stack=m1)
                for gt in range(32):
                    TS(mask[:, gt * 32:(gt + 1) * 32], Ls[:, gt, :], m8s[:, gt, 3:4], None, ALU.is_ge, None,
                       [("Ls", gt), ("m8s", gt)], ["mask"])
                for hf in range(2):
                    cs = slice(hf * 512, (hf + 1) * 512)
                    pt, pk = nps()
                    MM(pt[:, :], triS[:], mask[:, cs], True, True, ["triS", "mask"], [pk])
                    CP(R1[:, cs], pt[:, :], [pk], ["R1"])
                    pt, pk = nps()
                    MM(pt[:, :], ones_f[:], mask[:, cs], True, True, ["ones_f", "mask"], [pk])
                    CP(cnt[:, cs], pt[:, :], [pk], ["cnt"])
                CP(cA[:], cnt[:], ["cnt"], ["cA"])
                a_, ak, b_, bk = cA, "cA", cB, "cB"
                for sft in (1, 2, 4, 8, 16):
                    w_ = sft * 32
                    CP(b_[:, 0:w_], a_[:, 0:w_], [ak], [bk])
                    TT(b_[:, w_:1024], a_[:, w_:1024], a_[:, 0:1024 - w_], ALU.add, [ak], [bk])
                    a_, ak, b_, bk = b_, bk, a_, ak
                incl, inck = a_, ak
                CP(tot[:], incl[:, 31 * 32:32 * 32], [inck], ["tot"])
                TT(dest[:], incl[:], cnt[:], ALU.subtract, [inck, "cnt"], ["dest"])
                TT(dest[:], dest[:], R1[:], ALU.add, ["dest", "R1"], ["dest"])
                MSET(nblk[:], 0.0, ["nblk"])
                for j in range(MAXB):
                    STT(nblk[:], tot[:], float(j * BLK), nblk[:], ALU.is_gt, ALU.add, ["tot", "nblk"], ["nblk"])
                CP(pA[:], nblk[:], ["nblk"], ["pA"])
                a_, ak, b_, bk = pA, "pA", pB, "pB"
                for sft in (1, 2, 4, 8, 16):
                    CP(b_[:, 0:sft], a_[:, 0:sft], [ak], [bk])
                    TT(b_[:, sft:32], a_[:, sft:32], a_[:, 0:32 - sft], ALU.add, [ak], [bk])
                    a_, ak, b_, bk = b_, bk, a_, ak
                pend, pendk = a_, ak
                TT(pstart[:], pend[:], nblk[:], ALU.subtract, [pendk, "nblk"], ["pstart"])
                TS(pstart[:], pstart[:], float(BLK), None, ALU.mult, None, ["pstart"], ["pstart"])
                for gt in range(32):
                    TT(dest[:, gt * 32:(gt + 1) * 32], dest[:, gt * 32:(gt + 1) * 32], pstart[:], ALU.add,
                       ["dest", "pstart"], ["dest"])
                for gt in range(32):
                    for k in range(4):
                        STT(junk[:], Ls[:, gt, :], m8s[:, gt, k:k + 1], dest[:, gt * 32:(gt + 1) * 32], ALU.is_equal, ALU.mult,
                            [("Ls", gt), ("m8s", gt), "dest"], ["junk"])
                        RED(destk[:, gt * 4 + k:gt * 4 + k + 1], junk[:], ["junk"], ["destk"])
                TS(destk[:], destk[:], 0.0, float(NB * BLK - 1), ALU.max, ALU.min, ["destk"], ["destk"])
                CP(desti[:], destk[:], ["destk"], ["desti"])
                TS(nm[:], m8s[:, :, 0], -1.0, None, ALU.mult, None, m8K, ["nm"])
                for gt in range(32):
                    ACTV(exs[:, gt, :], m8s[:, gt, 0:4], AF.Exp, [("m8s", gt), "nm"], ["exs"], bias=nm[:, gt:gt + 1])
                RED(ssum[:], exs[:], ["exs"], ["ssum"])
                RECIP(ssum[:], ssum[:], ["ssum"], ["ssum"])
                for gt in range(32):
                    TS(wk[:, gt, :], exs[:, gt, :], ssum[:, gt:gt + 1], None, ALU.mult, None, ["exs", "ssum"], ["wk"])
                for j in range(NB):
                    TS(junk[:], pend[:], float(j), None, ALU.is_le, None, [pendk], ["junk"])
                    RED(ejf[:, j:j + 1], junk[:], ["junk"], ["ejf"])
                TS(ejf[:], ejf[:], 31.0, None, ALU.min, None, ["ejf"], ["ejf"])
                CP(bidx[:], ejf[:], ["ejf"], ["bidx"])
                TS(wif[:], ejf[:], 128.0, pidx[:, 0:1], ALU.mult, ALU.add, ["ejf", "pidx"], ["wif"])
                CP(widx[:], wif[:], ["wif"], ["widx"])
                wif8 = sb("wif8", [128, 8, NB], stack=m1)
                for k in range(8):
                    TS(wif8[:, k, :], wif[:], 8.0, float(k), ALU.mult, ALU.add, ["wif"], ["wif8"])
                CP(widx8[:], wif8[:], ["wif8"], ["widx8"])
                dump("destk", destk[:], lambda d: d, ["destk"])
                dump("ejf", ejf[:], lambda d: d, ["ejf"])
                dump("wk", wk[:].rearrange("p a b -> p (a b)"), lambda d: d, ["wk"])
                dump("wif", wif[:], lambda d: d, ["wif"])
                h2t = [sb(f"h2t{i}", [128, 1024], BF16, stack=m1) for i in range(2)]
                for gt in range(32 if stop is None else 0):
                    ht, htk = h2t[gt % 2], ("h2t", gt % 2)
                    DMA(SP, ht[:], h2_d[gt * 128:(gt + 1) * 128, :], [("h2d", gt)], [htk])
                    for k in range(4):
                        c_ = gt * 4 + k
                        P.dma(POOL, lambda e, ht=ht, c_=c_: e.indirect_dma_start(
                            out=xs_d, out_offset=bass.IndirectOffsetOnAxis(ap=desti[:, c_:c_ + 1], axis=0),
                            in_=ht[:], in_offset=None),
                            [htk, "desti"], ["xs"])
            P.barrier()

        if stop is None:
            with contextlib.ExitStack() as m4:
                wgu = [sb(f"wgu{i}", [128, 8, 2048], BF16, stack=m4) for i in range(2)]
                wdn = [sb(f"wdn{i}", [128, 8, 1024], BF16, stack=m4) for i in range(2)]
                bgt = [sb(f"bgt{i}", [128, 16], stack=m4) for i in range(2)]
                bdt = [sb(f"bdt{i}", [128, 1024], stack=m4) for i in range(2)]
                xb = sb("xb", [128, 4, 1024], BF16, stack=m4)
                xT = sb("xT", [128, 8, 512], BF16, stack=m4)
                act = sb("act", [128, 8, 512], BF16, stack=m4)
                g1 = [sb(f"g1_{i}", [128, 512], stack=m4) for i in range(2)]
                sg = [sb(f"sg_{i}", [128, 512], stack=m4) for i in range(2)]
                u1 = [sb(f"u1_{i}", [128, 512], stack=m4) for i in range(2)]
                ybt = [sb(f"ybt{i}", [128, 1024], stack=m4) for i in range(2)]

                def load_w(j):
                    r_ = j % 2
                    ix = widx[:, j:j + 1]
                    for k in range(8):
                        P.dma(POOL, lambda e, k=k: e.indirect_dma_start(
                            out=wgu[r_][:, k, :], out_offset=None, in_=wgu_d,
                            in_offset=bass.IndirectOffsetOnAxis(ap=widx8[:, k, j:j + 1], axis=0)),
                            ["widx8"], [("wgu", r_, k)])
                    for k in range(8):
                        P.dma(POOL, lambda e, k=k: e.indirect_dma_start(
                            out=wdn[r_][:, k, :], out_offset=None, in_=wdn_d,
                            in_offset=bass.IndirectOffsetOnAxis(ap=widx8[:, k, j:j + 1], axis=0)),
                            ["widx8"], [("wdn", r_, k)])
                    P.dma(POOL, lambda e: e.indirect_dma_start(
                        out=bgt[r_][:], out_offset=None, in_=bgu_d,
                        in_offset=bass.IndirectOffsetOnAxis(ap=ix, axis=0)), ["widx"], [("bgt", r_)])
                    P.dma(POOL, lambda e: e.indirect_dma_start(
                        out=bdt[r_][:], out_offset=None, in_=bdn_d,
                        in_offset=bass.IndirectOffsetOnAxis(ap=bidx[:, j:j + 1], axis=0)), ["bidx"], [("bdt", r_)])

                load_w(0)
                rot = 0
                for j in range(NB):
                    r_ = j % 2
                    if j + 1 < NB:
                        load_w(j + 1)
                    DMA(SP, xb[:], xs_d[j * BLK:(j + 1) * BLK, :].rearrange("(a p) n -> p a n", p=128), ["xs"], ["xb"])
                    for a in range(4):
                        pb_, pbk = npsb()
                        for k in range(8):
                            TR(pb_[:, k * 128:(k + 1) * 128], xb[:, a, k * 128:(k + 1) * 128], ident_b[:], ["xb", "ident_b"], [pbk])
                        CP(xT[:, :, a * 128:(a + 1) * 128], pb_[:].rearrange("p (k t) -> p k t", k=8), [pbk], ["xT"],
                           eng=ACT if a % 2 == 0 else DVE)
                    for fb in range(8):
                        q_ = rot % 2
                        rot += 1
                        pg, pgk = nps()
                        for k in range(8):
                            MM(pg[:, :], wgu[r_][:, k, fb * 128:(fb + 1) * 128], xT[:, k, :], k == 0, k == 7,
                               [("wgu", r_, k), "xT"], [pgk])
                        pu, puk = nps()
                        for k in range(8):
                            MM(pu[:, :], wgu[r_][:, k, 1024 + fb * 128:1024 + (fb + 1) * 128], xT[:, k, :], k == 0, k == 7,
                               [("wgu", r_, k), "xT"], [puk])
                        TS(g1[q_][:], pg[:, :], bgt[r_][:, fb:fb + 1], 7.0, ALU.add, ALU.min, [pgk, ("bgt", r_)], [("g1", q_)])
                        ACTV(sg[q_][:], g1[q_][:], AF.Sigmoid, [("g1", q_)], [("sg", q_)], scale=1.702)
                        TS(u1[q_][:], pu[:, :], bgt[r_][:, 8 + fb:9 + fb], 7.0, ALU.add, ALU.min, [puk, ("bgt", r_)], [("u1", q_)])
                        TS(u1[q_][:], u1[q_][:], -7.0, 1.0, ALU.max, ALU.add, [("u1", q_)], [("u1", q_)])
                        TT(g1[q_][:], g1[q_][:], sg[q_][:], ALU.mult, [("g1", q_), ("sg", q_)], [("g1", q_)])
                        TT(act[:, fb, :], g1[q_][:], u1[q_][:], ALU.mult, [("g1", q_), ("u1", q_)], ["act"])
                    for a in range(4):
                        y_, yk = ybt[a % 2], ("ybt", a % 2)
                        for dh in range(2):
                            py, pyk = nps()
                            for fb in range(8):
                                MM(py[:, :], act[:, fb, a * 128:(a + 1) * 128], wdn[r_][:, fb, dh * 512:(dh + 1) * 512],
                                   fb == 0, fb == 7, ["act", ("wdn", r_, fb)], [pyk])
                            TT(y_[:, dh * 512:(dh + 1) * 512], py[:, :], bdt[r_][:, dh * 512:(dh + 1) * 512], ALU.add,
                               [pyk, ("bdt", r_)], [yk])
                        DMA(SP, yb_d[j * BLK + a * 128:j * BLK + (a + 1) * 128, :], y_[:], [yk], ["yb"])
            P.barrier()

            with contextlib.ExitStack() as m5:
                G2 = [sb(f"G2_{i}", [128, 1024], stack=m5) for i in range(2)]
                ybk = [sb(f"ybk{i}", [128, 1024], stack=m5) for i in range(8)]
                ym = sb("ym", [128, 1024], stack=m5)
                sq = sb("sq7", [128, 1024], stack=m5)
                x1r = [sb(f"x1r{i}", [128, 1024], stack=m5) for i in range(2)]
                ot = [sb(f"ot{i}", [128, 1024], stack=m5) for i in range(2)]
                for b in range(nb):
                    load_bc(G2[b], b * 6 + 5, ("G2", b))
                for gt in range(nb * 16):
                    b, lt = gt // 16, gt % 16
                    ys = []
                    for k in range(4):
                        i_ = (gt % 2) * 4 + k
                        c_ = gt * 4 + k
                        P.dma(POOL, lambda e, i_=i_, c_=c_: e.indirect_dma_start(
                            out=ybk[i_][:], out_offset=None, in_=yb_d,
                            in_offset=bass.IndirectOffsetOnAxis(ap=desti[:, c_:c_ + 1], axis=0)),
                            ["yb", "desti"], [("ybk", i_)])
                        ys.append((ybk[i_], ("ybk", i_)))
                    TS(ym[:], ys[0][0][:], wk[:, gt, 0:1], None, ALU.mult, None, [ys[0][1], "wk"], ["ym"])
                    for k in range(1, 4):
                        STT(ym[:], ys[k][0][:], wk[:, gt, k:k + 1], ym[:], ALU.mult, ALU.add, [ys[k][1], "wk", "ym"], ["ym"])
                    ACTV(sq[:], ym[:], AF.Square, ["ym"], ["sq7"])
                    sc, sk = small[:, 20 + lt:21 + lt], ("small", 20 + lt)
                    RED(sc, sq[:], ["sq7"], [sk])
                    rstd_from_ss(sc, sc, 1.0 / 1024, sk)
                    xr, xrk = x1r[gt % 2], ("x1r", gt % 2)
                    DMA(SP, xr[:], x1_d[b, lt * 128:(lt + 1) * 128, :], [("x1d", b, lt)], [xrk])
                    STT(sq[:], ym[:], sc, G2[b][:], ALU.mult, ALU.mult, ["ym", sk, ("G2", b), "sq7"], ["sq7"])
                    o_, ok_ = ot[gt % 2], ("ot", gt % 2)
                    TT(o_[:], sq[:], xr[:], ALU.add, ["sq7", xrk], [ok_])
                    out_toks.append(DMA(SP, out_d[b, lt * 128:(lt + 1) * 128, :], o_[:], [ok_], []))

        if mx is not None:
            mx.close()
        P.final_wait(SP, out_toks)
        P.emit()
    return nc


def _c(a):
    return np.ascontiguousarray(a, dtype=np.float32)


def prep_shared(inp):
    w_in = inp["w_in"][0]
    sh = {}
    sh["w_ada"] = _c(inp["w_ada"][0].reshape(8, 128, 12, 512).transpose(2, 1, 0, 3))
    sh["b_ada"] = _c(inp["b_ada"].reshape(1, 6144))
    sh["nrm"] = _c(np.concatenate([inp["norm_mix_pre"][0], inp["norm_mix_post"][0],
                                   inp["norm_ffn_pre"][0], inp["norm_ffn_post"][0]]).reshape(1, 4096))
    sh["w_in"] = _c(w_in[:, :6656].reshape(8, 128, 52, 128).transpose(2, 1, 0, 3))
    sh["w_mg"] = _c(w_in[:, 6656:].reshape(8, 128, 16).transpose(1, 0, 2))
    sh["b_in_col"] = _c(inp["b_in"][0, :6656].reshape(52, 128).T)
    sh["b_in_row"] = _c(inp["b_in"].reshape(1, 6672))
    sh["conv_col"] = _c(inp["conv_w"][0].reshape(9, 8, 128).transpose(2, 1, 0))
    sh["lb_raw"] = _c(inp["lb_raw"].reshape(1, 2048))
    sh["m_norm"] = _c(inp["m_norm"].reshape(1, 512))
    sh["h_norm"] = _c(inp["h_norm"].reshape(1, 512))
    sh["w_pa"] = _c(inp["w_pa"][0].reshape(4, 128, 1024).transpose(1, 0, 2))
    sh["w_pb"] = _c(inp["w_pb"][0].reshape(4, 128, 1024).transpose(1, 0, 2))
    sh["w_out"] = _c(inp["w_out"][0].reshape(8, 128, 1024).transpose(1, 0, 2))
    sh["w_router"] = _c(inp["w_router"][0].reshape(8, 128, 32).transpose(1, 0, 2))
    sh["b_router"] = _c(inp["b_router"].reshape(1, 32))
    sh["w_gu"] = _c(inp["w_gu"][0].reshape(32, 8, 128, 2048).transpose(0, 2, 1, 3)).reshape(32 * 128 * 8, 2048)
    sh["b_gu_col"] = _c(inp["b_gu"][0].reshape(32, 16, 128).transpose(0, 2, 1)).reshape(32 * 128, 16)
    sh["w_dn"] = _c(inp["w_dn"][0].reshape(32, 8, 128, 1024).transpose(0, 2, 1, 3)).reshape(32 * 128 * 8, 1024)
    sh["b_dn"] = _c(inp["b_dn"][0])
    sh["ident"] = np.eye(128, dtype=np.float32)
    sh["triF"] = np.triu(np.ones((128, 128), np.float32))
    sh["triB"] = np.tril(np.ones((128, 128), np.float32))
    sh["ones"] = np.ones((128, 128), np.float32)
    sh["pidx"] = np.arange(128, dtype=np.float32).reshape(128, 1)
    sh["triS"] = np.triu(np.ones((128, 128), np.float32), 1)
    return sh


def core_inputs(inp, sh, i):
    m = dict(sh)
    m["x"] = _c(inp["x"][2 * i:2 * i + 2])
    m["ctx"] = _c(inp["ctx"][2 * i:2 * i + 2])
    cv = np.stack([inp["c"][2 * i], inp["c"][2 * i + 1], inp["c_ctx"]])
    m["cvT"] = _c(cv.reshape(3, 8, 128).transpose(2, 0, 1).reshape(128, 24))
    return m


def kernel(**inputs):
    inp = {k: np.asarray(v) for k, v in inputs.items()}
    sh = prep_shared(inp)
    nc = build_nc()
    in_maps = [core_inputs(inp, sh, i) for i in range(N_CORES)]
    res = run_bass_kernel_spmd(nc, in_maps, core_ids=list(range(N_CORES)))
    out = np.concatenate([np.asarray(r["out"], dtype=np.float32) for r in res.results], axis=0)
    return out
```

```python
import contextlib
import numpy as np
import concourse.bass as bass
import concourse.mybir as mybir
from concourse.bass_utils import run_bass_kernel_spmd

F32 = mybir.dt.float32
BF16 = mybir.dt.bfloat16
I32 = mybir.dt.int32
AF = mybir.ActivationFunctionType
ALU = mybir.AluOpType
AX = mybir.AxisListType

PE, DVE, ACT, POOL, SP = "pe", "dve", "act", "pool", "sp"
COMPUTE = (PE, DVE, ACT, POOL)
EPOCH = 12000
N_DMA_SEM = 88
DMA_POOLS = {"sp": (0, 32), "act": (32, 8), "pool": (40, 48)}
N_EPOCH_SEM = 12
EPS = 1e-6
NT = 18
BLK = 512
NB = 4096 * 4 // BLK + 32
MAXB = 4096 // BLK
TOK = 2304
N_CORES = 8


class Prog:
    def __init__(self, nc, same_engine_sync=True):
        self.nc = nc
        self.same = same_engine_sync
        self.streams = {e: [] for e in (PE, DVE, ACT, POOL, SP)}
        self.count = {e: 0 for e in COMPUTE}
        self.waited = {}
        self.state = {}
        self.dma_tot = [0] * N_DMA_SEM
        self.dma_rr = {q: 0 for q in DMA_POOLS}
        self.barrier_toks = set()

    def barrier(self):
        toks = set()
        for e in COMPUTE:
            c = self.count[e]
            if c > 0:
                toks.add(((e, (c - 1) // EPOCH), ((c - 1) % EPOCH) + 1))
        for s_ in range(N_DMA_SEM):
            if self.dma_tot[s_] > 0:
                toks.add((("dma", s_), self.dma_tot[s_]))
        self.barrier_toks = toks

    def _deps(self, reads, writes):
        deps = set()
        for k in reads:
            st = self.state.get(k)
            if st and st[0]:
                deps.add(st[0])
        for k in writes:
            st = self.state.get(k)
            if st:
                if st[0]:
                    deps.add(st[0])
                deps.update(st[1])
        return deps

    def _commit(self, reads, writes, tok):
        for k in writes:
            self.state[k] = [tok, []]
        for k in reads:
            if k in writes:
                continue
            st = self.state.setdefault(k, [None, []])
            st[1].append(tok)
            if len(st[1]) > 64:
                best = {}
                for (key, val) in st[1]:
                    if best.get(key, 0) < val:
                        best[key] = val
                st[1] = list(best.items())

    def _waits(self, eng, deps, own_key=None):
        best = {}
        for (key, val) in deps:
            if key == own_key and not self.same:
                continue
            if self.waited.get((eng, key), 0) >= val:
                continue
            if best.get(key, 0) < val:
                best[key] = val
        out = []
        for key, val in best.items():
            self.waited[(eng, key)] = val
            out.append((key, val))
        return out

    def op(self, eng, fn, reads=(), writes=()):
        reads, writes = tuple(reads), tuple(writes)
        c = self.count[eng]
        own_key = (eng, c // EPOCH)
        deps = self._deps(reads, writes) | self.barrier_toks
        if eng == PE:
            deps = {d for d in deps if d[0][0] != PE}
        waits = self._waits(eng, deps, own_key)
        self.count[eng] = c + 1
        tok = (own_key, (c % EPOCH) + 1)
        self.streams[eng].append(("op", waits, fn, own_key))
        self._commit(reads, writes, tok)

    def dma(self, q, fn, reads=(), writes=()):
        reads, writes = tuple(reads), tuple(writes)
        first, cnt_ = DMA_POOLS[q]
        s = first + self.dma_rr[q]
        self.dma_rr[q] = (self.dma_rr[q] + 1) % cnt_
        key = ("dma", s)
        deps = self._deps(reads, writes) | self.barrier_toks
        if self.dma_tot[s] > 0:
            deps.add((key, self.dma_tot[s]))
        waits = self._waits(q, deps, None)
        self.dma_tot[s] += 16
        tok = (key, self.dma_tot[s])
        self.streams[q].append(("dma", waits, fn, key))
        self._commit(reads, writes, tok)
        return tok

    def final_wait(self, eng, toks):
        waits = self._waits(eng, set(toks), None)
        self.streams[eng].append(("wait", waits, None, None))

    def emit(self):
        nc = self.nc
        with contextlib.ExitStack() as es:
            sems = {}
            for e in COMPUTE:
                nep = self.count[e] // EPOCH + 1
                assert nep <= N_EPOCH_SEM, (e, self.count[e])
                for i in range(nep):
                    sems[(e, i)] = es.enter_context(nc.semaphore(f"s_{e}_{i}"))
            for s in range(N_DMA_SEM):
                if self.dma_tot[s] > 0:
                    sems[("dma", s)] = es.enter_context(nc.semaphore(f"s_dma_{s}"))
            block = es.enter_context(nc.Block())

            def run(eng_name):
                def body(engine):
                    for kind, waits, fn, key in self.streams[eng_name]:
                        for (k, v) in waits:
                            engine.wait_ge(sems[k], v)
                        if kind == "op":
                            fn(engine).then_inc(sems[key], 1)
                        elif kind == "dma":
                            fn(engine).then_inc(sems[key], 16)
                return body

            block.sync(run(SP))
            block.tensor(run(PE))
            block.vector(run(DVE))
            block.scalar(run(ACT))
            block.gpsimd(run(POOL))


def kk(name, idxs):
    return [(name, i) for i in idxs]


def build_nc(nb=2, dumps=(), stop=None, n_exp=32):
    nc = bass.Bass("TRN2", target_bir_lowering=False)
    P = Prog(nc)
    D = {}

    def din(name, shape, dt=F32):
        D[name] = nc.dram_tensor(name, list(shape), dt, kind="ExternalInput").ap()
        return D[name]

    x_d = din("x", [2, 2048, 1024])
    ctx_d = din("ctx", [2, 256, 1024])
    cvT_d = din("cvT", [128, 24])
    wada_d = din("w_ada", [12, 128, 8, 512])
    bada_d = din("b_ada", [1, 6144])
    nrm_d = din("nrm", [1, 4096])
    win_d = din("w_in", [52, 128, 8, 128])
    wmg_d = din("w_mg", [128, 8, 16])
    bcol_d = din("b_in_col", [128, 52])
    brow_d = din("b_in_row", [1, 6672])
    conv_d = din("conv_col", [128, 8, 9])
    lb_d = din("lb_raw", [1, 2048])
    mn_d = din("m_norm", [1, 512])
    hn_d = din("h_norm", [1, 512])
    wpa_d = din("w_pa", [128, 4, 1024])
    wpb_d = din("w_pb", [128, 4, 1024])
    wout_d = din("w_out", [128, 8, 1024])
    wr_d = din("w_router", [128, 8, 32])
    br_d = din("b_router", [1, 32])
    wgu_d = din("w_gu", [32 * 128 * 8, 2048])
    bgu_d = din("b_gu_col", [32 * 128, 16])
    wdn_d = din("w_dn", [32 * 128 * 8, 1024])
    bdn_d = din("b_dn", [32, 1024])
    ident_d = din("ident", [128, 128])
    triF_d = din("triF", [128, 128])
    triB_d = din("triB", [128, 128])
    ones_d = din("ones", [128, 128])
    pidx_d = din("pidx", [128, 1])
    triS_d = din("triS", [128, 128])
    out_d = nc.dram_tensor("out", [2, 2048, 1024], F32, kind="ExternalOutput").ap()
    bc_d = nc.dram_tensor("bc_scr", [14, 128, 1024], F32).ap()
    x1_d = nc.dram_tensor("x1_scr", [2, 2048, 1024], F32).ap()
    h2_d = nc.dram_tensor("h2_scr", [4096, 1024], BF16).ap()
    xs_d = nc.dram_tensor("xs_scr", [NB * BLK, 1024], BF16).ap()
    yb_d = nc.dram_tensor("yb_scr", [NB * BLK, 1024], F32).ap()
    dump_d = {}
    for (nm, shape) in dumps:
        dump_d[nm] = nc.dram_tensor("dbg_" + nm, list(shape), F32, kind="ExternalOutput").ap()
    out_toks = []

    def MM(out, lhsT, rhs, start, stop, reads, writes):
        P.op(PE, lambda e: e.matmul(out, lhsT=lhsT, rhs=rhs, start=start, stop=stop), reads, writes)

    def TR(out, in_, ident, reads, writes):
        P.op(PE, lambda e: e.transpose(out, in_, ident), reads, writes)

    def ACTV(out, in_, func, reads, writes, bias=None, scale=None):
        kw = {}
        if bias is not None:
            kw["bias"] = bias
        if scale is not None:
            kw["scale"] = scale
        P.op(ACT, lambda e: e.activation(out=out, in_=in_, func=func, **kw), reads, writes)

    def TS(out, in0, s1, s2, op0, op1, reads, writes, eng=DVE):
        if s2 is None:
            P.op(eng, lambda e: e.tensor_scalar(out=out, in0=in0, scalar1=s1, scalar2=None, op0=op0), reads, writes)
        else:
            P.op(eng, lambda e: e.tensor_scalar(out=out, in0=in0, scalar1=s1, scalar2=s2, op0=op0, op1=op1),
                 reads, writes)

    def TT(out, in0, in1, op, reads, writes, eng=DVE):
        P.op(eng, lambda e: e.tensor_tensor(out=out, in0=in0, in1=in1, op=op), reads, writes)

    def STT(out, in0, scalar, in1, op0, op1, reads, writes):
        P.op(DVE, lambda e: e.scalar_tensor_tensor(out=out, in0=in0, scalar=scalar, in1=in1, op0=op0, op1=op1),
             reads, writes)

    def CP(out, in_, reads, writes, eng=DVE):
        if eng == ACT:
            P.op(ACT, lambda e: e.activation(out=out, in_=in_, func=AF.Copy), reads, writes)
        else:
            P.op(eng, lambda e: e.tensor_copy(out=out, in_=in_), reads, writes)

    def RED(out, in_, reads, writes):
        P.op(DVE, lambda e: e.tensor_reduce(out=out, in_=in_, axis=AX.X, op=ALU.add), reads, writes)

    def RECIP(out, in_, reads, writes):
        P.op(DVE, lambda e: e.reciprocal(out=out, in_=in_), reads, writes)

    def MSET(ap, val, writes, eng=DVE):
        P.op(eng, lambda e: e.memset(ap, val), (), writes)

    def DMA(q, out, in_, reads, writes):
        return P.dma(q, lambda e: e.dma_start(out=out, in_=in_), reads, writes)

    def bcast(ap1n, n):
        return ap1n.partition_broadcast(128)

    def rstd_from_ss(rs, ss, inv_n, key):
        TS(rs, ss, inv_n, EPS, ALU.mult, ALU.add, [key], [key])
        ACTV(rs, rs, AF.Sqrt, [key], [key])
        RECIP(rs, rs, [key], [key])

    with contextlib.ExitStack() as top:
        uniq = [0]

        def sb(name, shape, dt=F32, stack=top):
            uniq[0] += 1
            return stack.enter_context(nc.sbuf_tensor(f"sb_{name}_{uniq[0]}", list(shape), dt))

        psf = [top.enter_context(nc.psum_tensor(f"psf{i}", [128, 512], F32)) for i in range(6)]
        psb = [top.enter_context(nc.psum_tensor(f"psb{i}", [128, 1024], BF16)) for i in range(2)]
        rr = {"f": 0, "b": 0, "t": 0}

        def nps():
            i = rr["f"]
            rr["f"] = (i + 1) % 6
            return psf[i], ("psf", i)

        def npsb():
            i = rr["b"]
            rr["b"] = (i + 1) % 2
            return psb[i], ("psb", i)

        ident_f = sb("ident_f", [128, 128])
        ident_b = sb("ident_b", [128, 128], BF16)
        triF = sb("triF", [128, 128])
        triB = sb("triB", [128, 128])
        ones_f = sb("ones_f", [128, 128])
        bcol = sb("bcol", [128, 52])
        convc = sb("convc", [128, 8, 9])
        lb_bc = sb("lb_bc", [128, 2, 512])
        oml_bc = sb("oml_bc", [128, 2, 512])
        mn_bc = sb("mn_bc", [128, 512])
        hn_bc = sb("hn_bc", [128, 512])
        br_bc = sb("br_bc", [128, 32])
        wr_sb = sb("wr_sb", [128, 8, 32], BF16)
        wmg_sb = sb("wmg_sb", [128, 8, 16], BF16)
        bmg_bc = sb("bmg_bc", [128, 16])
        DMA(SP, ident_f[:], ident_d, [], ["ident_f"])
        DMA(POOL, ident_b[:], ident_d, [], ["ident_b"])
        DMA(SP, triF[:], triF_d, [], ["triF"])
        DMA(SP, triB[:], triB_d, [], ["triB"])
        DMA(SP, ones_f[:], ones_d, [], ["ones_f"])
        DMA(SP, bcol[:], bcol_d, [], ["bcol"])
        DMA(SP, convc[:], conv_d, [], ["convc"])
        DMA(SP, mn_bc[:], bcast(mn_d, 512), [], ["mn_bc"])
        DMA(SP, hn_bc[:], bcast(hn_d, 512), [], ["hn_bc"])
        DMA(SP, br_bc[:], bcast(br_d, 32), [], ["br_bc"])
        DMA(POOL, wr_sb[:], wr_d, [], ["wr_sb"])
        DMA(POOL, wmg_sb[:], wmg_d, [], ["wmg_sb"])
        DMA(SP, bmg_bc[:], bcast(brow_d[:, 6656:6672], 16), [], ["bmg_bc"])
        lbf = lb_bc[:].rearrange("p a b -> p (a b)")
        omlf = oml_bc[:].rearrange("p a b -> p (a b)")
        with contextlib.ExitStack() as sl:
            lbr = sb("lbr", [128, 2048], stack=sl)
            DMA(SP, lbr[:], bcast(lb_d, 2048), [], ["lbr"])
            TT(lbf, lbr[:, 0:1024], lbr[:, 1024:2048], ALU.subtract, ["lbr"], ["lb_bc"])
            ACTV(lbf, lbf, AF.Sigmoid, ["lb_bc"], ["lb_bc"])
            TS(omlf, lbf, -1.0, 1.0, ALU.mult, ALU.add, ["lb_bc"], ["oml_bc"])
        P.barrier()

        with contextlib.ExitStack() as s0:
            cT = sb("cT", [128, 24], stack=s0)
            sg0 = sb("sg0", [128, 24], stack=s0)
            cb = sb("cb", [128, 24, 128], stack=s0)
            nrm_bc = sb("nrm_bc", [128, 4, 1024], stack=s0)
            wa = [sb(f"wa{i}", [128, 8, 512], stack=s0) for i in range(2)]
            ba = [sb(f"ba{i}", [128, 512], stack=s0) for i in range(2)]
            tA = [sb(f"tA{i}", [128, 512], stack=s0) for i in range(3)]
            tB = [sb(f"tB{i}", [128, 512], stack=s0) for i in range(3)]
            DMA(SP, cT[:], cvT_d, [], ["cT"])
            DMA(SP, nrm_bc[:].rearrange("p a b -> p (a b)"), bcast(nrm_d, 4096), [], ["nrm_bc"])
            ACTV(sg0[:], cT[:], AF.Sigmoid, ["cT"], ["sg0"])
            TT(cT[:], cT[:], sg0[:], ALU.mult, ["cT", "sg0"], ["cT"])
            for i in range(24):
                TS(cb[:, i, :], ones_f[:], cT[:, i:i + 1], None, ALU.mult, None, ["ones_f", "cT"], [("cb", i)])
            ti_rot = 0
            for cbi in range(12):
                w, half = cbi // 2, cbi % 2
                cs = slice(half * 512, (half + 1) * 512)
                wt, bt = wa[cbi % 2], ba[cbi % 2]
                DMA(SP, wt[:], wada_d[cbi], [], [("wa", cbi % 2)])
                DMA(SP, bt[:], bcast(bada_d[:, cbi * 512:(cbi + 1) * 512], 512), [], [("ba", cbi % 2)])
                for r in range(3):
                    if r == 2 and w > 1:
                        continue
                    pt, pk = nps()
                    for k in range(8):
                        MM(pt[:, :], cb[:, r * 8 + k, :], wt[:, k, :], k == 0, k == 7,
                           [("cb", r * 8 + k), ("wa", cbi % 2)], [pk])
                    a_, b_ = tA[ti_rot % 3], tB[ti_rot % 3]
                    ka, kb = ("tA", ti_rot % 3), ("tB", ti_rot % 3)
                    ti_rot += 1
                    TT(a_[:], pt[:, :], bt[:], ALU.add, [pk, ("ba", cbi % 2)], [ka])
                    if w in (1, 4):
                        STT(b_[:], a_[:], 1.0, nrm_bc[:, 0 if w == 1 else 2, cs], ALU.add, ALU.mult,
                            [ka, "nrm_bc"], [kb])
                        src, ksrc = b_, kb
                    elif w in (2, 5):
                        TT(b_[:], a_[:], nrm_bc[:, 1 if w == 2 else 3, cs], ALU.mult, [ka, "nrm_bc"], [kb])
                        src, ksrc = b_, kb
                    else:
                        src, ksrc = a_, ka
                    tidx = r * 6 + w if r < 2 else 12 + w
                    DMA(SP, bc_d[tidx, :, cs], src[:], [ksrc], [("bc", tidx, half)])

        P.barrier()

        def load_bc(dst, tidx, key):
            DMA(SP, dst[:], bc_d[tidx], [("bc", tidx, 0), ("bc", tidx, 1)], [key])

        Ls = sb("Ls", [128, 32, 32])
        m8s = sb("m8s", [128, 32, 8])
        pidx = sb("pidx", [128, 1])
        DMA(SP, pidx[:], pidx_d, [], ["pidx"])
        small = sb("small", [128, 64])

        def hTk(t0, t1):
            return kk("hT", range(t0 // 128, (t1 + 127) // 128))

        def dump(nm, ap_sb, dst, reads):
            if nm in dump_d:
                out_toks.append(DMA(POOL, dst(dump_d[nm]), ap_sb, reads, []))

        mx = None
        for b in range(nb):
            mx = contextlib.ExitStack()
            hT = sb("hT", [128, 8, TOK], BF16, stack=mx)
            aT = sb("aT", [128, 4, 2048], BF16, stack=mx)
            bT = sb("bT", [128, 4, 2048], BF16, stack=mx)
            with contextlib.ExitStack() as s1:
                A1 = sb("A1", [128, 1024], stack=s1)
                SH1 = sb("SH1", [128, 1024], stack=s1)
                A1c = sb("A1c", [128, 1024], stack=s1)
                SH1c = sb("SH1c", [128, 1024], stack=s1)
                xin = [sb(f"xin{i}", [128, 1024], stack=s1) for i in range(2)]
                sq = sb("sq", [128, 1024], stack=s1)
                tmp = sb("tmp1", [128, 1024], stack=s1)
                hb = [sb(f"hb{i}", [128, 1024], BF16, stack=s1) for i in range(2)]
                load_bc(A1, b * 6 + 1, "A1")
                load_bc(SH1, b * 6 + 0, "SH1")
                load_bc(A1c, 13, "A1c")
                load_bc(SH1c, 12, "SH1c")
                for ti in range(NT):
                    xi, xk = xin[ti % 2], ("xin", ti % 2)
                    src = ctx_d[b, ti * 128:(ti + 1) * 128, :] if ti < 2 else x_d[b, (ti - 2) * 128:(ti - 1) * 128, :]
                    DMA(SP, xi[:], src, [], [xk])
                    ACTV(sq[:], xi[:], AF.Square, [xk], ["sq"])
                    sc, sk = small[:, ti:ti + 1], ("small", ti)
                    RED(sc, sq[:], ["sq"], [sk])
                    rstd_from_ss(sc, sc, 1.0 / 1024, sk)
                    Am, Ak, Sm, Sk = (A1c, "A1c", SH1c, "SH1c") if ti < 2 else (A1, "A1", SH1, "SH1")
                    STT(tmp[:], xi[:], sc, Am[:], ALU.mult, ALU.mult, [xk, sk, Ak], ["tmp1"])
                    hbt, hbk = hb[ti % 2], ("hb", ti % 2)
                    TT(hbt[:], tmp[:], Sm[:], ALU.add, ["tmp1", Sk], [hbk])
                    pb_, pbk = npsb()
                    for k in range(8):
                        TR(pb_[:, k * 128:(k + 1) * 128], hbt[:, k * 128:(k + 1) * 128], ident_b[:],
                           [hbk, "ident_b"], [pbk])
                    CP(hT[:, :, ti * 128:(ti + 1) * 128], pb_[:].rearrange("p (k t) -> p k t", k=8),
                       [pbk], [("hT", ti)], eng=ACT)
            P.barrier()
            dump("hT", hT[:, 0, :], lambda d: d, kk("hT", range(NT)))
            if stop == "s1":
                break

            with contextlib.ExitStack() as sm:
                wfm = [sb(f"wfm{i}", [128, 8, 128], BF16, stack=sm) for i in range(2)]
                srcf = sb("srcf", [128, TOK], stack=sm)
                accf = sb("accf", [128, TOK], stack=sm)
                stm = [sb(f"stm{i}", [128, 128], BF16, stack=sm) for i in range(2)]
                Pst = [sb(f"Pst{i}", [128, 129], stack=sm) for i in range(2)]
                Sbf = [sb(f"Sbf{i}", [128, 129], BF16, stack=sm) for i in range(2)]
                ssh = sb("ssh", [128, 16], stack=sm)
                tmpf = sb("tmpf", [128, 16, 128], stack=sm)
                a_tm = sb("a_tm", [128, 16, 128], BF16, stack=sm)

                def proj_fm(chunk, wt, wk, dst, dk):
                    DMA(POOL, wt[:], win_d[chunk], [], [wk])
                    for g0 in range(0, TOK, 512):
                        n = min(512, TOK - g0)
                        pt, pk = nps()
                        for k in range(8):
                            MM(pt[:, 0:n], wt[:, k, :], hT[:, k, g0:g0 + n], k == 0, k == 7,
                               [wk] + hTk(g0, g0 + n), [pk])
                        ACTV(dst[:, g0:g0 + n], pt[:, 0:n], AF.Identity, [pk, "bcol"], [dk],
                             bias=bcol[:, chunk:chunk + 1])

                def scan(d, QT, qk, q_lat_only, KT, ktk, Ktm, ktmk, V, vk, nv, dec_of, deck, post):
                    order = [0, 1] + list(range(2, NT)) if d == 0 else [1, 0] + list(range(NT - 1, 1, -1))
                    mask, mkey = (triF, "triF") if d == 0 else (triB, "triB")
                    Pt, Pk, St, Sk = Pst[d], ("Pst", d), Sbf[d], ("Sbf", d)
                    prev = None
                    for idx, c in enumerate(order):
                        cs = slice(c * 128, (c + 1) * 128)
                        qs = slice((c - 2) * 128, (c - 1) * 128) if q_lat_only else cs
                        if c >= 2:
                            p1, p1k = nps()
                            MM(p1[:, 0:128], KT[:, cs], QT[:, qs], True, True, [ktk, qk], [p1k])
                            sm_, smk = stm[rr["t"] % 2], ("stm", rr["t"] % 2)
                            rr["t"] += 1
                            TT(sm_[:], p1[:, 0:128], mask[:], ALU.mult, [p1k, mkey], [smk])
                            po, pok = nps()
                            MM(po[:, 0:nv], sm_[:], V[:, c, 0:nv], True, prev is None, [smk, vk], [pok])
                            if prev is not None:
                                MM(po[:, 0:nv], QT[:, qs], St[:, 0:nv], False, True, [qk, Sk], [pok])
                            post(po, pok, c)
                        if idx < len(order) - 1:
                            pu, puk = nps()
                            MM(pu[:, 0:nv], Ktm[:, c, :], V[:, c, 0:nv], True, True, [ktmk, vk], [puk])
                            if prev is None:
                                CP(Pt[:, 0:nv], pu[:, 0:nv], [puk], [Pk])
                            else:
                                STT(Pt[:, 0:nv], Pt[:, 0:nv], dec_of(prev), pu[:, 0:nv], ALU.mult, ALU.add,
                                    [Pk, puk, deck], [Pk])
                            TS(St[:, 0:nv], Pt[:, 0:nv], dec_of(c), None, ALU.mult, None, [Pk, deck], [Sk])
                            prev = c
                        yield

                def head_norm_to_T(hs, hsk, nbc, nbk, h, gate, gk, dstT, dstk, tmpf, a_tm):
                    ACTV(tmpf[:], hs[:], AF.Square, list(hsk), ["hn_tmp"])
                    RED(ssh[:], tmpf[:], ["hn_tmp"], ["ssh"])
                    rstd_from_ss(ssh[:], ssh[:], 1.0 / 128, "ssh")
                    for ti in range(16):
                        STT(tmpf[:, ti, :], hs[:, ti, :], ssh[:, ti:ti + 1], nbc[:, h * 128:(h + 1) * 128],
                            ALU.mult, ALU.mult, list(hsk) + ["ssh", nbk, "hn_tmp"], ["hn_tmp"])
                    TT(a_tm[:], tmpf[:], gate[:], ALU.mult, ["hn_tmp", gk], ["a_tm"])
                    for t0 in (0, 8):
                        pb_, pbk = npsb()
                        for j in range(8):
                            TR(pb_[:, j * 128:(j + 1) * 128], a_tm[:, t0 + j, :], ident_b[:], ["a_tm", "ident_b"], [pbk])
                        CP(dstT[:, h, t0 * 128:(t0 + 8) * 128], pb_[:, :], [pbk], [dstk], eng=ACT)

                s2x = contextlib.ExitStack()
                gates = sb("gates", [128, 16, NT], stack=s2x)
                lf = sb("lf", [128, 8, NT], stack=s2x)
                bcum = sb("bcum", [128, 8, NT], stack=s2x)
                dec_m = sb("dec_m", [128, 8, NT], stack=s2x)
                w_m = sb("w_m", [128, 8, NT], stack=s2x)
                e_m = sb("e_m", [128, 8, NT], stack=s2x)
                for ti in range(NT):
                    pt, pk = nps()
                    for k in range(8):
                        MM(pt[:, 0:16], hT[:, k, ti * 128:(ti + 1) * 128], wmg_sb[:, k, :], k == 0, k == 7,
                           [("hT", ti), "wmg_sb"], [pk])
                    TT(gates[:, :, ti], pt[:, 0:16], bmg_bc[:], ALU.add, [pk, "bmg_bc"], ["gates"])
                ACTV(lf[:], gates[:, 8:16, :], AF.Sigmoid, ["gates"], ["lf"])
                ACTV(lf[:], lf[:], AF.Ln, ["lf"], ["lf"])
                pt, pk = nps()
                MM(pt[:, 0:72], triF[:], lf[:, 0:4, :].rearrange("p a b -> p (a b)"), True, True, ["triF", "lf"], [pk])
                MM(pt[:, 72:144], triB[:], lf[:, 4:8, :].rearrange("p a b -> p (a b)"), True, True,
                   ["triB", "lf"], [pk])
                CP(bcum[:].rearrange("p a b -> p (a b)"), pt[:, 0:144], [pk], ["bcum"])
                pt2, pk2 = nps()
                MM(pt2[:, 0:144], ones_f[:], lf[:].rearrange("p a b -> p (a b)"), True, True, ["ones_f", "lf"], [pk2])
                ACTV(dec_m[:].rearrange("p a b -> p (a b)"), pt2[:, 0:144], AF.Exp, [pk2], ["dec_m"])
                TT(w_m[:], gates[:, 0:8, :], bcum[:], ALU.subtract, ["gates", "bcum"], ["w_m"])
                ACTV(w_m[:], w_m[:], AF.Exp, ["w_m"], ["w_m"])
                ACTV(e_m[:], bcum[:], AF.Exp, ["bcum"], ["e_m"])
                dump("bcum", bcum[:].rearrange("p a b -> p (a b)"), lambda d: d, ["bcum"])

                with contextlib.ExitStack() as s3:
                    qT = sb("qT", [128, TOK], BF16, stack=s3)
                    kT = sb("kT", [128, TOK], BF16, stack=s3)
                    k_tm = sb("k_tm", [128, NT, 128], BF16, stack=s3)
                    wvo = sb("wvo", [128, 8, 256], BF16, stack=s3)
                    bvo = sb("bvo", [128, 256], stack=s3)
                    vp = sb("vp", [128, NT, 129], BF16, stack=s3)
                    v2_ = sb("v2_0", [128, NT, 129], BF16, stack=s3)
                    v2 = [v2_, v2_]
                    og = sb("og", [128, 16, 128], BF16, stack=s3)
                    hsum = sb("hsum", [128, 16, 128], stack=s3)
                    dsc = sb("dsc", [128, 4], stack=s3)
                    for h in range(4):
                        for which, dst, dkey, scale in ((0, qT, "qT", 128.0 ** -0.5), (1, kT, "kT", 1.0)):
                            chunk = which * 4 + h
                            proj_fm(chunk, wfm[which], ("wfm", which), srcf, "srcf")
                            wc = convc[:, chunk, :]
                            ls = srcf[:, 256:TOK].rearrange("p (r c) -> p r c", c=64)
                            la = accf[:, 256:TOK].rearrange("p (r c) -> p r c", c=64)
                            TS(accf[:], srcf[:], wc[:, 4:5], None, ALU.mult, None, ["srcf", "convc"], ["accf"])
                            for di in (-1, 0, 1):
                                for dj in (-1, 0, 1):
                                    if di == 0 and dj == 0:
                                        continue
                                    tap = (di + 1) * 3 + (dj + 1)
                                    r0, r1 = max(0, -di), 32 - max(0, di)
                                    c0, c1 = max(0, -dj), 64 - max(0, dj)
                                    STT(la[:, r0:r1, c0:c1], ls[:, r0 + di:r1 + di, c0 + dj:c1 + dj], wc[:, tap:tap + 1],
                                        la[:, r0:r1, c0:c1], ALU.mult, ALU.add, ["srcf", "convc", "accf"], ["accf"])
                            STT(accf[:, 1:256], srcf[:, 0:255], wc[:, 3:4], accf[:, 1:256], ALU.mult, ALU.add,
                                ["srcf", "convc", "accf"], ["accf"])
                            STT(accf[:, 0:255], srcf[:, 1:256], wc[:, 5:6], accf[:, 0:255], ALU.mult, ALU.add,
                                ["srcf", "convc", "accf"], ["accf"])
                            ACTV(srcf[:], accf[:], AF.Sigmoid, ["accf"], ["srcf"])
                            STT(dst[:], accf[:], scale, srcf[:], ALU.mult, ALU.mult, ["accf", "srcf"], [dkey])
                        if h == 0 and b == 0:
                            dump("qT", qT[:], lambda d: d, ["qT"])
                            dump("kT", kT[:], lambda d: d, ["kT"])
                        for t0 in range(0, NT, 8):
                            n = min(8, NT - t0)
                            pb_, pbk = npsb()
                            for j in range(n):
                                TR(pb_[:, j * 128:(j + 1) * 128], kT[:, (t0 + j) * 128:(t0 + j + 1) * 128], ident_b[:],
                                   ["kT", "ident_b"], [pbk])
                            CP(k_tm[:, t0:t0 + n, :].rearrange("p a b -> p (a b)"), pb_[:, 0:n * 128], [pbk], ["k_tm"],
                               eng=ACT)
                        DMA(POOL, wvo[:, :, 0:128], win_d[8 + h], [], ["wvo"])
                        DMA(POOL, wvo[:, :, 128:256], win_d[12 + h], [], ["wvo"])
                        DMA(SP, bvo[:, 0:128], bcast(brow_d[:, 1024 + h * 128:1024 + (h + 1) * 128], 128), [], ["bvo"])
                        DMA(SP, bvo[:, 128:256], bcast(brow_d[:, 1536 + h * 128:1536 + (h + 1) * 128], 128), [], ["bvo"])
                        MSET(vp[:, :, 128:129], 1.0, ["vp"])
                        for ti in range(NT):
                            pt, pk = nps()
                            for k in range(8):
                                MM(pt[:, 0:256], hT[:, k, ti * 128:(ti + 1) * 128], wvo[:, k, :], k == 0, k == 7,
                                   [("hT", ti), "wvo"], [pk])
                            TT(vp[:, ti, 0:128], pt[:, 0:128], bvo[:, 0:128], ALU.add, [pk, "bvo"], ["vp"])
                            if ti >= 2:
                                TT(og[:, ti - 2, :], pt[:, 128:256], bvo[:, 128:256], ALU.add, [pk, "bvo"], ["og"])
                        ACTV(og[:], og[:], AF.Sigmoid, ["og"], ["og"])
                        gens = []
                        for d in range(2):
                            col = d * 4 + h
                            vd, vdk = (v2_, "v2") if d == 0 else (vp, "vp")
                            for ti in range(NT):
                                TS(vd[:, ti, :], vp[:, ti, :], w_m[:, col, ti:ti + 1], None, ALU.mult, None,
                                   ["vp", "w_m"], [vdk])

                            def post(po, pok, c, d=d, col=col):
                                ecol = e_m[:, col, c:c + 1]
                                d1, d2 = dsc[:, 2 * d:2 * d + 1], dsc[:, 2 * d + 1:2 * d + 2]
                                dk_ = ("dsc", d)
                                ACTV(d1, po[:, 128:129], AF.Abs, [pok, "e_m"], [dk_], scale=ecol)
                                TS(d1, d1, 1.0, None, ALU.max, None, [dk_], [dk_])
                                RECIP(d1, d1, [dk_], [dk_])
                                TT(d2, d1, ecol, ALU.mult, [dk_, "e_m"], [dk_])
                                if (c <= 9) == (d == 0):
                                    TS(hsum[:, c - 2, :], po[:, 0:128], d2, None, ALU.mult, None, [pok, dk_], [("hsum", c)])
                                else:
                                    STT(hsum[:, c - 2, :], po[:, 0:128], d2, hsum[:, c - 2, :], ALU.mult, ALU.add,
                                        [pok, dk_, ("hsum", c)], [("hsum", c)])

                            gens.append(scan(d, qT, "qT", False, kT, "kT", k_tm, "k_tm", vd, vdk, 129,
                                             lambda c, col=col: dec_m[:, col, c:c + 1], "dec_m", post))
                        while gens:
                            for g_ in list(gens):
                                try:
                                    next(g_)
                                except StopIteration:
                                    gens.remove(g_)
                        if h == 0 and b == 0:
                            dump("hsum", hsum[:].rearrange("p a b -> p (a b)"), lambda d: d, kk("hsum", range(2, NT)))
                        head_norm_to_T(hsum, kk("hsum", range(2, NT)), mn_bc, "mn_bc", h, og, "og", aT, "aT", tmpf, a_tm)
                s2x.close()
                P.barrier()
                if stop == "s3":
                    break

                with contextlib.ExitStack() as s4:
                    hqT = sb("hqT", [128, TOK], BF16, stack=s4)
                    w4 = sb("w4", [128, 8, 384], BF16, stack=s4)
                    w4b = sb("w4b", [128, 8, 128], BF16, stack=s4)
                    b4 = sb("b4", [128, 512], stack=s4)
                    hi_tm = sb("hi_tm", [128, NT, 128], BF16, stack=s4)
                    g_tm = sb("g_tm", [128, 16, 128], BF16, stack=s4)
                    zf = srcf[:].rearrange("p (a b) -> p a b", b=128)
                    kkf = accf[:].rearrange("p (a b) -> p a b", b=128)
                    ktl = sb("ktl", [128, NT, 128], BF16, stack=s4)
                    ktlT = sb("ktlT", [128, TOK], BF16, stack=s4)
                    qtl = sb("qtl", [128, 2048], BF16, stack=s4)
                    dec_h = sb("dec_h", [128, 2, NT], stack=s4)
                    ex_ = sb("ex0", [128, 512], stack=s4)
                    ex = [ex_, ex_]
                    osum = sb("osum", [128, 16, 128], stack=s4)
                    exr = 0
                    for h in range(4):
                        proj_fm(16 + h, wfm[0], ("wfm", 0), srcf, "srcf")
                        ACTV(accf[:], srcf[:], AF.Sigmoid, ["srcf"], ["accf"])
                        TT(hqT[:], srcf[:], accf[:], ALU.mult, ["srcf", "accf"], ["hqT"])
                        for j, ch in enumerate((20 + h, 24 + h, 28 + h)):
                            DMA(POOL, w4[:, :, j * 128:(j + 1) * 128], win_d[ch], [], ["w4"])
                        DMA(POOL, w4b[:], win_d[32 + h], [], ["w4b"])
                        for j, ch in enumerate((20 + h, 24 + h, 28 + h, 32 + h)):
                            DMA(SP, b4[:, j * 128:(j + 1) * 128], bcast(brow_d[:, ch * 128:(ch + 1) * 128], 128), [], ["b4"])
                        for d in range(2):
                            hs = slice(h * 128, (h + 1) * 128)
                            for ti in range(NT):
                                pt, pk = nps()
                                if d == 0:
                                    for k in range(8):
                                        MM(pt[:, 0:384], hT[:, k, ti * 128:(ti + 1) * 128], w4[:, k, :], k == 0, k == 7,
                                           [("hT", ti), "w4"], [pk])
                                    TT(hi_tm[:, ti, :], pt[:, 0:128], b4[:, 0:128], ALU.add, [pk, "b4"], ["hi_tm"])
                                    if ti >= 2:
                                        TT(tmpf[:, ti - 2, :], pt[:, 128:256], b4[:, 128:256], ALU.add, [pk, "b4"], ["hn_tmp"])
                                    TT(zf[:, ti, :], pt[:, 256:384], b4[:, 256:384], ALU.add, [pk, "b4"], ["srcf"])
                                else:
                                    for k in range(8):
                                        MM(pt[:, 0:128], hT[:, k, ti * 128:(ti + 1) * 128], w4b[:, k, :], k == 0, k == 7,
                                           [("hT", ti), "w4b"], [pk])
                                    TT(zf[:, ti, :], pt[:, 0:128], b4[:, 384:512], ALU.add, [pk, "b4"], ["srcf"])
                            if d == 0:
                                ACTV(g_tm[:], tmpf[:], AF.Sigmoid, ["hn_tmp"], ["g_tm"])
                                TT(g_tm[:], g_tm[:], tmpf[:], ALU.mult, ["g_tm", "hn_tmp"], ["g_tm"])
                            ACTV(accf[:], srcf[:], AF.Sigmoid, ["srcf"], ["accf"])
                            for ti in range(NT):
                                TT(zf[:, ti, :], kkf[:, ti, :], oml_bc[:, d, hs], ALU.mult, ["accf", "oml_bc", "srcf"], ["srcf"])
                                TT(zf[:, ti, :], zf[:, ti, :], lb_bc[:, d, hs], ALU.add, ["srcf", "lb_bc"], ["srcf"])
                            TS(accf[:], srcf[:], -1.0, 1.0, ALU.mult, ALU.add, ["srcf"], ["accf"])
                            ACTV(srcf[:], srcf[:], AF.Ln, ["srcf"], ["srcf"])
                            tri, trik = (triF, "triF") if d == 0 else (triB, "triB")
                            for t0 in range(0, NT, 4):
                                n = min(4, NT - t0)
                                fs = slice(t0 * 128, (t0 + n) * 128)
                                pt, pk = nps()
                                MM(pt[:, 0:n * 128], tri[:], srcf[:, fs], True, True, [trik, "srcf"], [pk])
                                e_, ek = ex[0], ("ex", 0)
                                exr += 1
                                ACTV(e_[:, 0:n * 128], pt[:, 0:n * 128], AF.Exp, [pk], [ek], scale=-1.0)
                                TT(ktl[:, t0:t0 + n, :].rearrange("p a b -> p (a b)"), accf[:, fs], e_[:, 0:n * 128], ALU.mult,
                                   ["accf", ek], ["ktl"])
                                pt, pk = nps()
                                for j in range(n):
                                    MM(pt[:, j * 128:(j + 1) * 128], zf[:, t0 + j, :], tri[:], True, True, ["srcf", trik], [pk])
                                e_, ek = ex[0], ("ex", 0)
                                exr += 1
                                ACTV(e_[:, 0:n * 128], pt[:, 0:n * 128], AF.Exp, [pk], [ek])
                                lastc = 127 if d == 0 else 0
                                CP(dec_h[:, d, t0:t0 + n],
                                   e_[:, 0:n * 128].rearrange("p (a b) -> p a b", b=128)[:, :, lastc], [ek], ["dec_h"])
                                for j in range(n):
                                    ti = t0 + j
                                    if ti >= 2:
                                        TT(qtl[:, (ti - 2) * 128:(ti - 1) * 128], hqT[:, ti * 128:(ti + 1) * 128],
                                           e_[:, j * 128:(j + 1) * 128], ALU.mult, ["hqT", ek], ["qtl"])
                            for t0 in range(0, NT, 8):
                                n = min(8, NT - t0)
                                pb_, pbk = npsb()
                                for j in range(n):
                                    TR(pb_[:, j * 128:(j + 1) * 128], ktl[:, t0 + j, :], ident_b[:], ["ktl", "ident_b"], [pbk])
                                CP(ktlT[:, t0 * 128:(t0 + n) * 128], pb_[:, 0:n * 128], [pbk], ["ktlT"], eng=ACT)

                            if h == 0 and b == 0 and d == 0:
                                dump("ktl", ktl[:].rearrange("p a b -> p (a b)"), lambda dd: dd, ["ktl"])
                                dump("qtl", qtl[:], lambda dd: dd, ["qtl"])
                                dump("ktlT", ktlT[:], lambda dd: dd, ["ktlT"])
                                dump("dech", dec_h[:].rearrange("p a b -> p (a b)"), lambda dd: dd, ["dec_h"])
                                dump("hqT", hqT[:], lambda dd: dd, ["hqT"])
                                dump("hitm", hi_tm[:].rearrange("p a b -> p (a b)"), lambda dd: dd, ["hi_tm"])

                            def post(po, pok, c, d=d):
                                if d == 0:
                                    CP(osum[:, c - 2, :], po[:, 0:128], [pok], ["osum"])
                                else:
                                    TT(osum[:, c - 2, :], po[:, 0:128], osum[:, c - 2, :], ALU.add, [pok, "osum"], ["osum"])

                            for _ in scan(d, qtl, "qtl", True, ktlT, "ktlT", ktl, "ktl", hi_tm, "hi_tm",
                                          128, lambda c, d=d: dec_h[:, d, c:c + 1], "dec_h", post):
                                pass
                        if h == 0 and b == 0:
                            dump("osum", osum[:].rearrange("p a b -> p (a b)"), lambda d: d, ["osum"])
                        head_norm_to_T(osum, ["osum"], hn_bc, "hn_bc", h, g_tm, "g_tm", bT, "bT", tmpf, a_tm)
                P.barrier()
                if stop == "s4":
                    break

            P.barrier()
            with contextlib.ExitStack() as s5:
                G1 = sb("G1", [128, 1024], stack=s5)
                A2 = sb("A2", [128, 1024], stack=s5)
                SH2 = sb("SH2", [128, 1024], stack=s5)
                wpa = sb("wpa", [128, 4, 1024], BF16, stack=s5)
                wpb = sb("wpb", [128, 4, 1024], BF16, stack=s5)
                wout = sb("wout", [128, 8, 1024], BF16, stack=s5)
                wg = [sb(f"wg{i}", [128, 8, 128], BF16, stack=s5) for i in range(2)]
                yT = sb("yT", [128, 8, 512], BF16, stack=s5)
                sga = sb("sga", [128, 512], stack=s5)
                y1 = sb("y1", [128, 512], stack=s5)
                y2 = sb("y2", [128, 512], stack=s5)
                sgb = sga
                yl = sb("yl", [128, 1024], stack=s5)
                sq = sb("sq5", [128, 1024], stack=s5)
                t1 = sq
                xin = [sb(f"xin5{i}", [128, 1024], stack=s5) for i in range(1)]
                x1t = [sb(f"x1t{i}", [128, 1024], stack=s5) for i in range(1)]
                h2b = sb("h2b", [128, 1024], BF16, stack=s5)
                h2Tt = sb("h2Tt", [128, 8, 128], BF16, stack=s5)
                load_bc(G1, b * 6 + 2, "G1")
                load_bc(A2, b * 6 + 4, "A2")
                load_bc(SH2, b * 6 + 3, "SH2")
                DMA(POOL, wpa[:], wpa_d, [], ["wpa"])
                DMA(POOL, wpb[:], wpb_d, [], ["wpb"])
                DMA(POOL, wout[:], wout_d, [], ["wout"])
                wgr = 0
                for tg in range(4):
                    tok0 = tg * 512
                    for j in range(8):
                        wga, wgak = wg[0], ("wg", 0)
                        wgb, wgbk = wg[1], ("wg", 1)
                        wgr += 2
                        DMA(POOL, wga[:], win_d[36 + j], [], [wgak])
                        DMA(POOL, wgb[:], win_d[44 + j], [], [wgbk])
                        pa, pak = nps()
                        for k in range(4):
                            MM(pa[:, :], wpa[:, k, j * 128:(j + 1) * 128], aT[:, k, tok0:tok0 + 512], k == 0, k == 3,
                               ["wpa", "aT"], [pak])
                        pga, pgak = nps()
                        for k in range(8):
                            MM(pga[:, :], wga[:, k, :], hT[:, k, 256 + tok0:256 + tok0 + 512], k == 0, k == 7,
                               [wgak] + hTk(256 + tok0, 256 + tok0 + 512), [pgak])
                        ACTV(sga[:], pga[:, :], AF.Sigmoid, [pgak, "bcol"], ["sga"], bias=bcol[:, 36 + j:37 + j])
                        TT(y1[:], pa[:, :], sga[:], ALU.mult, [pak, "sga"], ["y1"])
                        pb2, pb2k = nps()
                        for k in range(4):
                            MM(pb2[:, :], wpb[:, k, j * 128:(j + 1) * 128], bT[:, k, tok0:tok0 + 512], k == 0, k == 3,
                               ["wpb", "bT"], [pb2k])
                        pgb, pgbk = nps()
                        for k in range(8):
                            MM(pgb[:, :], wgb[:, k, :], hT[:, k, 256 + tok0:256 + tok0 + 512], k == 0, k == 7,
                               [wgbk] + hTk(256 + tok0, 256 + tok0 + 512), [pgbk])
                        ACTV(sgb[:], pgb[:, :], AF.Sigmoid, [pgbk, "bcol"], ["sga"], bias=bcol[:, 44 + j:45 + j])
                        TT(y2[:], pb2[:, :], sgb[:], ALU.mult, [pb2k, "sga"], ["y2"])
                        TT(yT[:, j, :], y1[:], y2[:], ALU.add, ["y1", "y2"], [("yT", j)])
                    for tl in range(4):
                        lt = tg * 4 + tl
                        for half in range(2):
                            pt, pk = nps()
                            for k in range(8):
                                MM(pt[:, :], yT[:, k, tl * 128:(tl + 1) * 128], wout[:, k, half * 512:(half + 1) * 512],
                                   k == 0, k == 7, [("yT", k), "wout"], [pk])
                            CP(yl[:, half * 512:(half + 1) * 512], pt[:, :], [pk], ["yl"], eng=ACT)
                        ACTV(sq[:], yl[:], AF.Square, ["yl"], ["sq5"])
                        sc, sk = small[:, 20 + lt:21 + lt], ("small", 20 + lt)
                        RED(sc, sq[:], ["sq5"], [sk])
                        rstd_from_ss(sc, sc, 1.0 / 1024, sk)
                        xi, xk = xin[0], ("xin5", 0)
                        DMA(SP, xi[:], x_d[b, lt * 128:(lt + 1) * 128, :], [], [xk])
                        STT(t1[:], yl[:], sc, G1[:], ALU.mult, ALU.mult, ["yl", sk, "G1"], ["sq5"])
                        xo, xok = x1t[0], ("x1t", 0)
                        TT(xo[:], t1[:], xi[:], ALU.add, ["sq5", xk], [xok])
                        DMA(SP, x1_d[b, lt * 128:(lt + 1) * 128, :], xo[:], [xok], [("x1d", b, lt)])
                        ACTV(sq[:], xo[:], AF.Square, [xok], ["sq5"])
                        sc2, sk2 = small[:, 40 + lt:41 + lt], ("small", 40 + lt)
                        RED(sc2, sq[:], ["sq5"], [sk2])
                        rstd_from_ss(sc2, sc2, 1.0 / 1024, sk2)
                        STT(t1[:], xo[:], sc2, A2[:], ALU.mult, ALU.mult, [xok, sk2, "A2"], ["sq5"])
                        TT(h2b[:], t1[:], SH2[:], ALU.add, ["sq5", "SH2"], ["h2b"])
                        pb_, pbk = npsb()
                        for k in range(8):
                            TR(pb_[:, k * 128:(k + 1) * 128], h2b[:, k * 128:(k + 1) * 128], ident_b[:],
                               ["h2b", "ident_b"], [pbk])
                        gt = b * 16 + lt
                        DMA(SP, h2_d[gt * 128:(gt + 1) * 128, :], h2b[:], ["h2b"], [("h2d", gt)])
                        CP(h2Tt[:], pb_[:].rearrange("p (k t) -> p k t", k=8), [pbk], ["h2Tt"], eng=ACT)
                        pt, pk = nps()
                        for k in range(8):
                            MM(pt[:, 0:32], h2Tt[:, k, :], wr_sb[:, k, :], k == 0, k == 7, ["h2Tt", "wr_sb"], [pk])
                        TT(Ls[:, gt, :], pt[:, 0:32], br_bc[:], ALU.add, [pk, "br_bc"], [("Ls", gt)])
                        P.op(DVE, lambda e, gt=gt: e.max(out=m8s[:, gt, :], in_=Ls[:, gt, :]), [("Ls", gt)], [("m8s", gt)])
            mx.close()
            mx = None
            P.barrier()
            if stop == "s5":
                break

        LsK = kk("Ls", range(32))
        m8K = kk("m8s", range(32))
        desti = sb("desti", [128, 128], I32)
        wk = sb("wk", [128, 32, 4])
        widx = sb("widx", [128, NB], I32)
        widx8 = sb("widx8", [128, 8, NB], I32)
        bidx = sb("bidx", [128, NB], I32)
        if stop in (None, "m1"):
            with contextlib.ExitStack() as m1:
                triS = sb("triS", [128, 128], stack=m1)
                DMA(SP, triS[:], triS_d, [], ["triS"])
                mask = sb("mask", [128, 1024], stack=m1)
                R1 = sb("R1", [128, 1024], stack=m1)
                cA = sb("cA", [128, 1024], stack=m1)
                cB = sb("cB", [128, 1024], stack=m1)
                cnt = sb("cnt", [128, 1024], stack=m1)
                dest = sb("dest", [128, 1024], stack=m1)
                tot = sb("tot", [128, 32], stack=m1)
                nblk = sb("nblk", [128, 32], stack=m1)
                pA = sb("pA", [128, 32], stack=m1)
                pB = sb("pB", [128, 32], stack=m1)
                pstart = sb("pstart", [128, 32], stack=m1)
                junk = sb("junk", [128, 32], stack=m1)
                destk = sb("destk", [128, 128], stack=m1)
                ejf = sb("ejf", [128, NB], stack=m1)
                wif = sb("wif", [128, NB], stack=m1)
                nm = sb("nm", [128, 32], stack=m1)
                exs = sb("exs", [128, 32, 4], stack=m1)
                ssum = sb("ssum", [128, 32], stack=m1)
                for gt in range(32):
                    TS(mask[:, gt * 32:(gt + 1) * 32], Ls[:, gt, :], m8s[:, gt, 3:4], None, ALU.is_ge, None,
                       [("Ls", gt), ("m8s", gt)], ["mask"])
                for hf in range(2):
                    cs = slice(hf * 512, (hf + 1) * 512)
                    pt, pk = nps()
                    MM(pt[:, :], triS[:], mask[:, cs], True, True, ["triS", "mask"], [pk])
                    CP(R1[:, cs], pt[:, :], [pk], ["R1"])
                    pt, pk = nps()
                    MM(pt[:, :], ones_f[:], mask[:, cs], True, True, ["ones_f", "mask"], [pk])
                    CP(cnt[:, cs], pt[:, :], [pk], ["cnt"])
                CP(cA[:], cnt[:], ["cnt"], ["cA"])
                a_, ak, b_, bk = cA, "cA", cB, "cB"
                for sft in (1, 2, 4, 8, 16):
                    w_ = sft * 32
                    CP(b_[:, 0:w_], a_[:, 0:w_], [ak], [bk])
                    TT(b_[:, w_:1024], a_[:, w_:1024], a_[:, 0:1024 - w_], ALU.add, [ak], [bk])
                    a_, ak, b_, bk = b_, bk, a_, ak
                incl, inck = a_, ak
                CP(tot[:], incl[:, 31 * 32:32 * 32], [inck], ["tot"])
                TT(dest[:], incl[:], cnt[:], ALU.subtract, [inck, "cnt"], ["dest"])
                TT(dest[:], dest[:], R1[:], ALU.add, ["dest", "R1"], ["dest"])
                MSET(nblk[:], 0.0, ["nblk"])
                for j in range(MAXB):
                    STT(nblk[:], tot[:], float(j * BLK), nblk[:], ALU.is_gt, ALU.add, ["tot", "nblk"], ["nblk"])
                CP(pA[:], nblk[:], ["nblk"], ["pA"])
                a_, ak, b_, bk = pA, "pA", pB, "pB"
                for sft in (1, 2, 4, 8, 16):
                    CP(b_[:, 0:sft], a_[:, 0:sft], [ak], [bk])
                    TT(b_[:, sft:32], a_[:, sft:32], a_[:, 0:32 - sft], ALU.add, [ak], [bk])
                    a_, ak, b_, bk = b_, bk, a_, ak
                pend, pendk = a_, ak
                TT(pstart[:], pend[:], nblk[:], ALU.subtract, [pendk, "nblk"], ["pstart"])
                TS(pstart[:], pstart[:], float(BLK), None, ALU.mult, None, ["pstart"], ["pstart"])
                for gt in range(32):
                    TT(dest[:, gt * 32:(gt + 1) * 32], dest[:, gt * 32:(gt + 1) * 32], pstart[:], ALU.add,
                       ["dest", "pstart"], ["dest"])
                for gt in range(32):
                    for k in range(4):
                        STT(junk[:], Ls[:, gt, :], m8s[:, gt, k:k + 1], dest[:, gt * 32:(gt + 1) * 32], ALU.is_equal, ALU.mult,
                            [("Ls", gt), ("m8s", gt), "dest"], ["junk"])
                        RED(destk[:, gt * 4 + k:gt * 4 + k + 1], junk[:], ["junk"], ["destk"])
                TS(destk[:], destk[:], 0.0, float(NB * BLK - 1), ALU.max, ALU.min, ["destk"], ["destk"])
                CP(desti[:], destk[:], ["destk"], ["desti"])
                TS(nm[:], m8s[:, :, 0], -1.0, None, ALU.mult, None, m8K, ["nm"])
                for gt in range(32):
                    ACTV(exs[:, gt, :], m8s[:, gt, 0:4], AF.Exp, [("m8s", gt), "nm"], ["exs"], bias=nm[:, gt:gt + 1])
                RED(ssum[:], exs[:], ["exs"], ["ssum"])
                RECIP(ssum[:], ssum[:], ["ssum"], ["ssum"])
                for gt in range(32):
                    TS(wk[:, gt, :], exs[:, gt, :], ssum[:, gt:gt + 1], None, ALU.mult, None, ["exs", "ssum"], ["wk"])
                for j in range(NB):
                    TS(junk[:], pend[:], float(j), None, ALU.is_le, None, [pendk], ["junk"])
                    RED(ejf[:, j:j + 1], junk[:], ["junk"], ["ejf"])
                TS(ejf[:], ejf[:], 31.0, None, ALU.min, None, ["ejf"], ["ejf"])
                CP(bidx[:], ejf[:], ["ejf"], ["bidx"])
                TS(wif[:], ejf[:], 128.0, pidx[:, 0:1], ALU.mult, ALU.add, ["ejf", "pidx"], ["wif"])
                CP(widx[:], wif[:], ["wif"], ["widx"])
                wif8 = sb("wif8", [128, 8, NB], stack=m1)
                for k in range(8):
                    TS(wif8[:, k, :], wif[:], 8.0, float(k), ALU.mult, ALU.add, ["wif"], ["wif8"])
                CP(widx8[:], wif8[:], ["wif8"], ["widx8"])
                dump("destk", destk[:], lambda d: d, ["destk"])
                dump("ejf", ejf[:], lambda d: d, ["ejf"])
                dump("wk", wk[:].rearrange("p a b -> p (a b)"), lambda d: d, ["wk"])
                dump("wif", wif[:], lambda d: d, ["wif"])
                h2t = [sb(f"h2t{i}", [128, 1024], BF16, stack=m1) for i in range(2)]
                for gt in range(32 if stop is None else 0):
                    ht, htk = h2t[gt % 2], ("h2t", gt % 2)
                    DMA(SP, ht[:], h2_d[gt * 128:(gt + 1) * 128, :], [("h2d", gt)], [htk])
                    for k in range(4):
                        c_ = gt * 4 + k
                        P.dma(POOL, lambda e, ht=ht, c_=c_: e.indirect_dma_start(
                            out=xs_d, out_offset=bass.IndirectOffsetOnAxis(ap=desti[:, c_:c_ + 1], axis=0),
                            in_=ht[:], in_offset=None),
                            [htk, "desti"], ["xs"])
            P.barrier()

        if stop is None:
            with contextlib.ExitStack() as m4:
                wgu = [sb(f"wgu{i}", [128, 8, 2048], BF16, stack=m4) for i in range(2)]
                wdn = [sb(f"wdn{i}", [128, 8, 1024], BF16, stack=m4) for i in range(2)]
                bgt = [sb(f"bgt{i}", [128, 16], stack=m4) for i in range(2)]
                bdt = [sb(f"bdt{i}", [128, 1024], stack=m4) for i in range(2)]
                xbs = [sb(f"xb{i}", [128, 4, 1024], BF16, stack=m4) for i in range(2)]
                xT = sb("xT", [128, 8, 512], BF16, stack=m4)
                act = sb("act", [128, 8, 512], BF16, stack=m4)
                g1 = [sb(f"g1_{i}", [128, 512], stack=m4) for i in range(2)]
                sg = [sb(f"sg_{i}", [128, 512], stack=m4) for i in range(2)]
                u1 = [sb(f"u1_{i}", [128, 512], stack=m4) for i in range(2)]
                ybt = [sb(f"ybt{i}", [128, 1024], stack=m4) for i in range(2)]

                def load_w(j):
                    r_ = j % 2
                    ix = widx[:, j:j + 1]
                    for k in range(8):
                        P.dma(POOL, lambda e, k=k: e.indirect_dma_start(
                            out=wgu[r_][:, k, :], out_offset=None, in_=wgu_d,
                            in_offset=bass.IndirectOffsetOnAxis(ap=widx8[:, k, j:j + 1], axis=0)),
                            ["widx8"], [("wgu", r_, k)])
                    for k in range(8):
                        P.dma(POOL, lambda e, k=k: e.indirect_dma_start(
                            out=wdn[r_][:, k, :], out_offset=None, in_=wdn_d,
                            in_offset=bass.IndirectOffsetOnAxis(ap=widx8[:, k, j:j + 1], axis=0)),
                            ["widx8"], [("wdn", r_, k)])
                    P.dma(POOL, lambda e: e.indirect_dma_start(
                        out=bgt[r_][:], out_offset=None, in_=bgu_d,
                        in_offset=bass.IndirectOffsetOnAxis(ap=ix, axis=0)), ["widx"], [("bgt", r_)])
                    P.dma(POOL, lambda e: e.indirect_dma_start(
                        out=bdt[r_][:], out_offset=None, in_=bdn_d,
                        in_offset=bass.IndirectOffsetOnAxis(ap=bidx[:, j:j + 1], axis=0)), ["bidx"], [("bdt", r_)])

                load_w(0)
                rot = 0
                for j in range(NB):
                    r_ = j % 2
                    if j + 1 < NB:
                        load_w(j + 1)
                    xb, xbk = xbs[j % 2], ("xb", j % 2)
                    if j == 0:
                        DMA(SP, xb[:], xs_d[0:BLK, :].rearrange("(a p) n -> p a n", p=128), ["xs"], [xbk])
                    if j + 1 < NB:
                        DMA(SP, xbs[(j + 1) % 2][:], xs_d[(j + 1) * BLK:(j + 2) * BLK, :].rearrange("(a p) n -> p a n", p=128),
                            ["xs"], [("xb", (j + 1) % 2)])
                    for a in range(4):
                        pb_, pbk = npsb()
                        for k in range(8):
                            TR(pb_[:, k * 128:(k + 1) * 128], xb[:, a, k * 128:(k + 1) * 128], ident_b[:], [xbk, "ident_b"], [pbk])
                        CP(xT[:, :, a * 128:(a + 1) * 128], pb_[:].rearrange("p (k t) -> p k t", k=8), [pbk], ["xT"],
                           eng=ACT if a % 2 == 0 else DVE)
                    for fb in range(8):
                        q_ = rot % 2
                        rot += 1
                        pg, pgk = nps()
                        for k in range(8):
                            MM(pg[:, :], wgu[r_][:, k, fb * 128:(fb + 1) * 128], xT[:, k, :], k == 0, k == 7,
                               [("wgu", r_, k), "xT"], [pgk])
                        pu, puk = nps()
                        for k in range(8):
                            MM(pu[:, :], wgu[r_][:, k, 1024 + fb * 128:1024 + (fb + 1) * 128], xT[:, k, :], k == 0, k == 7,
                               [("wgu", r_, k), "xT"], [puk])
                        TS(g1[q_][:], pg[:, :], bgt[r_][:, fb:fb + 1], 7.0, ALU.add, ALU.min, [pgk, ("bgt", r_)], [("g1", q_)])
                        ACTV(sg[q_][:], g1[q_][:], AF.Sigmoid, [("g1", q_)], [("sg", q_)], scale=1.702)
                        TS(u1[q_][:], pu[:, :], bgt[r_][:, 8 + fb:9 + fb], 7.0, ALU.add, ALU.min, [puk, ("bgt", r_)], [("u1", q_)])
                        TS(u1[q_][:], u1[q_][:], -7.0, 1.0, ALU.max, ALU.add, [("u1", q_)], [("u1", q_)])
                        TT(g1[q_][:], g1[q_][:], sg[q_][:], ALU.mult, [("g1", q_), ("sg", q_)], [("g1", q_)])
                        TT(act[:, fb, :], g1[q_][:], u1[q_][:], ALU.mult, [("g1", q_), ("u1", q_)], ["act"])
                    for a in range(4):
                        y_, yk = ybt[a % 2], ("ybt", a % 2)
                        for dh in range(2):
                            py, pyk = nps()
                            for fb in range(8):
                                MM(py[:, :], act[:, fb, a * 128:(a + 1) * 128], wdn[r_][:, fb, dh * 512:(dh + 1) * 512],
                                   fb == 0, fb == 7, ["act", ("wdn", r_, fb)], [pyk])
                            TT(y_[:, dh * 512:(dh + 1) * 512], py[:, :], bdt[r_][:, dh * 512:(dh + 1) * 512], ALU.add,
                               [pyk, ("bdt", r_)], [yk])
                        DMA(SP, yb_d[j * BLK + a * 128:j * BLK + (a + 1) * 128, :], y_[:], [yk], ["yb"])
            P.barrier()

            with contextlib.ExitStack() as m5:
                G2 = [sb(f"G2_{i}", [128, 1024], stack=m5) for i in range(2)]
                ybk = [sb(f"ybk{i}", [128, 1024], stack=m5) for i in range(8)]
                ym = sb("ym", [128, 1024], stack=m5)
                sq = sb("sq7", [128, 1024], stack=m5)
                x1r = [sb(f"x1r{i}", [128, 1024], stack=m5) for i in range(2)]
                ot = [sb(f"ot{i}", [128, 1024], stack=m5) for i in range(2)]
                for b in range(nb):
                    load_bc(G2[b], b * 6 + 5, ("G2", b))
                for gt in range(nb * 16):
                    b, lt = gt // 16, gt % 16
                    ys = []
                    for k in range(4):
                        i_ = (gt % 2) * 4 + k
                        c_ = gt * 4 + k
                        P.dma(POOL, lambda e, i_=i_, c_=c_: e.indirect_dma_start(
                            out=ybk[i_][:], out_offset=None, in_=yb_d,
                            in_offset=bass.IndirectOffsetOnAxis(ap=desti[:, c_:c_ + 1], axis=0)),
                            ["yb", "desti"], [("ybk", i_)])
                        ys.append((ybk[i_], ("ybk", i_)))
                    TS(ym[:], ys[0][0][:], wk[:, gt, 0:1], None, ALU.mult, None, [ys[0][1], "wk"], ["ym"])
                    for k in range(1, 4):
                        STT(ym[:], ys[k][0][:], wk[:, gt, k:k + 1], ym[:], ALU.mult, ALU.add, [ys[k][1], "wk", "ym"], ["ym"])
                    ACTV(sq[:], ym[:], AF.Square, ["ym"], ["sq7"])
                    sc, sk = small[:, 20 + lt:21 + lt], ("small", 20 + lt)
                    RED(sc, sq[:], ["sq7"], [sk])
                    rstd_from_ss(sc, sc, 1.0 / 1024, sk)
                    xr, xrk = x1r[gt % 2], ("x1r", gt % 2)
                    DMA(SP, xr[:], x1_d[b, lt * 128:(lt + 1) * 128, :], [("x1d", b, lt)], [xrk])
                    STT(sq[:], ym[:], sc, G2[b][:], ALU.mult, ALU.mult, ["ym", sk, ("G2", b), "sq7"], ["sq7"])
                    o_, ok_ = ot[gt % 2], ("ot", gt % 2)
                    TT(o_[:], sq[:], xr[:], ALU.add, ["sq7", xrk], [ok_])
                    out_toks.append(DMA(SP, out_d[b, lt * 128:(lt + 1) * 128, :], o_[:], [ok_], []))

        if mx is not None:
            mx.close()
        P.final_wait(SP, out_toks)
        P.emit()
    return nc


def _c(a):
    return np.ascontiguousarray(a, dtype=np.float32)


def prep_shared(inp):
    w_in = inp["w_in"][0]
    sh = {}
    sh["w_ada"] = _c(inp["w_ada"][0].reshape(8, 128, 12, 512).transpose(2, 1, 0, 3))
    sh["b_ada"] = _c(inp["b_ada"].reshape(1, 6144))
    sh["nrm"] = _c(np.concatenate([inp["norm_mix_pre"][0], inp["norm_mix_post"][0],
                                   inp["norm_ffn_pre"][0], inp["norm_ffn_post"][0]]).reshape(1, 4096))
    sh["w_in"] = _c(w_in[:, :6656].reshape(8, 128, 52, 128).transpose(2, 1, 0, 3))
    sh["w_mg"] = _c(w_in[:, 6656:].reshape(8, 128, 16).transpose(1, 0, 2))
    sh["b_in_col"] = _c(inp["b_in"][0, :6656].reshape(52, 128).T)
    sh["b_in_row"] = _c(inp["b_in"].reshape(1, 6672))
    sh["conv_col"] = _c(inp["conv_w"][0].reshape(9, 8, 128).transpose(2, 1, 0))
    sh["lb_raw"] = _c(inp["lb_raw"].reshape(1, 2048))
    sh["m_norm"] = _c(inp["m_norm"].reshape(1, 512))
    sh["h_norm"] = _c(inp["h_norm"].reshape(1, 512))
    sh["w_pa"] = _c(inp["w_pa"][0].reshape(4, 128, 1024).transpose(1, 0, 2))
    sh["w_pb"] = _c(inp["w_pb"][0].reshape(4, 128, 1024).transpose(1, 0, 2))
    sh["w_out"] = _c(inp["w_out"][0].reshape(8, 128, 1024).transpose(1, 0, 2))
    sh["w_router"] = _c(inp["w_router"][0].reshape(8, 128, 32).transpose(1, 0, 2))
    sh["b_router"] = _c(inp["b_router"].reshape(1, 32))
    sh["w_gu"] = _c(inp["w_gu"][0].reshape(32, 8, 128, 2048).transpose(0, 2, 1, 3)).reshape(32 * 128 * 8, 2048)
    sh["b_gu_col"] = _c(inp["b_gu"][0].reshape(32, 16, 128).transpose(0, 2, 1)).reshape(32 * 128, 16)
    sh["w_dn"] = _c(inp["w_dn"][0].reshape(32, 8, 128, 1024).transpose(0, 2, 1, 3)).reshape(32 * 128 * 8, 1024)
    sh["b_dn"] = _c(inp["b_dn"][0])
    sh["ident"] = np.eye(128, dtype=np.float32)
    sh["triF"] = np.triu(np.ones((128, 128), np.float32))
    sh["triB"] = np.tril(np.ones((128, 128), np.float32))
    sh["ones"] = np.ones((128, 128), np.float32)
    sh["pidx"] = np.arange(128, dtype=np.float32).reshape(128, 1)
    sh["triS"] = np.triu(np.ones((128, 128), np.float32), 1)
    return sh


def core_inputs(inp, sh, i):
    m = dict(sh)
    m["x"] = _c(inp["x"][2 * i:2 * i + 2])
    m["ctx"] = _c(inp["ctx"][2 * i:2 * i + 2])
    cv = np.stack([inp["c"][2 * i], inp["c"][2 * i + 1], inp["c_ctx"]])
    m["cvT"] = _c(cv.reshape(3, 8, 128).transpose(2, 0, 1).reshape(128, 24))
    return m


def kernel(**inputs):
    inp = {k: np.asarray(v) for k, v in inputs.items()}
    sh = prep_shared(inp)
    nc = build_nc()
    in_maps = [core_inputs(inp, sh, i) for i in range(N_CORES)]
    res = run_bass_kernel_spmd(nc, in_maps, core_ids=list(range(N_CORES)))
    out = np.concatenate([np.asarray(r["out"], dtype=np.float32) for r in res.results], axis=0)
    return out
```

```python
import contextlib
import numpy as np
import concourse.bass as bass
import concourse.mybir as mybir
from concourse.bass_utils import run_bass_kernel_spmd

F32 = mybir.dt.float32
BF16 = mybir.dt.bfloat16
I32 = mybir.dt.int32
AF = mybir.ActivationFunctionType
ALU = mybir.AluOpType
AX = mybir.AxisListType

PE, DVE, ACT, POOL, SP = "pe", "dve", "act", "pool", "sp"
COMPUTE = (PE, DVE, ACT, POOL)
EPOCH = 12000
N_DMA_SEM = 88
DMA_POOLS = {"sp": (0, 32), "act": (32, 8), "pool": (40, 48)}
N_EPOCH_SEM = 12
EPS = 1e-6
NT = 18
BLK = 512
NB = 4096 * 4 // BLK + 32
MAXB = 4096 // BLK
TOK = 2304
N_CORES = 8


class Prog:
    def __init__(self, nc, same_engine_sync=True):
        self.nc = nc
        self.same = same_engine_sync
        self.streams = {e: [] for e in (PE, DVE, ACT, POOL, SP)}
        self.count = {e: 0 for e in COMPUTE}
        self.waited = {}
        self.state = {}
        self.dma_tot = [0] * N_DMA_SEM
        self.dma_rr = {q: 0 for q in DMA_POOLS}
        self.barrier_toks = set()

    def barrier(self):
        toks = set()
        for e in COMPUTE:
            c = self.count[e]
            if c > 0:
                toks.add(((e, (c - 1) // EPOCH), ((c - 1) % EPOCH) + 1))
        for s_ in range(N_DMA_SEM):
            if self.dma_tot[s_] > 0:
                toks.add((("dma", s_), self.dma_tot[s_]))
        self.barrier_toks = toks

    def _deps(self, reads, writes):
        deps = set()
        for k in reads:
            st = self.state.get(k)
            if st and st[0]:
                deps.add(st[0])
        for k in writes:
            st = self.state.get(k)
            if st:
                if st[0]:
                    deps.add(st[0])
                deps.update(st[1])
        return deps

    def _commit(self, reads, writes, tok):
        for k in writes:
            self.state[k] = [tok, []]
        for k in reads:
            if k in writes:
                continue
            st = self.state.setdefault(k, [None, []])
            st[1].append(tok)
            if len(st[1]) > 64:
                best = {}
                for (key, val) in st[1]:
                    if best.get(key, 0) < val:
                        best[key] = val
                st[1] = list(best.items())

    def _waits(self, eng, deps, own_key=None):
        best = {}
        for (key, val) in deps:
            if key == own_key and not self.same:
                continue
            if self.waited.get((eng, key), 0) >= val:
                continue
            if best.get(key, 0) < val:
                best[key] = val
        out = []
        for key, val in best.items():
            self.waited[(eng, key)] = val
            out.append((key, val))
        return out

    def op(self, eng, fn, reads=(), writes=()):
        reads, writes = tuple(reads), tuple(writes)
        c = self.count[eng]
        own_key = (eng, c // EPOCH)
        deps = self._deps(reads, writes) | self.barrier_toks
        if eng == PE:
            deps = {d for d in deps if d[0][0] != PE}
        waits = self._waits(eng, deps, own_key)
        self.count[eng] = c + 1
        tok = (own_key, (c % EPOCH) + 1)
        self.streams[eng].append(("op", waits, fn, own_key))
        self._commit(reads, writes, tok)

    def dma(self, q, fn, reads=(), writes=()):
        reads, writes = tuple(reads), tuple(writes)
        first, cnt_ = DMA_POOLS[q]
        s = first + self.dma_rr[q]
        self.dma_rr[q] = (self.dma_rr[q] + 1) % cnt_
        key = ("dma", s)
        deps = self._deps(reads, writes) | self.barrier_toks
        if self.dma_tot[s] > 0:
            deps.add((key, self.dma_tot[s]))
        waits = self._waits(q, deps, None)
        self.dma_tot[s] += 16
        tok = (key, self.dma_tot[s])
        self.streams[q].append(("dma", waits, fn, key))
        self._commit(reads, writes, tok)
        return tok

    def final_wait(self, eng, toks):
        waits = self._waits(eng, set(toks), None)
        self.streams[eng].append(("wait", waits, None, None))

    def emit(self):
        nc = self.nc
        with contextlib.ExitStack() as es:
            sems = {}
            for e in COMPUTE:
                nep = self.count[e] // EPOCH + 1
                assert nep <= N_EPOCH_SEM, (e, self.count[e])
                for i in range(nep):
                    sems[(e, i)] = es.enter_context(nc.semaphore(f"s_{e}_{i}"))
            for s in range(N_DMA_SEM):
                if self.dma_tot[s] > 0:
                    sems[("dma", s)] = es.enter_context(nc.semaphore(f"s_dma_{s}"))
            block = es.enter_context(nc.Block())

            def run(eng_name):
                def body(engine):
                    for kind, waits, fn, key in self.streams[eng_name]:
                        for (k, v) in waits:
                            engine.wait_ge(sems[k], v)
                        if kind == "op":
                            fn(engine).then_inc(sems[key], 1)
                        elif kind == "dma":
                            fn(engine).then_inc(sems[key], 16)
                return body

            block.sync(run(SP))
            block.tensor(run(PE))
            block.vector(run(DVE))
            block.scalar(run(ACT))
            block.gpsimd(run(POOL))


def kk(name, idxs):
    return [(name, i) for i in idxs]


def build_nc(nb=2, dumps=(), stop=None, n_exp=32):
    nc = bass.Bass("TRN2", target_bir_lowering=False)
    P = Prog(nc)
    D = {}

    def din(name, shape, dt=F32):
        D[name] = nc.dram_tensor(name, list(shape), dt, kind="ExternalInput").ap()
        return D[name]

    x_d = din("x", [2, 2048, 1024])
    ctx_d = din("ctx", [2, 256, 1024])
    cvT_d = din("cvT", [128, 24])
    wada_d = din("w_ada", [12, 128, 8, 512])
    bada_d = din("b_ada", [1, 6144])
    nrm_d = din("nrm", [1, 4096])
    win_d = din("w_in", [52, 128, 8, 128])
    wmg_d = din("w_mg", [128, 8, 16])
    bcol_d = din("b_in_col", [128, 52])
    brow_d = din("b_in_row", [1, 6672])
    conv_d = din("conv_col", [128, 8, 9])
    lb_d = din("lb_raw", [1, 2048])
    mn_d = din("m_norm", [1, 512])
    hn_d = din("h_norm", [1, 512])
    wpa_d = din("w_pa", [128, 4, 1024])
    wpb_d = din("w_pb", [128, 4, 1024])
    wout_d = din("w_out", [128, 8, 1024])
    wr_d = din("w_router", [128, 8, 32])
    br_d = din("b_router", [1, 32])
    wgu_d = din("w_gu", [32 * 128 * 8, 2048])
    bgu_d = din("b_gu_col", [32 * 128, 16])
    wdn_d = din("w_dn", [32 * 128 * 8, 1024])
    bdn_d = din("b_dn", [32, 1024])
    ident_d = din("ident", [128, 128])
    triF_d = din("triF", [128, 128])
    triB_d = din("triB", [128, 128])
    ones_d = din("ones", [128, 128])
    pidx_d = din("pidx", [128, 1])
    triS_d = din("triS", [128, 128])
    out_d = nc.dram_tensor("out", [2, 2048, 1024], F32, kind="ExternalOutput").ap()
    bc_d = nc.dram_tensor("bc_scr", [14, 128, 1024], F32).ap()
    x1_d = nc.dram_tensor("x1_scr", [2, 2048, 1024], F32).ap()
    h2_d = nc.dram_tensor("h2_scr", [4096, 1024], BF16).ap()
    xs_d = nc.dram_tensor("xs_scr", [NB * BLK, 1024], BF16).ap()
    yb_d = nc.dram_tensor("yb_scr", [NB * BLK, 1024], F32).ap()
    dump_d = {}
    for (nm, shape) in dumps:
        dump_d[nm] = nc.dram_tensor("dbg_" + nm, list(shape), F32, kind="ExternalOutput").ap()
    out_toks = []

    def MM(out, lhsT, rhs, start, stop, reads, writes):
        P.op(PE, lambda e: e.matmul(out, lhsT=lhsT, rhs=rhs, start=start, stop=stop), reads, writes)

    def TR(out, in_, ident, reads, writes):
        P.op(PE, lambda e: e.transpose(out, in_, ident), reads, writes)

    def ACTV(out, in_, func, reads, writes, bias=None, scale=None):
        kw = {}
        if bias is not None:
            kw["bias"] = bias
        if scale is not None:
            kw["scale"] = scale
        P.op(ACT, lambda e: e.activation(out=out, in_=in_, func=func, **kw), reads, writes)

    def TS(out, in0, s1, s2, op0, op1, reads, writes, eng=DVE):
        if s2 is None:
            P.op(eng, lambda e: e.tensor_scalar(out=out, in0=in0, scalar1=s1, scalar2=None, op0=op0), reads, writes)
        else:
            P.op(eng, lambda e: e.tensor_scalar(out=out, in0=in0, scalar1=s1, scalar2=s2, op0=op0, op1=op1),
                 reads, writes)

    def TT(out, in0, in1, op, reads, writes, eng=DVE):
        P.op(eng, lambda e: e.tensor_tensor(out=out, in0=in0, in1=in1, op=op), reads, writes)

    def STT(out, in0, scalar, in1, op0, op1, reads, writes):
        P.op(DVE, lambda e: e.scalar_tensor_tensor(out=out, in0=in0, scalar=scalar, in1=in1, op0=op0, op1=op1),
             reads, writes)

    def CP(out, in_, reads, writes, eng=DVE):
        if eng == ACT:
            P.op(ACT, lambda e: e.activation(out=out, in_=in_, func=AF.Copy), reads, writes)
        else:
            P.op(eng, lambda e: e.tensor_copy(out=out, in_=in_), reads, writes)

    def RED(out, in_, reads, writes):
        P.op(DVE, lambda e: e.tensor_reduce(out=out, in_=in_, axis=AX.X, op=ALU.add), reads, writes)

    def RECIP(out, in_, reads, writes):
        P.op(DVE, lambda e: e.reciprocal(out=out, in_=in_), reads, writes)

    def MSET(ap, val, writes, eng=DVE):
        P.op(eng, lambda e: e.memset(ap, val), (), writes)

    def DMA(q, out, in_, reads, writes):
        return P.dma(q, lambda e: e.dma_start(out=out, in_=in_), reads, writes)

    def bcast(ap1n, n):
        return ap1n.partition_broadcast(128)

    def rstd_from_ss(rs, ss, inv_n, key):
        TS(rs, ss, inv_n, EPS, ALU.mult, ALU.add, [key], [key])
        ACTV(rs, rs, AF.Sqrt, [key], [key])
        RECIP(rs, rs, [key], [key])

    with contextlib.ExitStack() as top:
        uniq = [0]

        def sb(name, shape, dt=F32, stack=top):
            uniq[0] += 1
            return stack.enter_context(nc.sbuf_tensor(f"sb_{name}_{uniq[0]}", list(shape), dt))

        psf = [top.enter_context(nc.psum_tensor(f"psf{i}", [128, 512], F32)) for i in range(6)]
        psb = [top.enter_context(nc.psum_tensor(f"psb{i}", [128, 1024], BF16)) for i in range(2)]
        rr = {"f": 0, "b": 0, "t": 0}

        def nps():
            i = rr["f"]
            rr["f"] = (i + 1) % 6
            return psf[i], ("psf", i)

        def npsb():
            i = rr["b"]
            rr["b"] = (i + 1) % 2
            return psb[i], ("psb", i)

        ident_f = sb("ident_f", [128, 128])
        ident_b = sb("ident_b", [128, 128], BF16)
        triF = sb("triF", [128, 128])
        triB = sb("triB", [128, 128])
        ones_f = sb("ones_f", [128, 128])
        bcol = sb("bcol", [128, 52])
        convc = sb("convc", [128, 8, 9])
        lb_bc = sb("lb_bc", [128, 2, 512])
        oml_bc = sb("oml_bc", [128, 2, 512])
        mn_bc = sb("mn_bc", [128, 512])
        hn_bc = sb("hn_bc", [128, 512])
        br_bc = sb("br_bc", [128, 32])
        wr_sb = sb("wr_sb", [128, 8, 32], BF16)
        wmg_sb = sb("wmg_sb", [128, 8, 16], BF16)
        bmg_bc = sb("bmg_bc", [128, 16])
        DMA(SP, ident_f[:], ident_d, [], ["ident_f"])
        DMA(POOL, ident_b[:], ident_d, [], ["ident_b"])
        DMA(SP, triF[:], triF_d, [], ["triF"])
        DMA(SP, triB[:], triB_d, [], ["triB"])
        DMA(SP, ones_f[:], ones_d, [], ["ones_f"])
        DMA(SP, bcol[:], bcol_d, [], ["bcol"])
        DMA(SP, convc[:], conv_d, [], ["convc"])
        DMA(SP, mn_bc[:], bcast(mn_d, 512), [], ["mn_bc"])
        DMA(SP, hn_bc[:], bcast(hn_d, 512), [], ["hn_bc"])
        DMA(SP, br_bc[:], bcast(br_d, 32), [], ["br_bc"])
        DMA(POOL, wr_sb[:], wr_d, [], ["wr_sb"])
        DMA(POOL, wmg_sb[:], wmg_d, [], ["wmg_sb"])
        DMA(SP, bmg_bc[:], bcast(brow_d[:, 6656:6672], 16), [], ["bmg_bc"])
        lbf = lb_bc[:].rearrange("p a b -> p (a b)")
        omlf = oml_bc[:].rearrange("p a b -> p (a b)")
        with contextlib.ExitStack() as sl:
            lbr = sb("lbr", [128, 2048], stack=sl)
            DMA(SP, lbr[:], bcast(lb_d, 2048), [], ["lbr"])
            TT(lbf, lbr[:, 0:1024], lbr[:, 1024:2048], ALU.subtract, ["lbr"], ["lb_bc"])
            ACTV(lbf, lbf, AF.Sigmoid, ["lb_bc"], ["lb_bc"])
            TS(omlf, lbf, -1.0, 1.0, ALU.mult, ALU.add, ["lb_bc"], ["oml_bc"])
        P.barrier()

        with contextlib.ExitStack() as s0:
            cT = sb("cT", [128, 24], stack=s0)
            sg0 = sb("sg0", [128, 24], stack=s0)
            cb = sb("cb", [128, 24, 128], stack=s0)
            nrm_bc = sb("nrm_bc", [128, 4, 1024], stack=s0)
            wa = [sb(f"wa{i}", [128, 8, 512], stack=s0) for i in range(2)]
            ba = [sb(f"ba{i}", [128, 512], stack=s0) for i in range(2)]
            tA = [sb(f"tA{i}", [128, 512], stack=s0) for i in range(3)]
            tB = [sb(f"tB{i}", [128, 512], stack=s0) for i in range(3)]
            DMA(SP, cT[:], cvT_d, [], ["cT"])
            DMA(SP, nrm_bc[:].rearrange("p a b -> p (a b)"), bcast(nrm_d, 4096), [], ["nrm_bc"])
            ACTV(sg0[:], cT[:], AF.Sigmoid, ["cT"], ["sg0"])
            TT(cT[:], cT[:], sg0[:], ALU.mult, ["cT", "sg0"], ["cT"])
            for i in range(24):
                TS(cb[:, i, :], ones_f[:], cT[:, i:i + 1], None, ALU.mult, None, ["ones_f", "cT"], [("cb", i)])
            ti_rot = 0
            for cbi in range(12):
                w, half = cbi // 2, cbi % 2
                cs = slice(half * 512, (half + 1) * 512)
                wt, bt = wa[cbi % 2], ba[cbi % 2]
                DMA(SP, wt[:], wada_d[cbi], [], [("wa", cbi % 2)])
                DMA(SP, bt[:], bcast(bada_d[:, cbi * 512:(cbi + 1) * 512], 512), [], [("ba", cbi % 2)])
                for r in range(3):
                    if r == 2 and w > 1:
                        continue
                    pt, pk = nps()
                    for k in range(8):
                        MM(pt[:, :], cb[:, r * 8 + k, :], wt[:, k, :], k == 0, k == 7,
                           [("cb", r * 8 + k), ("wa", cbi % 2)], [pk])
                    a_, b_ = tA[ti_rot % 3], tB[ti_rot % 3]
                    ka, kb = ("tA", ti_rot % 3), ("tB", ti_rot % 3)
                    ti_rot += 1
                    TT(a_[:], pt[:, :], bt[:], ALU.add, [pk, ("ba", cbi % 2)], [ka])
                    if w in (1, 4):
                        STT(b_[:], a_[:], 1.0, nrm_bc[:, 0 if w == 1 else 2, cs], ALU.add, ALU.mult,
                            [ka, "nrm_bc"], [kb])
                        src, ksrc = b_, kb
                    elif w in (2, 5):
                        TT(b_[:], a_[:], nrm_bc[:, 1 if w == 2 else 3, cs], ALU.mult, [ka, "nrm_bc"], [kb])
                        src, ksrc = b_, kb
                    else:
                        src, ksrc = a_, ka
                    tidx = r * 6 + w if r < 2 else 12 + w
                    DMA(SP, bc_d[tidx, :, cs], src[:], [ksrc], [("bc", tidx, half)])

        P.barrier()

        def load_bc(dst, tidx, key):
            DMA(SP, dst[:], bc_d[tidx], [("bc", tidx, 0), ("bc", tidx, 1)], [key])

        Ls = sb("Ls", [128, 32, 32])
        m8s = sb("m8s", [128, 32, 8])
        pidx = sb("pidx", [128, 1])
        DMA(SP, pidx[:], pidx_d, [], ["pidx"])
        small = sb("small", [128, 64])

        def hTk(t0, t1):
            return kk("hT", range(t0 // 128, (t1 + 127) // 128))

        def dump(nm, ap_sb, dst, reads):
            if nm in dump_d:
                out_toks.append(DMA(POOL, dst(dump_d[nm]), ap_sb, reads, []))

        mx = None
        for b in range(nb):
            mx = contextlib.ExitStack()
            hT = sb("hT", [128, 8, TOK], BF16, stack=mx)
            aT = sb("aT", [128, 4, 2048], BF16, stack=mx)
            bT = sb("bT", [128, 4, 2048], BF16, stack=mx)
            with contextlib.ExitStack() as s1:
                A1 = sb("A1", [128, 1024], stack=s1)
                SH1 = sb("SH1", [128, 1024], stack=s1)
                A1c = sb("A1c", [128, 1024], stack=s1)
                SH1c = sb("SH1c", [128, 1024], stack=s1)
                xin = [sb(f"xin{i}", [128, 1024], stack=s1) for i in range(2)]
                sq = sb("sq", [128, 1024], stack=s1)
                tmp = sb("tmp1", [128, 1024], stack=s1)
                hb = [sb(f"hb{i}", [128, 1024], BF16, stack=s1) for i in range(2)]
                load_bc(A1, b * 6 + 1, "A1")
                load_bc(SH1, b * 6 + 0, "SH1")
                load_bc(A1c, 13, "A1c")
                load_bc(SH1c, 12, "SH1c")
                for ti in range(NT):
                    xi, xk = xin[ti % 2], ("xin", ti % 2)
                    src = ctx_d[b, ti * 128:(ti + 1) * 128, :] if ti < 2 else x_d[b, (ti - 2) * 128:(ti - 1) * 128, :]
                    DMA(SP, xi[:], src, [], [xk])
                    ACTV(sq[:], xi[:], AF.Square, [xk], ["sq"])
                    sc, sk = small[:, ti:ti + 1], ("small", ti)
                    RED(sc, sq[:], ["sq"], [sk])
                    rstd_from_ss(sc, sc, 1.0 / 1024, sk)
                    Am, Ak, Sm, Sk = (A1c, "A1c", SH1c, "SH1c") if ti < 2 else (A1, "A1", SH1, "SH1")
                    STT(tmp[:], xi[:], sc, Am[:], ALU.mult, ALU.mult, [xk, sk, Ak], ["tmp1"])
                    hbt, hbk = hb[ti % 2], ("hb", ti % 2)
                    TT(hbt[:], tmp[:], Sm[:], ALU.add, ["tmp1", Sk], [hbk])
                    pb_, pbk = npsb()
                    for k in range(8):
                        TR(pb_[:, k * 128:(k + 1) * 128], hbt[:, k * 128:(k + 1) * 128], ident_b[:],
                           [hbk, "ident_b"], [pbk])
                    CP(hT[:, :, ti * 128:(ti + 1) * 128], pb_[:].rearrange("p (k t) -> p k t", k=8),
                       [pbk], [("hT", ti)], eng=ACT)
            P.barrier()
            dump("hT", hT[:, 0, :], lambda d: d, kk("hT", range(NT)))
            if stop == "s1":
                break

            with contextlib.ExitStack() as sm:
                wfm = [sb(f"wfm{i}", [128, 8, 128], BF16, stack=sm) for i in range(2)]
                srcf = sb("srcf", [128, TOK], stack=sm)
                accf = sb("accf", [128, TOK], stack=sm)
                stm = [sb(f"stm{i}", [128, 128], BF16, stack=sm) for i in range(2)]
                Pst = [sb(f"Pst{i}", [128, 129], stack=sm) for i in range(2)]
                Sbf = [sb(f"Sbf{i}", [128, 129], BF16, stack=sm) for i in range(2)]
                Sbf2 = [sb(f"Sbf2{i}", [128, 129], BF16, stack=sm) for i in range(2)]
                ssh = sb("ssh", [128, 16], stack=sm)
                tmpf = sb("tmpf", [128, 16, 128], stack=sm)
                a_tm = sb("a_tm", [128, 16, 128], BF16, stack=sm)

                def proj_fm(chunk, wt, wk, dst, dk):
                    DMA(POOL, wt[:], win_d[chunk], [], [wk])
                    for g0 in range(0, TOK, 512):
                        n = min(512, TOK - g0)
                        pt, pk = nps()
                        for k in range(8):
                            MM(pt[:, 0:n], wt[:, k, :], hT[:, k, g0:g0 + n], k == 0, k == 7,
                               [wk] + hTk(g0, g0 + n), [pk])
                        ACTV(dst[:, g0:g0 + n], pt[:, 0:n], AF.Identity, [pk, "bcol"], [dk],
                             bias=bcol[:, chunk:chunk + 1])

                def scan(d, QT, qk, q_lat_only, KT, ktk, Ktm, ktmk, V, vk, nv, dec_of, deck, post):
                    order = [0, 1] + list(range(2, NT)) if d == 0 else [1, 0] + list(range(NT - 1, 1, -1))
                    n_ = len(order)
                    mask, mkey = (triF, "triF") if d == 0 else (triB, "triB")
                    Pt, Pk = Pst[d], ("Pst", d)
                    Sts = [(Sbf[d], ("Sbf", d)), (Sbf2[d], ("Sbf2", d))]

                    def emit_U(i):
                        c_ = order[i]
                        pu, puk = nps()
                        MM(pu[:, 0:nv], Ktm[:, c_, :], V[:, c_, 0:nv], True, True, [ktmk, vk], [puk])
                        return pu, puk

                    nxt = emit_U(0)
                    for idx, c in enumerate(order):
                        cs = slice(c * 128, (c + 1) * 128)
                        qs = slice((c - 2) * 128, (c - 1) * 128) if q_lat_only else cs
                        if idx < n_ - 1:
                            pu, puk = nxt
                            St, Sk = Sts[idx % 2]
                            if idx == 0:
                                CP(Pt[:, 0:nv], pu[:, 0:nv], [puk], [Pk])
                            else:
                                STT(Pt[:, 0:nv], Pt[:, 0:nv], dec_of(order[idx - 1]), pu[:, 0:nv], ALU.mult, ALU.add,
                                    [Pk, puk, deck], [Pk])
                            TS(St[:, 0:nv], Pt[:, 0:nv], dec_of(c), None, ALU.mult, None, [Pk, deck], [Sk])
                            if idx + 1 < n_ - 1:
                                nxt = emit_U(idx + 1)
                        if c >= 2:
                            p1, p1k = nps()
                            MM(p1[:, 0:128], KT[:, cs], QT[:, qs], True, True, [ktk, qk], [p1k])
                            sm_, smk = stm[rr["t"] % 2], ("stm", rr["t"] % 2)
                            rr["t"] += 1
                            TT(sm_[:], p1[:, 0:128], mask[:], ALU.mult, [p1k, mkey], [smk])
                            po, pok = nps()
                            MM(po[:, 0:nv], sm_[:], V[:, c, 0:nv], True, idx == 0, [smk, vk], [pok])
                            if idx > 0:
                                Sp, Spk = Sts[(idx - 1) % 2]
                                MM(po[:, 0:nv], QT[:, qs], Sp[:, 0:nv], False, True, [qk, Spk], [pok])
                            post(po, pok, c)
                        yield

                def head_norm_to_T(hs, hsk, nbc, nbk, h, gate, gk, dstT, dstk, tmpf, a_tm):
                    ACTV(tmpf[:], hs[:], AF.Square, list(hsk), ["hn_tmp"])
                    RED(ssh[:], tmpf[:], ["hn_tmp"], ["ssh"])
                    rstd_from_ss(ssh[:], ssh[:], 1.0 / 128, "ssh")
                    for ti in range(16):
                        STT(tmpf[:, ti, :], hs[:, ti, :], ssh[:, ti:ti + 1], nbc[:, h * 128:(h + 1) * 128],
                            ALU.mult, ALU.mult, list(hsk) + ["ssh", nbk, "hn_tmp"], ["hn_tmp"])
                    TT(a_tm[:], tmpf[:], gate[:], ALU.mult, ["hn_tmp", gk], ["a_tm"])
                    for t0 in (0, 8):
                        pb_, pbk = npsb()
                        for j in range(8):
                            TR(pb_[:, j * 128:(j + 1) * 128], a_tm[:, t0 + j, :], ident_b[:], ["a_tm", "ident_b"], [pbk])
                        CP(dstT[:, h, t0 * 128:(t0 + 8) * 128], pb_[:, :], [pbk], [dstk], eng=ACT)

                s2x = contextlib.ExitStack()
                gates = sb("gates", [128, 16, NT], stack=s2x)
                lf = sb("lf", [128, 8, NT], stack=s2x)
                bcum = sb("bcum", [128, 8, NT], stack=s2x)
                dec_m = sb("dec_m", [128, 8, NT], stack=s2x)
                w_m = sb("w_m", [128, 8, NT], stack=s2x)
                e_m = sb("e_m", [128, 8, NT], stack=s2x)
                for ti in range(NT):
                    pt, pk = nps()
                    for k in range(8):
                        MM(pt[:, 0:16], hT[:, k, ti * 128:(ti + 1) * 128], wmg_sb[:, k, :], k == 0, k == 7,
                           [("hT", ti), "wmg_sb"], [pk])
                    TT(gates[:, :, ti], pt[:, 0:16], bmg_bc[:], ALU.add, [pk, "bmg_bc"], ["gates"])
                ACTV(lf[:], gates[:, 8:16, :], AF.Sigmoid, ["gates"], ["lf"])
                ACTV(lf[:], lf[:], AF.Ln, ["lf"], ["lf"])
                pt, pk = nps()
                MM(pt[:, 0:72], triF[:], lf[:, 0:4, :].rearrange("p a b -> p (a b)"), True, True, ["triF", "lf"], [pk])
                MM(pt[:, 72:144], triB[:], lf[:, 4:8, :].rearrange("p a b -> p (a b)"), True, True,
                   ["triB", "lf"], [pk])
                CP(bcum[:].rearrange("p a b -> p (a b)"), pt[:, 0:144], [pk], ["bcum"])
                pt2, pk2 = nps()
                MM(pt2[:, 0:144], ones_f[:], lf[:].rearrange("p a b -> p (a b)"), True, True, ["ones_f", "lf"], [pk2])
                ACTV(dec_m[:].rearrange("p a b -> p (a b)"), pt2[:, 0:144], AF.Exp, [pk2], ["dec_m"])
                TT(w_m[:], gates[:, 0:8, :], bcum[:], ALU.subtract, ["gates", "bcum"], ["w_m"])
                ACTV(w_m[:], w_m[:], AF.Exp, ["w_m"], ["w_m"])
                ACTV(e_m[:], bcum[:], AF.Exp, ["bcum"], ["e_m"])
                dump("bcum", bcum[:].rearrange("p a b -> p (a b)"), lambda d: d, ["bcum"])

                with contextlib.ExitStack() as s3:
                    qT = sb("qT", [128, TOK], BF16, stack=s3)
                    kT = sb("kT", [128, TOK], BF16, stack=s3)
                    k_tm = sb("k_tm", [128, NT, 128], BF16, stack=s3)
                    wvo = sb("wvo", [128, 8, 256], BF16, stack=s3)
                    bvo = sb("bvo", [128, 256], stack=s3)
                    vp = sb("vp", [128, NT, 129], BF16, stack=s3)
                    v2_ = sb("v2_0", [128, NT, 129], BF16, stack=s3)
                    v2 = [v2_, v2_]
                    og = sb("og", [128, 16, 128], BF16, stack=s3)
                    hsum = sb("hsum", [128, 16, 128], stack=s3)
                    dsc = sb("dsc", [128, 4], stack=s3)
                    for h in range(4):
                        for which, dst, dkey, scale in ((0, qT, "qT", 128.0 ** -0.5), (1, kT, "kT", 1.0)):
                            chunk = which * 4 + h
                            proj_fm(chunk, wfm[which], ("wfm", which), srcf, "srcf")
                            wc = convc[:, chunk, :]
                            ls = srcf[:, 256:TOK].rearrange("p (r c) -> p r c", c=64)
                            la = accf[:, 256:TOK].rearrange("p (r c) -> p r c", c=64)
                            TS(accf[:], srcf[:], wc[:, 4:5], None, ALU.mult, None, ["srcf", "convc"], ["accf"])
                            for di in (-1, 0, 1):
                                for dj in (-1, 0, 1):
                                    if di == 0 and dj == 0:
                                        continue
                                    tap = (di + 1) * 3 + (dj + 1)
                                    r0, r1 = max(0, -di), 32 - max(0, di)
                                    c0, c1 = max(0, -dj), 64 - max(0, dj)
                                    STT(la[:, r0:r1, c0:c1], ls[:, r0 + di:r1 + di, c0 + dj:c1 + dj], wc[:, tap:tap + 1],
                                        la[:, r0:r1, c0:c1], ALU.mult, ALU.add, ["srcf", "convc", "accf"], ["accf"])
                            STT(accf[:, 1:256], srcf[:, 0:255], wc[:, 3:4], accf[:, 1:256], ALU.mult, ALU.add,
                                ["srcf", "convc", "accf"], ["accf"])
                            STT(accf[:, 0:255], srcf[:, 1:256], wc[:, 5:6], accf[:, 0:255], ALU.mult, ALU.add,
                                ["srcf", "convc", "accf"], ["accf"])
                            ACTV(srcf[:], accf[:], AF.Sigmoid, ["accf"], ["srcf"])
                            STT(dst[:], accf[:], scale, srcf[:], ALU.mult, ALU.mult, ["accf", "srcf"], [dkey])
                        if h == 0 and b == 0:
                            dump("qT", qT[:], lambda d: d, ["qT"])
                            dump("kT", kT[:], lambda d: d, ["kT"])
                        for t0 in range(0, NT, 8):
                            n = min(8, NT - t0)
                            pb_, pbk = npsb()
                            for j in range(n):
                                TR(pb_[:, j * 128:(j + 1) * 128], kT[:, (t0 + j) * 128:(t0 + j + 1) * 128], ident_b[:],
                                   ["kT", "ident_b"], [pbk])
                            CP(k_tm[:, t0:t0 + n, :].rearrange("p a b -> p (a b)"), pb_[:, 0:n * 128], [pbk], ["k_tm"],
                               eng=ACT)
                        DMA(POOL, wvo[:, :, 0:128], win_d[8 + h], [], ["wvo"])
                        DMA(POOL, wvo[:, :, 128:256], win_d[12 + h], [], ["wvo"])
                        DMA(SP, bvo[:, 0:128], bcast(brow_d[:, 1024 + h * 128:1024 + (h + 1) * 128], 128), [], ["bvo"])
                        DMA(SP, bvo[:, 128:256], bcast(brow_d[:, 1536 + h * 128:1536 + (h + 1) * 128], 128), [], ["bvo"])
                        MSET(vp[:, :, 128:129], 1.0, ["vp"])
                        for ti in range(NT):
                            pt, pk = nps()
                            for k in range(8):
                                MM(pt[:, 0:256], hT[:, k, ti * 128:(ti + 1) * 128], wvo[:, k, :], k == 0, k == 7,
                                   [("hT", ti), "wvo"], [pk])
                            TT(vp[:, ti, 0:128], pt[:, 0:128], bvo[:, 0:128], ALU.add, [pk, "bvo"], ["vp"])
                            if ti >= 2:
                                TT(og[:, ti - 2, :], pt[:, 128:256], bvo[:, 128:256], ALU.add, [pk, "bvo"], ["og"])
                        ACTV(og[:], og[:], AF.Sigmoid, ["og"], ["og"])
                        gens = []
                        for d in range(2):
                            col = d * 4 + h
                            vd, vdk = (v2_, "v2") if d == 0 else (vp, "vp")
                            for ti in range(NT):
                                TS(vd[:, ti, :], vp[:, ti, :], w_m[:, col, ti:ti + 1], None, ALU.mult, None,
                                   ["vp", "w_m"], [vdk])

                            def post(po, pok, c, d=d, col=col):
                                ecol = e_m[:, col, c:c + 1]
                                d1, d2 = dsc[:, 2 * d:2 * d + 1], dsc[:, 2 * d + 1:2 * d + 2]
                                dk_ = ("dsc", d)
                                ACTV(d1, po[:, 128:129], AF.Abs, [pok, "e_m"], [dk_], scale=ecol)
                                TS(d1, d1, 1.0, None, ALU.max, None, [dk_], [dk_])
                                RECIP(d1, d1, [dk_], [dk_])
                                TT(d2, d1, ecol, ALU.mult, [dk_, "e_m"], [dk_])
                                if (c <= 9) == (d == 0):
                                    TS(hsum[:, c - 2, :], po[:, 0:128], d2, None, ALU.mult, None, [pok, dk_], [("hsum", c)])
                                else:
                                    STT(hsum[:, c - 2, :], po[:, 0:128], d2, hsum[:, c - 2, :], ALU.mult, ALU.add,
                                        [pok, dk_, ("hsum", c)], [("hsum", c)])

                            gens.append(scan(d, qT, "qT", False, kT, "kT", k_tm, "k_tm", vd, vdk, 129,
                                             lambda c, col=col: dec_m[:, col, c:c + 1], "dec_m", post))
                        while gens:
                            for g_ in list(gens):
                                try:
                                    next(g_)
                                except StopIteration:
                                    gens.remove(g_)
                        if h == 0 and b == 0:
                            dump("hsum", hsum[:].rearrange("p a b -> p (a b)"), lambda d: d, kk("hsum", range(2, NT)))
                        head_norm_to_T(hsum, kk("hsum", range(2, NT)), mn_bc, "mn_bc", h, og, "og", aT, "aT", tmpf, a_tm)
                s2x.close()
                P.barrier()
                if stop == "s3":
                    break

                with contextlib.ExitStack() as s4:
                    hqT = sb("hqT", [128, TOK], BF16, stack=s4)
                    w4 = sb("w4", [128, 8, 384], BF16, stack=s4)
                    w4b = sb("w4b", [128, 8, 128], BF16, stack=s4)
                    b4 = sb("b4", [128, 512], stack=s4)
                    hi_tm = sb("hi_tm", [128, NT, 128], BF16, stack=s4)
                    g_tm = sb("g_tm", [128, 16, 128], BF16, stack=s4)
                    zf = srcf[:].rearrange("p (a b) -> p a b", b=128)
                    kkf = accf[:].rearrange("p (a b) -> p a b", b=128)
                    ktl = sb("ktl", [128, NT, 128], BF16, stack=s4)
                    ktlT = sb("ktlT", [128, TOK], BF16, stack=s4)
                    qtl = sb("qtl", [128, 2048], BF16, stack=s4)
                    dec_h = sb("dec_h", [128, 2, NT], stack=s4)
                    ex_ = sb("ex0", [128, 512], stack=s4)
                    ex = [ex_, ex_]
                    osum = sb("osum", [128, 16, 128], stack=s4)
                    exr = 0
                    for h in range(4):
                        proj_fm(16 + h, wfm[0], ("wfm", 0), srcf, "srcf")
                        ACTV(accf[:], srcf[:], AF.Sigmoid, ["srcf"], ["accf"])
                        TT(hqT[:], srcf[:], accf[:], ALU.mult, ["srcf", "accf"], ["hqT"])
                        for j, ch in enumerate((20 + h, 24 + h, 28 + h)):
                            DMA(POOL, w4[:, :, j * 128:(j + 1) * 128], win_d[ch], [], ["w4"])
                        DMA(POOL, w4b[:], win_d[32 + h], [], ["w4b"])
                        for j, ch in enumerate((20 + h, 24 + h, 28 + h, 32 + h)):
                            DMA(SP, b4[:, j * 128:(j + 1) * 128], bcast(brow_d[:, ch * 128:(ch + 1) * 128], 128), [], ["b4"])
                        for d in range(2):
                            hs = slice(h * 128, (h + 1) * 128)
                            for ti in range(NT):
                                pt, pk = nps()
                                if d == 0:
                                    for k in range(8):
                                        MM(pt[:, 0:384], hT[:, k, ti * 128:(ti + 1) * 128], w4[:, k, :], k == 0, k == 7,
                                           [("hT", ti), "w4"], [pk])
                                    TT(hi_tm[:, ti, :], pt[:, 0:128], b4[:, 0:128], ALU.add, [pk, "b4"], ["hi_tm"])
                                    if ti >= 2:
                                        TT(tmpf[:, ti - 2, :], pt[:, 128:256], b4[:, 128:256], ALU.add, [pk, "b4"], ["hn_tmp"])
                                    TT(zf[:, ti, :], pt[:, 256:384], b4[:, 256:384], ALU.add, [pk, "b4"], ["srcf"])
                                else:
                                    for k in range(8):
                                        MM(pt[:, 0:128], hT[:, k, ti * 128:(ti + 1) * 128], w4b[:, k, :], k == 0, k == 7,
                                           [("hT", ti), "w4b"], [pk])
                                    TT(zf[:, ti, :], pt[:, 0:128], b4[:, 384:512], ALU.add, [pk, "b4"], ["srcf"])
                            if d == 0:
                                ACTV(g_tm[:], tmpf[:], AF.Sigmoid, ["hn_tmp"], ["g_tm"])
                                TT(g_tm[:], g_tm[:], tmpf[:], ALU.mult, ["g_tm", "hn_tmp"], ["g_tm"])
                            ACTV(accf[:], srcf[:], AF.Sigmoid, ["srcf"], ["accf"])
                            for ti in range(NT):
                                TT(zf[:, ti, :], kkf[:, ti, :], oml_bc[:, d, hs], ALU.mult, ["accf", "oml_bc", "srcf"], ["srcf"])
                                TT(zf[:, ti, :], zf[:, ti, :], lb_bc[:, d, hs], ALU.add, ["srcf", "lb_bc"], ["srcf"])
                            TS(accf[:], srcf[:], -1.0, 1.0, ALU.mult, ALU.add, ["srcf"], ["accf"])
                            ACTV(srcf[:], srcf[:], AF.Ln, ["srcf"], ["srcf"])
                            tri, trik = (triF, "triF") if d == 0 else (triB, "triB")
                            for t0 in range(0, NT, 4):
                                n = min(4, NT - t0)
                                fs = slice(t0 * 128, (t0 + n) * 128)
                                pt, pk = nps()
                                MM(pt[:, 0:n * 128], tri[:], srcf[:, fs], True, True, [trik, "srcf"], [pk])
                                e_, ek = ex[0], ("ex", 0)
                                exr += 1
                                ACTV(e_[:, 0:n * 128], pt[:, 0:n * 128], AF.Exp, [pk], [ek], scale=-1.0)
                                TT(ktl[:, t0:t0 + n, :].rearrange("p a b -> p (a b)"), accf[:, fs], e_[:, 0:n * 128], ALU.mult,
                                   ["accf", ek], ["ktl"])
                                pt, pk = nps()
                                for j in range(n):
                                    MM(pt[:, j * 128:(j + 1) * 128], zf[:, t0 + j, :], tri[:], True, True, ["srcf", trik], [pk])
                                e_, ek = ex[0], ("ex", 0)
                                exr += 1
                                ACTV(e_[:, 0:n * 128], pt[:, 0:n * 128], AF.Exp, [pk], [ek])
                                lastc = 127 if d == 0 else 0
                                CP(dec_h[:, d, t0:t0 + n],
                                   e_[:, 0:n * 128].rearrange("p (a b) -> p a b", b=128)[:, :, lastc], [ek], ["dec_h"])
                                for j in range(n):
                                    ti = t0 + j
                                    if ti >= 2:
                                        TT(qtl[:, (ti - 2) * 128:(ti - 1) * 128], hqT[:, ti * 128:(ti + 1) * 128],
                                           e_[:, j * 128:(j + 1) * 128], ALU.mult, ["hqT", ek], ["qtl"])
                            for t0 in range(0, NT, 8):
                                n = min(8, NT - t0)
                                pb_, pbk = npsb()
                                for j in range(n):
                                    TR(pb_[:, j * 128:(j + 1) * 128], ktl[:, t0 + j, :], ident_b[:], ["ktl", "ident_b"], [pbk])
                                CP(ktlT[:, t0 * 128:(t0 + n) * 128], pb_[:, 0:n * 128], [pbk], ["ktlT"], eng=ACT)

                            if h == 0 and b == 0 and d == 0:
                                dump("ktl", ktl[:].rearrange("p a b -> p (a b)"), lambda dd: dd, ["ktl"])
                                dump("qtl", qtl[:], lambda dd: dd, ["qtl"])
                                dump("ktlT", ktlT[:], lambda dd: dd, ["ktlT"])
                                dump("dech", dec_h[:].rearrange("p a b -> p (a b)"), lambda dd: dd, ["dec_h"])
                                dump("hqT", hqT[:], lambda dd: dd, ["hqT"])
                                dump("hitm", hi_tm[:].rearrange("p a b -> p (a b)"), lambda dd: dd, ["hi_tm"])

                            def post(po, pok, c, d=d):
                                if d == 0:
                                    CP(osum[:, c - 2, :], po[:, 0:128], [pok], ["osum"])
                                else:
                                    TT(osum[:, c - 2, :], po[:, 0:128], osum[:, c - 2, :], ALU.add, [pok, "osum"], ["osum"])

                            for _ in scan(d, qtl, "qtl", True, ktlT, "ktlT", ktl, "ktl", hi_tm, "hi_tm",
                                          128, lambda c, d=d: dec_h[:, d, c:c + 1], "dec_h", post):
                                pass
                        if h == 0 and b == 0:
                            dump("osum", osum[:].rearrange("p a b -> p (a b)"), lambda d: d, ["osum"])
                        head_norm_to_T(osum, ["osum"], hn_bc, "hn_bc", h, g_tm, "g_tm", bT, "bT", tmpf, a_tm)
                P.barrier()
                if stop == "s4":
                    break

            P.barrier()
            with contextlib.ExitStack() as s5:
                G1 = sb("G1", [128, 1024], stack=s5)
                A2 = sb("A2", [128, 1024], stack=s5)
                SH2 = sb("SH2", [128, 1024], stack=s5)
                wpa = sb("wpa", [128, 4, 1024], BF16, stack=s5)
                wpb = sb("wpb", [128, 4, 1024], BF16, stack=s5)
                wout = sb("wout", [128, 8, 1024], BF16, stack=s5)
                wg = [sb(f"wg{i}", [128, 8, 128], BF16, stack=s5) for i in range(2)]
                yT = sb("yT", [128, 8, 512], BF16, stack=s5)
                sga = sb("sga", [128, 512], stack=s5)
                y1 = sb("y1", [128, 512], stack=s5)
                y2 = sb("y2", [128, 512], stack=s5)
                sgb = sga
                yl = sb("yl", [128, 1024], stack=s5)
                sq = sb("sq5", [128, 1024], stack=s5)
                t1 = sq
                xin = [sb(f"xin5{i}", [128, 1024], stack=s5) for i in range(1)]
                x1t = [sb(f"x1t{i}", [128, 1024], stack=s5) for i in range(1)]
                h2b = sb("h2b", [128, 1024], BF16, stack=s5)
                h2Tt = sb("h2Tt", [128, 8, 128], BF16, stack=s5)
                load_bc(G1, b * 6 + 2, "G1")
                load_bc(A2, b * 6 + 4, "A2")
                load_bc(SH2, b * 6 + 3, "SH2")
                DMA(POOL, wpa[:], wpa_d, [], ["wpa"])
                DMA(POOL, wpb[:], wpb_d, [], ["wpb"])
                DMA(POOL, wout[:], wout_d, [], ["wout"])
                wgr = 0
                for tg in range(4):
                    tok0 = tg * 512
                    for j in range(8):
                        wga, wgak = wg[0], ("wg", 0)
                        wgb, wgbk = wg[1], ("wg", 1)
                        wgr += 2
                        DMA(POOL, wga[:], win_d[36 + j], [], [wgak])
                        DMA(POOL, wgb[:], win_d[44 + j], [], [wgbk])
                        pa, pak = nps()
                        for k in range(4):
                            MM(pa[:, :], wpa[:, k, j * 128:(j + 1) * 128], aT[:, k, tok0:tok0 + 512], k == 0, k == 3,
                               ["wpa", "aT"], [pak])
                        pga, pgak = nps()
                        for k in range(8):
                            MM(pga[:, :], wga[:, k, :], hT[:, k, 256 + tok0:256 + tok0 + 512], k == 0, k == 7,
                               [wgak] + hTk(256 + tok0, 256 + tok0 + 512), [pgak])
                        ACTV(sga[:], pga[:, :], AF.Sigmoid, [pgak, "bcol"], ["sga"], bias=bcol[:, 36 + j:37 + j])
                        TT(y1[:], pa[:, :], sga[:], ALU.mult, [pak, "sga"], ["y1"])
                        pb2, pb2k = nps()
                        for k in range(4):
                            MM(pb2[:, :], wpb[:, k, j * 128:(j + 1) * 128], bT[:, k, tok0:tok0 + 512], k == 0, k == 3,
                               ["wpb", "bT"], [pb2k])
                        pgb, pgbk = nps()
                        for k in range(8):
                            MM(pgb[:, :], wgb[:, k, :], hT[:, k, 256 + tok0:256 + tok0 + 512], k == 0, k == 7,
                               [wgbk] + hTk(256 + tok0, 256 + tok0 + 512), [pgbk])
                        ACTV(sgb[:], pgb[:, :], AF.Sigmoid, [pgbk, "bcol"], ["sga"], bias=bcol[:, 44 + j:45 + j])
                        TT(y2[:], pb2[:, :], sgb[:], ALU.mult, [pb2k, "sga"], ["y2"])
                        TT(yT[:, j, :], y1[:], y2[:], ALU.add, ["y1", "y2"], [("yT", j)])
                    for tl in range(4):
                        lt = tg * 4 + tl
                        for half in range(2):
                            pt, pk = nps()
                            for k in range(8):
                                MM(pt[:, :], yT[:, k, tl * 128:(tl + 1) * 128], wout[:, k, half * 512:(half + 1) * 512],
                                   k == 0, k == 7, [("yT", k), "wout"], [pk])
                            CP(yl[:, half * 512:(half + 1) * 512], pt[:, :], [pk], ["yl"], eng=ACT)
                        ACTV(sq[:], yl[:], AF.Square, ["yl"], ["sq5"])
                        sc, sk = small[:, 20 + lt:21 + lt], ("small", 20 + lt)
                        RED(sc, sq[:], ["sq5"], [sk])
                        rstd_from_ss(sc, sc, 1.0 / 1024, sk)
                        xi, xk = xin[0], ("xin5", 0)
                        DMA(SP, xi[:], x_d[b, lt * 128:(lt + 1) * 128, :], [], [xk])
                        STT(t1[:], yl[:], sc, G1[:], ALU.mult, ALU.mult, ["yl", sk, "G1"], ["sq5"])
                        xo, xok = x1t[0], ("x1t", 0)
                        TT(xo[:], t1[:], xi[:], ALU.add, ["sq5", xk], [xok])
                        DMA(SP, x1_d[b, lt * 128:(lt + 1) * 128, :], xo[:], [xok], [("x1d", b, lt)])
                        ACTV(sq[:], xo[:], AF.Square, [xok], ["sq5"])
                        sc2, sk2 = small[:, 40 + lt:41 + lt], ("small", 40 + lt)
                        RED(sc2, sq[:], ["sq5"], [sk2])
                        rstd_from_ss(sc2, sc2, 1.0 / 1024, sk2)
                        STT(t1[:], xo[:], sc2, A2[:], ALU.mult, ALU.mult, [xok, sk2, "A2"], ["sq5"])
                        TT(h2b[:], t1[:], SH2[:], ALU.add, ["sq5", "SH2"], ["h2b"])
                        pb_, pbk = npsb()
                        for k in range(8):
                            TR(pb_[:, k * 128:(k + 1) * 128], h2b[:, k * 128:(k + 1) * 128], ident_b[:],
                               ["h2b", "ident_b"], [pbk])
                        gt = b * 16 + lt
                        DMA(SP, h2_d[gt * 128:(gt + 1) * 128, :], h2b[:], ["h2b"], [("h2d", gt)])
                        CP(h2Tt[:], pb_[:].rearrange("p (k t) -> p k t", k=8), [pbk], ["h2Tt"], eng=ACT)
                        pt, pk = nps()
                        for k in range(8):
                            MM(pt[:, 0:32], h2Tt[:, k, :], wr_sb[:, k, :], k == 0, k == 7, ["h2Tt", "wr_sb"], [pk])
                        TT(Ls[:, gt, :], pt[:, 0:32], br_bc[:], ALU.add, [pk, "br_bc"], [("Ls", gt)])
                        P.op(DVE, lambda e, gt=gt: e.max(out=m8s[:, gt, :], in_=Ls[:, gt, :]), [("Ls", gt)], [("m8s", gt)])
            mx.close()
            mx = None
            P.barrier()
            if stop == "s5":
                break

        LsK = kk("Ls", range(32))
        m8K = kk("m8s", range(32))
        desti = sb("desti", [128, 128], I32)
        wk = sb("wk", [128, 32, 4])
        widx = sb("widx", [128, NB], I32)
        widx8 = sb("widx8", [128, 8, NB], I32)
        bidx = sb("bidx", [128, NB], I32)
        if stop in (None, "m1"):
            with contextlib.ExitStack() as m1:
                triS = sb("triS", [128, 128], stack=m1)
                DMA(SP, triS[:], triS_d, [], ["triS"])
                mask = sb("mask", [128, 1024], stack=m1)
                R1 = sb("R1", [128, 1024], stack=m1)
                cA = sb("cA", [128, 1024], stack=m1)
                cB = sb("cB", [128, 1024], stack=m1)
                cnt = sb("cnt", [128, 1024], stack=m1)
                dest = sb("dest", [128, 1024], stack=m1)
                tot = sb("tot", [128, 32], stack=m1)
                nblk = sb("nblk", [128, 32], stack=m1)
                pA = sb("pA", [128, 32], stack=m1)
                pB = sb("pB", [128, 32], stack=m1)
                pstart = sb("pstart", [128, 32], stack=m1)
                junk = sb("junk", [128, 32], stack=m1)
                destk = sb("destk", [128, 128], stack=m1)
                ejf = sb("ejf", [128, NB], stack=m1)
                wif = sb("wif", [128, NB], stack=m1)
                nm = sb("nm", [128, 32], stack=m1)
                exs = sb("exs", [128, 32, 4], stack=m1)
                ssum = sb("ssum", [128, 32], stack=m1)
                for gt in range(32):
                    TS(mask[:, gt * 32:(gt + 1) * 32], Ls[:, gt, :], m8s[:, gt, 3:4], None, ALU.is_ge, None,
                       [("Ls", gt), ("m8s", gt)], ["mask"])
                for hf in range(2):
                    cs = slice(hf * 512, (hf + 1) * 512)
                    pt, pk = nps()
                    MM(pt[:, :], triS[:], mask[:, cs], True, True, ["triS", "mask"], [pk])
                    CP(R1[:, cs], pt[:, :], [pk], ["R1"])
                    pt, pk = nps()
                    MM(pt[:, :], ones_f[:], mask[:, cs], True, True, ["ones_f", "mask"], [pk])
                    CP(cnt[:, cs], pt[:, :], [pk], ["cnt"])
                CP(cA[:], cnt[:], ["cnt"], ["cA"])
                a_, ak, b_, bk = cA, "cA", cB, "cB"
                for sft in (1, 2, 4, 8, 16):
                    w_ = sft * 32
                    CP(b_[:, 0:w_], a_[:, 0:w_], [ak], [bk])
                    TT(b_[:, w_:1024], a_[:, w_:1024], a_[:, 0:1024 - w_], ALU.add, [ak], [bk])
                    a_, ak, b_, bk = b_, bk, a_, ak
                incl, inck = a_, ak
                CP(tot[:], incl[:, 31 * 32:32 * 32], [inck], ["tot"])
                TT(dest[:], incl[:], cnt[:], ALU.subtract, [inck, "cnt"], ["dest"])
                TT(dest[:], dest[:], R1[:], ALU.add, ["dest", "R1"], ["dest"])
                MSET(nblk[:], 0.0, ["nblk"])
                for j in range(MAXB):
                    STT(nblk[:], tot[:], float(j * BLK), nblk[:], ALU.is_gt, ALU.add, ["tot", "nblk"], ["nblk"])
                CP(pA[:], nblk[:], ["nblk"], ["pA"])
                a_, ak, b_, bk = pA, "pA", pB, "pB"
                for sft in (1, 2, 4, 8, 16):
                    CP(b_[:, 0:sft], a_[:, 0:sft], [ak], [bk])
                    TT(b_[:, sft:32], a_[:, sft:32], a_[:, 0:32 - sft], ALU.add, [ak], [bk])
                    a_, ak, b_, bk = b_, bk, a_, ak
                pend, pendk = a_, ak
                TT(pstart[:], pend[:], nblk[:], ALU.subtract, [pendk, "nblk"], ["pstart"])
                TS(pstart[:], pstart[:], float(BLK), None, ALU.mult, None, ["pstart"], ["pstart"])
                for gt in range(32):
                    TT(dest[:, gt * 32:(gt + 1) * 32], dest[:, gt * 32:(gt + 1) * 32], pstart[:], ALU.add,
                       ["dest", "pstart"], ["dest"])
                for gt in range(32):
                    for k in range(4):
                        STT(junk[:], Ls[:, gt, :], m8s[:, gt, k:k + 1], dest[:, gt * 32:(gt + 1) * 32], ALU.is_equal, ALU.mult,
                            [("Ls", gt), ("m8s", gt), "dest"], ["junk"])
                        RED(destk[:, gt * 4 + k:gt * 4 + k + 1], junk[:], ["junk"], ["destk"])
                TS(destk[:], destk[:], 0.0, float(NB * BLK - 1), ALU.max, ALU.min, ["destk"], ["destk"])
                CP(desti[:], destk[:], ["destk"], ["desti"])
                TS(nm[:], m8s[:, :, 0], -1.0, None, ALU.mult, None, m8K, ["nm"])
                for gt in range(32):
                    ACTV(exs[:, gt, :], m8s[:, gt, 0:4], AF.Exp, [("m8s", gt), "nm"], ["exs"], bias=nm[:, gt:gt + 1])
                RED(ssum[:], exs[:], ["exs"], ["ssum"])
                RECIP(ssum[:], ssum[:], ["ssum"], ["ssum"])
                for gt in range(32):
                    TS(wk[:, gt, :], exs[:, gt, :], ssum[:, gt:gt + 1], None, ALU.mult, None, ["exs", "ssum"], ["wk"])
                for j in range(NB):
                    TS(junk[:], pend[:], float(j), None, ALU.is_le, None, [pendk], ["junk"])
                    RED(ejf[:, j:j + 1], junk[:], ["junk"], ["ejf"])
                TS(ejf[:], ejf[:], 31.0, None, ALU.min, None, ["ejf"], ["ejf"])
                CP(bidx[:], ejf[:], ["ejf"], ["bidx"])
                TS(wif[:], ejf[:], 128.0, pidx[:, 0:1], ALU.mult, ALU.add, ["ejf", "pidx"], ["wif"])
                CP(widx[:], wif[:], ["wif"], ["widx"])
                wif8 = sb("wif8", [128, 8, NB], stack=m1)
                for k in range(8):
                    TS(wif8[:, k, :], wif[:], 8.0, float(k), ALU.mult, ALU.add, ["wif"], ["wif8"])
                CP(widx8[:], wif8[:], ["wif8"], ["widx8"])
                dump("destk", destk[:], lambda d: d, ["destk"])
                dump("ejf", ejf[:], lambda d: d, ["ejf"])
                dump("wk", wk[:].rearrange("p a b -> p (a b)"), lambda d: d, ["wk"])
                dump("wif", wif[:], lambda d: d, ["wif"])
                h2t = [sb(f"h2t{i}", [128, 1024], BF16, stack=m1) for i in range(2)]
                for gt in range(32 if stop is None else 0):
                    ht, htk = h2t[gt % 2], ("h2t", gt % 2)
                    DMA(SP, ht[:], h2_d[gt * 128:(gt + 1) * 128, :], [("h2d", gt)], [htk])
                    for k in range(4):
                        c_ = gt * 4 + k
                        P.dma(POOL, lambda e, ht=ht, c_=c_: e.indirect_dma_start(
                            out=xs_d, out_offset=bass.IndirectOffsetOnAxis(ap=desti[:, c_:c_ + 1], axis=0),
                            in_=ht[:], in_offset=None),
                            [htk, "desti"], ["xs"])
            P.barrier()

        if stop is None:
            with contextlib.ExitStack() as m4:
                wgu = [sb(f"wgu{i}", [128, 8, 2048], BF16, stack=m4) for i in range(2)]
                wdn = [sb(f"wdn{i}", [128, 8, 1024], BF16, stack=m4) for i in range(2)]
                bgt = [sb(f"bgt{i}", [128, 16], stack=m4) for i in range(2)]
                bdt = [sb(f"bdt{i}", [128, 1024], stack=m4) for i in range(2)]
                xbs = [sb(f"xb{i}", [128, 4, 1024], BF16, stack=m4) for i in range(2)]
                xT = sb("xT", [128, 8, 512], BF16, stack=m4)
                act = sb("act", [128, 8, 512], BF16, stack=m4)
                g1 = [sb(f"g1_{i}", [128, 512], stack=m4) for i in range(2)]
                sg = [sb(f"sg_{i}", [128, 512], stack=m4) for i in range(2)]
                u1 = [sb(f"u1_{i}", [128, 512], stack=m4) for i in range(2)]
                ybt = [sb(f"ybt{i}", [128, 1024], stack=m4) for i in range(2)]

                def load_w(j):
                    r_ = j % 2
                    ix = widx[:, j:j + 1]
                    for k in range(8):
                        P.dma(POOL, lambda e, k=k: e.indirect_dma_start(
                            out=wgu[r_][:, k, :], out_offset=None, in_=wgu_d,
                            in_offset=bass.IndirectOffsetOnAxis(ap=widx8[:, k, j:j + 1], axis=0)),
                            ["widx8"], [("wgu", r_, k)])
                    for k in range(8):
                        P.dma(POOL, lambda e, k=k: e.indirect_dma_start(
                            out=wdn[r_][:, k, :], out_offset=None, in_=wdn_d,
                            in_offset=bass.IndirectOffsetOnAxis(ap=widx8[:, k, j:j + 1], axis=0)),
                            ["widx8"], [("wdn", r_, k)])
                    P.dma(POOL, lambda e: e.indirect_dma_start(
                        out=bgt[r_][:], out_offset=None, in_=bgu_d,
                        in_offset=bass.IndirectOffsetOnAxis(ap=ix, axis=0)), ["widx"], [("bgt", r_)])
                    P.dma(POOL, lambda e: e.indirect_dma_start(
                        out=bdt[r_][:], out_offset=None, in_=bdn_d,
                        in_offset=bass.IndirectOffsetOnAxis(ap=bidx[:, j:j + 1], axis=0)), ["bidx"], [("bdt", r_)])

                load_w(0)
                rot = 0
                for j in range(NB):
                    r_ = j % 2
                    if j + 1 < NB:
                        load_w(j + 1)
                    xb, xbk = xbs[j % 2], ("xb", j % 2)
                    if j == 0:
                        DMA(SP, xb[:], xs_d[0:BLK, :].rearrange("(a p) n -> p a n", p=128), ["xs"], [xbk])
                    if j + 1 < NB:
                        DMA(SP, xbs[(j + 1) % 2][:], xs_d[(j + 1) * BLK:(j + 2) * BLK, :].rearrange("(a p) n -> p a n", p=128),
                            ["xs"], [("xb", (j + 1) % 2)])
                    for a in range(4):
                        pb_, pbk = npsb()
                        for k in range(8):
                            TR(pb_[:, k * 128:(k + 1) * 128], xb[:, a, k * 128:(k + 1) * 128], ident_b[:], [xbk, "ident_b"], [pbk])
                        CP(xT[:, :, a * 128:(a + 1) * 128], pb_[:].rearrange("p (k t) -> p k t", k=8), [pbk], ["xT"],
                           eng=ACT if a % 2 == 0 else DVE)
                    for fb in range(8):
                        q_ = rot % 2
                        rot += 1
                        pg, pgk = nps()
                        for k in range(8):
                            MM(pg[:, :], wgu[r_][:, k, fb * 128:(fb + 1) * 128], xT[:, k, :], k == 0, k == 7,
                               [("wgu", r_, k), "xT"], [pgk])
                        pu, puk = nps()
                        for k in range(8):
                            MM(pu[:, :], wgu[r_][:, k, 1024 + fb * 128:1024 + (fb + 1) * 128], xT[:, k, :], k == 0, k == 7,
                               [("wgu", r_, k), "xT"], [puk])
                        TS(g1[q_][:], pg[:, :], bgt[r_][:, fb:fb + 1], 7.0, ALU.add, ALU.min, [pgk, ("bgt", r_)], [("g1", q_)])
                        ACTV(sg[q_][:], g1[q_][:], AF.Sigmoid, [("g1", q_)], [("sg", q_)], scale=1.702)
                        TS(u1[q_][:], pu[:, :], bgt[r_][:, 8 + fb:9 + fb], 7.0, ALU.add, ALU.min, [puk, ("bgt", r_)], [("u1", q_)])
                        TS(u1[q_][:], u1[q_][:], -7.0, 1.0, ALU.max, ALU.add, [("u1", q_)], [("u1", q_)])
                        TT(g1[q_][:], g1[q_][:], sg[q_][:], ALU.mult, [("g1", q_), ("sg", q_)], [("g1", q_)])
                        TT(act[:, fb, :], g1[q_][:], u1[q_][:], ALU.mult, [("g1", q_), ("u1", q_)], ["act"])
                    for a in range(4):
                        y_, yk = ybt[a % 2], ("ybt", a % 2)
                        for dh in range(2):
                            py, pyk = nps()
                            for fb in range(8):
                                MM(py[:, :], act[:, fb, a * 128:(a + 1) * 128], wdn[r_][:, fb, dh * 512:(dh + 1) * 512],
                                   fb == 0, fb == 7, ["act", ("wdn", r_, fb)], [pyk])
                            TT(y_[:, dh * 512:(dh + 1) * 512], py[:, :], bdt[r_][:, dh * 512:(dh + 1) * 512], ALU.add,
                               [pyk, ("bdt", r_)], [yk])
                        DMA(SP, yb_d[j * BLK + a * 128:j * BLK + (a + 1) * 128, :], y_[:], [yk], ["yb"])
            P.barrier()

            with contextlib.ExitStack() as m5:
                G2 = [sb(f"G2_{i}", [128, 1024], stack=m5) for i in range(2)]
                ybk = [sb(f"ybk{i}", [128, 1024], stack=m5) for i in range(8)]
                ym = sb("ym", [128, 1024], stack=m5)
                sq = sb("sq7", [128, 1024], stack=m5)
                x1r = [sb(f"x1r{i}", [128, 1024], stack=m5) for i in range(2)]
                ot = [sb(f"ot{i}", [128, 1024], stack=m5) for i in range(2)]
                for b in range(nb):
                    load_bc(G2[b], b * 6 + 5, ("G2", b))
                for gt in range(nb * 16):
                    b, lt = gt // 16, gt % 16
                    ys = []
                    for k in range(4):
                        i_ = (gt % 2) * 4 + k
                        c_ = gt * 4 + k
                        P.dma(POOL, lambda e, i_=i_, c_=c_: e.indirect_dma_start(
                            out=ybk[i_][:], out_offset=None, in_=yb_d,
                            in_offset=bass.IndirectOffsetOnAxis(ap=desti[:, c_:c_ + 1], axis=0)),
                            ["yb", "desti"], [("ybk", i_)])
                        ys.append((ybk[i_], ("ybk", i_)))
                    TS(ym[:], ys[0][0][:], wk[:, gt, 0:1], None, ALU.mult, None, [ys[0][1], "wk"], ["ym"])
                    for k in range(1, 4):
                        STT(ym[:], ys[k][0][:], wk[:, gt, k:k + 1], ym[:], ALU.mult, ALU.add, [ys[k][1], "wk", "ym"], ["ym"])
                    ACTV(sq[:], ym[:], AF.Square, ["ym"], ["sq7"])
                    sc, sk = small[:, 20 + lt:21 + lt], ("small", 20 + lt)
                    RED(sc, sq[:], ["sq7"], [sk])
                    rstd_from_ss(sc, sc, 1.0 / 1024, sk)
                    xr, xrk = x1r[gt % 2], ("x1r", gt % 2)
                    DMA(SP, xr[:], x1_d[b, lt * 128:(lt + 1) * 128, :], [("x1d", b, lt)], [xrk])
                    STT(sq[:], ym[:], sc, G2[b][:], ALU.mult, ALU.mult, ["ym", sk, ("G2", b), "sq7"], ["sq7"])
                    o_, ok_ = ot[gt % 2], ("ot", gt % 2)
                    TT(o_[:], sq[:], xr[:], ALU.add, ["sq7", xrk], [ok_])
                    out_toks.append(DMA(SP, out_d[b, lt * 128:(lt + 1) * 128, :], o_[:], [ok_], []))

        if mx is not None:
            mx.close()
        P.final_wait(SP, out_toks)
        P.emit()
    return nc


def _c(a):
    return np.ascontiguousarray(a, dtype=np.float32)


def prep_shared(inp):
    w_in = inp["w_in"][0]
    sh = {}
    sh["w_ada"] = _c(inp["w_ada"][0].reshape(8, 128, 12, 512).transpose(2, 1, 0, 3))
    sh["b_ada"] = _c(inp["b_ada"].reshape(1, 6144))
    sh["nrm"] = _c(np.concatenate([inp["norm_mix_pre"][0], inp["norm_mix_post"][0],
                                   inp["norm_ffn_pre"][0], inp["norm_ffn_post"][0]]).reshape(1, 4096))
    sh["w_in"] = _c(w_in[:, :6656].reshape(8, 128, 52, 128).transpose(2, 1, 0, 3))
    sh["w_mg"] = _c(w_in[:, 6656:].reshape(8, 128, 16).transpose(1, 0, 2))
    sh["b_in_col"] = _c(inp["b_in"][0, :6656].reshape(52, 128).T)
    sh["b_in_row"] = _c(inp["b_in"].reshape(1, 6672))
    sh["conv_col"] = _c(inp["conv_w"][0].reshape(9, 8, 128).transpose(2, 1, 0))
    sh["lb_raw"] = _c(inp["lb_raw"].reshape(1, 2048))
    sh["m_norm"] = _c(inp["m_norm"].reshape(1, 512))
    sh["h_norm"] = _c(inp["h_norm"].reshape(1, 512))
    sh["w_pa"] = _c(inp["w_pa"][0].reshape(4, 128, 1024).transpose(1, 0, 2))
    sh["w_pb"] = _c(inp["w_pb"][0].reshape(4, 128, 1024).transpose(1, 0, 2))
    sh["w_out"] = _c(inp["w_out"][0].reshape(8, 128, 1024).transpose(1, 0, 2))
    sh["w_router"] = _c(inp["w_router"][0].reshape(8, 128, 32).transpose(1, 0, 2))
    sh["b_router"] = _c(inp["b_router"].reshape(1, 32))
    sh["w_gu"] = _c(inp["w_gu"][0].reshape(32, 8, 128, 2048).transpose(0, 2, 1, 3)).reshape(32 * 128 * 8, 2048)
    sh["b_gu_col"] = _c(inp["b_gu"][0].reshape(32, 16, 128).transpose(0, 2, 1)).reshape(32 * 128, 16)
    sh["w_dn"] = _c(inp["w_dn"][0].reshape(32, 8, 128, 1024).transpose(0, 2, 1, 3)).reshape(32 * 128 * 8, 1024)
    sh["b_dn"] = _c(inp["b_dn"][0])
    sh["ident"] = np.eye(128, dtype=np.float32)
    sh["triF"] = np.triu(np.ones((128, 128), np.float32))
    sh["triB"] = np.tril(np.ones((128, 128), np.float32))
    sh["ones"] = np.ones((128, 128), np.float32)
    sh["pidx"] = np.arange(128, dtype=np.float32).reshape(128, 1)
    sh["triS"] = np.triu(np.ones((128, 128), np.float32), 1)
    return sh


def core_inputs(inp, sh, i):
    m = dict(sh)
    m["x"] = _c(inp["x"][2 * i:2 * i + 2])
    m["ctx"] = _c(inp["ctx"][2 * i:2 * i + 2])
    cv = np.stack([inp["c"][2 * i], inp["c"][2 * i + 1], inp["c_ctx"]])
    m["cvT"] = _c(cv.reshape(3, 8, 128).transpose(2, 0, 1).reshape(128, 24))
    return m


def kernel(**inputs):
    inp = {k: np.asarray(v) for k, v in inputs.items()}
    sh = prep_shared(inp)
    nc = build_nc()
    in_maps = [core_inputs(inp, sh, i) for i in range(N_CORES)]
    res = run_bass_kernel_spmd(nc, in_maps, core_ids=list(range(N_CORES)))
    out = np.concatenate([np.asarray(r["out"], dtype=np.float32) for r in res.results], axis=0)
    return out
```

```python
import contextlib
import numpy as np
import concourse.bass as bass
import concourse.mybir as mybir
from concourse.bass_utils import run_bass_kernel_spmd

F32 = mybir.dt.float32
BF16 = mybir.dt.bfloat16
I32 = mybir.dt.int32
AF = mybir.ActivationFunctionType
ALU = mybir.AluOpType
AX = mybir.AxisListType

PE, DVE, ACT, POOL, SP = "pe", "dve", "act", "pool", "sp"
COMPUTE = (PE, DVE, ACT, POOL)
EPOCH = 12000
N_DMA_SEM = 88
DMA_POOLS = {"sp": (0, 32), "act": (32, 8), "pool": (40, 48)}
N_EPOCH_SEM = 12
EPS = 1e-6
NT = 18
BLK = 512
NB = 4096 * 4 // BLK + 32
MAXB = 4096 // BLK
TOK = 2304
N_CORES = 8


class Prog:
    def __init__(self, nc, same_engine_sync=True):
        self.nc = nc
        self.same = same_engine_sync
        self.streams = {e: [] for e in (PE, DVE, ACT, POOL, SP)}
        self.count = {e: 0 for e in COMPUTE}
        self.waited = {}
        self.state = {}
        self.dma_tot = [0] * N_DMA_SEM
        self.dma_rr = {q: 0 for q in DMA_POOLS}
        self.barrier_toks = set()

    def barrier(self):
        toks = set()
        for e in COMPUTE:
            c = self.count[e]
            if c > 0:
                toks.add(((e, (c - 1) // EPOCH), ((c - 1) % EPOCH) + 1))
        for s_ in range(N_DMA_SEM):
            if self.dma_tot[s_] > 0:
                toks.add((("dma", s_), self.dma_tot[s_]))
        self.barrier_toks = toks

    def _deps(self, reads, writes):
        deps = set()
        for k in reads:
            st = self.state.get(k)
            if st and st[0]:
                deps.add(st[0])
        for k in writes:
            st = self.state.get(k)
            if st:
                if st[0]:
                    deps.add(st[0])
                deps.update(st[1])
        return deps

    def _commit(self, reads, writes, tok):
        for k in writes:
            self.state[k] = [tok, []]
        for k in reads:
            if k in writes:
                continue
            st = self.state.setdefault(k, [None, []])
            st[1].append(tok)
            if len(st[1]) > 64:
                best = {}
                for (key, val) in st[1]:
                    if best.get(key, 0) < val:
                        best[key] = val
                st[1] = list(best.items())

    def _waits(self, eng, deps, own_key=None):
        best = {}
        for (key, val) in deps:
            if key == own_key and not self.same:
                continue
            if self.waited.get((eng, key), 0) >= val:
                continue
            if best.get(key, 0) < val:
                best[key] = val
        out = []
        for key, val in best.items():
            self.waited[(eng, key)] = val
            out.append((key, val))
        return out

    def op(self, eng, fn, reads=(), writes=()):
        reads, writes = tuple(reads), tuple(writes)
        c = self.count[eng]
        own_key = (eng, c // EPOCH)
        deps = self._deps(reads, writes) | self.barrier_toks
        if eng == PE:
            deps = {d for d in deps if d[0][0] != PE}
        waits = self._waits(eng, deps, own_key)
        self.count[eng] = c + 1
        tok = (own_key, (c % EPOCH) + 1)
        self.streams[eng].append(("op", waits, fn, own_key))
        self._commit(reads, writes, tok)

    def dma(self, q, fn, reads=(), writes=()):
        reads, writes = tuple(reads), tuple(writes)
        first, cnt_ = DMA_POOLS[q]
        s = first + self.dma_rr[q]
        self.dma_rr[q] = (self.dma_rr[q] + 1) % cnt_
        key = ("dma", s)
        deps = self._deps(reads, writes) | self.barrier_toks
        if self.dma_tot[s] > 0:
            deps.add((key, self.dma_tot[s]))
        waits = self._waits(q, deps, None)
        self.dma_tot[s] += 16
        tok = (key, self.dma_tot[s])
        self.streams[q].append(("dma", waits, fn, key))
        self._commit(reads, writes, tok)
        return tok

    def final_wait(self, eng, toks):
        waits = self._waits(eng, set(toks), None)
        self.streams[eng].append(("wait", waits, None, None))

    def emit(self):
        nc = self.nc
        with contextlib.ExitStack() as es:
            sems = {}
            for e in COMPUTE:
                nep = self.count[e] // EPOCH + 1
                assert nep <= N_EPOCH_SEM, (e, self.count[e])
                for i in range(nep):
                    sems[(e, i)] = es.enter_context(nc.semaphore(f"s_{e}_{i}"))
            for s in range(N_DMA_SEM):
                if self.dma_tot[s] > 0:
                    sems[("dma", s)] = es.enter_context(nc.semaphore(f"s_dma_{s}"))
            block = es.enter_context(nc.Block())

            def run(eng_name):
                def body(engine):
                    for kind, waits, fn, key in self.streams[eng_name]:
                        for (k, v) in waits:
                            engine.wait_ge(sems[k], v)
                        if kind == "op":
                            fn(engine).then_inc(sems[key], 1)
                        elif kind == "dma":
                            fn(engine).then_inc(sems[key], 16)
                return body

            block.sync(run(SP))
            block.tensor(run(PE))
            block.vector(run(DVE))
            block.scalar(run(ACT))
            block.gpsimd(run(POOL))


def kk(name, idxs):
    return [(name, i) for i in idxs]


def build_nc(nb=2, dumps=(), stop=None, n_exp=32):
    nc = bass.Bass("TRN2", target_bir_lowering=False)
    P = Prog(nc)
    D = {}

    def din(name, shape, dt=F32):
        D[name] = nc.dram_tensor(name, list(shape), dt, kind="ExternalInput").ap()
        return D[name]

    x_d = din("x", [2, 2048, 1024])
    ctx_d = din("ctx", [2, 256, 1024])
    cvT_d = din("cvT", [128, 24])
    wada_d = din("w_ada", [12, 128, 8, 512])
    bada_d = din("b_ada", [1, 6144])
    nrm_d = din("nrm", [1, 4096])
    win_d = din("w_in", [52, 128, 8, 128])
    wmg_d = din("w_mg", [128, 8, 16])
    bcol_d = din("b_in_col", [128, 52])
    brow_d = din("b_in_row", [1, 6672])
    conv_d = din("conv_col", [128, 8, 9])
    lb_d = din("lb_raw", [1, 2048])
    mn_d = din("m_norm", [1, 512])
    hn_d = din("h_norm", [1, 512])
    wpa_d = din("w_pa", [128, 4, 1024])
    wpb_d = din("w_pb", [128, 4, 1024])
    wout_d = din("w_out", [128, 8, 1024])
    wr_d = din("w_router", [128, 8, 32])
    br_d = din("b_router", [1, 32])
    wgu_d = din("w_gu", [32 * 128 * 8, 2048])
    bgu_d = din("b_gu_col", [32 * 128, 16])
    wdn_d = din("w_dn", [32 * 128 * 8, 1024])
    bdn_d = din("b_dn", [32, 1024])
    ident_d = din("ident", [128, 128])
    triF_d = din("triF", [128, 128])
    triB_d = din("triB", [128, 128])
    ones_d = din("ones", [128, 128])
    pidx_d = din("pidx", [128, 1])
    jidx_d = din("jidx", [1, NB])
    triS_d = din("triS", [128, 128])
    out_d = nc.dram_tensor("out", [2, 2048, 1024], F32, kind="ExternalOutput").ap()
    bc_d = nc.dram_tensor("bc_scr", [14, 128, 1024], F32).ap()
    x1_d = nc.dram_tensor("x1_scr", [2, 2048, 1024], F32).ap()
    h2_d = nc.dram_tensor("h2_scr", [4096, 1024], BF16).ap()
    xs_d = nc.dram_tensor("xs_scr", [NB * BLK, 1024], BF16).ap()
    yb_d = nc.dram_tensor("yb_scr", [NB * BLK, 1024], F32).ap()
    dump_d = {}
    for (nm, shape) in dumps:
        dump_d[nm] = nc.dram_tensor("dbg_" + nm, list(shape), F32, kind="ExternalOutput").ap()
    out_toks = []

    def MM(out, lhsT, rhs, start, stop, reads, writes):
        P.op(PE, lambda e: e.matmul(out, lhsT=lhsT, rhs=rhs, start=start, stop=stop), reads, writes)

    def TR(out, in_, ident, reads, writes):
        P.op(PE, lambda e: e.transpose(out, in_, ident), reads, writes)

    def ACTV(out, in_, func, reads, writes, bias=None, scale=None):
        kw = {}
        if bias is not None:
            kw["bias"] = bias
        if scale is not None:
            kw["scale"] = scale
        P.op(ACT, lambda e: e.activation(out=out, in_=in_, func=func, **kw), reads, writes)

    def TS(out, in0, s1, s2, op0, op1, reads, writes, eng=DVE):
        if s2 is None:
            P.op(eng, lambda e: e.tensor_scalar(out=out, in0=in0, scalar1=s1, scalar2=None, op0=op0), reads, writes)
        else:
            P.op(eng, lambda e: e.tensor_scalar(out=out, in0=in0, scalar1=s1, scalar2=s2, op0=op0, op1=op1),
                 reads, writes)

    def TT(out, in0, in1, op, reads, writes, eng=DVE):
        P.op(eng, lambda e: e.tensor_tensor(out=out, in0=in0, in1=in1, op=op), reads, writes)

    def STT(out, in0, scalar, in1, op0, op1, reads, writes):
        P.op(DVE, lambda e: e.scalar_tensor_tensor(out=out, in0=in0, scalar=scalar, in1=in1, op0=op0, op1=op1),
             reads, writes)

    def CP(out, in_, reads, writes, eng=DVE):
        if eng == ACT:
            P.op(ACT, lambda e: e.activation(out=out, in_=in_, func=AF.Copy), reads, writes)
        else:
            P.op(eng, lambda e: e.tensor_copy(out=out, in_=in_), reads, writes)

    def RED(out, in_, reads, writes):
        P.op(DVE, lambda e: e.tensor_reduce(out=out, in_=in_, axis=AX.X, op=ALU.add), reads, writes)

    def RECIP(out, in_, reads, writes):
        P.op(DVE, lambda e: e.reciprocal(out=out, in_=in_), reads, writes)

    def MSET(ap, val, writes, eng=DVE):
        P.op(eng, lambda e: e.memset(ap, val), (), writes)

    def DMA(q, out, in_, reads, writes):
        return P.dma(q, lambda e: e.dma_start(out=out, in_=in_), reads, writes)

    def bcast(ap1n, n):
        return ap1n.partition_broadcast(128)

    def rstd_from_ss(rs, ss, inv_n, key):
        TS(rs, ss, inv_n, EPS, ALU.mult, ALU.add, [key], [key])
        ACTV(rs, rs, AF.Sqrt, [key], [key])
        RECIP(rs, rs, [key], [key])

    with contextlib.ExitStack() as top:
        uniq = [0]

        def sb(name, shape, dt=F32, stack=top):
            uniq[0] += 1
            return stack.enter_context(nc.sbuf_tensor(f"sb_{name}_{uniq[0]}", list(shape), dt))

        psf = [top.enter_context(nc.psum_tensor(f"psf{i}", [128, 512], F32)) for i in range(6)]
        psb = [top.enter_context(nc.psum_tensor(f"psb{i}", [128, 1024], BF16)) for i in range(2)]
        rr = {"f": 0, "b": 0, "t": 0}

        def nps():
            i = rr["f"]
            rr["f"] = (i + 1) % 6
            return psf[i], ("psf", i)

        def npsb():
            i = rr["b"]
            rr["b"] = (i + 1) % 2
            return psb[i], ("psb", i)

        ident_f = sb("ident_f", [128, 128])
        ident_b = sb("ident_b", [128, 128], BF16)
        triF = sb("triF", [128, 128])
        triB = sb("triB", [128, 128])
        ones_f = sb("ones_f", [128, 128])
        bcol = sb("bcol", [128, 52])
        convc = sb("convc", [128, 8, 9])
        lb_bc = sb("lb_bc", [128, 2, 512])
        oml_bc = sb("oml_bc", [128, 2, 512])
        mn_bc = sb("mn_bc", [128, 512])
        hn_bc = sb("hn_bc", [128, 512])
        br_bc = sb("br_bc", [128, 32])
        wr_sb = sb("wr_sb", [128, 8, 32], BF16)
        wmg_sb = sb("wmg_sb", [128, 8, 16], BF16)
        bmg_bc = sb("bmg_bc", [128, 16])
        DMA(SP, ident_f[:], ident_d, [], ["ident_f"])
        DMA(POOL, ident_b[:], ident_d, [], ["ident_b"])
        DMA(SP, triF[:], triF_d, [], ["triF"])
        DMA(SP, triB[:], triB_d, [], ["triB"])
        DMA(SP, ones_f[:], ones_d, [], ["ones_f"])
        DMA(SP, bcol[:], bcol_d, [], ["bcol"])
        DMA(SP, convc[:], conv_d, [], ["convc"])
        DMA(SP, mn_bc[:], bcast(mn_d, 512), [], ["mn_bc"])
        DMA(SP, hn_bc[:], bcast(hn_d, 512), [], ["hn_bc"])
        DMA(SP, br_bc[:], bcast(br_d, 32), [], ["br_bc"])
        DMA(POOL, wr_sb[:], wr_d, [], ["wr_sb"])
        DMA(POOL, wmg_sb[:], wmg_d, [], ["wmg_sb"])
        DMA(SP, bmg_bc[:], bcast(brow_d[:, 6656:6672], 16), [], ["bmg_bc"])
        lbf = lb_bc[:].rearrange("p a b -> p (a b)")
        omlf = oml_bc[:].rearrange("p a b -> p (a b)")
        with contextlib.ExitStack() as sl:
            lbr = sb("lbr", [128, 2048], stack=sl)
            DMA(SP, lbr[:], bcast(lb_d, 2048), [], ["lbr"])
            TT(lbf, lbr[:, 0:1024], lbr[:, 1024:2048], ALU.subtract, ["lbr"], ["lb_bc"])
            ACTV(lbf, lbf, AF.Sigmoid, ["lb_bc"], ["lb_bc"])
            TS(omlf, lbf, -1.0, 1.0, ALU.mult, ALU.add, ["lb_bc"], ["oml_bc"])
        P.barrier()

        with contextlib.ExitStack() as s0:
            cT = sb("cT", [128, 24], stack=s0)
            sg0 = sb("sg0", [128, 24], stack=s0)
            cb = sb("cb", [128, 24, 128], stack=s0)
            nrm_bc = sb("nrm_bc", [128, 4, 1024], stack=s0)
            wa = [sb(f"wa{i}", [128, 8, 512], stack=s0) for i in range(2)]
            ba = [sb(f"ba{i}", [128, 512], stack=s0) for i in range(2)]
            tA = [sb(f"tA{i}", [128, 512], stack=s0) for i in range(3)]
            tB = [sb(f"tB{i}", [128, 512], stack=s0) for i in range(3)]
            DMA(SP, cT[:], cvT_d, [], ["cT"])
            DMA(SP, nrm_bc[:].rearrange("p a b -> p (a b)"), bcast(nrm_d, 4096), [], ["nrm_bc"])
            ACTV(sg0[:], cT[:], AF.Sigmoid, ["cT"], ["sg0"])
            TT(cT[:], cT[:], sg0[:], ALU.mult, ["cT", "sg0"], ["cT"])
            for i in range(24):
                TS(cb[:, i, :], ones_f[:], cT[:, i:i + 1], None, ALU.mult, None, ["ones_f", "cT"], [("cb", i)])
            ti_rot = 0
            for cbi in range(12):
                w, half = cbi // 2, cbi % 2
                cs = slice(half * 512, (half + 1) * 512)
                wt, bt = wa[cbi % 2], ba[cbi % 2]
                DMA(SP, wt[:], wada_d[cbi], [], [("wa", cbi % 2)])
                DMA(SP, bt[:], bcast(bada_d[:, cbi * 512:(cbi + 1) * 512], 512), [], [("ba", cbi % 2)])
                for r in range(3):
                    if r == 2 and w > 1:
                        continue
                    pt, pk = nps()
                    for k in range(8):
                        MM(pt[:, :], cb[:, r * 8 + k, :], wt[:, k, :], k == 0, k == 7,
                           [("cb", r * 8 + k), ("wa", cbi % 2)], [pk])
                    a_, b_ = tA[ti_rot % 3], tB[ti_rot % 3]
                    ka, kb = ("tA", ti_rot % 3), ("tB", ti_rot % 3)
                    ti_rot += 1
                    TT(a_[:], pt[:, :], bt[:], ALU.add, [pk, ("ba", cbi % 2)], [ka])
                    if w in (1, 4):
                        STT(b_[:], a_[:], 1.0, nrm_bc[:, 0 if w == 1 else 2, cs], ALU.add, ALU.mult,
                            [ka, "nrm_bc"], [kb])
                        src, ksrc = b_, kb
                    elif w in (2, 5):
                        TT(b_[:], a_[:], nrm_bc[:, 1 if w == 2 else 3, cs], ALU.mult, [ka, "nrm_bc"], [kb])
                        src, ksrc = b_, kb
                    else:
                        src, ksrc = a_, ka
                    tidx = r * 6 + w if r < 2 else 12 + w
                    DMA(SP, bc_d[tidx, :, cs], src[:], [ksrc], [("bc", tidx, half)])

        P.barrier()

        def load_bc(dst, tidx, key):
            DMA(SP, dst[:], bc_d[tidx], [("bc", tidx, 0), ("bc", tidx, 1)], [key])

        Ls = sb("Ls", [128, 32, 32])
        m8s = sb("m8s", [128, 32, 8])
        pidx = sb("pidx", [128, 1])
        DMA(SP, pidx[:], pidx_d, [], ["pidx"])
        small = sb("small", [128, 64])

        def hTk(t0, t1):
            return kk("hT", range(t0 // 128, (t1 + 127) // 128))

        def dump(nm, ap_sb, dst, reads):
            if nm in dump_d:
                out_toks.append(DMA(POOL, dst(dump_d[nm]), ap_sb, reads, []))

        mx = None
        for b in range(nb):
            mx = contextlib.ExitStack()
            hT = sb("hT", [128, 8, TOK], BF16, stack=mx)
            aT = sb("aT", [128, 4, 2048], BF16, stack=mx)
            bT = sb("bT", [128, 4, 2048], BF16, stack=mx)
            with contextlib.ExitStack() as s1:
                A1 = sb("A1", [128, 1024], stack=s1)
                SH1 = sb("SH1", [128, 1024], stack=s1)
                A1c = sb("A1c", [128, 1024], stack=s1)
                SH1c = sb("SH1c", [128, 1024], stack=s1)
                xin = [sb(f"xin{i}", [128, 1024], stack=s1) for i in range(2)]
                sq = sb("sq", [128, 1024], stack=s1)
                tmp = sb("tmp1", [128, 1024], stack=s1)
                hb = [sb(f"hb{i}", [128, 1024], BF16, stack=s1) for i in range(2)]
                load_bc(A1, b * 6 + 1, "A1")
                load_bc(SH1, b * 6 + 0, "SH1")
                load_bc(A1c, 13, "A1c")
                load_bc(SH1c, 12, "SH1c")
                for ti in range(NT):
                    xi, xk = xin[ti % 2], ("xin", ti % 2)
                    src = ctx_d[b, ti * 128:(ti + 1) * 128, :] if ti < 2 else x_d[b, (ti - 2) * 128:(ti - 1) * 128, :]
                    DMA(SP, xi[:], src, [], [xk])
                    ACTV(sq[:], xi[:], AF.Square, [xk], ["sq"])
                    sc, sk = small[:, ti:ti + 1], ("small", ti)
                    RED(sc, sq[:], ["sq"], [sk])
                    rstd_from_ss(sc, sc, 1.0 / 1024, sk)
                    Am, Ak, Sm, Sk = (A1c, "A1c", SH1c, "SH1c") if ti < 2 else (A1, "A1", SH1, "SH1")
                    STT(tmp[:], xi[:], sc, Am[:], ALU.mult, ALU.mult, [xk, sk, Ak], ["tmp1"])
                    hbt, hbk = hb[ti % 2], ("hb", ti % 2)
                    TT(hbt[:], tmp[:], Sm[:], ALU.add, ["tmp1", Sk], [hbk])
                    pb_, pbk = npsb()
                    for k in range(8):
                        TR(pb_[:, k * 128:(k + 1) * 128], hbt[:, k * 128:(k + 1) * 128], ident_b[:],
                           [hbk, "ident_b"], [pbk])
                    CP(hT[:, :, ti * 128:(ti + 1) * 128], pb_[:].rearrange("p (k t) -> p k t", k=8),
                       [pbk], [("hT", ti)], eng=ACT)
            P.barrier()
            dump("hT", hT[:, 0, :], lambda d: d, kk("hT", range(NT)))
            if stop == "s1":
                break

            with contextlib.ExitStack() as sm:
                wfm = [sb(f"wfm{i}", [128, 8, 128], BF16, stack=sm) for i in range(2)]
                srcf = sb("srcf", [128, TOK], stack=sm)
                accf = sb("accf", [128, TOK], stack=sm)
                stm = [sb(f"stm{i}", [128, 128], BF16, stack=sm) for i in range(2)]
                Pst = [sb(f"Pst{i}", [128, 129], stack=sm) for i in range(2)]
                Sbf = [sb(f"Sbf{i}", [128, 129], BF16, stack=sm) for i in range(2)]
                Sbf2 = [sb(f"Sbf2{i}", [128, 129], BF16, stack=sm) for i in range(2)]
                ssh = sb("ssh", [128, 16], stack=sm)
                tmpf = sb("tmpf", [128, 16, 128], stack=sm)
                a_tm = sb("a_tm", [128, 16, 128], BF16, stack=sm)

                def proj_fm(chunk, wt, wk, dst, dk):
                    DMA(POOL, wt[:], win_d[chunk], [], [wk])
                    for g0 in range(0, TOK, 512):
                        n = min(512, TOK - g0)
                        pt, pk = nps()
                        for k in range(8):
                            MM(pt[:, 0:n], wt[:, k, :], hT[:, k, g0:g0 + n], k == 0, k == 7,
                               [wk] + hTk(g0, g0 + n), [pk])
                        ACTV(dst[:, g0:g0 + n], pt[:, 0:n], AF.Identity, [pk, "bcol"], [dk],
                             bias=bcol[:, chunk:chunk + 1])

                def scan(d, QT, qk, q_lat_only, KT, ktk, Ktm, ktmk, V, vk, nv, dec_of, deck, post):
                    order = [0, 1] + list(range(2, NT)) if d == 0 else [1, 0] + list(range(NT - 1, 1, -1))
                    n_ = len(order)
                    mask, mkey = (triF, "triF") if d == 0 else (triB, "triB")
                    Pt, Pk = Pst[d], ("Pst", d)
                    Sts = [(Sbf[d], ("Sbf", d)), (Sbf2[d], ("Sbf2", d))]

                    def emit_U(i):
                        c_ = order[i]
                        pu, puk = nps()
                        MM(pu[:, 0:nv], Ktm[:, c_, :], V[:, c_, 0:nv], True, True, [ktmk, vk], [puk])
                        return pu, puk

                    nxt = emit_U(0)
                    for idx, c in enumerate(order):
                        cs = slice(c * 128, (c + 1) * 128)
                        qs = slice((c - 2) * 128, (c - 1) * 128) if q_lat_only else cs
                        if idx < n_ - 1:
                            pu, puk = nxt
                            St, Sk = Sts[idx % 2]
                            if idx == 0:
                                CP(Pt[:, 0:nv], pu[:, 0:nv], [puk], [Pk])
                            else:
                                STT(Pt[:, 0:nv], Pt[:, 0:nv], dec_of(order[idx - 1]), pu[:, 0:nv], ALU.mult, ALU.add,
                                    [Pk, puk, deck], [Pk])
                            TS(St[:, 0:nv], Pt[:, 0:nv], dec_of(c), None, ALU.mult, None, [Pk, deck], [Sk])
                            if idx + 1 < n_ - 1:
                                nxt = emit_U(idx + 1)
                        if c >= 2:
                            p1, p1k = nps()
                            MM(p1[:, 0:128], KT[:, cs], QT[:, qs], True, True, [ktk, qk], [p1k])
                            sm_, smk = stm[rr["t"] % 2], ("stm", rr["t"] % 2)
                            rr["t"] += 1
                            TT(sm_[:], p1[:, 0:128], mask[:], ALU.mult, [p1k, mkey], [smk])
                            po, pok = nps()
                            MM(po[:, 0:nv], sm_[:], V[:, c, 0:nv], True, idx == 0, [smk, vk], [pok])
                            if idx > 0:
                                Sp, Spk = Sts[(idx - 1) % 2]
                                MM(po[:, 0:nv], QT[:, qs], Sp[:, 0:nv], False, True, [qk, Spk], [pok])
                            post(po, pok, c)
                        yield

                def head_norm_to_T(hs, hsk, nbc, nbk, h, gate, gk, dstT, dstk, tmpf, a_tm):
                    ACTV(tmpf[:], hs[:], AF.Square, list(hsk), ["hn_tmp"])
                    RED(ssh[:], tmpf[:], ["hn_tmp"], ["ssh"])
                    rstd_from_ss(ssh[:], ssh[:], 1.0 / 128, "ssh")
                    for ti in range(16):
                        STT(tmpf[:, ti, :], hs[:, ti, :], ssh[:, ti:ti + 1], nbc[:, h * 128:(h + 1) * 128],
                            ALU.mult, ALU.mult, list(hsk) + ["ssh", nbk, "hn_tmp"], ["hn_tmp"])
                    TT(a_tm[:], tmpf[:], gate[:], ALU.mult, ["hn_tmp", gk], ["a_tm"])
                    for t0 in (0, 8):
                        pb_, pbk = npsb()
                        for j in range(8):
                            TR(pb_[:, j * 128:(j + 1) * 128], a_tm[:, t0 + j, :], ident_b[:], ["a_tm", "ident_b"], [pbk])
                        CP(dstT[:, h, t0 * 128:(t0 + 8) * 128], pb_[:, :], [pbk], [dstk], eng=ACT)

                s2x = contextlib.ExitStack()
                gates = sb("gates", [128, 16, NT], stack=s2x)
                lf = sb("lf", [128, 8, NT], stack=s2x)
                bcum = sb("bcum", [128, 8, NT], stack=s2x)
                dec_m = sb("dec_m", [128, 8, NT], stack=s2x)
                w_m = sb("w_m", [128, 8, NT], stack=s2x)
                e_m = sb("e_m", [128, 8, NT], stack=s2x)
                for ti in range(NT):
                    pt, pk = nps()
                    for k in range(8):
                        MM(pt[:, 0:16], hT[:, k, ti * 128:(ti + 1) * 128], wmg_sb[:, k, :], k == 0, k == 7,
                           [("hT", ti), "wmg_sb"], [pk])
                    TT(gates[:, :, ti], pt[:, 0:16], bmg_bc[:], ALU.add, [pk, "bmg_bc"], ["gates"])
                ACTV(lf[:], gates[:, 8:16, :], AF.Sigmoid, ["gates"], ["lf"])
                ACTV(lf[:], lf[:], AF.Ln, ["lf"], ["lf"])
                pt, pk = nps()
                MM(pt[:, 0:72], triF[:], lf[:, 0:4, :].rearrange("p a b -> p (a b)"), True, True, ["triF", "lf"], [pk])
                MM(pt[:, 72:144], triB[:], lf[:, 4:8, :].rearrange("p a b -> p (a b)"), True, True,
                   ["triB", "lf"], [pk])
                CP(bcum[:].rearrange("p a b -> p (a b)"), pt[:, 0:144], [pk], ["bcum"])
                pt2, pk2 = nps()
                MM(pt2[:, 0:144], ones_f[:], lf[:].rearrange("p a b -> p (a b)"), True, True, ["ones_f", "lf"], [pk2])
                ACTV(dec_m[:].rearrange("p a b -> p (a b)"), pt2[:, 0:144], AF.Exp, [pk2], ["dec_m"])
                TT(w_m[:], gates[:, 0:8, :], bcum[:], ALU.subtract, ["gates", "bcum"], ["w_m"])
                ACTV(w_m[:], w_m[:], AF.Exp, ["w_m"], ["w_m"])
                ACTV(e_m[:], bcum[:], AF.Exp, ["bcum"], ["e_m"])
                dump("bcum", bcum[:].rearrange("p a b -> p (a b)"), lambda d: d, ["bcum"])

                with contextlib.ExitStack() as s3:
                    qT = sb("qT", [128, TOK], BF16, stack=s3)
                    kT = sb("kT", [128, TOK], BF16, stack=s3)
                    k_tm = sb("k_tm", [128, NT, 128], BF16, stack=s3)
                    wvo = sb("wvo", [128, 8, 256], BF16, stack=s3)
                    bvo = sb("bvo", [128, 256], stack=s3)
                    vp = sb("vp", [128, NT, 129], BF16, stack=s3)
                    v2_ = sb("v2_0", [128, NT, 129], BF16, stack=s3)
                    v2 = [v2_, v2_]
                    og = sb("og", [128, 16, 128], BF16, stack=s3)
                    hsum = sb("hsum", [128, 16, 128], stack=s3)
                    dsc = sb("dsc", [128, 4], stack=s3)
                    for h in range(4):
                        for which, dst, dkey, scale in ((0, qT, "qT", 128.0 ** -0.5), (1, kT, "kT", 1.0)):
                            chunk = which * 4 + h
                            proj_fm(chunk, wfm[which], ("wfm", which), srcf, "srcf")
                            wc = convc[:, chunk, :]
                            ls = srcf[:, 256:TOK].rearrange("p (r c) -> p r c", c=64)
                            la = accf[:, 256:TOK].rearrange("p (r c) -> p r c", c=64)
                            TS(accf[:], srcf[:], wc[:, 4:5], None, ALU.mult, None, ["srcf", "convc"], ["accf"])
                            for di in (-1, 0, 1):
                                for dj in (-1, 0, 1):
                                    if di == 0 and dj == 0:
                                        continue
                                    tap = (di + 1) * 3 + (dj + 1)
                                    r0, r1 = max(0, -di), 32 - max(0, di)
                                    c0, c1 = max(0, -dj), 64 - max(0, dj)
                                    STT(la[:, r0:r1, c0:c1], ls[:, r0 + di:r1 + di, c0 + dj:c1 + dj], wc[:, tap:tap + 1],
                                        la[:, r0:r1, c0:c1], ALU.mult, ALU.add, ["srcf", "convc", "accf"], ["accf"])
                            STT(accf[:, 1:256], srcf[:, 0:255], wc[:, 3:4], accf[:, 1:256], ALU.mult, ALU.add,
                                ["srcf", "convc", "accf"], ["accf"])
                            STT(accf[:, 0:255], srcf[:, 1:256], wc[:, 5:6], accf[:, 0:255], ALU.mult, ALU.add,
                                ["srcf", "convc", "accf"], ["accf"])
                            ACTV(srcf[:], accf[:], AF.Sigmoid, ["accf"], ["srcf"])
                            STT(dst[:], accf[:], scale, srcf[:], ALU.mult, ALU.mult, ["accf", "srcf"], [dkey])
                        if h == 0 and b == 0:
                            dump("qT", qT[:], lambda d: d, ["qT"])
                            dump("kT", kT[:], lambda d: d, ["kT"])
                        for t0 in range(0, NT, 8):
                            n = min(8, NT - t0)
                            pb_, pbk = npsb()
                            for j in range(n):
                                TR(pb_[:, j * 128:(j + 1) * 128], kT[:, (t0 + j) * 128:(t0 + j + 1) * 128], ident_b[:],
                                   ["kT", "ident_b"], [pbk])
                            CP(k_tm[:, t0:t0 + n, :].rearrange("p a b -> p (a b)"), pb_[:, 0:n * 128], [pbk], ["k_tm"],
                               eng=ACT)
                        DMA(POOL, wvo[:, :, 0:128], win_d[8 + h], [], ["wvo"])
                        DMA(POOL, wvo[:, :, 128:256], win_d[12 + h], [], ["wvo"])
                        DMA(SP, bvo[:, 0:128], bcast(brow_d[:, 1024 + h * 128:1024 + (h + 1) * 128], 128), [], ["bvo"])
                        DMA(SP, bvo[:, 128:256], bcast(brow_d[:, 1536 + h * 128:1536 + (h + 1) * 128], 128), [], ["bvo"])
                        MSET(vp[:, :, 128:129], 1.0, ["vp"])
                        for ti in range(NT):
                            pt, pk = nps()
                            for k in range(8):
                                MM(pt[:, 0:256], hT[:, k, ti * 128:(ti + 1) * 128], wvo[:, k, :], k == 0, k == 7,
                                   [("hT", ti), "wvo"], [pk])
                            TT(vp[:, ti, 0:128], pt[:, 0:128], bvo[:, 0:128], ALU.add, [pk, "bvo"], ["vp"])
                            if ti >= 2:
                                TT(og[:, ti - 2, :], pt[:, 128:256], bvo[:, 128:256], ALU.add, [pk, "bvo"], ["og"])
                        ACTV(og[:], og[:], AF.Sigmoid, ["og"], ["og"])
                        gens = []
                        for d in range(2):
                            col = d * 4 + h
                            vd, vdk = (v2_, "v2") if d == 0 else (vp, "vp")
                            for ti in range(NT):
                                TS(vd[:, ti, :], vp[:, ti, :], w_m[:, col, ti:ti + 1], None, ALU.mult, None,
                                   ["vp", "w_m"], [vdk])

                            def post(po, pok, c, d=d, col=col):
                                ecol = e_m[:, col, c:c + 1]
                                d1, d2 = dsc[:, 2 * d:2 * d + 1], dsc[:, 2 * d + 1:2 * d + 2]
                                dk_ = ("dsc", d)
                                ACTV(d1, po[:, 128:129], AF.Abs, [pok, "e_m"], [dk_], scale=ecol)
                                TS(d1, d1, 1.0, None, ALU.max, None, [dk_], [dk_])
                                RECIP(d1, d1, [dk_], [dk_])
                                TT(d2, d1, ecol, ALU.mult, [dk_, "e_m"], [dk_])
                                if (c <= 9) == (d == 0):
                                    TS(hsum[:, c - 2, :], po[:, 0:128], d2, None, ALU.mult, None, [pok, dk_], [("hsum", c)])
                                else:
                                    STT(hsum[:, c - 2, :], po[:, 0:128], d2, hsum[:, c - 2, :], ALU.mult, ALU.add,
                                        [pok, dk_, ("hsum", c)], [("hsum", c)])

                            gens.append(scan(d, qT, "qT", False, kT, "kT", k_tm, "k_tm", vd, vdk, 129,
                                             lambda c, col=col: dec_m[:, col, c:c + 1], "dec_m", post))
                        while gens:
                            for g_ in list(gens):
                                try:
                                    next(g_)
                                except StopIteration:
                                    gens.remove(g_)
                        if h == 0 and b == 0:
                            dump("hsum", hsum[:].rearrange("p a b -> p (a b)"), lambda d: d, kk("hsum", range(2, NT)))
                        head_norm_to_T(hsum, kk("hsum", range(2, NT)), mn_bc, "mn_bc", h, og, "og", aT, "aT", tmpf, a_tm)
                s2x.close()
                P.barrier()
                if stop == "s3":
                    break

                with contextlib.ExitStack() as s4:
                    hqT = sb("hqT", [128, TOK], BF16, stack=s4)
                    w4 = sb("w4", [128, 8, 384], BF16, stack=s4)
                    w4b = sb("w4b", [128, 8, 128], BF16, stack=s4)
                    b4 = sb("b4", [128, 512], stack=s4)
                    hi_tm = sb("hi_tm", [128, NT, 128], BF16, stack=s4)
                    g_tm = sb("g_tm", [128, 16, 128], BF16, stack=s4)
                    zf = srcf[:].rearrange("p (a b) -> p a b", b=128)
                    kkf = accf[:].rearrange("p (a b) -> p a b", b=128)
                    ktl = sb("ktl", [128, NT, 128], BF16, stack=s4)
                    ktlT = sb("ktlT", [128, TOK], BF16, stack=s4)
                    qtl = sb("qtl", [128, 2048], BF16, stack=s4)
                    dec_h = sb("dec_h", [128, 2, NT], stack=s4)
                    ex_ = sb("ex0", [128, 512], stack=s4)
                    ex = [ex_, ex_]
                    osum = sb("osum", [128, 16, 128], stack=s4)
                    exr = 0
                    for h in range(4):
                        proj_fm(16 + h, wfm[0], ("wfm", 0), srcf, "srcf")
                        ACTV(accf[:], srcf[:], AF.Sigmoid, ["srcf"], ["accf"])
                        TT(hqT[:], srcf[:], accf[:], ALU.mult, ["srcf", "accf"], ["hqT"])
                        for j, ch in enumerate((20 + h, 24 + h, 28 + h)):
                            DMA(POOL, w4[:, :, j * 128:(j + 1) * 128], win_d[ch], [], ["w4"])
                        DMA(POOL, w4b[:], win_d[32 + h], [], ["w4b"])
                        for j, ch in enumerate((20 + h, 24 + h, 28 + h, 32 + h)):
                            DMA(SP, b4[:, j * 128:(j + 1) * 128], bcast(brow_d[:, ch * 128:(ch + 1) * 128], 128), [], ["b4"])
                        for d in range(2):
                            hs = slice(h * 128, (h + 1) * 128)
                            for ti in range(NT):
                                pt, pk = nps()
                                if d == 0:
                                    for k in range(8):
                                        MM(pt[:, 0:384], hT[:, k, ti * 128:(ti + 1) * 128], w4[:, k, :], k == 0, k == 7,
                                           [("hT", ti), "w4"], [pk])
                                    TT(hi_tm[:, ti, :], pt[:, 0:128], b4[:, 0:128], ALU.add, [pk, "b4"], ["hi_tm"])
                                    if ti >= 2:
                                        TT(tmpf[:, ti - 2, :], pt[:, 128:256], b4[:, 128:256], ALU.add, [pk, "b4"], ["hn_tmp"])
                                    TT(zf[:, ti, :], pt[:, 256:384], b4[:, 256:384], ALU.add, [pk, "b4"], ["srcf"])
                                else:
                                    for k in range(8):
                                        MM(pt[:, 0:128], hT[:, k, ti * 128:(ti + 1) * 128], w4b[:, k, :], k == 0, k == 7,
                                           [("hT", ti), "w4b"], [pk])
                                    TT(zf[:, ti, :], pt[:, 0:128], b4[:, 384:512], ALU.add, [pk, "b4"], ["srcf"])
                            if d == 0:
                                ACTV(g_tm[:], tmpf[:], AF.Sigmoid, ["hn_tmp"], ["g_tm"])
                                TT(g_tm[:], g_tm[:], tmpf[:], ALU.mult, ["g_tm", "hn_tmp"], ["g_tm"])
                            ACTV(accf[:], srcf[:], AF.Sigmoid, ["srcf"], ["accf"])
                            for ti in range(NT):
                                TT(zf[:, ti, :], kkf[:, ti, :], oml_bc[:, d, hs], ALU.mult, ["accf", "oml_bc", "srcf"], ["srcf"])
                                TT(zf[:, ti, :], zf[:, ti, :], lb_bc[:, d, hs], ALU.add, ["srcf", "lb_bc"], ["srcf"])
                            TS(accf[:], srcf[:], -1.0, 1.0, ALU.mult, ALU.add, ["srcf"], ["accf"])
                            ACTV(srcf[:], srcf[:], AF.Ln, ["srcf"], ["srcf"])
                            tri, trik = (triF, "triF") if d == 0 else (triB, "triB")
                            for t0 in range(0, NT, 4):
                                n = min(4, NT - t0)
                                fs = slice(t0 * 128, (t0 + n) * 128)
                                pt, pk = nps()
                                MM(pt[:, 0:n * 128], tri[:], srcf[:, fs], True, True, [trik, "srcf"], [pk])
                                e_, ek = ex[0], ("ex", 0)
                                exr += 1
                                ACTV(e_[:, 0:n * 128], pt[:, 0:n * 128], AF.Exp, [pk], [ek], scale=-1.0)
                                TT(ktl[:, t0:t0 + n, :].rearrange("p a b -> p (a b)"), accf[:, fs], e_[:, 0:n * 128], ALU.mult,
                                   ["accf", ek], ["ktl"])
                                pt, pk = nps()
                                for j in range(n):
                                    MM(pt[:, j * 128:(j + 1) * 128], zf[:, t0 + j, :], tri[:], True, True, ["srcf", trik], [pk])
                                e_, ek = ex[0], ("ex", 0)
                                exr += 1
                                ACTV(e_[:, 0:n * 128], pt[:, 0:n * 128], AF.Exp, [pk], [ek])
                                lastc = 127 if d == 0 else 0
                                CP(dec_h[:, d, t0:t0 + n],
                                   e_[:, 0:n * 128].rearrange("p (a b) -> p a b", b=128)[:, :, lastc], [ek], ["dec_h"])
                                for j in range(n):
                                    ti = t0 + j
                                    if ti >= 2:
                                        TT(qtl[:, (ti - 2) * 128:(ti - 1) * 128], hqT[:, ti * 128:(ti + 1) * 128],
                                           e_[:, j * 128:(j + 1) * 128], ALU.mult, ["hqT", ek], ["qtl"])
                            for t0 in range(0, NT, 8):
                                n = min(8, NT - t0)
                                pb_, pbk = npsb()
                                for j in range(n):
                                    TR(pb_[:, j * 128:(j + 1) * 128], ktl[:, t0 + j, :], ident_b[:], ["ktl", "ident_b"], [pbk])
                                CP(ktlT[:, t0 * 128:(t0 + n) * 128], pb_[:, 0:n * 128], [pbk], ["ktlT"], eng=ACT)

                            if h == 0 and b == 0 and d == 0:
                                dump("ktl", ktl[:].rearrange("p a b -> p (a b)"), lambda dd: dd, ["ktl"])
                                dump("qtl", qtl[:], lambda dd: dd, ["qtl"])
                                dump("ktlT", ktlT[:], lambda dd: dd, ["ktlT"])
                                dump("dech", dec_h[:].rearrange("p a b -> p (a b)"), lambda dd: dd, ["dec_h"])
                                dump("hqT", hqT[:], lambda dd: dd, ["hqT"])
                                dump("hitm", hi_tm[:].rearrange("p a b -> p (a b)"), lambda dd: dd, ["hi_tm"])

                            def post(po, pok, c, d=d):
                                if d == 0:
                                    CP(osum[:, c - 2, :], po[:, 0:128], [pok], ["osum"])
                                else:
                                    TT(osum[:, c - 2, :], po[:, 0:128], osum[:, c - 2, :], ALU.add, [pok, "osum"], ["osum"])

                            for _ in scan(d, qtl, "qtl", True, ktlT, "ktlT", ktl, "ktl", hi_tm, "hi_tm",
                                          128, lambda c, d=d: dec_h[:, d, c:c + 1], "dec_h", post):
                                pass
                        if h == 0 and b == 0:
                            dump("osum", osum[:].rearrange("p a b -> p (a b)"), lambda d: d, ["osum"])
                        head_norm_to_T(osum, ["osum"], hn_bc, "hn_bc", h, g_tm, "g_tm", bT, "bT", tmpf, a_tm)
                P.barrier()
                if stop == "s4":
                    break

            P.barrier()
            with contextlib.ExitStack() as s5:
                G1 = sb("G1", [128, 1024], stack=s5)
                A2 = sb("A2", [128, 1024], stack=s5)
                SH2 = sb("SH2", [128, 1024], stack=s5)
                wpa = sb("wpa", [128, 4, 1024], BF16, stack=s5)
                wpb = sb("wpb", [128, 4, 1024], BF16, stack=s5)
                wout = sb("wout", [128, 8, 1024], BF16, stack=s5)
                wg = [sb(f"wg{i}", [128, 8, 128], BF16, stack=s5) for i in range(2)]
                yT = sb("yT", [128, 8, 512], BF16, stack=s5)
                sga = sb("sga", [128, 512], stack=s5)
                y1 = sb("y1", [128, 512], stack=s5)
                y2 = sb("y2", [128, 512], stack=s5)
                sgb = sga
                yl = sb("yl", [128, 1024], stack=s5)
                sq = sb("sq5", [128, 1024], stack=s5)
                t1 = sq
                xin = [sb(f"xin5{i}", [128, 1024], stack=s5) for i in range(1)]
                x1t = [sb(f"x1t{i}", [128, 1024], stack=s5) for i in range(1)]
                h2b = sb("h2b", [128, 1024], BF16, stack=s5)
                h2Tt = sb("h2Tt", [128, 8, 128], BF16, stack=s5)
                load_bc(G1, b * 6 + 2, "G1")
                load_bc(A2, b * 6 + 4, "A2")
                load_bc(SH2, b * 6 + 3, "SH2")
                DMA(POOL, wpa[:], wpa_d, [], ["wpa"])
                DMA(POOL, wpb[:], wpb_d, [], ["wpb"])
                DMA(POOL, wout[:], wout_d, [], ["wout"])
                wgr = 0
                for tg in range(4):
                    tok0 = tg * 512
                    for j in range(8):
                        wga, wgak = wg[0], ("wg", 0)
                        wgb, wgbk = wg[1], ("wg", 1)
                        wgr += 2
                        DMA(POOL, wga[:], win_d[36 + j], [], [wgak])
                        DMA(POOL, wgb[:], win_d[44 + j], [], [wgbk])
                        pa, pak = nps()
                        for k in range(4):
                            MM(pa[:, :], wpa[:, k, j * 128:(j + 1) * 128], aT[:, k, tok0:tok0 + 512], k == 0, k == 3,
                               ["wpa", "aT"], [pak])
                        pga, pgak = nps()
                        for k in range(8):
                            MM(pga[:, :], wga[:, k, :], hT[:, k, 256 + tok0:256 + tok0 + 512], k == 0, k == 7,
                               [wgak] + hTk(256 + tok0, 256 + tok0 + 512), [pgak])
                        ACTV(sga[:], pga[:, :], AF.Sigmoid, [pgak, "bcol"], ["sga"], bias=bcol[:, 36 + j:37 + j])
                        TT(y1[:], pa[:, :], sga[:], ALU.mult, [pak, "sga"], ["y1"])
                        pb2, pb2k = nps()
                        for k in range(4):
                            MM(pb2[:, :], wpb[:, k, j * 128:(j + 1) * 128], bT[:, k, tok0:tok0 + 512], k == 0, k == 3,
                               ["wpb", "bT"], [pb2k])
                        pgb, pgbk = nps()
                        for k in range(8):
                            MM(pgb[:, :], wgb[:, k, :], hT[:, k, 256 + tok0:256 + tok0 + 512], k == 0, k == 7,
                               [wgbk] + hTk(256 + tok0, 256 + tok0 + 512), [pgbk])
                        ACTV(sgb[:], pgb[:, :], AF.Sigmoid, [pgbk, "bcol"], ["sga"], bias=bcol[:, 44 + j:45 + j])
                        TT(y2[:], pb2[:, :], sgb[:], ALU.mult, [pb2k, "sga"], ["y2"])
                        TT(yT[:, j, :], y1[:], y2[:], ALU.add, ["y1", "y2"], [("yT", j)])
                    for tl in range(4):
                        lt = tg * 4 + tl
                        for half in range(2):
                            pt, pk = nps()
                            for k in range(8):
                                MM(pt[:, :], yT[:, k, tl * 128:(tl + 1) * 128], wout[:, k, half * 512:(half + 1) * 512],
                                   k == 0, k == 7, [("yT", k), "wout"], [pk])
                            CP(yl[:, half * 512:(half + 1) * 512], pt[:, :], [pk], ["yl"], eng=ACT)
                        ACTV(sq[:], yl[:], AF.Square, ["yl"], ["sq5"])
                        sc, sk = small[:, 20 + lt:21 + lt], ("small", 20 + lt)
                        RED(sc, sq[:], ["sq5"], [sk])
                        rstd_from_ss(sc, sc, 1.0 / 1024, sk)
                        xi, xk = xin[0], ("xin5", 0)
                        DMA(SP, xi[:], x_d[b, lt * 128:(lt + 1) * 128, :], [], [xk])
                        STT(t1[:], yl[:], sc, G1[:], ALU.mult, ALU.mult, ["yl", sk, "G1"], ["sq5"])
                        xo, xok = x1t[0], ("x1t", 0)
                        TT(xo[:], t1[:], xi[:], ALU.add, ["sq5", xk], [xok])
                        DMA(SP, x1_d[b, lt * 128:(lt + 1) * 128, :], xo[:], [xok], [("x1d", b, lt)])
                        ACTV(sq[:], xo[:], AF.Square, [xok], ["sq5"])
                        sc2, sk2 = small[:, 40 + lt:41 + lt], ("small", 40 + lt)
                        RED(sc2, sq[:], ["sq5"], [sk2])
                        rstd_from_ss(sc2, sc2, 1.0 / 1024, sk2)
                        STT(t1[:], xo[:], sc2, A2[:], ALU.mult, ALU.mult, [xok, sk2, "A2"], ["sq5"])
                        TT(h2b[:], t1[:], SH2[:], ALU.add, ["sq5", "SH2"], ["h2b"])
                        pb_, pbk = npsb()
                        for k in range(8):
                            TR(pb_[:, k * 128:(k + 1) * 128], h2b[:, k * 128:(k + 1) * 128], ident_b[:],
                               ["h2b", "ident_b"], [pbk])
                        gt = b * 16 + lt
                        DMA(SP, h2_d[gt * 128:(gt + 1) * 128, :], h2b[:], ["h2b"], [("h2d", gt)])
                        CP(h2Tt[:], pb_[:].rearrange("p (k t) -> p k t", k=8), [pbk], ["h2Tt"], eng=ACT)
                        pt, pk = nps()
                        for k in range(8):
                            MM(pt[:, 0:32], h2Tt[:, k, :], wr_sb[:, k, :], k == 0, k == 7, ["h2Tt", "wr_sb"], [pk])
                        TT(Ls[:, gt, :], pt[:, 0:32], br_bc[:], ALU.add, [pk, "br_bc"], [("Ls", gt)])
                        P.op(DVE, lambda e, gt=gt: e.max(out=m8s[:, gt, :], in_=Ls[:, gt, :]), [("Ls", gt)], [("m8s", gt)])
            mx.close()
            mx = None
            P.barrier()
            if stop == "s5":
                break

        LsK = kk("Ls", range(32))
        m8K = kk("m8s", range(32))
        desti = sb("desti", [128, 128], I32)
        wk = sb("wk", [128, 32, 4])
        widx = sb("widx", [128, NB], I32)
        widx8 = sb("widx8", [128, 8, NB], I32)
        bidx = sb("bidx", [128, NB], I32)
        if stop in (None, "m1"):
            with contextlib.ExitStack() as m1:
                triS = sb("triS", [128, 128], stack=m1)
                DMA(SP, triS[:], triS_d, [], ["triS"])
                mask = sb("mask", [128, 1024], stack=m1)
                R1 = sb("R1", [128, 1024], stack=m1)
                cA = sb("cA", [128, 1024], stack=m1)
                cB = sb("cB", [128, 1024], stack=m1)
                cnt = sb("cnt", [128, 1024], stack=m1)
                dest = sb("dest", [128, 1024], stack=m1)
                tot = sb("tot", [128, 32], stack=m1)
                nblk = sb("nblk", [128, 32], stack=m1)
                pA = sb("pA", [128, 32], stack=m1)
                pB = sb("pB", [128, 32], stack=m1)
                pstart = sb("pstart", [128, 32], stack=m1)
                junk = sb("junk", [128, 32], stack=m1)
                destk = sb("destk", [128, 128], stack=m1)
                ejf = sb("ejf", [128, NB], stack=m1)
                wif = sb("wif", [128, NB], stack=m1)
                nm = sb("nm", [128, 32], stack=m1)
                exs = sb("exs", [128, 32, 4], stack=m1)
                ssum = sb("ssum", [128, 32], stack=m1)
                m3 = lambda t: t[:].rearrange("p (a b) -> p a b", b=32)
                TT(m3(mask), Ls[:], m8s[:, :, 3:4].to_broadcast([128, 32, 32]), ALU.is_ge, LsK + m8K, ["mask"])
                for hf in range(2):
                    cs = slice(hf * 512, (hf + 1) * 512)
                    pt, pk = nps()
                    MM(pt[:, :], triS[:], mask[:, cs], True, True, ["triS", "mask"], [pk])
                    CP(R1[:, cs], pt[:, :], [pk], ["R1"])
                    pt, pk = nps()
                    MM(pt[:, :], ones_f[:], mask[:, cs], True, True, ["ones_f", "mask"], [pk])
                    CP(cnt[:, cs], pt[:, :], [pk], ["cnt"])
                CP(cA[:], cnt[:], ["cnt"], ["cA"])
                a_, ak, b_, bk = cA, "cA", cB, "cB"
                for sft in (1, 2, 4, 8, 16):
                    w_ = sft * 32
                    CP(b_[:, 0:w_], a_[:, 0:w_], [ak], [bk])
                    TT(b_[:, w_:1024], a_[:, w_:1024], a_[:, 0:1024 - w_], ALU.add, [ak], [bk])
                    a_, ak, b_, bk = b_, bk, a_, ak
                incl, inck = a_, ak
                CP(tot[:], incl[:, 31 * 32:32 * 32], [inck], ["tot"])
                TT(dest[:], incl[:], cnt[:], ALU.subtract, [inck, "cnt"], ["dest"])
                TT(dest[:], dest[:], R1[:], ALU.add, ["dest", "R1"], ["dest"])
                MSET(nblk[:], 0.0, ["nblk"])
                for j in range(MAXB):
                    STT(nblk[:], tot[:], float(j * BLK), nblk[:], ALU.is_gt, ALU.add, ["tot", "nblk"], ["nblk"])
                CP(pA[:], nblk[:], ["nblk"], ["pA"])
                a_, ak, b_, bk = pA, "pA", pB, "pB"
                for sft in (1, 2, 4, 8, 16):
                    CP(b_[:, 0:sft], a_[:, 0:sft], [ak], [bk])
                    TT(b_[:, sft:32], a_[:, sft:32], a_[:, 0:32 - sft], ALU.add, [ak], [bk])
                    a_, ak, b_, bk = b_, bk, a_, ak
                pend, pendk = a_, ak
                TT(pstart[:], pend[:], nblk[:], ALU.subtract, [pendk, "nblk"], ["pstart"])
                TS(pstart[:], pstart[:], float(BLK), None, ALU.mult, None, ["pstart"], ["pstart"])
                TT(m3(dest), m3(dest), pstart[:].rearrange("p (a e) -> p a e", a=1).to_broadcast([128, 32, 32]), ALU.add,
                   ["dest", "pstart"], ["dest"])
                for k in range(4):
                    TT(m3(cA), Ls[:], m8s[:, :, k:k + 1].to_broadcast([128, 32, 32]), ALU.is_equal, LsK + m8K + ["cA"], ["cA"])
                    TT(cA[:], cA[:], dest[:], ALU.mult, ["cA", "dest"], ["cA"])
                    RED(destk[:, k * 32:(k + 1) * 32], m3(cA), ["cA"], ["destk"])
                TS(destk[:], destk[:], 0.0, float(NB * BLK - 1), ALU.max, ALU.min, ["destk"], ["destk"])
                CP(desti[:], destk[:], ["destk"], ["desti"])
                TT(exs[:], m8s[:, :, 0:4], m8s[:, :, 0:1].to_broadcast([128, 32, 4]), ALU.subtract, m8K, ["exs"])
                ACTV(exs[:], exs[:], AF.Exp, ["exs"], ["exs"])
                RED(ssum[:], exs[:], ["exs"], ["ssum"])
                RECIP(ssum[:], ssum[:], ["ssum"], ["ssum"])
                TT(wk[:], exs[:], ssum[:].rearrange("p (a b) -> p a b", b=1).to_broadcast([128, 32, 4]), ALU.mult,
                   ["exs", "ssum"], ["wk"])
                jix = sb("jix", [128, NB], stack=m1)
                cmpb = sb("cmpb", [128, NB, 32], stack=m1)
                DMA(SP, jix[:], bcast(jidx_d, NB), [], ["jix"])
                TT(cmpb[:], pend[:].rearrange("p (a e) -> p a e", a=1).to_broadcast([128, NB, 32]),
                   jix[:].rearrange("p (a b) -> p a b", b=1).to_broadcast([128, NB, 32]), ALU.is_le, [pendk, "jix"], ["cmpb"])
                RED(ejf[:], cmpb[:], ["cmpb"], ["ejf"])
                TS(ejf[:], ejf[:], 31.0, None, ALU.min, None, ["ejf"], ["ejf"])
                CP(bidx[:], ejf[:], ["ejf"], ["bidx"])
                TS(wif[:], ejf[:], 128.0, pidx[:, 0:1], ALU.mult, ALU.add, ["ejf", "pidx"], ["wif"])
                CP(widx[:], wif[:], ["wif"], ["widx"])
                wif8 = sb("wif8", [128, 8, NB], stack=m1)
                for k in range(8):
                    TS(wif8[:, k, :], wif[:], 8.0, float(k), ALU.mult, ALU.add, ["wif"], ["wif8"])
                CP(widx8[:], wif8[:], ["wif8"], ["widx8"])
                dump("destk", destk[:], lambda d: d, ["destk"])
                dump("ejf", ejf[:], lambda d: d, ["ejf"])
                dump("wk", wk[:].rearrange("p a b -> p (a b)"), lambda d: d, ["wk"])
                dump("wif", wif[:], lambda d: d, ["wif"])
                h2t = [sb(f"h2t{i}", [128, 1024], BF16, stack=m1) for i in range(2)]
                for gt in range(32 if stop is None else 0):
                    ht, htk = h2t[gt % 2], ("h2t", gt % 2)
                    DMA(SP, ht[:], h2_d[gt * 128:(gt + 1) * 128, :], [("h2d", gt)], [htk])
                    for k in range(4):
                        c_ = k * 32 + gt
                        P.dma(POOL, lambda e, ht=ht, c_=c_: e.indirect_dma_start(
                            out=xs_d, out_offset=bass.IndirectOffsetOnAxis(ap=desti[:, c_:c_ + 1], axis=0),
                            in_=ht[:], in_offset=None),
                            [htk, "desti"], ["xs"])
            P.barrier()

        if stop is None:
            with contextlib.ExitStack() as m4:
                wgu = [sb(f"wgu{i}", [128, 8, 2048], BF16, stack=m4) for i in range(2)]
                wdn = [sb(f"wdn{i}", [128, 8, 1024], BF16, stack=m4) for i in range(2)]
                bgt = [sb(f"bgt{i}", [128, 16], stack=m4) for i in range(2)]
                bdt = [sb(f"bdt{i}", [128, 1024], stack=m4) for i in range(2)]
                xbs = [sb(f"xb{i}", [128, 4, 1024], BF16, stack=m4) for i in range(2)]
                xT = sb("xT", [128, 8, 512], BF16, stack=m4)
                act = sb("act", [128, 8, 512], BF16, stack=m4)
                g1 = [sb(f"g1_{i}", [128, 512], stack=m4) for i in range(2)]
                sg = [sb(f"sg_{i}", [128, 512], stack=m4) for i in range(2)]
                u1 = [sb(f"u1_{i}", [128, 512], stack=m4) for i in range(2)]
                ybt = [sb(f"ybt{i}", [128, 1024], stack=m4) for i in range(2)]

                def load_w(j):
                    r_ = j % 2
                    ix = widx[:, j:j + 1]
                    for k in range(8):
                        P.dma(POOL, lambda e, k=k: e.indirect_dma_start(
                            out=wgu[r_][:, k, :], out_offset=None, in_=wgu_d,
                            in_offset=bass.IndirectOffsetOnAxis(ap=widx8[:, k, j:j + 1], axis=0)),
                            ["widx8"], [("wgu", r_, k)])
                    for k in range(8):
                        P.dma(POOL, lambda e, k=k: e.indirect_dma_start(
                            out=wdn[r_][:, k, :], out_offset=None, in_=wdn_d,
                            in_offset=bass.IndirectOffsetOnAxis(ap=widx8[:, k, j:j + 1], axis=0)),
                            ["widx8"], [("wdn", r_, k)])
                    P.dma(POOL, lambda e: e.indirect_dma_start(
                        out=bgt[r_][:], out_offset=None, in_=bgu_d,
                        in_offset=bass.IndirectOffsetOnAxis(ap=ix, axis=0)), ["widx"], [("bgt", r_)])
                    P.dma(POOL, lambda e: e.indirect_dma_start(
                        out=bdt[r_][:], out_offset=None, in_=bdn_d,
                        in_offset=bass.IndirectOffsetOnAxis(ap=bidx[:, j:j + 1], axis=0)), ["bidx"], [("bdt", r_)])

                load_w(0)
                rot = 0
                for j in range(NB):
                    r_ = j % 2
                    if j + 1 < NB:
                        load_w(j + 1)
                    xb, xbk = xbs[j % 2], ("xb", j % 2)
                    if j == 0:
                        DMA(SP, xb[:], xs_d[0:BLK, :].rearrange("(a p) n -> p a n", p=128), ["xs"], [xbk])
                    if j + 1 < NB:
                        DMA(SP, xbs[(j + 1) % 2][:], xs_d[(j + 1) * BLK:(j + 2) * BLK, :].rearrange("(a p) n -> p a n", p=128),
                            ["xs"], [("xb", (j + 1) % 2)])
                    for a in range(4):
                        pb_, pbk = npsb()
                        for k in range(8):
                            TR(pb_[:, k * 128:(k + 1) * 128], xb[:, a, k * 128:(k + 1) * 128], ident_b[:], [xbk, "ident_b"], [pbk])
                        CP(xT[:, :, a * 128:(a + 1) * 128], pb_[:].rearrange("p (k t) -> p k t", k=8), [pbk], ["xT"],
                           eng=ACT if a % 2 == 0 else DVE)
                    for fb in range(8):
                        q_ = rot % 2
                        rot += 1
                        pg, pgk = nps()
                        for k in range(8):
                            MM(pg[:, :], wgu[r_][:, k, fb * 128:(fb + 1) * 128], xT[:, k, :], k == 0, k == 7,
                               [("wgu", r_, k), "xT"], [pgk])
                        pu, puk = nps()
                        for k in range(8):
                            MM(pu[:, :], wgu[r_][:, k, 1024 + fb * 128:1024 + (fb + 1) * 128], xT[:, k, :], k == 0, k == 7,
                               [("wgu", r_, k), "xT"], [puk])
                        TS(g1[q_][:], pg[:, :], bgt[r_][:, fb:fb + 1], 7.0, ALU.add, ALU.min, [pgk, ("bgt", r_)], [("g1", q_)])
                        ACTV(sg[q_][:], g1[q_][:], AF.Sigmoid, [("g1", q_)], [("sg", q_)], scale=1.702)
                        TS(u1[q_][:], pu[:, :], bgt[r_][:, 8 + fb:9 + fb], 7.0, ALU.add, ALU.min, [puk, ("bgt", r_)], [("u1", q_)])
                        TS(u1[q_][:], u1[q_][:], -7.0, 1.0, ALU.max, ALU.add, [("u1", q_)], [("u1", q_)])
                        TT(g1[q_][:], g1[q_][:], sg[q_][:], ALU.mult, [("g1", q_), ("sg", q_)], [("g1", q_)])
                        TT(act[:, fb, :], g1[q_][:], u1[q_][:], ALU.mult, [("g1", q_), ("u1", q_)], ["act"])
                    for a in range(4):
                        y_, yk = ybt[a % 2], ("ybt", a % 2)
                        for dh in range(2):
                            py, pyk = nps()
                            for fb in range(8):
                                MM(py[:, :], act[:, fb, a * 128:(a + 1) * 128], wdn[r_][:, fb, dh * 512:(dh + 1) * 512],
                                   fb == 0, fb == 7, ["act", ("wdn", r_, fb)], [pyk])
                            TT(y_[:, dh * 512:(dh + 1) * 512], py[:, :], bdt[r_][:, dh * 512:(dh + 1) * 512], ALU.add,
                               [pyk, ("bdt", r_)], [yk])
                        DMA(SP, yb_d[j * BLK + a * 128:j * BLK + (a + 1) * 128, :], y_[:], [yk], ["yb"])
            P.barrier()

            with contextlib.ExitStack() as m5:
                G2 = [sb(f"G2_{i}", [128, 1024], stack=m5) for i in range(2)]
                ybk = [sb(f"ybk{i}", [128, 1024], stack=m5) for i in range(8)]
                ym = sb("ym", [128, 1024], stack=m5)
                sq = sb("sq7", [128, 1024], stack=m5)
                x1r = [sb(f"x1r{i}", [128, 1024], stack=m5) for i in range(2)]
                ot = [sb(f"ot{i}", [128, 1024], stack=m5) for i in range(2)]
                for b in range(nb):
                    load_bc(G2[b], b * 6 + 5, ("G2", b))
                for gt in range(nb * 16):
                    b, lt = gt // 16, gt % 16
                    ys = []
                    for k in range(4):
                        i_ = (gt % 2) * 4 + k
                        c_ = k * 32 + gt
                        P.dma(POOL, lambda e, i_=i_, c_=c_: e.indirect_dma_start(
                            out=ybk[i_][:], out_offset=None, in_=yb_d,
                            in_offset=bass.IndirectOffsetOnAxis(ap=desti[:, c_:c_ + 1], axis=0)),
                            ["yb", "desti"], [("ybk", i_)])
                        ys.append((ybk[i_], ("ybk", i_)))
                    TS(ym[:], ys[0][0][:], wk[:, gt, 0:1], None, ALU.mult, None, [ys[0][1], "wk"], ["ym"])
                    for k in range(1, 4):
                        STT(ym[:], ys[k][0][:], wk[:, gt, k:k + 1], ym[:], ALU.mult, ALU.add, [ys[k][1], "wk", "ym"], ["ym"])
                    ACTV(sq[:], ym[:], AF.Square, ["ym"], ["sq7"])
                    sc, sk = small[:, 20 + lt:21 + lt], ("small", 20 + lt)
                    RED(sc, sq[:], ["sq7"], [sk])
                    rstd_from_ss(sc, sc, 1.0 / 1024, sk)
                    xr, xrk = x1r[gt % 2], ("x1r", gt % 2)
                    DMA(SP, xr[:], x1_d[b, lt * 128:(lt + 1) * 128, :], [("x1d", b, lt)], [xrk])
                    STT(sq[:], ym[:], sc, G2[b][:], ALU.mult, ALU.mult, ["ym", sk, ("G2", b), "sq7"], ["sq7"])
                    o_, ok_ = ot[gt % 2], ("ot", gt % 2)
                    TT(o_[:], sq[:], xr[:], ALU.add, ["sq7", xrk], [ok_])
                    out_toks.append(DMA(SP, out_d[b, lt * 128:(lt + 1) * 128, :], o_[:], [ok_], []))

        if mx is not None:
            mx.close()
        P.final_wait(SP, out_toks)
        P.emit()
    return nc


def _c(a):
    return np.ascontiguousarray(a, dtype=np.float32)


def prep_shared(inp):
    w_in = inp["w_in"][0]
    sh = {}
    sh["w_ada"] = _c(inp["w_ada"][0].reshape(8, 128, 12, 512).transpose(2, 1, 0, 3))
    sh["b_ada"] = _c(inp["b_ada"].reshape(1, 6144))
    sh["nrm"] = _c(np.concatenate([inp["norm_mix_pre"][0], inp["norm_mix_post"][0],
                                   inp["norm_ffn_pre"][0], inp["norm_ffn_post"][0]]).reshape(1, 4096))
    sh["w_in"] = _c(w_in[:, :6656].reshape(8, 128, 52, 128).transpose(2, 1, 0, 3))
    sh["w_mg"] = _c(w_in[:, 6656:].reshape(8, 128, 16).transpose(1, 0, 2))
    sh["b_in_col"] = _c(inp["b_in"][0, :6656].reshape(52, 128).T)
    sh["b_in_row"] = _c(inp["b_in"].reshape(1, 6672))
    sh["conv_col"] = _c(inp["conv_w"][0].reshape(9, 8, 128).transpose(2, 1, 0))
    sh["lb_raw"] = _c(inp["lb_raw"].reshape(1, 2048))
    sh["m_norm"] = _c(inp["m_norm"].reshape(1, 512))
    sh["h_norm"] = _c(inp["h_norm"].reshape(1, 512))
    sh["w_pa"] = _c(inp["w_pa"][0].reshape(4, 128, 1024).transpose(1, 0, 2))
    sh["w_pb"] = _c(inp["w_pb"][0].reshape(4, 128, 1024).transpose(1, 0, 2))
    sh["w_out"] = _c(inp["w_out"][0].reshape(8, 128, 1024).transpose(1, 0, 2))
    sh["w_router"] = _c(inp["w_router"][0].reshape(8, 128, 32).transpose(1, 0, 2))
    sh["b_router"] = _c(inp["b_router"].reshape(1, 32))
    sh["w_gu"] = _c(inp["w_gu"][0].reshape(32, 8, 128, 2048).transpose(0, 2, 1, 3)).reshape(32 * 128 * 8, 2048)
    sh["b_gu_col"] = _c(inp["b_gu"][0].reshape(32, 16, 128).transpose(0, 2, 1)).reshape(32 * 128, 16)
    sh["w_dn"] = _c(inp["w_dn"][0].reshape(32, 8, 128, 1024).transpose(0, 2, 1, 3)).reshape(32 * 128 * 8, 1024)
    sh["b_dn"] = _c(inp["b_dn"][0])
    sh["ident"] = np.eye(128, dtype=np.float32)
    sh["triF"] = np.triu(np.ones((128, 128), np.float32))
    sh["triB"] = np.tril(np.ones((128, 128), np.float32))
    sh["ones"] = np.ones((128, 128), np.float32)
    sh["pidx"] = np.arange(128, dtype=np.float32).reshape(128, 1)
    sh["jidx"] = np.arange(NB, dtype=np.float32).reshape(1, NB)
    sh["triS"] = np.triu(np.ones((128, 128), np.float32), 1)
    return sh


def core_inputs(inp, sh, i):
    m = dict(sh)
    m["x"] = _c(inp["x"][2 * i:2 * i + 2])
    m["ctx"] = _c(inp["ctx"][2 * i:2 * i + 2])
    cv = np.stack([inp["c"][2 * i], inp["c"][2 * i + 1], inp["c_ctx"]])
    m["cvT"] = _c(cv.reshape(3, 8, 128).transpose(2, 0, 1).reshape(128, 24))
    return m


def kernel(**inputs):
    inp = {k: np.asarray(v) for k, v in inputs.items()}
    sh = prep_shared(inp)
    nc = build_nc()
    in_maps = [core_inputs(inp, sh, i) for i in range(N_CORES)]
    res = run_bass_kernel_spmd(nc, in_maps, core_ids=list(range(N_CORES)))
    out = np.concatenate([np.asarray(r["out"], dtype=np.float32) for r in res.results], axis=0)
    return out
```

```python
import contextlib
import numpy as np
import concourse.bass as bass
import concourse.mybir as mybir
from concourse.bass_utils import run_bass_kernel_spmd

F32 = mybir.dt.float32
BF16 = mybir.dt.bfloat16
I32 = mybir.dt.int32
AF = mybir.ActivationFunctionType
ALU = mybir.AluOpType
AX = mybir.AxisListType

PE, DVE, ACT, POOL, SP = "pe", "dve", "act", "pool", "sp"
COMPUTE = (PE, DVE, ACT, POOL)
EPOCH = 12000
N_DMA_SEM = 88
DMA_POOLS = {"sp": (0, 32), "act": (32, 8), "pool": (40, 48)}
N_EPOCH_SEM = 12
EPS = 1e-6
NT = 18
BLK = 512
NB = 4096 * 4 // BLK + 32
MAXB = 4096 // BLK
TOK = 2304
N_CORES = 8


class Prog:
    def __init__(self, nc, same_engine_sync=True):
        self.nc = nc
        self.same = same_engine_sync
        self.streams = {e: [] for e in (PE, DVE, ACT, POOL, SP)}
        self.count = {e: 0 for e in COMPUTE}
        self.waited = {}
        self.state = {}
        self.dma_tot = [0] * N_DMA_SEM
        self.dma_rr = {q: 0 for q in DMA_POOLS}
        self.barrier_toks = set()

    def barrier(self):
        toks = set()
        for e in COMPUTE:
            c = self.count[e]
            if c > 0:
                toks.add(((e, (c - 1) // EPOCH), ((c - 1) % EPOCH) + 1))
        for s_ in range(N_DMA_SEM):
            if self.dma_tot[s_] > 0:
                toks.add((("dma", s_), self.dma_tot[s_]))
        self.barrier_toks = toks

    def _deps(self, reads, writes, own_eng=None):
        deps = set()
        for k in reads:
            st = self.state.get(k)
            if st and st[0]:
                deps.add(st[0])
        for k in writes:
            st = self.state.get(k)
            if st:
                if st[0] and st[0][0][0] != own_eng:
                    deps.add(st[0])
                deps.update(t for t in st[1] if t[0][0] != own_eng)
        return deps

    def _commit(self, reads, writes, tok):
        for k in writes:
            self.state[k] = [tok, []]
        for k in reads:
            if k in writes:
                continue
            st = self.state.setdefault(k, [None, []])
            st[1].append(tok)
            if len(st[1]) > 64:
                best = {}
                for (key, val) in st[1]:
                    if best.get(key, 0) < val:
                        best[key] = val
                st[1] = list(best.items())

    def _waits(self, eng, deps, own_key=None):
        best = {}
        for (key, val) in deps:
            if key == own_key and not self.same:
                continue
            if self.waited.get((eng, key), 0) >= val:
                continue
            if best.get(key, 0) < val:
                best[key] = val
        out = []
        for key, val in best.items():
            self.waited[(eng, key)] = val
            out.append((key, val))
        return out

    def op(self, eng, fn, reads=(), writes=()):
        reads, writes = tuple(reads), tuple(writes)
        c = self.count[eng]
        own_key = (eng, c // EPOCH)
        deps = self._deps(reads, writes, eng) | self.barrier_toks
        if eng == PE:
            deps = {d for d in deps if d[0][0] != PE}
        waits = self._waits(eng, deps, own_key)
        self.count[eng] = c + 1
        tok = (own_key, (c % EPOCH) + 1)
        self.streams[eng].append(("op", waits, fn, own_key))
        self._commit(reads, writes, tok)

    def dma(self, q, fn, reads=(), writes=()):
        reads, writes = tuple(reads), tuple(writes)
        first, cnt_ = DMA_POOLS[q]
        s = first + self.dma_rr[q]
        self.dma_rr[q] = (self.dma_rr[q] + 1) % cnt_
        key = ("dma", s)
        deps = self._deps(reads, writes) | self.barrier_toks
        if self.dma_tot[s] > 0:
            deps.add((key, self.dma_tot[s]))
        waits = self._waits(q, deps, None)
        self.dma_tot[s] += 16
        tok = (key, self.dma_tot[s])
        self.streams[q].append(("dma", waits, fn, key))
        self._commit(reads, writes, tok)
        return tok

    def final_wait(self, eng, toks):
        waits = self._waits(eng, set(toks), None)
        self.streams[eng].append(("wait", waits, None, None))

    def emit(self):
        nc = self.nc
        with contextlib.ExitStack() as es:
            sems = {}
            for e in COMPUTE:
                nep = self.count[e] // EPOCH + 1
                assert nep <= N_EPOCH_SEM, (e, self.count[e])
                for i in range(nep):
                    sems[(e, i)] = es.enter_context(nc.semaphore(f"s_{e}_{i}"))
            for s in range(N_DMA_SEM):
                if self.dma_tot[s] > 0:
                    sems[("dma", s)] = es.enter_context(nc.semaphore(f"s_dma_{s}"))
            block = es.enter_context(nc.Block())

            def run(eng_name):
                def body(engine):
                    for kind, waits, fn, key in self.streams[eng_name]:
                        for (k, v) in waits:
                            engine.wait_ge(sems[k], v)
                        if kind == "op":
                            fn(engine).then_inc(sems[key], 1)
                        elif kind == "dma":
                            fn(engine).then_inc(sems[key], 16)
                return body

            block.sync(run(SP))
            block.tensor(run(PE))
            block.vector(run(DVE))
            block.scalar(run(ACT))
            block.gpsimd(run(POOL))


def kk(name, idxs):
    return [(name, i) for i in idxs]


def build_nc(nb=2, dumps=(), stop=None, n_exp=32):
    nc = bass.Bass("TRN2", target_bir_lowering=False)
    P = Prog(nc)
    D = {}

    def din(name, shape, dt=F32):
        D[name] = nc.dram_tensor(name, list(shape), dt, kind="ExternalInput").ap()
        return D[name]

    x_d = din("x", [2, 2048, 1024])
    ctx_d = din("ctx", [2, 256, 1024])
    cvT_d = din("cvT", [128, 24])
    wada_d = din("w_ada", [12, 128, 8, 512])
    bada_d = din("b_ada", [1, 6144])
    nrm_d = din("nrm", [1, 4096])
    win_d = din("w_in", [52, 128, 8, 128])
    wmg_d = din("w_mg", [128, 8, 16])
    bcol_d = din("b_in_col", [128, 52])
    brow_d = din("b_in_row", [1, 6672])
    conv_d = din("conv_col", [128, 8, 9])
    lb_d = din("lb_raw", [1, 2048])
    mn_d = din("m_norm", [1, 512])
    hn_d = din("h_norm", [1, 512])
    wpa_d = din("w_pa", [128, 4, 1024])
    wpb_d = din("w_pb", [128, 4, 1024])
    wout_d = din("w_out", [128, 8, 1024])
    wr_d = din("w_router", [128, 8, 32])
    br_d = din("b_router", [1, 32])
    wgu_d = din("w_gu", [32 * 128 * 8, 2048])
    bgu_d = din("b_gu_col", [32 * 128, 16])
    wdn_d = din("w_dn", [32 * 128 * 8, 1024])
    bdn_d = din("b_dn", [32, 1024])
    ident_d = din("ident", [128, 128])
    triF_d = din("triF", [128, 128])
    triB_d = din("triB", [128, 128])
    ones_d = din("ones", [128, 128])
    pidx_d = din("pidx", [128, 1])
    jidx_d = din("jidx", [1, NB])
    triS_d = din("triS", [128, 128])
    out_d = nc.dram_tensor("out", [2, 2048, 1024], F32, kind="ExternalOutput").ap()
    bc_d = nc.dram_tensor("bc_scr", [14, 128, 1024], F32).ap()
    x1_d = nc.dram_tensor("x1_scr", [2, 2048, 1024], F32).ap()
    h2_d = nc.dram_tensor("h2_scr", [4096, 1024], BF16).ap()
    xs_d = nc.dram_tensor("xs_scr", [NB * BLK, 1024], BF16).ap()
    yb_d = nc.dram_tensor("yb_scr", [NB * BLK, 1024], F32).ap()
    dump_d = {}
    for (nm, shape) in dumps:
        dump_d[nm] = nc.dram_tensor("dbg_" + nm, list(shape), F32, kind="ExternalOutput").ap()
    out_toks = []

    def MM(out, lhsT, rhs, start, stop, reads, writes):
        P.op(PE, lambda e: e.matmul(out, lhsT=lhsT, rhs=rhs, start=start, stop=stop), reads, writes)

    def TR(out, in_, ident, reads, writes):
        P.op(PE, lambda e: e.transpose(out, in_, ident), reads, writes)

    def ACTV(out, in_, func, reads, writes, bias=None, scale=None):
        kw = {}
        if bias is not None:
            kw["bias"] = bias
        if scale is not None:
            kw["scale"] = scale
        P.op(ACT, lambda e: e.activation(out=out, in_=in_, func=func, **kw), reads, writes)

    def TS(out, in0, s1, s2, op0, op1, reads, writes, eng=DVE):
        if s2 is None:
            P.op(eng, lambda e: e.tensor_scalar(out=out, in0=in0, scalar1=s1, scalar2=None, op0=op0), reads, writes)
        else:
            P.op(eng, lambda e: e.tensor_scalar(out=out, in0=in0, scalar1=s1, scalar2=s2, op0=op0, op1=op1),
                 reads, writes)

    def TT(out, in0, in1, op, reads, writes, eng=DVE):
        P.op(eng, lambda e: e.tensor_tensor(out=out, in0=in0, in1=in1, op=op), reads, writes)

    def STT(out, in0, scalar, in1, op0, op1, reads, writes):
        P.op(DVE, lambda e: e.scalar_tensor_tensor(out=out, in0=in0, scalar=scalar, in1=in1, op0=op0, op1=op1),
             reads, writes)

    def CP(out, in_, reads, writes, eng=DVE):
        if eng == ACT:
            P.op(ACT, lambda e: e.activation(out=out, in_=in_, func=AF.Copy), reads, writes)
        else:
            P.op(eng, lambda e: e.tensor_copy(out=out, in_=in_), reads, writes)

    def RED(out, in_, reads, writes):
        P.op(DVE, lambda e: e.tensor_reduce(out=out, in_=in_, axis=AX.X, op=ALU.add), reads, writes)

    def RECIP(out, in_, reads, writes):
        P.op(DVE, lambda e: e.reciprocal(out=out, in_=in_), reads, writes)

    def MSET(ap, val, writes, eng=DVE):
        P.op(eng, lambda e: e.memset(ap, val), (), writes)

    def DMA(q, out, in_, reads, writes):
        return P.dma(q, lambda e: e.dma_start(out=out, in_=in_), reads, writes)

    def bcast(ap1n, n):
        return ap1n.partition_broadcast(128)

    def rstd_from_ss(rs, ss, inv_n, key):
        TS(rs, ss, inv_n, EPS, ALU.mult, ALU.add, [key], [key])
        ACTV(rs, rs, AF.Sqrt, [key], [key])
        RECIP(rs, rs, [key], [key])

    with contextlib.ExitStack() as top:
        uniq = [0]

        def sb(name, shape, dt=F32, stack=top):
            uniq[0] += 1
            return stack.enter_context(nc.sbuf_tensor(f"sb_{name}_{uniq[0]}", list(shape), dt))

        psf = [top.enter_context(nc.psum_tensor(f"psf{i}", [128, 512], F32)) for i in range(6)]
        psb = [top.enter_context(nc.psum_tensor(f"psb{i}", [128, 1024], BF16)) for i in range(2)]
        rr = {"f": 0, "b": 0, "t": 0}

        def nps():
            i = rr["f"]
            rr["f"] = (i + 1) % 6
            return psf[i], ("psf", i)

        def npsb():
            i = rr["b"]
            rr["b"] = (i + 1) % 2
            return psb[i], ("psb", i)

        ident_f = sb("ident_f", [128, 128])
        ident_b = sb("ident_b", [128, 128], BF16)
        triF = sb("triF", [128, 128])
        triB = sb("triB", [128, 128])
        ones_f = sb("ones_f", [128, 128])
        bcol = sb("bcol", [128, 52])
        convc = sb("convc", [128, 8, 9])
        lb_bc = sb("lb_bc", [128, 2, 512])
        oml_bc = sb("oml_bc", [128, 2, 512])
        mn_bc = sb("mn_bc", [128, 512])
        hn_bc = sb("hn_bc", [128, 512])
        br_bc = sb("br_bc", [128, 32])
        wr_sb = sb("wr_sb", [128, 8, 32], BF16)
        wmg_sb = sb("wmg_sb", [128, 8, 16], BF16)
        bmg_bc = sb("bmg_bc", [128, 16])
        DMA(SP, ident_f[:], ident_d, [], ["ident_f"])
        DMA(POOL, ident_b[:], ident_d, [], ["ident_b"])
        DMA(SP, triF[:], triF_d, [], ["triF"])
        DMA(SP, triB[:], triB_d, [], ["triB"])
        DMA(SP, ones_f[:], ones_d, [], ["ones_f"])
        DMA(SP, bcol[:], bcol_d, [], ["bcol"])
        DMA(SP, convc[:], conv_d, [], ["convc"])
        DMA(SP, mn_bc[:], bcast(mn_d, 512), [], ["mn_bc"])
        DMA(SP, hn_bc[:], bcast(hn_d, 512), [], ["hn_bc"])
        DMA(SP, br_bc[:], bcast(br_d, 32), [], ["br_bc"])
        DMA(POOL, wr_sb[:], wr_d, [], ["wr_sb"])
        DMA(POOL, wmg_sb[:], wmg_d, [], ["wmg_sb"])
        DMA(SP, bmg_bc[:], bcast(brow_d[:, 6656:6672], 16), [], ["bmg_bc"])
        lbf = lb_bc[:].rearrange("p a b -> p (a b)")
        omlf = oml_bc[:].rearrange("p a b -> p (a b)")
        with contextlib.ExitStack() as sl:
            lbr = sb("lbr", [128, 2048], stack=sl)
            DMA(SP, lbr[:], bcast(lb_d, 2048), [], ["lbr"])
            TT(lbf, lbr[:, 0:1024], lbr[:, 1024:2048], ALU.subtract, ["lbr"], ["lb_bc"])
            ACTV(lbf, lbf, AF.Sigmoid, ["lb_bc"], ["lb_bc"])
            TS(omlf, lbf, -1.0, 1.0, ALU.mult, ALU.add, ["lb_bc"], ["oml_bc"])
        P.barrier()

        with contextlib.ExitStack() as s0:
            cT = sb("cT", [128, 24], stack=s0)
            sg0 = sb("sg0", [128, 24], stack=s0)
            cb = sb("cb", [128, 24, 128], stack=s0)
            nrm_bc = sb("nrm_bc", [128, 4, 1024], stack=s0)
            wa = [sb(f"wa{i}", [128, 8, 512], stack=s0) for i in range(2)]
            ba = [sb(f"ba{i}", [128, 512], stack=s0) for i in range(2)]
            tA = [sb(f"tA{i}", [128, 512], stack=s0) for i in range(3)]
            tB = [sb(f"tB{i}", [128, 512], stack=s0) for i in range(3)]
            DMA(SP, cT[:], cvT_d, [], ["cT"])
            DMA(SP, nrm_bc[:].rearrange("p a b -> p (a b)"), bcast(nrm_d, 4096), [], ["nrm_bc"])
            ACTV(sg0[:], cT[:], AF.Sigmoid, ["cT"], ["sg0"])
            TT(cT[:], cT[:], sg0[:], ALU.mult, ["cT", "sg0"], ["cT"])
            for i in range(24):
                TS(cb[:, i, :], ones_f[:], cT[:, i:i + 1], None, ALU.mult, None, ["ones_f", "cT"], [("cb", i)])
            ti_rot = 0
            for cbi in range(12):
                w, half = cbi // 2, cbi % 2
                cs = slice(half * 512, (half + 1) * 512)
                wt, bt = wa[cbi % 2], ba[cbi % 2]
                DMA(SP, wt[:], wada_d[cbi], [], [("wa", cbi % 2)])
                DMA(SP, bt[:], bcast(bada_d[:, cbi * 512:(cbi + 1) * 512], 512), [], [("ba", cbi % 2)])
                for r in range(3):
                    if r == 2 and w > 1:
                        continue
                    pt, pk = nps()
                    for k in range(8):
                        MM(pt[:, :], cb[:, r * 8 + k, :], wt[:, k, :], k == 0, k == 7,
                           [("cb", r * 8 + k), ("wa", cbi % 2)], [pk])
                    a_, b_ = tA[ti_rot % 3], tB[ti_rot % 3]
                    ka, kb = ("tA", ti_rot % 3), ("tB", ti_rot % 3)
                    ti_rot += 1
                    TT(a_[:], pt[:, :], bt[:], ALU.add, [pk, ("ba", cbi % 2)], [ka])
                    if w in (1, 4):
                        STT(b_[:], a_[:], 1.0, nrm_bc[:, 0 if w == 1 else 2, cs], ALU.add, ALU.mult,
                            [ka, "nrm_bc"], [kb])
                        src, ksrc = b_, kb
                    elif w in (2, 5):
                        TT(b_[:], a_[:], nrm_bc[:, 1 if w == 2 else 3, cs], ALU.mult, [ka, "nrm_bc"], [kb])
                        src, ksrc = b_, kb
                    else:
                        src, ksrc = a_, ka
                    tidx = r * 6 + w if r < 2 else 12 + w
                    DMA(SP, bc_d[tidx, :, cs], src[:], [ksrc], [("bc", tidx, half)])

        P.barrier()

        def load_bc(dst, tidx, key):
            DMA(SP, dst[:], bc_d[tidx], [("bc", tidx, 0), ("bc", tidx, 1)], [key])

        Ls = sb("Ls", [128, 32, 32])
        m8s = sb("m8s", [128, 32, 8])
        pidx = sb("pidx", [128, 1])
        DMA(SP, pidx[:], pidx_d, [], ["pidx"])
        small = sb("small", [128, 64])

        def hTk(t0, t1):
            return kk("hT", range(t0 // 128, (t1 + 127) // 128))

        def dump(nm, ap_sb, dst, reads):
            if nm in dump_d:
                out_toks.append(DMA(POOL, dst(dump_d[nm]), ap_sb, reads, []))

        mx = None
        for b in range(nb):
            mx = contextlib.ExitStack()
            hT = sb("hT", [128, 8, TOK], BF16, stack=mx)
            aT = sb("aT", [128, 4, 2048], BF16, stack=mx)
            bT = sb("bT", [128, 4, 2048], BF16, stack=mx)
            with contextlib.ExitStack() as s1:
                A1 = sb("A1", [128, 1024], stack=s1)
                SH1 = sb("SH1", [128, 1024], stack=s1)
                A1c = sb("A1c", [128, 1024], stack=s1)
                SH1c = sb("SH1c", [128, 1024], stack=s1)
                xin = [sb(f"xin{i}", [128, 1024], stack=s1) for i in range(2)]
                sq = sb("sq", [128, 1024], stack=s1)
                tmp = sb("tmp1", [128, 1024], stack=s1)
                hb = [sb(f"hb{i}", [128, 1024], BF16, stack=s1) for i in range(2)]
                load_bc(A1, b * 6 + 1, "A1")
                load_bc(SH1, b * 6 + 0, "SH1")
                load_bc(A1c, 13, "A1c")
                load_bc(SH1c, 12, "SH1c")
                for ti in range(NT):
                    xi, xk = xin[ti % 2], ("xin", ti % 2)
                    src = ctx_d[b, ti * 128:(ti + 1) * 128, :] if ti < 2 else x_d[b, (ti - 2) * 128:(ti - 1) * 128, :]
                    DMA(SP, xi[:], src, [], [xk])
                    ACTV(sq[:], xi[:], AF.Square, [xk], ["sq"])
                    sc, sk = small[:, ti:ti + 1], ("small", ti)
                    RED(sc, sq[:], ["sq"], [sk])
                    rstd_from_ss(sc, sc, 1.0 / 1024, sk)
                    Am, Ak, Sm, Sk = (A1c, "A1c", SH1c, "SH1c") if ti < 2 else (A1, "A1", SH1, "SH1")
                    STT(tmp[:], xi[:], sc, Am[:], ALU.mult, ALU.mult, [xk, sk, Ak], ["tmp1"])
                    hbt, hbk = hb[ti % 2], ("hb", ti % 2)
                    TT(hbt[:], tmp[:], Sm[:], ALU.add, ["tmp1", Sk], [hbk])
                    pb_, pbk = npsb()
                    for k in range(8):
                        TR(pb_[:, k * 128:(k + 1) * 128], hbt[:, k * 128:(k + 1) * 128], ident_b[:],
                           [hbk, "ident_b"], [pbk])
                    CP(hT[:, :, ti * 128:(ti + 1) * 128], pb_[:].rearrange("p (k t) -> p k t", k=8),
                       [pbk], [("hT", ti)], eng=ACT)
            P.barrier()
            dump("hT", hT[:, 0, :], lambda d: d, kk("hT", range(NT)))
            if stop == "s1":
                break

            with contextlib.ExitStack() as sm:
                wfm = [sb(f"wfm{i}", [128, 8, 128], BF16, stack=sm) for i in range(2)]
                srcf = sb("srcf", [128, TOK], stack=sm)
                accf = sb("accf", [128, TOK], stack=sm)
                stm = [sb(f"stm{i}", [128, 128], BF16, stack=sm) for i in range(2)]
                Pst = [sb(f"Pst{i}", [128, 129], stack=sm) for i in range(2)]
                Sbf = [sb(f"Sbf{i}", [128, 129], BF16, stack=sm) for i in range(2)]
                Sbf2 = [sb(f"Sbf2{i}", [128, 129], BF16, stack=sm) for i in range(2)]
                ssh = sb("ssh", [128, 16], stack=sm)
                tmpf = sb("tmpf", [128, 16, 128], stack=sm)
                a_tm = sb("a_tm", [128, 16, 128], BF16, stack=sm)

                def proj_fm(chunk, wt, wk, dst, dk):
                    DMA(POOL, wt[:], win_d[chunk], [], [wk])
                    for g0 in range(0, TOK, 512):
                        n = min(512, TOK - g0)
                        pt, pk = nps()
                        for k in range(8):
                            MM(pt[:, 0:n], wt[:, k, :], hT[:, k, g0:g0 + n], k == 0, k == 7,
                               [wk] + hTk(g0, g0 + n), [pk])
                        ACTV(dst[:, g0:g0 + n], pt[:, 0:n], AF.Identity, [pk, "bcol"], [dk],
                             bias=bcol[:, chunk:chunk + 1])

                def scan(d, QT, qk, q_lat_only, KT, ktk, Ktm, ktmk, V, vk, nv, dec_of, deck, post):
                    order = [0, 1] + list(range(2, NT)) if d == 0 else [1, 0] + list(range(NT - 1, 1, -1))
                    n_ = len(order)
                    mask, mkey = (triF, "triF") if d == 0 else (triB, "triB")
                    Pt, Pk = Pst[d], ("Pst", d)
                    Sts = [(Sbf[d], ("Sbf", d)), (Sbf2[d], ("Sbf2", d))]

                    def emit_U(i):
                        c_ = order[i]
                        pu, puk = nps()
                        MM(pu[:, 0:nv], Ktm[:, c_, :], V[:, c_, 0:nv], True, True, [ktmk, vk], [puk])
                        return pu, puk

                    nxt = emit_U(0)
                    for idx, c in enumerate(order):
                        cs = slice(c * 128, (c + 1) * 128)
                        qs = slice((c - 2) * 128, (c - 1) * 128) if q_lat_only else cs
                        if idx < n_ - 1:
                            pu, puk = nxt
                            St, Sk = Sts[idx % 2]
                            if idx == 0:
                                CP(Pt[:, 0:nv], pu[:, 0:nv], [puk], [Pk])
                            else:
                                STT(Pt[:, 0:nv], Pt[:, 0:nv], dec_of(order[idx - 1]), pu[:, 0:nv], ALU.mult, ALU.add,
                                    [Pk, puk, deck], [Pk])
                            TS(St[:, 0:nv], Pt[:, 0:nv], dec_of(c), None, ALU.mult, None, [Pk, deck], [Sk])
                            if idx + 1 < n_ - 1:
                                nxt = emit_U(idx + 1)
                        if c >= 2:
                            p1, p1k = nps()
                            MM(p1[:, 0:128], KT[:, cs], QT[:, qs], True, True, [ktk, qk], [p1k])
                            sm_, smk = stm[rr["t"] % 2], ("stm", rr["t"] % 2)
                            rr["t"] += 1
                            TT(sm_[:], p1[:, 0:128], mask[:], ALU.mult, [p1k, mkey], [smk])
                            po, pok = nps()
                            MM(po[:, 0:nv], sm_[:], V[:, c, 0:nv], True, idx == 0, [smk, vk], [pok])
                            if idx > 0:
                                Sp, Spk = Sts[(idx - 1) % 2]
                                MM(po[:, 0:nv], QT[:, qs], Sp[:, 0:nv], False, True, [qk, Spk], [pok])
                            post(po, pok, c)
                        yield

                def head_norm_to_T(hs, hsk, nbc, nbk, h, gate, gk, dstT, dstk, tmpf, a_tm):
                    ACTV(tmpf[:], hs[:], AF.Square, list(hsk), ["hn_tmp"])
                    RED(ssh[:], tmpf[:], ["hn_tmp"], ["ssh"])
                    rstd_from_ss(ssh[:], ssh[:], 1.0 / 128, "ssh")
                    for ti in range(16):
                        STT(tmpf[:, ti, :], hs[:, ti, :], ssh[:, ti:ti + 1], nbc[:, h * 128:(h + 1) * 128],
                            ALU.mult, ALU.mult, list(hsk) + ["ssh", nbk, "hn_tmp"], ["hn_tmp"])
                    TT(a_tm[:], tmpf[:], gate[:], ALU.mult, ["hn_tmp", gk], ["a_tm"])
                    for t0 in (0, 8):
                        pb_, pbk = npsb()
                        for j in range(8):
                            TR(pb_[:, j * 128:(j + 1) * 128], a_tm[:, t0 + j, :], ident_b[:], ["a_tm", "ident_b"], [pbk])
                        CP(dstT[:, h, t0 * 128:(t0 + 8) * 128], pb_[:, :], [pbk], [dstk], eng=ACT)

                s2x = contextlib.ExitStack()
                gates = sb("gates", [128, 16, NT], stack=s2x)
                lf = sb("lf", [128, 8, NT], stack=s2x)
                bcum = sb("bcum", [128, 8, NT], stack=s2x)
                dec_m = sb("dec_m", [128, 8, NT], stack=s2x)
                w_m = sb("w_m", [128, 8, NT], stack=s2x)
                e_m = sb("e_m", [128, 8, NT], stack=s2x)
                for ti in range(NT):
                    pt, pk = nps()
                    for k in range(8):
                        MM(pt[:, 0:16], hT[:, k, ti * 128:(ti + 1) * 128], wmg_sb[:, k, :], k == 0, k == 7,
                           [("hT", ti), "wmg_sb"], [pk])
                    TT(gates[:, :, ti], pt[:, 0:16], bmg_bc[:], ALU.add, [pk, "bmg_bc"], ["gates"])
                ACTV(lf[:], gates[:, 8:16, :], AF.Sigmoid, ["gates"], ["lf"])
                ACTV(lf[:], lf[:], AF.Ln, ["lf"], ["lf"])
                pt, pk = nps()
                MM(pt[:, 0:72], triF[:], lf[:, 0:4, :].rearrange("p a b -> p (a b)"), True, True, ["triF", "lf"], [pk])
                MM(pt[:, 72:144], triB[:], lf[:, 4:8, :].rearrange("p a b -> p (a b)"), True, True,
                   ["triB", "lf"], [pk])
                CP(bcum[:].rearrange("p a b -> p (a b)"), pt[:, 0:144], [pk], ["bcum"])
                pt2, pk2 = nps()
                MM(pt2[:, 0:144], ones_f[:], lf[:].rearrange("p a b -> p (a b)"), True, True, ["ones_f", "lf"], [pk2])
                ACTV(dec_m[:].rearrange("p a b -> p (a b)"), pt2[:, 0:144], AF.Exp, [pk2], ["dec_m"])
                TT(w_m[:], gates[:, 0:8, :], bcum[:], ALU.subtract, ["gates", "bcum"], ["w_m"])
                ACTV(w_m[:], w_m[:], AF.Exp, ["w_m"], ["w_m"])
                ACTV(e_m[:], bcum[:], AF.Exp, ["bcum"], ["e_m"])
                dump("bcum", bcum[:].rearrange("p a b -> p (a b)"), lambda d: d, ["bcum"])

                with contextlib.ExitStack() as s3:
                    qT = sb("qT", [128, TOK], BF16, stack=s3)
                    kT = sb("kT", [128, TOK], BF16, stack=s3)
                    k_tm = sb("k_tm", [128, NT, 128], BF16, stack=s3)
                    wvo = sb("wvo", [128, 8, 256], BF16, stack=s3)
                    bvo = sb("bvo", [128, 256], stack=s3)
                    vp = sb("vp", [128, NT, 129], BF16, stack=s3)
                    v2_ = sb("v2_0", [128, NT, 129], BF16, stack=s3)
                    v2 = [v2_, v2_]
                    og = sb("og", [128, 16, 128], BF16, stack=s3)
                    hsum = sb("hsum", [128, 16, 128], stack=s3)
                    dsc = sb("dsc", [128, 4], stack=s3)
                    for h in range(4):
                        for which, dst, dkey, scale in ((0, qT, "qT", 128.0 ** -0.5), (1, kT, "kT", 1.0)):
                            chunk = which * 4 + h
                            proj_fm(chunk, wfm[which], ("wfm", which), srcf, "srcf")
                            wc = convc[:, chunk, :]
                            ls = srcf[:, 256:TOK].rearrange("p (r c) -> p r c", c=64)
                            la = accf[:, 256:TOK].rearrange("p (r c) -> p r c", c=64)
                            TS(accf[:], srcf[:], wc[:, 4:5], None, ALU.mult, None, ["srcf", "convc"], ["accf"])
                            for di in (-1, 0, 1):
                                for dj in (-1, 0, 1):
                                    if di == 0 and dj == 0:
                                        continue
                                    tap = (di + 1) * 3 + (dj + 1)
                                    r0, r1 = max(0, -di), 32 - max(0, di)
                                    c0, c1 = max(0, -dj), 64 - max(0, dj)
                                    STT(la[:, r0:r1, c0:c1], ls[:, r0 + di:r1 + di, c0 + dj:c1 + dj], wc[:, tap:tap + 1],
                                        la[:, r0:r1, c0:c1], ALU.mult, ALU.add, ["srcf", "convc", "accf"], ["accf"])
                            STT(accf[:, 1:256], srcf[:, 0:255], wc[:, 3:4], accf[:, 1:256], ALU.mult, ALU.add,
                                ["srcf", "convc", "accf"], ["accf"])
                            STT(accf[:, 0:255], srcf[:, 1:256], wc[:, 5:6], accf[:, 0:255], ALU.mult, ALU.add,
                                ["srcf", "convc", "accf"], ["accf"])
                            ACTV(srcf[:], accf[:], AF.Sigmoid, ["accf"], ["srcf"])
                            STT(dst[:], accf[:], scale, srcf[:], ALU.mult, ALU.mult, ["accf", "srcf"], [dkey])
                        if h == 0 and b == 0:
                            dump("qT", qT[:], lambda d: d, ["qT"])
                            dump("kT", kT[:], lambda d: d, ["kT"])
                        for t0 in range(0, NT, 8):
                            n = min(8, NT - t0)
                            pb_, pbk = npsb()
                            for j in range(n):
                                TR(pb_[:, j * 128:(j + 1) * 128], kT[:, (t0 + j) * 128:(t0 + j + 1) * 128], ident_b[:],
                                   ["kT", "ident_b"], [pbk])
                            CP(k_tm[:, t0:t0 + n, :].rearrange("p a b -> p (a b)"), pb_[:, 0:n * 128], [pbk], ["k_tm"],
                               eng=ACT)
                        DMA(POOL, wvo[:, :, 0:128], win_d[8 + h], [], ["wvo"])
                        DMA(POOL, wvo[:, :, 128:256], win_d[12 + h], [], ["wvo"])
                        DMA(SP, bvo[:, 0:128], bcast(brow_d[:, 1024 + h * 128:1024 + (h + 1) * 128], 128), [], ["bvo"])
                        DMA(SP, bvo[:, 128:256], bcast(brow_d[:, 1536 + h * 128:1536 + (h + 1) * 128], 128), [], ["bvo"])
                        MSET(vp[:, :, 128:129], 1.0, ["vp"])
                        for ti in range(NT):
                            pt, pk = nps()
                            for k in range(8):
                                MM(pt[:, 0:256], hT[:, k, ti * 128:(ti + 1) * 128], wvo[:, k, :], k == 0, k == 7,
                                   [("hT", ti), "wvo"], [pk])
                            TT(vp[:, ti, 0:128], pt[:, 0:128], bvo[:, 0:128], ALU.add, [pk, "bvo"], ["vp"])
                            if ti >= 2:
                                TT(og[:, ti - 2, :], pt[:, 128:256], bvo[:, 128:256], ALU.add, [pk, "bvo"], ["og"])
                        ACTV(og[:], og[:], AF.Sigmoid, ["og"], ["og"])
                        gens = []
                        for d in range(2):
                            col = d * 4 + h
                            vd, vdk = (v2_, "v2") if d == 0 else (vp, "vp")
                            for ti in range(NT):
                                TS(vd[:, ti, :], vp[:, ti, :], w_m[:, col, ti:ti + 1], None, ALU.mult, None,
                                   ["vp", "w_m"], [vdk])

                            def post(po, pok, c, d=d, col=col):
                                ecol = e_m[:, col, c:c + 1]
                                d1, d2 = dsc[:, 2 * d:2 * d + 1], dsc[:, 2 * d + 1:2 * d + 2]
                                dk_ = ("dsc", d)
                                ACTV(d1, po[:, 128:129], AF.Abs, [pok, "e_m"], [dk_], scale=ecol)
                                TS(d1, d1, 1.0, None, ALU.max, None, [dk_], [dk_])
                                RECIP(d1, d1, [dk_], [dk_])
                                TT(d2, d1, ecol, ALU.mult, [dk_, "e_m"], [dk_])
                                if (c <= 9) == (d == 0):
                                    TS(hsum[:, c - 2, :], po[:, 0:128], d2, None, ALU.mult, None, [pok, dk_], [("hsum", c)])
                                else:
                                    STT(hsum[:, c - 2, :], po[:, 0:128], d2, hsum[:, c - 2, :], ALU.mult, ALU.add,
                                        [pok, dk_, ("hsum", c)], [("hsum", c)])

                            gens.append(scan(d, qT, "qT", False, kT, "kT", k_tm, "k_tm", vd, vdk, 129,
                                             lambda c, col=col: dec_m[:, col, c:c + 1], "dec_m", post))
                        while gens:
                            for g_ in list(gens):
                                try:
                                    next(g_)
                                except StopIteration:
                                    gens.remove(g_)
                        if h == 0 and b == 0:
                            dump("hsum", hsum[:].rearrange("p a b -> p (a b)"), lambda d: d, kk("hsum", range(2, NT)))
                        head_norm_to_T(hsum, kk("hsum", range(2, NT)), mn_bc, "mn_bc", h, og, "og", aT, "aT", tmpf, a_tm)
                s2x.close()
                P.barrier()
                if stop == "s3":
                    break

                with contextlib.ExitStack() as s4:
                    hqT = sb("hqT", [128, TOK], BF16, stack=s4)
                    w4 = sb("w4", [128, 8, 384], BF16, stack=s4)
                    w4b = sb("w4b", [128, 8, 128], BF16, stack=s4)
                    b4 = sb("b4", [128, 512], stack=s4)
                    hi_tm = sb("hi_tm", [128, NT, 128], BF16, stack=s4)
                    g_tm = sb("g_tm", [128, 16, 128], BF16, stack=s4)
                    zf = srcf[:].rearrange("p (a b) -> p a b", b=128)
                    kkf = accf[:].rearrange("p (a b) -> p a b", b=128)
                    ktl = sb("ktl", [128, NT, 128], BF16, stack=s4)
                    ktlT = sb("ktlT", [128, TOK], BF16, stack=s4)
                    qtl = sb("qtl", [128, 2048], BF16, stack=s4)
                    dec_h = sb("dec_h", [128, 2, NT], stack=s4)
                    ex_ = sb("ex0", [128, 512], stack=s4)
                    ex = [ex_, ex_]
                    osum = sb("osum", [128, 16, 128], stack=s4)
                    exr = 0
                    for h in range(4):
                        proj_fm(16 + h, wfm[0], ("wfm", 0), srcf, "srcf")
                        ACTV(accf[:], srcf[:], AF.Sigmoid, ["srcf"], ["accf"])
                        TT(hqT[:], srcf[:], accf[:], ALU.mult, ["srcf", "accf"], ["hqT"])
                        for j, ch in enumerate((20 + h, 24 + h, 28 + h)):
                            DMA(POOL, w4[:, :, j * 128:(j + 1) * 128], win_d[ch], [], ["w4"])
                        DMA(POOL, w4b[:], win_d[32 + h], [], ["w4b"])
                        for j, ch in enumerate((20 + h, 24 + h, 28 + h, 32 + h)):
                            DMA(SP, b4[:, j * 128:(j + 1) * 128], bcast(brow_d[:, ch * 128:(ch + 1) * 128], 128), [], ["b4"])
                        for d in range(2):
                            hs = slice(h * 128, (h + 1) * 128)
                            for ti in range(NT):
                                pt, pk = nps()
                                if d == 0:
                                    for k in range(8):
                                        MM(pt[:, 0:384], hT[:, k, ti * 128:(ti + 1) * 128], w4[:, k, :], k == 0, k == 7,
                                           [("hT", ti), "w4"], [pk])
                                    TT(hi_tm[:, ti, :], pt[:, 0:128], b4[:, 0:128], ALU.add, [pk, "b4"], ["hi_tm"])
                                    if ti >= 2:
                                        TT(tmpf[:, ti - 2, :], pt[:, 128:256], b4[:, 128:256], ALU.add, [pk, "b4"], ["hn_tmp"])
                                    TT(zf[:, ti, :], pt[:, 256:384], b4[:, 256:384], ALU.add, [pk, "b4"], ["srcf"])
                                else:
                                    for k in range(8):
                                        MM(pt[:, 0:128], hT[:, k, ti * 128:(ti + 1) * 128], w4b[:, k, :], k == 0, k == 7,
                                           [("hT", ti), "w4b"], [pk])
                                    TT(zf[:, ti, :], pt[:, 0:128], b4[:, 384:512], ALU.add, [pk, "b4"], ["srcf"])
                            if d == 0:
                                ACTV(g_tm[:], tmpf[:], AF.Sigmoid, ["hn_tmp"], ["g_tm"])
                                TT(g_tm[:], g_tm[:], tmpf[:], ALU.mult, ["g_tm", "hn_tmp"], ["g_tm"])
                            ACTV(accf[:], srcf[:], AF.Sigmoid, ["srcf"], ["accf"])
                            for ti in range(NT):
                                TT(zf[:, ti, :], kkf[:, ti, :], oml_bc[:, d, hs], ALU.mult, ["accf", "oml_bc", "srcf"], ["srcf"])
                                TT(zf[:, ti, :], zf[:, ti, :], lb_bc[:, d, hs], ALU.add, ["srcf", "lb_bc"], ["srcf"])
                            TS(accf[:], srcf[:], -1.0, 1.0, ALU.mult, ALU.add, ["srcf"], ["accf"])
                            ACTV(srcf[:], srcf[:], AF.Ln, ["srcf"], ["srcf"])
                            tri, trik = (triF, "triF") if d == 0 else (triB, "triB")
                            for t0 in range(0, NT, 4):
                                n = min(4, NT - t0)
                                fs = slice(t0 * 128, (t0 + n) * 128)
                                pt, pk = nps()
                                MM(pt[:, 0:n * 128], tri[:], srcf[:, fs], True, True, [trik, "srcf"], [pk])
                                e_, ek = ex[0], ("ex", 0)
                                exr += 1
                                ACTV(e_[:, 0:n * 128], pt[:, 0:n * 128], AF.Exp, [pk], [ek], scale=-1.0)
                                TT(ktl[:, t0:t0 + n, :].rearrange("p a b -> p (a b)"), accf[:, fs], e_[:, 0:n * 128], ALU.mult,
                                   ["accf", ek], ["ktl"])
                                pt, pk = nps()
                                for j in range(n):
                                    MM(pt[:, j * 128:(j + 1) * 128], zf[:, t0 + j, :], tri[:], True, True, ["srcf", trik], [pk])
                                e_, ek = ex[0], ("ex", 0)
                                exr += 1
                                ACTV(e_[:, 0:n * 128], pt[:, 0:n * 128], AF.Exp, [pk], [ek])
                                lastc = 127 if d == 0 else 0
                                CP(dec_h[:, d, t0:t0 + n],
                                   e_[:, 0:n * 128].rearrange("p (a b) -> p a b", b=128)[:, :, lastc], [ek], ["dec_h"])
                                for j in range(n):
                                    ti = t0 + j
                                    if ti >= 2:
                                        TT(qtl[:, (ti - 2) * 128:(ti - 1) * 128], hqT[:, ti * 128:(ti + 1) * 128],
                                           e_[:, j * 128:(j + 1) * 128], ALU.mult, ["hqT", ek], ["qtl"])
                            for t0 in range(0, NT, 8):
                                n = min(8, NT - t0)
                                pb_, pbk = npsb()
                                for j in range(n):
                                    TR(pb_[:, j * 128:(j + 1) * 128], ktl[:, t0 + j, :], ident_b[:], ["ktl", "ident_b"], [pbk])
                                CP(ktlT[:, t0 * 128:(t0 + n) * 128], pb_[:, 0:n * 128], [pbk], ["ktlT"], eng=ACT)

                            if h == 0 and b == 0 and d == 0:
                                dump("ktl", ktl[:].rearrange("p a b -> p (a b)"), lambda dd: dd, ["ktl"])
                                dump("qtl", qtl[:], lambda dd: dd, ["qtl"])
                                dump("ktlT", ktlT[:], lambda dd: dd, ["ktlT"])
                                dump("dech", dec_h[:].rearrange("p a b -> p (a b)"), lambda dd: dd, ["dec_h"])
                                dump("hqT", hqT[:], lambda dd: dd, ["hqT"])
                                dump("hitm", hi_tm[:].rearrange("p a b -> p (a b)"), lambda dd: dd, ["hi_tm"])

                            def post(po, pok, c, d=d):
                                if d == 0:
                                    CP(osum[:, c - 2, :], po[:, 0:128], [pok], ["osum"])
                                else:
                                    TT(osum[:, c - 2, :], po[:, 0:128], osum[:, c - 2, :], ALU.add, [pok, "osum"], ["osum"])

                            for _ in scan(d, qtl, "qtl", True, ktlT, "ktlT", ktl, "ktl", hi_tm, "hi_tm",
                                          128, lambda c, d=d: dec_h[:, d, c:c + 1], "dec_h", post):
                                pass
                        if h == 0 and b == 0:
                            dump("osum", osum[:].rearrange("p a b -> p (a b)"), lambda d: d, ["osum"])
                        head_norm_to_T(osum, ["osum"], hn_bc, "hn_bc", h, g_tm, "g_tm", bT, "bT", tmpf, a_tm)
                P.barrier()
                if stop == "s4":
                    break

            P.barrier()
            with contextlib.ExitStack() as s5:
                G1 = sb("G1", [128, 1024], stack=s5)
                A2 = sb("A2", [128, 1024], stack=s5)
                SH2 = sb("SH2", [128, 1024], stack=s5)
                wpa = sb("wpa", [128, 4, 1024], BF16, stack=s5)
                wpb = sb("wpb", [128, 4, 1024], BF16, stack=s5)
                wout = sb("wout", [128, 8, 1024], BF16, stack=s5)
                wg = [sb(f"wg{i}", [128, 8, 128], BF16, stack=s5) for i in range(2)]
                yT = sb("yT", [128, 8, 512], BF16, stack=s5)
                sga = sb("sga", [128, 512], stack=s5)
                y1 = sb("y1", [128, 512], stack=s5)
                y2 = sb("y2", [128, 512], stack=s5)
                sgb = sga
                yl = sb("yl", [128, 1024], stack=s5)
                sq = sb("sq5", [128, 1024], stack=s5)
                t1 = sq
                xin = [sb(f"xin5{i}", [128, 1024], stack=s5) for i in range(1)]
                x1t = [sb(f"x1t{i}", [128, 1024], stack=s5) for i in range(1)]
                h2b = sb("h2b", [128, 1024], BF16, stack=s5)
                h2Tt = sb("h2Tt", [128, 8, 128], BF16, stack=s5)
                load_bc(G1, b * 6 + 2, "G1")
                load_bc(A2, b * 6 + 4, "A2")
                load_bc(SH2, b * 6 + 3, "SH2")
                DMA(POOL, wpa[:], wpa_d, [], ["wpa"])
                DMA(POOL, wpb[:], wpb_d, [], ["wpb"])
                DMA(POOL, wout[:], wout_d, [], ["wout"])
                wgr = 0
                for tg in range(4):
                    tok0 = tg * 512
                    for j in range(8):
                        wga, wgak = wg[0], ("wg", 0)
                        wgb, wgbk = wg[1], ("wg", 1)
                        wgr += 2
                        DMA(POOL, wga[:], win_d[36 + j], [], [wgak])
                        DMA(POOL, wgb[:], win_d[44 + j], [], [wgbk])
                        pa, pak = nps()
                        for k in range(4):
                            MM(pa[:, :], wpa[:, k, j * 128:(j + 1) * 128], aT[:, k, tok0:tok0 + 512], k == 0, k == 3,
                               ["wpa", "aT"], [pak])
                        pga, pgak = nps()
                        for k in range(8):
                            MM(pga[:, :], wga[:, k, :], hT[:, k, 256 + tok0:256 + tok0 + 512], k == 0, k == 7,
                               [wgak] + hTk(256 + tok0, 256 + tok0 + 512), [pgak])
                        ACTV(sga[:], pga[:, :], AF.Sigmoid, [pgak, "bcol"], ["sga"], bias=bcol[:, 36 + j:37 + j])
                        TT(y1[:], pa[:, :], sga[:], ALU.mult, [pak, "sga"], ["y1"])
                        pb2, pb2k = nps()
                        for k in range(4):
                            MM(pb2[:, :], wpb[:, k, j * 128:(j + 1) * 128], bT[:, k, tok0:tok0 + 512], k == 0, k == 3,
                               ["wpb", "bT"], [pb2k])
                        pgb, pgbk = nps()
                        for k in range(8):
                            MM(pgb[:, :], wgb[:, k, :], hT[:, k, 256 + tok0:256 + tok0 + 512], k == 0, k == 7,
                               [wgbk] + hTk(256 + tok0, 256 + tok0 + 512), [pgbk])
                        ACTV(sgb[:], pgb[:, :], AF.Sigmoid, [pgbk, "bcol"], ["sga"], bias=bcol[:, 44 + j:45 + j])
                        TT(y2[:], pb2[:, :], sgb[:], ALU.mult, [pb2k, "sga"], ["y2"])
                        TT(yT[:, j, :], y1[:], y2[:], ALU.add, ["y1", "y2"], [("yT", j)])
                    for tl in range(4):
                        lt = tg * 4 + tl
                        for half in range(2):
                            pt, pk = nps()
                            for k in range(8):
                                MM(pt[:, :], yT[:, k, tl * 128:(tl + 1) * 128], wout[:, k, half * 512:(half + 1) * 512],
                                   k == 0, k == 7, [("yT", k), "wout"], [pk])
                            CP(yl[:, half * 512:(half + 1) * 512], pt[:, :], [pk], ["yl"], eng=ACT)
                        ACTV(sq[:], yl[:], AF.Square, ["yl"], ["sq5"])
                        sc, sk = small[:, 20 + lt:21 + lt], ("small", 20 + lt)
                        RED(sc, sq[:], ["sq5"], [sk])
                        rstd_from_ss(sc, sc, 1.0 / 1024, sk)
                        xi, xk = xin[0], ("xin5", 0)
                        DMA(SP, xi[:], x_d[b, lt * 128:(lt + 1) * 128, :], [], [xk])
                        STT(t1[:], yl[:], sc, G1[:], ALU.mult, ALU.mult, ["yl", sk, "G1"], ["sq5"])
                        xo, xok = x1t[0], ("x1t", 0)
                        TT(xo[:], t1[:], xi[:], ALU.add, ["sq5", xk], [xok])
                        DMA(SP, x1_d[b, lt * 128:(lt + 1) * 128, :], xo[:], [xok], [("x1d", b, lt)])
                        ACTV(sq[:], xo[:], AF.Square, [xok], ["sq5"])
                        sc2, sk2 = small[:, 40 + lt:41 + lt], ("small", 40 + lt)
                        RED(sc2, sq[:], ["sq5"], [sk2])
                        rstd_from_ss(sc2, sc2, 1.0 / 1024, sk2)
                        STT(t1[:], xo[:], sc2, A2[:], ALU.mult, ALU.mult, [xok, sk2, "A2"], ["sq5"])
                        TT(h2b[:], t1[:], SH2[:], ALU.add, ["sq5", "SH2"], ["h2b"])
                        pb_, pbk = npsb()
                        for k in range(8):
                            TR(pb_[:, k * 128:(k + 1) * 128], h2b[:, k * 128:(k + 1) * 128], ident_b[:],
                               ["h2b", "ident_b"], [pbk])
                        gt = b * 16 + lt
                        DMA(SP, h2_d[gt * 128:(gt + 1) * 128, :], h2b[:], ["h2b"], [("h2d", gt)])
                        CP(h2Tt[:], pb_[:].rearrange("p (k t) -> p k t", k=8), [pbk], ["h2Tt"], eng=ACT)
                        pt, pk = nps()
                        for k in range(8):
                            MM(pt[:, 0:32], h2Tt[:, k, :], wr_sb[:, k, :], k == 0, k == 7, ["h2Tt", "wr_sb"], [pk])
                        TT(Ls[:, gt, :], pt[:, 0:32], br_bc[:], ALU.add, [pk, "br_bc"], [("Ls", gt)])
                        P.op(DVE, lambda e, gt=gt: e.max(out=m8s[:, gt, :], in_=Ls[:, gt, :]), [("Ls", gt)], [("m8s", gt)])
            mx.close()
            mx = None
            P.barrier()
            if stop == "s5":
                break

        LsK = kk("Ls", range(32))
        m8K = kk("m8s", range(32))
        desti = sb("desti", [128, 128], I32)
        wk = sb("wk", [128, 32, 4])
        widx = sb("widx", [128, NB], I32)
        widx8 = sb("widx8", [128, 8, NB], I32)
        bidx = sb("bidx", [128, NB], I32)
        if stop in (None, "m1"):
            with contextlib.ExitStack() as m1:
                triS = sb("triS", [128, 128], stack=m1)
                DMA(SP, triS[:], triS_d, [], ["triS"])
                mask = sb("mask", [128, 1024], stack=m1)
                R1 = sb("R1", [128, 1024], stack=m1)
                cA = sb("cA", [128, 1024], stack=m1)
                cB = sb("cB", [128, 1024], stack=m1)
                cnt = sb("cnt", [128, 1024], stack=m1)
                dest = sb("dest", [128, 1024], stack=m1)
                tot = sb("tot", [128, 32], stack=m1)
                nblk = sb("nblk", [128, 32], stack=m1)
                pA = sb("pA", [128, 32], stack=m1)
                pB = sb("pB", [128, 32], stack=m1)
                pstart = sb("pstart", [128, 32], stack=m1)
                junk = sb("junk", [128, 32], stack=m1)
                destk = sb("destk", [128, 128], stack=m1)
                ejf = sb("ejf", [128, NB], stack=m1)
                wif = sb("wif", [128, NB], stack=m1)
                nm = sb("nm", [128, 32], stack=m1)
                exs = sb("exs", [128, 32, 4], stack=m1)
                ssum = sb("ssum", [128, 32], stack=m1)
                m3 = lambda t: t[:].rearrange("p (a b) -> p a b", b=32)
                TT(m3(mask), Ls[:], m8s[:, :, 3:4].to_broadcast([128, 32, 32]), ALU.is_ge, LsK + m8K, ["mask"])
                for hf in range(2):
                    cs = slice(hf * 512, (hf + 1) * 512)
                    pt, pk = nps()
                    MM(pt[:, :], triS[:], mask[:, cs], True, True, ["triS", "mask"], [pk])
                    CP(R1[:, cs], pt[:, :], [pk], ["R1"])
                    pt, pk = nps()
                    MM(pt[:, :], ones_f[:], mask[:, cs], True, True, ["ones_f", "mask"], [pk])
                    CP(cnt[:, cs], pt[:, :], [pk], ["cnt"])
                CP(cA[:], cnt[:], ["cnt"], ["cA"])
                a_, ak, b_, bk = cA, "cA", cB, "cB"
                for sft in (1, 2, 4, 8, 16):
                    w_ = sft * 32
                    CP(b_[:, 0:w_], a_[:, 0:w_], [ak], [bk])
                    TT(b_[:, w_:1024], a_[:, w_:1024], a_[:, 0:1024 - w_], ALU.add, [ak], [bk])
                    a_, ak, b_, bk = b_, bk, a_, ak
                incl, inck = a_, ak
                CP(tot[:], incl[:, 31 * 32:32 * 32], [inck], ["tot"])
                TT(dest[:], incl[:], cnt[:], ALU.subtract, [inck, "cnt"], ["dest"])
                TT(dest[:], dest[:], R1[:], ALU.add, ["dest", "R1"], ["dest"])
                MSET(nblk[:], 0.0, ["nblk"])
                for j in range(MAXB):
                    STT(nblk[:], tot[:], float(j * BLK), nblk[:], ALU.is_gt, ALU.add, ["tot", "nblk"], ["nblk"])
                CP(pA[:], nblk[:], ["nblk"], ["pA"])
                a_, ak, b_, bk = pA, "pA", pB, "pB"
                for sft in (1, 2, 4, 8, 16):
                    CP(b_[:, 0:sft], a_[:, 0:sft], [ak], [bk])
                    TT(b_[:, sft:32], a_[:, sft:32], a_[:, 0:32 - sft], ALU.add, [ak], [bk])
                    a_, ak, b_, bk = b_, bk, a_, ak
                pend, pendk = a_, ak
                TT(pstart[:], pend[:], nblk[:], ALU.subtract, [pendk, "nblk"], ["pstart"])
                TS(pstart[:], pstart[:], float(BLK), None, ALU.mult, None, ["pstart"], ["pstart"])
                TT(m3(dest), m3(dest), pstart[:].rearrange("p (a e) -> p a e", a=1).to_broadcast([128, 32, 32]), ALU.add,
                   ["dest", "pstart"], ["dest"])
                for k in range(4):
                    TT(m3(cA), Ls[:], m8s[:, :, k:k + 1].to_broadcast([128, 32, 32]), ALU.is_equal, LsK + m8K + ["cA"], ["cA"])
                    TT(cA[:], cA[:], dest[:], ALU.mult, ["cA", "dest"], ["cA"])
                    RED(destk[:, k * 32:(k + 1) * 32], m3(cA), ["cA"], ["destk"])
                TS(destk[:], destk[:], 0.0, float(NB * BLK - 1), ALU.max, ALU.min, ["destk"], ["destk"])
                CP(desti[:], destk[:], ["destk"], ["desti"])
                TT(exs[:], m8s[:, :, 0:4], m8s[:, :, 0:1].to_broadcast([128, 32, 4]), ALU.subtract, m8K, ["exs"])
                ACTV(exs[:], exs[:], AF.Exp, ["exs"], ["exs"])
                RED(ssum[:], exs[:], ["exs"], ["ssum"])
                RECIP(ssum[:], ssum[:], ["ssum"], ["ssum"])
                TT(wk[:], exs[:], ssum[:].rearrange("p (a b) -> p a b", b=1).to_broadcast([128, 32, 4]), ALU.mult,
                   ["exs", "ssum"], ["wk"])
                jix = sb("jix", [128, NB], stack=m1)
                cmpb = sb("cmpb", [128, NB, 32], stack=m1)
                DMA(SP, jix[:], bcast(jidx_d, NB), [], ["jix"])
                TT(cmpb[:], pend[:].rearrange("p (a e) -> p a e", a=1).to_broadcast([128, NB, 32]),
                   jix[:].rearrange("p (a b) -> p a b", b=1).to_broadcast([128, NB, 32]), ALU.is_le, [pendk, "jix"], ["cmpb"])
                RED(ejf[:], cmpb[:], ["cmpb"], ["ejf"])
                TS(ejf[:], ejf[:], 31.0, None, ALU.min, None, ["ejf"], ["ejf"])
                CP(bidx[:], ejf[:], ["ejf"], ["bidx"])
                TS(wif[:], ejf[:], 128.0, pidx[:, 0:1], ALU.mult, ALU.add, ["ejf", "pidx"], ["wif"])
                CP(widx[:], wif[:], ["wif"], ["widx"])
                wif8 = sb("wif8", [128, 8, NB], stack=m1)
                for k in range(8):
                    TS(wif8[:, k, :], wif[:], 8.0, float(k), ALU.mult, ALU.add, ["wif"], ["wif8"])
                CP(widx8[:], wif8[:], ["wif8"], ["widx8"])
                dump("destk", destk[:], lambda d: d, ["destk"])
                dump("ejf", ejf[:], lambda d: d, ["ejf"])
                dump("wk", wk[:].rearrange("p a b -> p (a b)"), lambda d: d, ["wk"])
                dump("wif", wif[:], lambda d: d, ["wif"])
                h2t = [sb(f"h2t{i}", [128, 1024], BF16, stack=m1) for i in range(2)]
                for gt in range(32 if stop is None else 0):
                    ht, htk = h2t[gt % 2], ("h2t", gt % 2)
                    DMA(SP, ht[:], h2_d[gt * 128:(gt + 1) * 128, :], [("h2d", gt)], [htk])
                    for k in range(4):
                        c_ = k * 32 + gt
                        P.dma(POOL, lambda e, ht=ht, c_=c_: e.indirect_dma_start(
                            out=xs_d, out_offset=bass.IndirectOffsetOnAxis(ap=desti[:, c_:c_ + 1], axis=0),
                            in_=ht[:], in_offset=None),
                            [htk, "desti"], ["xs"])
            P.barrier()

        if stop is None:
            with contextlib.ExitStack() as m4:
                wgu = [sb(f"wgu{i}", [128, 8, 2048], BF16, stack=m4) for i in range(2)]
                wdn = [sb(f"wdn{i}", [128, 8, 1024], BF16, stack=m4) for i in range(2)]
                bgt = [sb(f"bgt{i}", [128, 16], stack=m4) for i in range(2)]
                bdt = [sb(f"bdt{i}", [128, 1024], stack=m4) for i in range(2)]
                xbs = [sb(f"xb{i}", [128, 4, 1024], BF16, stack=m4) for i in range(2)]
                xT = sb("xT", [128, 8, 512], BF16, stack=m4)
                act = sb("act", [128, 8, 512], BF16, stack=m4)
                g1 = [sb(f"g1_{i}", [128, 512], stack=m4) for i in range(2)]
                sg = [sb(f"sg_{i}", [128, 512], stack=m4) for i in range(2)]
                u1 = [sb(f"u1_{i}", [128, 512], stack=m4) for i in range(2)]
                ybt = [sb(f"ybt{i}", [128, 1024], stack=m4) for i in range(2)]

                def load_w(j):
                    r_ = j % 2
                    ix = widx[:, j:j + 1]
                    for k in range(8):
                        P.dma(POOL, lambda e, k=k: e.indirect_dma_start(
                            out=wgu[r_][:, k, :], out_offset=None, in_=wgu_d,
                            in_offset=bass.IndirectOffsetOnAxis(ap=widx8[:, k, j:j + 1], axis=0)),
                            ["widx8"], [("wgu", r_, k)])
                    for k in range(8):
                        P.dma(POOL, lambda e, k=k: e.indirect_dma_start(
                            out=wdn[r_][:, k, :], out_offset=None, in_=wdn_d,
                            in_offset=bass.IndirectOffsetOnAxis(ap=widx8[:, k, j:j + 1], axis=0)),
                            ["widx8"], [("wdn", r_, k)])
                    P.dma(POOL, lambda e: e.indirect_dma_start(
                        out=bgt[r_][:], out_offset=None, in_=bgu_d,
                        in_offset=bass.IndirectOffsetOnAxis(ap=ix, axis=0)), ["widx"], [("bgt", r_)])
                    P.dma(POOL, lambda e: e.indirect_dma_start(
                        out=bdt[r_][:], out_offset=None, in_=bdn_d,
                        in_offset=bass.IndirectOffsetOnAxis(ap=bidx[:, j:j + 1], axis=0)), ["bidx"], [("bdt", r_)])

                load_w(0)
                rot = 0
                for j in range(NB):
                    r_ = j % 2
                    if j + 1 < NB:
                        load_w(j + 1)
                    xb, xbk = xbs[j % 2], ("xb", j % 2)
                    if j == 0:
                        DMA(SP, xb[:], xs_d[0:BLK, :].rearrange("(a p) n -> p a n", p=128), ["xs"], [xbk])
                    if j + 1 < NB:
                        DMA(SP, xbs[(j + 1) % 2][:], xs_d[(j + 1) * BLK:(j + 2) * BLK, :].rearrange("(a p) n -> p a n", p=128),
                            ["xs"], [("xb", (j + 1) % 2)])
                    for a in range(4):
                        pb_, pbk = npsb()
                        for k in range(8):
                            TR(pb_[:, k * 128:(k + 1) * 128], xb[:, a, k * 128:(k + 1) * 128], ident_b[:], [xbk, "ident_b"], [pbk])
                        CP(xT[:, :, a * 128:(a + 1) * 128], pb_[:].rearrange("p (k t) -> p k t", k=8), [pbk], ["xT"],
                           eng=ACT if a % 2 == 0 else DVE)
                    for fb in range(8):
                        q_ = rot % 2
                        rot += 1
                        pg, pgk = nps()
                        for k in range(8):
                            MM(pg[:, :], wgu[r_][:, k, fb * 128:(fb + 1) * 128], xT[:, k, :], k == 0, k == 7,
                               [("wgu", r_, k), "xT"], [pgk])
                        pu, puk = nps()
                        for k in range(8):
                            MM(pu[:, :], wgu[r_][:, k, 1024 + fb * 128:1024 + (fb + 1) * 128], xT[:, k, :], k == 0, k == 7,
                               [("wgu", r_, k), "xT"], [puk])
                        TS(g1[q_][:], pg[:, :], bgt[r_][:, fb:fb + 1], 7.0, ALU.add, ALU.min, [pgk, ("bgt", r_)], [("g1", q_)])
                        ACTV(sg[q_][:], g1[q_][:], AF.Sigmoid, [("g1", q_)], [("sg", q_)], scale=1.702)
                        TS(u1[q_][:], pu[:, :], bgt[r_][:, 8 + fb:9 + fb], 7.0, ALU.add, ALU.min, [puk, ("bgt", r_)], [("u1", q_)])
                        TS(u1[q_][:], u1[q_][:], -7.0, 1.0, ALU.max, ALU.add, [("u1", q_)], [("u1", q_)])
                        TT(g1[q_][:], g1[q_][:], sg[q_][:], ALU.mult, [("g1", q_), ("sg", q_)], [("g1", q_)])
                        TT(act[:, fb, :], g1[q_][:], u1[q_][:], ALU.mult, [("g1", q_), ("u1", q_)], ["act"])
                    for a in range(4):
                        y_, yk = ybt[a % 2], ("ybt", a % 2)
                        for dh in range(2):
                            py, pyk = nps()
                            for fb in range(8):
                                MM(py[:, :], act[:, fb, a * 128:(a + 1) * 128], wdn[r_][:, fb, dh * 512:(dh + 1) * 512],
                                   fb == 0, fb == 7, ["act", ("wdn", r_, fb)], [pyk])
                            TT(y_[:, dh * 512:(dh + 1) * 512], py[:, :], bdt[r_][:, dh * 512:(dh + 1) * 512], ALU.add,
                               [pyk, ("bdt", r_)], [yk])
                        DMA(SP, yb_d[j * BLK + a * 128:j * BLK + (a + 1) * 128, :], y_[:], [yk], ["yb"])
            P.barrier()

            with contextlib.ExitStack() as m5:
                G2 = [sb(f"G2_{i}", [128, 1024], stack=m5) for i in range(2)]
                ybk = [sb(f"ybk{i}", [128, 1024], stack=m5) for i in range(8)]
                ym = sb("ym", [128, 1024], stack=m5)
                sq = sb("sq7", [128, 1024], stack=m5)
                x1r = [sb(f"x1r{i}", [128, 1024], stack=m5) for i in range(2)]
                ot = [sb(f"ot{i}", [128, 1024], stack=m5) for i in range(2)]
                for b in range(nb):
                    load_bc(G2[b], b * 6 + 5, ("G2", b))
                for gt in range(nb * 16):
                    b, lt = gt // 16, gt % 16
                    ys = []
                    for k in range(4):
                        i_ = (gt % 2) * 4 + k
                        c_ = k * 32 + gt
                        P.dma(POOL, lambda e, i_=i_, c_=c_: e.indirect_dma_start(
                            out=ybk[i_][:], out_offset=None, in_=yb_d,
                            in_offset=bass.IndirectOffsetOnAxis(ap=desti[:, c_:c_ + 1], axis=0)),
                            ["yb", "desti"], [("ybk", i_)])
                        ys.append((ybk[i_], ("ybk", i_)))
                    TS(ym[:], ys[0][0][:], wk[:, gt, 0:1], None, ALU.mult, None, [ys[0][1], "wk"], ["ym"])
                    for k in range(1, 4):
                        STT(ym[:], ys[k][0][:], wk[:, gt, k:k + 1], ym[:], ALU.mult, ALU.add, [ys[k][1], "wk", "ym"], ["ym"])
                    ACTV(sq[:], ym[:], AF.Square, ["ym"], ["sq7"])
                    sc, sk = small[:, 20 + lt:21 + lt], ("small", 20 + lt)
                    RED(sc, sq[:], ["sq7"], [sk])
                    rstd_from_ss(sc, sc, 1.0 / 1024, sk)
                    xr, xrk = x1r[gt % 2], ("x1r", gt % 2)
                    DMA(SP, xr[:], x1_d[b, lt * 128:(lt + 1) * 128, :], [("x1d", b, lt)], [xrk])
                    STT(sq[:], ym[:], sc, G2[b][:], ALU.mult, ALU.mult, ["ym", sk, ("G2", b), "sq7"], ["sq7"])
                    o_, ok_ = ot[gt % 2], ("ot", gt % 2)
                    TT(o_[:], sq[:], xr[:], ALU.add, ["sq7", xrk], [ok_])
                    out_toks.append(DMA(SP, out_d[b, lt * 128:(lt + 1) * 128, :], o_[:], [ok_], []))

        if mx is not None:
            mx.close()
        P.final_wait(SP, out_toks)
        P.emit()
    return nc


def _c(a):
    return np.ascontiguousarray(a, dtype=np.float32)


def prep_shared(inp):
    w_in = inp["w_in"][0]
    sh = {}
    sh["w_ada"] = _c(inp["w_ada"][0].reshape(8, 128, 12, 512).transpose(2, 1, 0, 3))
    sh["b_ada"] = _c(inp["b_ada"].reshape(1, 6144))
    sh["nrm"] = _c(np.concatenate([inp["norm_mix_pre"][0], inp["norm_mix_post"][0],
                                   inp["norm_ffn_pre"][0], inp["norm_ffn_post"][0]]).reshape(1, 4096))
    sh["w_in"] = _c(w_in[:, :6656].reshape(8, 128, 52, 128).transpose(2, 1, 0, 3))
    sh["w_mg"] = _c(w_in[:, 6656:].reshape(8, 128, 16).transpose(1, 0, 2))
    sh["b_in_col"] = _c(inp["b_in"][0, :6656].reshape(52, 128).T)
    sh["b_in_row"] = _c(inp["b_in"].reshape(1, 6672))
    sh["conv_col"] = _c(inp["conv_w"][0].reshape(9, 8, 128).transpose(2, 1, 0))
    sh["lb_raw"] = _c(inp["lb_raw"].reshape(1, 2048))
    sh["m_norm"] = _c(inp["m_norm"].reshape(1, 512))
    sh["h_norm"] = _c(inp["h_norm"].reshape(1, 512))
    sh["w_pa"] = _c(inp["w_pa"][0].reshape(4, 128, 1024).transpose(1, 0, 2))
    sh["w_pb"] = _c(inp["w_pb"][0].reshape(4, 128, 1024).transpose(1, 0, 2))
    sh["w_out"] = _c(inp["w_out"][0].reshape(8, 128, 1024).transpose(1, 0, 2))
    sh["w_router"] = _c(inp["w_router"][0].reshape(8, 128, 32).transpose(1, 0, 2))
    sh["b_router"] = _c(inp["b_router"].reshape(1, 32))
    sh["w_gu"] = _c(inp["w_gu"][0].reshape(32, 8, 128, 2048).transpose(0, 2, 1, 3)).reshape(32 * 128 * 8, 2048)
    sh["b_gu_col"] = _c(inp["b_gu"][0].reshape(32, 16, 128).transpose(0, 2, 1)).reshape(32 * 128, 16)
    sh["w_dn"] = _c(inp["w_dn"][0].reshape(32, 8, 128, 1024).transpose(0, 2, 1, 3)).reshape(32 * 128 * 8, 1024)
    sh["b_dn"] = _c(inp["b_dn"][0])
    sh["ident"] = np.eye(128, dtype=np.float32)
    sh["triF"] = np.triu(np.ones((128, 128), np.float32))
    sh["triB"] = np.tril(np.ones((128, 128), np.float32))
    sh["ones"] = np.ones((128, 128), np.float32)
    sh["pidx"] = np.arange(128, dtype=np.float32).reshape(128, 1)
    sh["jidx"] = np.arange(NB, dtype=np.float32).reshape(1, NB)
    sh["triS"] = np.triu(np.ones((128, 128), np.float32), 1)
    return sh


def core_inputs(inp, sh, i):
    m = dict(sh)
    m["x"] = _c(inp["x"][2 * i:2 * i + 2])
    m["ctx"] = _c(inp["ctx"][2 * i:2 * i + 2])
    cv = np.stack([inp["c"][2 * i], inp["c"][2 * i + 1], inp["c_ctx"]])
    m["cvT"] = _c(cv.reshape(3, 8, 128).transpose(2, 0, 1).reshape(128, 24))
    return m


def kernel(**inputs):
    inp = {k: np.asarray(v) for k, v in inputs.items()}
    sh = prep_shared(inp)
    nc = build_nc()
    in_maps = [core_inputs(inp, sh, i) for i in range(N_CORES)]
    res = run_bass_kernel_spmd(nc, in_maps, core_ids=list(range(N_CORES)))
    out = np.concatenate([np.asarray(r["out"], dtype=np.float32) for r in res.results], axis=0)
    return out
```

```python
import contextlib
import numpy as np
import concourse.bass as bass
import concourse.mybir as mybir
from concourse.bass_utils import run_bass_kernel_spmd

F32 = mybir.dt.float32
BF16 = mybir.dt.bfloat16
I32 = mybir.dt.int32
AF = mybir.ActivationFunctionType
ALU = mybir.AluOpType
AX = mybir.AxisListType

PE, DVE, ACT, POOL, SP = "pe", "dve", "act", "pool", "sp"
COMPUTE = (PE, DVE, ACT, POOL)
EPOCH = 12000
N_DMA_SEM = 88
DMA_POOLS = {"sp": (0, 32), "act": (32, 8), "pool": (40, 48)}
N_EPOCH_SEM = 12
EPS = 1e-6
NT = 18
BLK = 512
NB = 4096 * 4 // BLK + 32
MAXB = 4096 // BLK
TOK = 2304
N_CORES = 8


class Prog:
    def __init__(self, nc, same_engine_sync=True):
        self.nc = nc
        self.same = same_engine_sync
        self.streams = {e: [] for e in (PE, DVE, ACT, POOL, SP)}
        self.count = {e: 0 for e in COMPUTE}
        self.waited = {}
        self.state = {}
        self.dma_tot = [0] * N_DMA_SEM
        self.dma_rr = {q: 0 for q in DMA_POOLS}
        self.barrier_toks = set()

    def barrier(self):
        toks = set()
        for e in COMPUTE:
            c = self.count[e]
            if c > 0:
                toks.add(((e, (c - 1) // EPOCH), ((c - 1) % EPOCH) + 1))
        for s_ in range(N_DMA_SEM):
            if self.dma_tot[s_] > 0:
                toks.add((("dma", s_), self.dma_tot[s_]))
        self.barrier_toks = toks

    def _deps(self, reads, writes):
        deps = set()
        for k in reads:
            st = self.state.get(k)
            if st and st[0]:
                deps.add(st[0])
        for k in writes:
            st = self.state.get(k)
            if st:
                if st[0]:
                    deps.add(st[0])
                deps.update(st[1])
        return deps

    def _commit(self, reads, writes, tok):
        for k in writes:
            self.state[k] = [tok, []]
        for k in reads:
            if k in writes:
                continue
            st = self.state.setdefault(k, [None, []])
            st[1].append(tok)
            if len(st[1]) > 64:
                best = {}
                for (key, val) in st[1]:
                    if best.get(key, 0) < val:
                        best[key] = val
                st[1] = list(best.items())

    def _waits(self, eng, deps, own_key=None):
        best = {}
        for (key, val) in deps:
            if key == own_key and not self.same:
                continue
            if self.waited.get((eng, key), 0) >= val:
                continue
            if best.get(key, 0) < val:
                best[key] = val
        out = []
        for key, val in best.items():
            self.waited[(eng, key)] = val
            out.append((key, val))
        return out

    def op(self, eng, fn, reads=(), writes=()):
        reads, writes = tuple(reads), tuple(writes)
        c = self.count[eng]
        own_key = (eng, c // EPOCH)
        deps = self._deps(reads, writes) | self.barrier_toks
        if eng == PE:
            deps = {d for d in deps if d[0][0] != PE}
        waits = self._waits(eng, deps, own_key)
        self.count[eng] = c + 1
        tok = (own_key, (c % EPOCH) + 1)
        self.streams[eng].append(("op", waits, fn, own_key))
        self._commit(reads, writes, tok)

    def dma(self, q, fn, reads=(), writes=()):
        reads, writes = tuple(reads), tuple(writes)
        first, cnt_ = DMA_POOLS[q]
        s = first + self.dma_rr[q]
        self.dma_rr[q] = (self.dma_rr[q] + 1) % cnt_
        key = ("dma", s)
        deps = self._deps(reads, writes) | self.barrier_toks
        if self.dma_tot[s] > 0:
            deps.add((key, self.dma_tot[s]))
        waits = self._waits(q, deps, None)
        self.dma_tot[s] += 16
        tok = (key, self.dma_tot[s])
        self.streams[q].append(("dma", waits, fn, key))
        self._commit(reads, writes, tok)
        return tok

    def final_wait(self, eng, toks):
        waits = self._waits(eng, set(toks), None)
        self.streams[eng].append(("wait", waits, None, None))

    def emit(self):
        nc = self.nc
        with contextlib.ExitStack() as es:
            sems = {}
            for e in COMPUTE:
                nep = self.count[e] // EPOCH + 1
                assert nep <= N_EPOCH_SEM, (e, self.count[e])
                for i in range(nep):
                    sems[(e, i)] = es.enter_context(nc.semaphore(f"s_{e}_{i}"))
            for s in range(N_DMA_SEM):
                if self.dma_tot[s] > 0:
                    sems[("dma", s)] = es.enter_context(nc.semaphore(f"s_dma_{s}"))
            block = es.enter_context(nc.Block())

            def run(eng_name):
                def body(engine):
                    for kind, waits, fn, key in self.streams[eng_name]:
                        for (k, v) in waits:
                            engine.wait_ge(sems[k], v)
                        if kind == "op":
                            fn(engine).then_inc(sems[key], 1)
                        elif kind == "dma":
                            fn(engine).then_inc(sems[key], 16)
                return body

            block.sync(run(SP))
            block.tensor(run(PE))
            block.vector(run(DVE))
            block.scalar(run(ACT))
            block.gpsimd(run(POOL))


def kk(name, idxs):
    return [(name, i) for i in idxs]


def build_nc(nb=2, dumps=(), stop=None, n_exp=32):
    nc = bass.Bass("TRN2", target_bir_lowering=False)
    P = Prog(nc)
    D = {}

    def din(name, shape, dt=F32):
        D[name] = nc.dram_tensor(name, list(shape), dt, kind="ExternalInput").ap()
        return D[name]

    x_d = din("x", [2, 2048, 1024])
    ctx_d = din("ctx", [2, 256, 1024])
    cvT_d = din("cvT", [128, 24])
    wada_d = din("w_ada", [12, 128, 8, 512])
    bada_d = din("b_ada", [1, 6144])
    nrm_d = din("nrm", [1, 4096])
    win_d = din("w_in", [52, 128, 8, 128])
    wmg_d = din("w_mg", [128, 8, 16])
    bcol_d = din("b_in_col", [128, 52])
    brow_d = din("b_in_row", [1, 6672])
    conv_d = din("conv_col", [128, 8, 9])
    lb_d = din("lb_raw", [1, 2048])
    mn_d = din("m_norm", [1, 512])
    hn_d = din("h_norm", [1, 512])
    wpa_d = din("w_pa", [128, 4, 1024])
    wpb_d = din("w_pb", [128, 4, 1024])
    wout_d = din("w_out", [128, 8, 1024])
    wr_d = din("w_router", [128, 8, 32])
    br_d = din("b_router", [1, 32])
    wgu_d = din("w_gu", [32 * 128 * 8, 2048])
    bgu_d = din("b_gu_col", [32 * 128, 16])
    wdn_d = din("w_dn", [32 * 128 * 4, 2048])
    bdn_d = din("b_dn", [32, 1024])
    ident_d = din("ident", [128, 128])
    triF_d = din("triF", [128, 128])
    triB_d = din("triB", [128, 128])
    ones_d = din("ones", [128, 128])
    pidx_d = din("pidx", [128, 1])
    jidx_d = din("jidx", [1, NB])
    triS_d = din("triS", [128, 128])
    out_d = nc.dram_tensor("out", [2, 2048, 1024], F32, kind="ExternalOutput").ap()
    bc_d = nc.dram_tensor("bc_scr", [14, 128, 1024], F32).ap()
    x1_d = nc.dram_tensor("x1_scr", [2, 2048, 1024], F32).ap()
    h2_d = nc.dram_tensor("h2_scr", [4096, 1024], BF16).ap()
    xs_d = nc.dram_tensor("xs_scr", [NB * BLK, 1024], BF16).ap()
    yb_d = nc.dram_tensor("yb_scr", [NB * BLK, 1024], F32).ap()
    dump_d = {}
    for (nm, shape) in dumps:
        dump_d[nm] = nc.dram_tensor("dbg_" + nm, list(shape), F32, kind="ExternalOutput").ap()
    out_toks = []

    def MM(out, lhsT, rhs, start, stop, reads, writes):
        P.op(PE, lambda e: e.matmul(out, lhsT=lhsT, rhs=rhs, start=start, stop=stop), reads, writes)

    def TR(out, in_, ident, reads, writes):
        P.op(PE, lambda e: e.transpose(out, in_, ident), reads, writes)

    def ACTV(out, in_, func, reads, writes, bias=None, scale=None):
        kw = {}
        if bias is not None:
            kw["bias"] = bias
        if scale is not None:
            kw["scale"] = scale
        P.op(ACT, lambda e: e.activation(out=out, in_=in_, func=func, **kw), reads, writes)

    def TS(out, in0, s1, s2, op0, op1, reads, writes, eng=DVE):
        if s2 is None:
            P.op(eng, lambda e: e.tensor_scalar(out=out, in0=in0, scalar1=s1, scalar2=None, op0=op0), reads, writes)
        else:
            P.op(eng, lambda e: e.tensor_scalar(out=out, in0=in0, scalar1=s1, scalar2=s2, op0=op0, op1=op1),
                 reads, writes)

    def TT(out, in0, in1, op, reads, writes, eng=DVE):
        P.op(eng, lambda e: e.tensor_tensor(out=out, in0=in0, in1=in1, op=op), reads, writes)

    def STT(out, in0, scalar, in1, op0, op1, reads, writes):
        P.op(DVE, lambda e: e.scalar_tensor_tensor(out=out, in0=in0, scalar=scalar, in1=in1, op0=op0, op1=op1),
             reads, writes)

    def CP(out, in_, reads, writes, eng=DVE):
        if eng == ACT:
            P.op(ACT, lambda e: e.activation(out=out, in_=in_, func=AF.Copy), reads, writes)
        else:
            P.op(eng, lambda e: e.tensor_copy(out=out, in_=in_), reads, writes)

    def RED(out, in_, reads, writes):
        P.op(DVE, lambda e: e.tensor_reduce(out=out, in_=in_, axis=AX.X, op=ALU.add), reads, writes)

    def RECIP(out, in_, reads, writes):
        P.op(DVE, lambda e: e.reciprocal(out=out, in_=in_), reads, writes)

    def MSET(ap, val, writes, eng=DVE):
        P.op(eng, lambda e: e.memset(ap, val), (), writes)

    def DMA(q, out, in_, reads, writes):
        return P.dma(q, lambda e: e.dma_start(out=out, in_=in_), reads, writes)

    def bcast(ap1n, n):
        return ap1n.partition_broadcast(128)

    def rstd_from_ss(rs, ss, inv_n, key):
        TS(rs, ss, inv_n, EPS, ALU.mult, ALU.add, [key], [key])
        ACTV(rs, rs, AF.Sqrt, [key], [key])
        RECIP(rs, rs, [key], [key])

    with contextlib.ExitStack() as top:
        uniq = [0]

        def sb(name, shape, dt=F32, stack=top):
            uniq[0] += 1
            return stack.enter_context(nc.sbuf_tensor(f"sb_{name}_{uniq[0]}", list(shape), dt))

        psf = [top.enter_context(nc.psum_tensor(f"psf{i}", [128, 512], F32)) for i in range(6)]
        psb = [top.enter_context(nc.psum_tensor(f"psb{i}", [128, 1024], BF16)) for i in range(2)]
        rr = {"f": 0, "b": 0, "t": 0}

        def nps():
            i = rr["f"]
            rr["f"] = (i + 1) % 6
            return psf[i], ("psf", i)

        def npsb():
            i = rr["b"]
            rr["b"] = (i + 1) % 2
            return psb[i], ("psb", i)

        ident_f = sb("ident_f", [128, 128])
        ident_b = sb("ident_b", [128, 128], BF16)
        triF = sb("triF", [128, 128])
        triB = sb("triB", [128, 128])
        ones_f = sb("ones_f", [128, 128])
        bcol = sb("bcol", [128, 52])
        convc = sb("convc", [128, 8, 9])
        lb_bc = sb("lb_bc", [128, 2, 512])
        oml_bc = sb("oml_bc", [128, 2, 512])
        mn_bc = sb("mn_bc", [128, 512])
        hn_bc = sb("hn_bc", [128, 512])
        br_bc = sb("br_bc", [128, 32])
        wr_sb = sb("wr_sb", [128, 8, 32], BF16)
        wmg_sb = sb("wmg_sb", [128, 8, 16], BF16)
        bmg_bc = sb("bmg_bc", [128, 16])
        DMA(SP, ident_f[:], ident_d, [], ["ident_f"])
        DMA(POOL, ident_b[:], ident_d, [], ["ident_b"])
        DMA(SP, triF[:], triF_d, [], ["triF"])
        DMA(SP, triB[:], triB_d, [], ["triB"])
        DMA(SP, ones_f[:], ones_d, [], ["ones_f"])
        DMA(SP, bcol[:], bcol_d, [], ["bcol"])
        DMA(SP, convc[:], conv_d, [], ["convc"])
        DMA(SP, mn_bc[:], bcast(mn_d, 512), [], ["mn_bc"])
        DMA(SP, hn_bc[:], bcast(hn_d, 512), [], ["hn_bc"])
        DMA(SP, br_bc[:], bcast(br_d, 32), [], ["br_bc"])
        DMA(POOL, wr_sb[:], wr_d, [], ["wr_sb"])
        DMA(POOL, wmg_sb[:], wmg_d, [], ["wmg_sb"])
        DMA(SP, bmg_bc[:], bcast(brow_d[:, 6656:6672], 16), [], ["bmg_bc"])
        lbf = lb_bc[:].rearrange("p a b -> p (a b)")
        omlf = oml_bc[:].rearrange("p a b -> p (a b)")
        with contextlib.ExitStack() as sl:
            lbr = sb("lbr", [128, 2048], stack=sl)
            DMA(SP, lbr[:], bcast(lb_d, 2048), [], ["lbr"])
            TT(lbf, lbr[:, 0:1024], lbr[:, 1024:2048], ALU.subtract, ["lbr"], ["lb_bc"])
            ACTV(lbf, lbf, AF.Sigmoid, ["lb_bc"], ["lb_bc"])
            TS(omlf, lbf, -1.0, 1.0, ALU.mult, ALU.add, ["lb_bc"], ["oml_bc"])
        P.barrier()

        with contextlib.ExitStack() as s0:
            cT = sb("cT", [128, 24], stack=s0)
            sg0 = sb("sg0", [128, 24], stack=s0)
            cb = sb("cb", [128, 24, 128], stack=s0)
            nrm_bc = sb("nrm_bc", [128, 4, 1024], stack=s0)
            wa = [sb(f"wa{i}", [128, 8, 512], stack=s0) for i in range(2)]
            ba = [sb(f"ba{i}", [128, 512], stack=s0) for i in range(2)]
            tA = [sb(f"tA{i}", [128, 512], stack=s0) for i in range(3)]
            tB = [sb(f"tB{i}", [128, 512], stack=s0) for i in range(3)]
            DMA(SP, cT[:], cvT_d, [], ["cT"])
            DMA(SP, nrm_bc[:].rearrange("p a b -> p (a b)"), bcast(nrm_d, 4096), [], ["nrm_bc"])
            ACTV(sg0[:], cT[:], AF.Sigmoid, ["cT"], ["sg0"])
            TT(cT[:], cT[:], sg0[:], ALU.mult, ["cT", "sg0"], ["cT"])
            for i in range(24):
                TS(cb[:, i, :], ones_f[:], cT[:, i:i + 1], None, ALU.mult, None, ["ones_f", "cT"], [("cb", i)])
            ti_rot = 0
            for cbi in range(12):
                w, half = cbi // 2, cbi % 2
                cs = slice(half * 512, (half + 1) * 512)
                wt, bt = wa[cbi % 2], ba[cbi % 2]
                DMA(SP, wt[:], wada_d[cbi], [], [("wa", cbi % 2)])
                DMA(SP, bt[:], bcast(bada_d[:, cbi * 512:(cbi + 1) * 512], 512), [], [("ba", cbi % 2)])
                for r in range(3):
                    if r == 2 and w > 1:
                        continue
                    pt, pk = nps()
                    for k in range(8):
                        MM(pt[:, :], cb[:, r * 8 + k, :], wt[:, k, :], k == 0, k == 7,
                           [("cb", r * 8 + k), ("wa", cbi % 2)], [pk])
                    a_, b_ = tA[ti_rot % 3], tB[ti_rot % 3]
                    ka, kb = ("tA", ti_rot % 3), ("tB", ti_rot % 3)
                    ti_rot += 1
                    TT(a_[:], pt[:, :], bt[:], ALU.add, [pk, ("ba", cbi % 2)], [ka])
                    if w in (1, 4):
                        STT(b_[:], a_[:], 1.0, nrm_bc[:, 0 if w == 1 else 2, cs], ALU.add, ALU.mult,
                            [ka, "nrm_bc"], [kb])
                        src, ksrc = b_, kb
                    elif w in (2, 5):
                        TT(b_[:], a_[:], nrm_bc[:, 1 if w == 2 else 3, cs], ALU.mult, [ka, "nrm_bc"], [kb])
                        src, ksrc = b_, kb
                    else:
                        src, ksrc = a_, ka
                    tidx = r * 6 + w if r < 2 else 12 + w
                    DMA(SP, bc_d[tidx, :, cs], src[:], [ksrc], [("bc", tidx, half)])

        P.barrier()

        def load_bc(dst, tidx, key):
            DMA(SP, dst[:], bc_d[tidx], [("bc", tidx, 0), ("bc", tidx, 1)], [key])

        Ls = sb("Ls", [128, 32, 32])
        m8s = sb("m8s", [128, 32, 8])
        pidx = sb("pidx", [128, 1])
        DMA(SP, pidx[:], pidx_d, [], ["pidx"])
        small = sb("small", [128, 64])

        def hTk(t0, t1):
            return kk("hT", range(t0 // 128, (t1 + 127) // 128))

        def dump(nm, ap_sb, dst, reads):
            if nm in dump_d:
                out_toks.append(DMA(POOL, dst(dump_d[nm]), ap_sb, reads, []))

        mx = None
        for b in range(nb):
            mx = contextlib.ExitStack()
            hT = sb("hT", [128, 8, TOK], BF16, stack=mx)
            aT = sb("aT", [128, 4, 2048], BF16, stack=mx)
            bT = sb("bT", [128, 4, 2048], BF16, stack=mx)
            with contextlib.ExitStack() as s1:
                A1 = sb("A1", [128, 1024], stack=s1)
                SH1 = sb("SH1", [128, 1024], stack=s1)
                A1c = sb("A1c", [128, 1024], stack=s1)
                SH1c = sb("SH1c", [128, 1024], stack=s1)
                xin = [sb(f"xin{i}", [128, 1024], stack=s1) for i in range(2)]
                sq = sb("sq", [128, 1024], stack=s1)
                tmp = sb("tmp1", [128, 1024], stack=s1)
                hb = [sb(f"hb{i}", [128, 1024], BF16, stack=s1) for i in range(2)]
                load_bc(A1, b * 6 + 1, "A1")
                load_bc(SH1, b * 6 + 0, "SH1")
                load_bc(A1c, 13, "A1c")
                load_bc(SH1c, 12, "SH1c")
                for ti in range(NT):
                    xi, xk = xin[ti % 2], ("xin", ti % 2)
                    src = ctx_d[b, ti * 128:(ti + 1) * 128, :] if ti < 2 else x_d[b, (ti - 2) * 128:(ti - 1) * 128, :]
                    DMA(SP, xi[:], src, [], [xk])
                    ACTV(sq[:], xi[:], AF.Square, [xk], ["sq"])
                    sc, sk = small[:, ti:ti + 1], ("small", ti)
                    RED(sc, sq[:], ["sq"], [sk])
                    rstd_from_ss(sc, sc, 1.0 / 1024, sk)
                    Am, Ak, Sm, Sk = (A1c, "A1c", SH1c, "SH1c") if ti < 2 else (A1, "A1", SH1, "SH1")
                    STT(tmp[:], xi[:], sc, Am[:], ALU.mult, ALU.mult, [xk, sk, Ak], ["tmp1"])
                    hbt, hbk = hb[ti % 2], ("hb", ti % 2)
                    TT(hbt[:], tmp[:], Sm[:], ALU.add, ["tmp1", Sk], [hbk])
                    pb_, pbk = npsb()
                    for k in range(8):
                        TR(pb_[:, k * 128:(k + 1) * 128], hbt[:, k * 128:(k + 1) * 128], ident_b[:],
                           [hbk, "ident_b"], [pbk])
                    CP(hT[:, :, ti * 128:(ti + 1) * 128], pb_[:].rearrange("p (k t) -> p k t", k=8),
                       [pbk], [("hT", ti)], eng=ACT)
            P.barrier()
            dump("hT", hT[:, 0, :], lambda d: d, kk("hT", range(NT)))
            if stop == "s1":
                break

            with contextlib.ExitStack() as sm:
                wfm = [sb(f"wfm{i}", [128, 8, 128], BF16, stack=sm) for i in range(2)]
                srcf = sb("srcf", [128, TOK], stack=sm)
                accf = sb("accf", [128, TOK], stack=sm)
                stm = [sb(f"stm{i}", [128, 128], BF16, stack=sm) for i in range(2)]
                Pst = [sb(f"Pst{i}", [128, 129], stack=sm) for i in range(2)]
                Sbf = [sb(f"Sbf{i}", [128, 129], BF16, stack=sm) for i in range(2)]
                Sbf2 = [sb(f"Sbf2{i}", [128, 129], BF16, stack=sm) for i in range(2)]
                ssh = sb("ssh", [128, 16], stack=sm)
                tmpf = sb("tmpf", [128, 16, 128], stack=sm)
                a_tm = sb("a_tm", [128, 16, 128], BF16, stack=sm)

                def proj_fm(chunk, wt, wk, dst, dk):
                    DMA(POOL, wt[:], win_d[chunk], [], [wk])
                    for g0 in range(0, TOK, 512):
                        n = min(512, TOK - g0)
                        pt, pk = nps()
                        for k in range(8):
                            MM(pt[:, 0:n], wt[:, k, :], hT[:, k, g0:g0 + n], k == 0, k == 7,
                               [wk] + hTk(g0, g0 + n), [pk])
                        ACTV(dst[:, g0:g0 + n], pt[:, 0:n], AF.Identity, [pk, "bcol"], [dk],
                             bias=bcol[:, chunk:chunk + 1])

                def scan(d, QT, qk, q_lat_only, KT, ktk, Ktm, ktmk, V, vk, nv, dec_of, deck, post):
                    order = [0, 1] + list(range(2, NT)) if d == 0 else [1, 0] + list(range(NT - 1, 1, -1))
                    n_ = len(order)
                    mask, mkey = (triF, "triF") if d == 0 else (triB, "triB")
                    Pt, Pk = Pst[d], ("Pst", d)
                    Sts = [(Sbf[d], ("Sbf", d)), (Sbf2[d], ("Sbf2", d))]

                    def emit_U(i):
                        c_ = order[i]
                        pu, puk = nps()
                        MM(pu[:, 0:nv], Ktm[:, c_, :], V[:, c_, 0:nv], True, True, [ktmk, vk], [puk])
                        return pu, puk

                    nxt = emit_U(0)
                    for idx, c in enumerate(order):
                        cs = slice(c * 128, (c + 1) * 128)
                        qs = slice((c - 2) * 128, (c - 1) * 128) if q_lat_only else cs
                        if idx < n_ - 1:
                            pu, puk = nxt
                            St, Sk = Sts[idx % 2]
                            if idx == 0:
                                CP(Pt[:, 0:nv], pu[:, 0:nv], [puk], [Pk])
                            else:
                                STT(Pt[:, 0:nv], Pt[:, 0:nv], dec_of(order[idx - 1]), pu[:, 0:nv], ALU.mult, ALU.add,
                                    [Pk, puk, deck], [Pk])
                            TS(St[:, 0:nv], Pt[:, 0:nv], dec_of(c), None, ALU.mult, None, [Pk, deck], [Sk])
                            if idx + 1 < n_ - 1:
                                nxt = emit_U(idx + 1)
                        if c >= 2:
                            p1, p1k = nps()
                            MM(p1[:, 0:128], KT[:, cs], QT[:, qs], True, True, [ktk, qk], [p1k])
                            sm_, smk = stm[rr["t"] % 2], ("stm", rr["t"] % 2)
                            rr["t"] += 1
                            TT(sm_[:], p1[:, 0:128], mask[:], ALU.mult, [p1k, mkey], [smk])
                            po, pok = nps()
                            MM(po[:, 0:nv], sm_[:], V[:, c, 0:nv], True, idx == 0, [smk, vk], [pok])
                            if idx > 0:
                                Sp, Spk = Sts[(idx - 1) % 2]
                                MM(po[:, 0:nv], QT[:, qs], Sp[:, 0:nv], False, True, [qk, Spk], [pok])
                            post(po, pok, c)
                        yield

                def head_norm_to_T(hs, hsk, nbc, nbk, h, gate, gk, dstT, dstk, tmpf, a_tm):
                    ACTV(tmpf[:], hs[:], AF.Square, list(hsk), ["hn_tmp"])
                    RED(ssh[:], tmpf[:], ["hn_tmp"], ["ssh"])
                    rstd_from_ss(ssh[:], ssh[:], 1.0 / 128, "ssh")
                    for ti in range(16):
                        STT(tmpf[:, ti, :], hs[:, ti, :], ssh[:, ti:ti + 1], nbc[:, h * 128:(h + 1) * 128],
                            ALU.mult, ALU.mult, list(hsk) + ["ssh", nbk, "hn_tmp"], ["hn_tmp"])
                    TT(a_tm[:], tmpf[:], gate[:], ALU.mult, ["hn_tmp", gk], ["a_tm"])
                    for t0 in (0, 8):
                        pb_, pbk = npsb()
                        for j in range(8):
                            TR(pb_[:, j * 128:(j + 1) * 128], a_tm[:, t0 + j, :], ident_b[:], ["a_tm", "ident_b"], [pbk])
                        CP(dstT[:, h, t0 * 128:(t0 + 8) * 128], pb_[:, :], [pbk], [dstk], eng=ACT)

                s2x = contextlib.ExitStack()
                gates = sb("gates", [128, 16, NT], stack=s2x)
                lf = sb("lf", [128, 8, NT], stack=s2x)
                bcum = sb("bcum", [128, 8, NT], stack=s2x)
                dec_m = sb("dec_m", [128, 8, NT], stack=s2x)
                w_m = sb("w_m", [128, 8, NT], stack=s2x)
                e_m = sb("e_m", [128, 8, NT], stack=s2x)
                for ti in range(NT):
                    pt, pk = nps()
                    for k in range(8):
                        MM(pt[:, 0:16], hT[:, k, ti * 128:(ti + 1) * 128], wmg_sb[:, k, :], k == 0, k == 7,
                           [("hT", ti), "wmg_sb"], [pk])
                    TT(gates[:, :, ti], pt[:, 0:16], bmg_bc[:], ALU.add, [pk, "bmg_bc"], ["gates"])
                ACTV(lf[:], gates[:, 8:16, :], AF.Sigmoid, ["gates"], ["lf"])
                ACTV(lf[:], lf[:], AF.Ln, ["lf"], ["lf"])
                pt, pk = nps()
                MM(pt[:, 0:72], triF[:], lf[:, 0:4, :].rearrange("p a b -> p (a b)"), True, True, ["triF", "lf"], [pk])
                MM(pt[:, 72:144], triB[:], lf[:, 4:8, :].rearrange("p a b -> p (a b)"), True, True,
                   ["triB", "lf"], [pk])
                CP(bcum[:].rearrange("p a b -> p (a b)"), pt[:, 0:144], [pk], ["bcum"])
                pt2, pk2 = nps()
                MM(pt2[:, 0:144], ones_f[:], lf[:].rearrange("p a b -> p (a b)"), True, True, ["ones_f", "lf"], [pk2])
                ACTV(dec_m[:].rearrange("p a b -> p (a b)"), pt2[:, 0:144], AF.Exp, [pk2], ["dec_m"])
                TT(w_m[:], gates[:, 0:8, :], bcum[:], ALU.subtract, ["gates", "bcum"], ["w_m"])
                ACTV(w_m[:], w_m[:], AF.Exp, ["w_m"], ["w_m"])
                ACTV(e_m[:], bcum[:], AF.Exp, ["bcum"], ["e_m"])
                dump("bcum", bcum[:].rearrange("p a b -> p (a b)"), lambda d: d, ["bcum"])

                with contextlib.ExitStack() as s3:
                    qT = sb("qT", [128, TOK], BF16, stack=s3)
                    kT = sb("kT", [128, TOK], BF16, stack=s3)
                    k_tm = sb("k_tm", [128, NT, 128], BF16, stack=s3)
                    wvo = sb("wvo", [128, 8, 256], BF16, stack=s3)
                    bvo = sb("bvo", [128, 256], stack=s3)
                    vp = sb("vp", [128, NT, 129], BF16, stack=s3)
                    v2_ = sb("v2_0", [128, NT, 129], BF16, stack=s3)
                    v2 = [v2_, v2_]
                    og = sb("og", [128, 16, 128], BF16, stack=s3)
                    hsum = sb("hsum", [128, 16, 128], stack=s3)
                    dsc = sb("dsc", [128, 4], stack=s3)
                    for h in range(4):
                        for which, dst, dkey, scale in ((0, qT, "qT", 128.0 ** -0.5), (1, kT, "kT", 1.0)):
                            chunk = which * 4 + h
                            proj_fm(chunk, wfm[which], ("wfm", which), srcf, "srcf")
                            wc = convc[:, chunk, :]
                            ls = srcf[:, 256:TOK].rearrange("p (r c) -> p r c", c=64)
                            la = accf[:, 256:TOK].rearrange("p (r c) -> p r c", c=64)
                            TS(accf[:], srcf[:], wc[:, 4:5], None, ALU.mult, None, ["srcf", "convc"], ["accf"])
                            for di in (-1, 0, 1):
                                for dj in (-1, 0, 1):
                                    if di == 0 and dj == 0:
                                        continue
                                    tap = (di + 1) * 3 + (dj + 1)
                                    r0, r1 = max(0, -di), 32 - max(0, di)
                                    c0, c1 = max(0, -dj), 64 - max(0, dj)
                                    STT(la[:, r0:r1, c0:c1], ls[:, r0 + di:r1 + di, c0 + dj:c1 + dj], wc[:, tap:tap + 1],
                                        la[:, r0:r1, c0:c1], ALU.mult, ALU.add, ["srcf", "convc", "accf"], ["accf"])
                            STT(accf[:, 1:256], srcf[:, 0:255], wc[:, 3:4], accf[:, 1:256], ALU.mult, ALU.add,
                                ["srcf", "convc", "accf"], ["accf"])
                            STT(accf[:, 0:255], srcf[:, 1:256], wc[:, 5:6], accf[:, 0:255], ALU.mult, ALU.add,
                                ["srcf", "convc", "accf"], ["accf"])
                            ACTV(srcf[:], accf[:], AF.Sigmoid, ["accf"], ["srcf"])
                            STT(dst[:], accf[:], scale, srcf[:], ALU.mult, ALU.mult, ["accf", "srcf"], [dkey])
                        if h == 0 and b == 0:
                            dump("qT", qT[:], lambda d: d, ["qT"])
                            dump("kT", kT[:], lambda d: d, ["kT"])
                        for t0 in range(0, NT, 8):
                            n = min(8, NT - t0)
                            pb_, pbk = npsb()
                            for j in range(n):
                                TR(pb_[:, j * 128:(j + 1) * 128], kT[:, (t0 + j) * 128:(t0 + j + 1) * 128], ident_b[:],
                                   ["kT", "ident_b"], [pbk])
                            CP(k_tm[:, t0:t0 + n, :].rearrange("p a b -> p (a b)"), pb_[:, 0:n * 128], [pbk], ["k_tm"],
                               eng=ACT)
                        DMA(POOL, wvo[:, :, 0:128], win_d[8 + h], [], ["wvo"])
                        DMA(POOL, wvo[:, :, 128:256], win_d[12 + h], [], ["wvo"])
                        DMA(SP, bvo[:, 0:128], bcast(brow_d[:, 1024 + h * 128:1024 + (h + 1) * 128], 128), [], ["bvo"])
                        DMA(SP, bvo[:, 128:256], bcast(brow_d[:, 1536 + h * 128:1536 + (h + 1) * 128], 128), [], ["bvo"])
                        MSET(vp[:, :, 128:129], 1.0, ["vp"])
                        for ti in range(NT):
                            pt, pk = nps()
                            for k in range(8):
                                MM(pt[:, 0:256], hT[:, k, ti * 128:(ti + 1) * 128], wvo[:, k, :], k == 0, k == 7,
                                   [("hT", ti), "wvo"], [pk])
                            TT(vp[:, ti, 0:128], pt[:, 0:128], bvo[:, 0:128], ALU.add, [pk, "bvo"], ["vp"])
                            if ti >= 2:
                                TT(og[:, ti - 2, :], pt[:, 128:256], bvo[:, 128:256], ALU.add, [pk, "bvo"], ["og"])
                        ACTV(og[:], og[:], AF.Sigmoid, ["og"], ["og"])
                        gens = []
                        for d in range(2):
                            col = d * 4 + h
                            vd, vdk = (v2_, "v2") if d == 0 else (vp, "vp")
                            for ti in range(NT):
                                TS(vd[:, ti, :], vp[:, ti, :], w_m[:, col, ti:ti + 1], None, ALU.mult, None,
                                   ["vp", "w_m"], [vdk])

                            def post(po, pok, c, d=d, col=col):
                                ecol = e_m[:, col, c:c + 1]
                                d1, d2 = dsc[:, 2 * d:2 * d + 1], dsc[:, 2 * d + 1:2 * d + 2]
                                dk_ = ("dsc", d)
                                ACTV(d1, po[:, 128:129], AF.Abs, [pok, "e_m"], [dk_], scale=ecol)
                                TS(d1, d1, 1.0, None, ALU.max, None, [dk_], [dk_])
                                RECIP(d1, d1, [dk_], [dk_])
                                TT(d2, d1, ecol, ALU.mult, [dk_, "e_m"], [dk_])
                                if (c <= 9) == (d == 0):
                                    TS(hsum[:, c - 2, :], po[:, 0:128], d2, None, ALU.mult, None, [pok, dk_], [("hsum", c)])
                                else:
                                    STT(hsum[:, c - 2, :], po[:, 0:128], d2, hsum[:, c - 2, :], ALU.mult, ALU.add,
                                        [pok, dk_, ("hsum", c)], [("hsum", c)])

                            gens.append(scan(d, qT, "qT", False, kT, "kT", k_tm, "k_tm", vd, vdk, 129,
                                             lambda c, col=col: dec_m[:, col, c:c + 1], "dec_m", post))
                        while gens:
                            for g_ in list(gens):
                                try:
                                    next(g_)
                                except StopIteration:
                                    gens.remove(g_)
                        if h == 0 and b == 0:
                            dump("hsum", hsum[:].rearrange("p a b -> p (a b)"), lambda d: d, kk("hsum", range(2, NT)))
                        head_norm_to_T(hsum, kk("hsum", range(2, NT)), mn_bc, "mn_bc", h, og, "og", aT, "aT", tmpf, a_tm)
                s2x.close()
                P.barrier()
                if stop == "s3":
                    break

                with contextlib.ExitStack() as s4:
                    hqT = sb("hqT", [128, TOK], BF16, stack=s4)
                    w4 = sb("w4", [128, 8, 384], BF16, stack=s4)
                    w4b = sb("w4b", [128, 8, 128], BF16, stack=s4)
                    b4 = sb("b4", [128, 512], stack=s4)
                    hi_tm = sb("hi_tm", [128, NT, 128], BF16, stack=s4)
                    g_tm = sb("g_tm", [128, 16, 128], BF16, stack=s4)
                    zf = srcf[:].rearrange("p (a b) -> p a b", b=128)
                    kkf = accf[:].rearrange("p (a b) -> p a b", b=128)
                    ktl = sb("ktl", [128, NT, 128], BF16, stack=s4)
                    ktlT = sb("ktlT", [128, TOK], BF16, stack=s4)
                    qtl = sb("qtl", [128, 2048], BF16, stack=s4)
                    dec_h = sb("dec_h", [128, 2, NT], stack=s4)
                    ex_ = sb("ex0", [128, 512], stack=s4)
                    ex = [ex_, ex_]
                    osum = sb("osum", [128, 16, 128], stack=s4)
                    exr = 0
                    for h in range(4):
                        proj_fm(16 + h, wfm[0], ("wfm", 0), srcf, "srcf")
                        ACTV(accf[:], srcf[:], AF.Sigmoid, ["srcf"], ["accf"])
                        TT(hqT[:], srcf[:], accf[:], ALU.mult, ["srcf", "accf"], ["hqT"])
                        for j, ch in enumerate((20 + h, 24 + h, 28 + h)):
                            DMA(POOL, w4[:, :, j * 128:(j + 1) * 128], win_d[ch], [], ["w4"])
                        DMA(POOL, w4b[:], win_d[32 + h], [], ["w4b"])
                        for j, ch in enumerate((20 + h, 24 + h, 28 + h, 32 + h)):
                            DMA(SP, b4[:, j * 128:(j + 1) * 128], bcast(brow_d[:, ch * 128:(ch + 1) * 128], 128), [], ["b4"])
                        for d in range(2):
                            hs = slice(h * 128, (h + 1) * 128)
                            for ti in range(NT):
                                pt, pk = nps()
                                if d == 0:
                                    for k in range(8):
                                        MM(pt[:, 0:384], hT[:, k, ti * 128:(ti + 1) * 128], w4[:, k, :], k == 0, k == 7,
                                           [("hT", ti), "w4"], [pk])
                                    TT(hi_tm[:, ti, :], pt[:, 0:128], b4[:, 0:128], ALU.add, [pk, "b4"], ["hi_tm"])
                                    if ti >= 2:
                                        TT(tmpf[:, ti - 2, :], pt[:, 128:256], b4[:, 128:256], ALU.add, [pk, "b4"], ["hn_tmp"])
                                    TT(zf[:, ti, :], pt[:, 256:384], b4[:, 256:384], ALU.add, [pk, "b4"], ["srcf"])
                                else:
                                    for k in range(8):
                                        MM(pt[:, 0:128], hT[:, k, ti * 128:(ti + 1) * 128], w4b[:, k, :], k == 0, k == 7,
                                           [("hT", ti), "w4b"], [pk])
                                    TT(zf[:, ti, :], pt[:, 0:128], b4[:, 384:512], ALU.add, [pk, "b4"], ["srcf"])
                            if d == 0:
                                ACTV(g_tm[:], tmpf[:], AF.Sigmoid, ["hn_tmp"], ["g_tm"])
                                TT(g_tm[:], g_tm[:], tmpf[:], ALU.mult, ["g_tm", "hn_tmp"], ["g_tm"])
                            ACTV(accf[:], srcf[:], AF.Sigmoid, ["srcf"], ["accf"])
                            for ti in range(NT):
                                TT(zf[:, ti, :], kkf[:, ti, :], oml_bc[:, d, hs], ALU.mult, ["accf", "oml_bc", "srcf"], ["srcf"])
                                TT(zf[:, ti, :], zf[:, ti, :], lb_bc[:, d, hs], ALU.add, ["srcf", "lb_bc"], ["srcf"])
                            TS(accf[:], srcf[:], -1.0, 1.0, ALU.mult, ALU.add, ["srcf"], ["accf"])
                            ACTV(srcf[:], srcf[:], AF.Ln, ["srcf"], ["srcf"])
                            tri, trik = (triF, "triF") if d == 0 else (triB, "triB")
                            for t0 in range(0, NT, 4):
                                n = min(4, NT - t0)
                                fs = slice(t0 * 128, (t0 + n) * 128)
                                pt, pk = nps()
                                MM(pt[:, 0:n * 128], tri[:], srcf[:, fs], True, True, [trik, "srcf"], [pk])
                                e_, ek = ex[0], ("ex", 0)
                                exr += 1
                                ACTV(e_[:, 0:n * 128], pt[:, 0:n * 128], AF.Exp, [pk], [ek], scale=-1.0)
                                TT(ktl[:, t0:t0 + n, :].rearrange("p a b -> p (a b)"), accf[:, fs], e_[:, 0:n * 128], ALU.mult,
                                   ["accf", ek], ["ktl"])
                                pt, pk = nps()
                                for j in range(n):
                                    MM(pt[:, j * 128:(j + 1) * 128], zf[:, t0 + j, :], tri[:], True, True, ["srcf", trik], [pk])
                                e_, ek = ex[0], ("ex", 0)
                                exr += 1
                                ACTV(e_[:, 0:n * 128], pt[:, 0:n * 128], AF.Exp, [pk], [ek])
                                lastc = 127 if d == 0 else 0
                                CP(dec_h[:, d, t0:t0 + n],
                                   e_[:, 0:n * 128].rearrange("p (a b) -> p a b", b=128)[:, :, lastc], [ek], ["dec_h"])
                                for j in range(n):
                                    ti = t0 + j
                                    if ti >= 2:
                                        TT(qtl[:, (ti - 2) * 128:(ti - 1) * 128], hqT[:, ti * 128:(ti + 1) * 128],
                                           e_[:, j * 128:(j + 1) * 128], ALU.mult, ["hqT", ek], ["qtl"])
                            for t0 in range(0, NT, 8):
                                n = min(8, NT - t0)
                                pb_, pbk = npsb()
                                for j in range(n):
                                    TR(pb_[:, j * 128:(j + 1) * 128], ktl[:, t0 + j, :], ident_b[:], ["ktl", "ident_b"], [pbk])
                                CP(ktlT[:, t0 * 128:(t0 + n) * 128], pb_[:, 0:n * 128], [pbk], ["ktlT"], eng=ACT)

                            if h == 0 and b == 0 and d == 0:
                                dump("ktl", ktl[:].rearrange("p a b -> p (a b)"), lambda dd: dd, ["ktl"])
                                dump("qtl", qtl[:], lambda dd: dd, ["qtl"])
                                dump("ktlT", ktlT[:], lambda dd: dd, ["ktlT"])
                                dump("dech", dec_h[:].rearrange("p a b -> p (a b)"), lambda dd: dd, ["dec_h"])
                                dump("hqT", hqT[:], lambda dd: dd, ["hqT"])
                                dump("hitm", hi_tm[:].rearrange("p a b -> p (a b)"), lambda dd: dd, ["hi_tm"])

                            def post(po, pok, c, d=d):
                                if d == 0:
                                    CP(osum[:, c - 2, :], po[:, 0:128], [pok], ["osum"])
                                else:
                                    TT(osum[:, c - 2, :], po[:, 0:128], osum[:, c - 2, :], ALU.add, [pok, "osum"], ["osum"])

                            for _ in scan(d, qtl, "qtl", True, ktlT, "ktlT", ktl, "ktl", hi_tm, "hi_tm",
                                          128, lambda c, d=d: dec_h[:, d, c:c + 1], "dec_h", post):
                                pass
                        if h == 0 and b == 0:
                            dump("osum", osum[:].rearrange("p a b -> p (a b)"), lambda d: d, ["osum"])
                        head_norm_to_T(osum, ["osum"], hn_bc, "hn_bc", h, g_tm, "g_tm", bT, "bT", tmpf, a_tm)
                P.barrier()
                if stop == "s4":
                    break

            P.barrier()
            with contextlib.ExitStack() as s5:
                G1 = sb("G1", [128, 1024], stack=s5)
                A2 = sb("A2", [128, 1024], stack=s5)
                SH2 = sb("SH2", [128, 1024], stack=s5)
                wpa = sb("wpa", [128, 4, 1024], BF16, stack=s5)
                wpb = sb("wpb", [128, 4, 1024], BF16, stack=s5)
                wout = sb("wout", [128, 8, 1024], BF16, stack=s5)
                wg = [sb(f"wg{i}", [128, 8, 128], BF16, stack=s5) for i in range(2)]
                yT = sb("yT", [128, 8, 512], BF16, stack=s5)
                sga = sb("sga", [128, 512], stack=s5)
                y1 = sb("y1", [128, 512], stack=s5)
                y2 = sb("y2", [128, 512], stack=s5)
                sgb = sga
                yl = sb("yl", [128, 1024], stack=s5)
                sq = sb("sq5", [128, 1024], stack=s5)
                t1 = sq
                xin = [sb(f"xin5{i}", [128, 1024], stack=s5) for i in range(1)]
                x1t = [sb(f"x1t{i}", [128, 1024], stack=s5) for i in range(1)]
                h2b = sb("h2b", [128, 1024], BF16, stack=s5)
                h2Tt = sb("h2Tt", [128, 8, 128], BF16, stack=s5)
                load_bc(G1, b * 6 + 2, "G1")
                load_bc(A2, b * 6 + 4, "A2")
                load_bc(SH2, b * 6 + 3, "SH2")
                DMA(POOL, wpa[:], wpa_d, [], ["wpa"])
                DMA(POOL, wpb[:], wpb_d, [], ["wpb"])
                DMA(POOL, wout[:], wout_d, [], ["wout"])
                wgr = 0
                for tg in range(4):
                    tok0 = tg * 512
                    for j in range(8):
                        wga, wgak = wg[0], ("wg", 0)
                        wgb, wgbk = wg[1], ("wg", 1)
                        wgr += 2
                        DMA(POOL, wga[:], win_d[36 + j], [], [wgak])
                        DMA(POOL, wgb[:], win_d[44 + j], [], [wgbk])
                        pa, pak = nps()
                        for k in range(4):
                            MM(pa[:, :], wpa[:, k, j * 128:(j + 1) * 128], aT[:, k, tok0:tok0 + 512], k == 0, k == 3,
                               ["wpa", "aT"], [pak])
                        pga, pgak = nps()
                        for k in range(8):
                            MM(pga[:, :], wga[:, k, :], hT[:, k, 256 + tok0:256 + tok0 + 512], k == 0, k == 7,
                               [wgak] + hTk(256 + tok0, 256 + tok0 + 512), [pgak])
                        ACTV(sga[:], pga[:, :], AF.Sigmoid, [pgak, "bcol"], ["sga"], bias=bcol[:, 36 + j:37 + j])
                        TT(y1[:], pa[:, :], sga[:], ALU.mult, [pak, "sga"], ["y1"])
                        pb2, pb2k = nps()
                        for k in range(4):
                            MM(pb2[:, :], wpb[:, k, j * 128:(j + 1) * 128], bT[:, k, tok0:tok0 + 512], k == 0, k == 3,
                               ["wpb", "bT"], [pb2k])
                        pgb, pgbk = nps()
                        for k in range(8):
                            MM(pgb[:, :], wgb[:, k, :], hT[:, k, 256 + tok0:256 + tok0 + 512], k == 0, k == 7,
                               [wgbk] + hTk(256 + tok0, 256 + tok0 + 512), [pgbk])
                        ACTV(sgb[:], pgb[:, :], AF.Sigmoid, [pgbk, "bcol"], ["sga"], bias=bcol[:, 44 + j:45 + j])
                        TT(y2[:], pb2[:, :], sgb[:], ALU.mult, [pb2k, "sga"], ["y2"])
                        TT(yT[:, j, :], y1[:], y2[:], ALU.add, ["y1", "y2"], [("yT", j)])
                    for tl in range(4):
                        lt = tg * 4 + tl
                        for half in range(2):
                            pt, pk = nps()
                            for k in range(8):
                                MM(pt[:, :], yT[:, k, tl * 128:(tl + 1) * 128], wout[:, k, half * 512:(half + 1) * 512],
                                   k == 0, k == 7, [("yT", k), "wout"], [pk])
                            CP(yl[:, half * 512:(half + 1) * 512], pt[:, :], [pk], ["yl"], eng=ACT)
                        ACTV(sq[:], yl[:], AF.Square, ["yl"], ["sq5"])
                        sc, sk = small[:, 20 + lt:21 + lt], ("small", 20 + lt)
                        RED(sc, sq[:], ["sq5"], [sk])
                        rstd_from_ss(sc, sc, 1.0 / 1024, sk)
                        xi, xk = xin[0], ("xin5", 0)
                        DMA(SP, xi[:], x_d[b, lt * 128:(lt + 1) * 128, :], [], [xk])
                        STT(t1[:], yl[:], sc, G1[:], ALU.mult, ALU.mult, ["yl", sk, "G1"], ["sq5"])
                        xo, xok = x1t[0], ("x1t", 0)
                        TT(xo[:], t1[:], xi[:], ALU.add, ["sq5", xk], [xok])
                        DMA(SP, x1_d[b, lt * 128:(lt + 1) * 128, :], xo[:], [xok], [("x1d", b, lt)])
                        ACTV(sq[:], xo[:], AF.Square, [xok], ["sq5"])
                        sc2, sk2 = small[:, 40 + lt:41 + lt], ("small", 40 + lt)
                        RED(sc2, sq[:], ["sq5"], [sk2])
                        rstd_from_ss(sc2, sc2, 1.0 / 1024, sk2)
                        STT(t1[:], xo[:], sc2, A2[:], ALU.mult, ALU.mult, [xok, sk2, "A2"], ["sq5"])
                        TT(h2b[:], t1[:], SH2[:], ALU.add, ["sq5", "SH2"], ["h2b"])
                        pb_, pbk = npsb()
                        for k in range(8):
                            TR(pb_[:, k * 128:(k + 1) * 128], h2b[:, k * 128:(k + 1) * 128], ident_b[:],
                               ["h2b", "ident_b"], [pbk])
                        gt = b * 16 + lt
                        DMA(SP, h2_d[gt * 128:(gt + 1) * 128, :], h2b[:], ["h2b"], [("h2d", gt)])
                        CP(h2Tt[:], pb_[:].rearrange("p (k t) -> p k t", k=8), [pbk], ["h2Tt"], eng=ACT)
                        pt, pk = nps()
                        for k in range(8):
                            MM(pt[:, 0:32], h2Tt[:, k, :], wr_sb[:, k, :], k == 0, k == 7, ["h2Tt", "wr_sb"], [pk])
                        TT(Ls[:, gt, :], pt[:, 0:32], br_bc[:], ALU.add, [pk, "br_bc"], [("Ls", gt)])
                        P.op(DVE, lambda e, gt=gt: e.max(out=m8s[:, gt, :], in_=Ls[:, gt, :]), [("Ls", gt)], [("m8s", gt)])
            mx.close()
            mx = None
            P.barrier()
            if stop == "s5":
                break

        LsK = kk("Ls", range(32))
        m8K = kk("m8s", range(32))
        desti = sb("desti", [128, 128], I32)
        wk = sb("wk", [128, 32, 4])
        widx = sb("widx", [128, NB], I32)
        widx8 = sb("widx8", [128, 8, NB], I32)
        widx4 = sb("widx4", [128, 4, NB], I32)
        bidx = sb("bidx", [128, NB], I32)
        if stop in (None, "m1"):
            with contextlib.ExitStack() as m1:
                triS = sb("triS", [128, 128], stack=m1)
                DMA(SP, triS[:], triS_d, [], ["triS"])
                mask = sb("mask", [128, 1024], stack=m1)
                R1 = sb("R1", [128, 1024], stack=m1)
                cA = sb("cA", [128, 1024], stack=m1)
                cB = sb("cB", [128, 1024], stack=m1)
                cnt = sb("cnt", [128, 1024], stack=m1)
                dest = sb("dest", [128, 1024], stack=m1)
                tot = sb("tot", [128, 32], stack=m1)
                nblk = sb("nblk", [128, 32], stack=m1)
                pA = sb("pA", [128, 32], stack=m1)
                pB = sb("pB", [128, 32], stack=m1)
                pstart = sb("pstart", [128, 32], stack=m1)
                junk = sb("junk", [128, 32], stack=m1)
                destk = sb("destk", [128, 128], stack=m1)
                ejf = sb("ejf", [128, NB], stack=m1)
                wif = sb("wif", [128, NB], stack=m1)
                nm = sb("nm", [128, 32], stack=m1)
                exs = sb("exs", [128, 32, 4], stack=m1)
                ssum = sb("ssum", [128, 32], stack=m1)
                m3 = lambda t: t[:].rearrange("p (a b) -> p a b", b=32)
                TT(m3(mask), Ls[:], m8s[:, :, 3:4].to_broadcast([128, 32, 32]), ALU.is_ge, LsK + m8K, ["mask"])
                for hf in range(2):
                    cs = slice(hf * 512, (hf + 1) * 512)
                    pt, pk = nps()
                    MM(pt[:, :], triS[:], mask[:, cs], True, True, ["triS", "mask"], [pk])
                    CP(R1[:, cs], pt[:, :], [pk], ["R1"])
                    pt, pk = nps()
                    MM(pt[:, :], ones_f[:], mask[:, cs], True, True, ["ones_f", "mask"], [pk])
                    CP(cnt[:, cs], pt[:, :], [pk], ["cnt"])
                CP(cA[:], cnt[:], ["cnt"], ["cA"])
                a_, ak, b_, bk = cA, "cA", cB, "cB"
                for sft in (1, 2, 4, 8, 16):
                    w_ = sft * 32
                    CP(b_[:, 0:w_], a_[:, 0:w_], [ak], [bk])
                    TT(b_[:, w_:1024], a_[:, w_:1024], a_[:, 0:1024 - w_], ALU.add, [ak], [bk])
                    a_, ak, b_, bk = b_, bk, a_, ak
                incl, inck = a_, ak
                CP(tot[:], incl[:, 31 * 32:32 * 32], [inck], ["tot"])
                TT(dest[:], incl[:], cnt[:], ALU.subtract, [inck, "cnt"], ["dest"])
                TT(dest[:], dest[:], R1[:], ALU.add, ["dest", "R1"], ["dest"])
                MSET(nblk[:], 0.0, ["nblk"])
                for j in range(MAXB):
                    STT(nblk[:], tot[:], float(j * BLK), nblk[:], ALU.is_gt, ALU.add, ["tot", "nblk"], ["nblk"])
                CP(pA[:], nblk[:], ["nblk"], ["pA"])
                a_, ak, b_, bk = pA, "pA", pB, "pB"
                for sft in (1, 2, 4, 8, 16):
                    CP(b_[:, 0:sft], a_[:, 0:sft], [ak], [bk])
                    TT(b_[:, sft:32], a_[:, sft:32], a_[:, 0:32 - sft], ALU.add, [ak], [bk])
                    a_, ak, b_, bk = b_, bk, a_, ak
                pend, pendk = a_, ak
                TT(pstart[:], pend[:], nblk[:], ALU.subtract, [pendk, "nblk"], ["pstart"])
                TS(pstart[:], pstart[:], float(BLK), None, ALU.mult, None, ["pstart"], ["pstart"])
                TT(m3(dest), m3(dest), pstart[:].rearrange("p (a e) -> p a e", a=1).to_broadcast([128, 32, 32]), ALU.add,
                   ["dest", "pstart"], ["dest"])
                for k in range(4):
                    TT(m3(cA), Ls[:], m8s[:, :, k:k + 1].to_broadcast([128, 32, 32]), ALU.is_equal, LsK + m8K + ["cA"], ["cA"])
                    TT(cA[:], cA[:], dest[:], ALU.mult, ["cA", "dest"], ["cA"])
                    RED(destk[:, k * 32:(k + 1) * 32], m3(cA), ["cA"], ["destk"])
                TS(destk[:], destk[:], 0.0, float(NB * BLK - 1), ALU.max, ALU.min, ["destk"], ["destk"])
                CP(desti[:], destk[:], ["destk"], ["desti"])
                TT(exs[:], m8s[:, :, 0:4], m8s[:, :, 0:1].to_broadcast([128, 32, 4]), ALU.subtract, m8K, ["exs"])
                ACTV(exs[:], exs[:], AF.Exp, ["exs"], ["exs"])
                RED(ssum[:], exs[:], ["exs"], ["ssum"])
                RECIP(ssum[:], ssum[:], ["ssum"], ["ssum"])
                TT(wk[:], exs[:], ssum[:].rearrange("p (a b) -> p a b", b=1).to_broadcast([128, 32, 4]), ALU.mult,
                   ["exs", "ssum"], ["wk"])
                jix = sb("jix", [128, NB], stack=m1)
                cmpb = sb("cmpb", [128, NB, 32], stack=m1)
                DMA(SP, jix[:], bcast(jidx_d, NB), [], ["jix"])
                TT(cmpb[:], pend[:].rearrange("p (a e) -> p a e", a=1).to_broadcast([128, NB, 32]),
                   jix[:].rearrange("p (a b) -> p a b", b=1).to_broadcast([128, NB, 32]), ALU.is_le, [pendk, "jix"], ["cmpb"])
                RED(ejf[:], cmpb[:], ["cmpb"], ["ejf"])
                TS(ejf[:], ejf[:], 31.0, None, ALU.min, None, ["ejf"], ["ejf"])
                CP(bidx[:], ejf[:], ["ejf"], ["bidx"])
                TS(wif[:], ejf[:], 128.0, pidx[:, 0:1], ALU.mult, ALU.add, ["ejf", "pidx"], ["wif"])
                CP(widx[:], wif[:], ["wif"], ["widx"])
                wif8 = sb("wif8", [128, 8, NB], stack=m1)
                for k in range(8):
                    TS(wif8[:, k, :], wif[:], 8.0, float(k), ALU.mult, ALU.add, ["wif"], ["wif8"])
                CP(widx8[:], wif8[:], ["wif8"], ["widx8"])
                for k in range(4):
                    TS(wif8[:, k, :], wif[:], 4.0, float(k), ALU.mult, ALU.add, ["wif", "wif8"], ["wif8"])
                CP(widx4[:], wif8[:, 0:4, :], ["wif8"], ["widx4"])
                dump("destk", destk[:], lambda d: d, ["destk"])
                dump("ejf", ejf[:], lambda d: d, ["ejf"])
                dump("wk", wk[:].rearrange("p a b -> p (a b)"), lambda d: d, ["wk"])
                dump("wif", wif[:], lambda d: d, ["wif"])
                h2t = [sb(f"h2t{i}", [128, 1024], BF16, stack=m1) for i in range(2)]
                for gt in range(32 if stop is None else 0):
                    ht, htk = h2t[gt % 2], ("h2t", gt % 2)
                    DMA(SP, ht[:], h2_d[gt * 128:(gt + 1) * 128, :], [("h2d", gt)], [htk])
                    for k in range(4):
                        c_ = k * 32 + gt
                        P.dma(POOL, lambda e, ht=ht, c_=c_: e.indirect_dma_start(
                            out=xs_d, out_offset=bass.IndirectOffsetOnAxis(ap=desti[:, c_:c_ + 1], axis=0),
                            in_=ht[:], in_offset=None),
                            [htk, "desti"], ["xs"])
            P.barrier()

        if stop is None:
            with contextlib.ExitStack() as m4:
                wgu = [sb(f"wgu{i}", [128, 8, 2048], BF16, stack=m4) for i in range(2)]
                wdn = [sb(f"wdn{i}", [128, 8, 1024], BF16, stack=m4) for i in range(2)]
                bgt = [sb(f"bgt{i}", [128, 16], stack=m4) for i in range(2)]
                bdt = [sb(f"bdt{i}", [128, 1024], stack=m4) for i in range(2)]
                xbs = [sb(f"xb{i}", [128, 4, 1024], BF16, stack=m4) for i in range(2)]
                xTs = [sb(f"xT{i}", [128, 8, 512], BF16, stack=m4) for i in range(2)]
                act = sb("act", [128, 8, 512], BF16, stack=m4)
                g1 = [sb(f"g1_{i}", [128, 512], stack=m4) for i in range(2)]
                sg = [sb(f"sg_{i}", [128, 512], stack=m4) for i in range(2)]
                u1 = [sb(f"u1_{i}", [128, 512], stack=m4) for i in range(2)]
                ybt = [sb(f"ybt{i}", [128, 1024], stack=m4) for i in range(2)]

                def load_w(j):
                    r_ = j % 2
                    ix = widx[:, j:j + 1]
                    for k in range(8):
                        P.dma(POOL, lambda e, k=k: e.indirect_dma_start(
                            out=wgu[r_][:, k, :], out_offset=None, in_=wgu_d,
                            in_offset=bass.IndirectOffsetOnAxis(ap=widx8[:, k, j:j + 1], axis=0)),
                            ["widx8"], [("wgu", r_, k)])
                    for k in range(4):
                        P.dma(POOL, lambda e, k=k: e.indirect_dma_start(
                            out=wdn[r_][:, 2 * k:2 * k + 2, :].rearrange("p a n -> p (a n)"), out_offset=None, in_=wdn_d,
                            in_offset=bass.IndirectOffsetOnAxis(ap=widx4[:, k, j:j + 1], axis=0)),
                            ["widx4"], [("wdn", r_, 2 * k), ("wdn", r_, 2 * k + 1)])
                    P.dma(POOL, lambda e: e.indirect_dma_start(
                        out=bgt[r_][:], out_offset=None, in_=bgu_d,
                        in_offset=bass.IndirectOffsetOnAxis(ap=ix, axis=0)), ["widx"], [("bgt", r_)])
                    P.dma(POOL, lambda e: e.indirect_dma_start(
                        out=bdt[r_][:], out_offset=None, in_=bdn_d,
                        in_offset=bass.IndirectOffsetOnAxis(ap=bidx[:, j:j + 1], axis=0)), ["bidx"], [("bdt", r_)])

                load_w(0)
                rot = 0
                for j in range(NB):
                    r_ = j % 2
                    if j + 1 < NB:
                        load_w(j + 1)
                    xb, xbk = xbs[j % 2], ("xb", j % 2)
                    if j == 0:
                        DMA(SP, xb[:], xs_d[0:BLK, :].rearrange("(a p) n -> p a n", p=128), ["xs"], [xbk])
                    if j + 1 < NB:
                        DMA(SP, xbs[(j + 1) % 2][:], xs_d[(j + 1) * BLK:(j + 2) * BLK, :].rearrange("(a p) n -> p a n", p=128),
                            ["xs"], [("xb", (j + 1) % 2)])
                    def transposes(jj):
                        xb_, xbk_ = xbs[jj % 2], ("xb", jj % 2)
                        xT_, xTk_ = xTs[jj % 2], ("xT", jj % 2)
                        for a in range(4):
                            pb_, pbk = npsb()
                            for k in range(8):
                                TR(pb_[:, k * 128:(k + 1) * 128], xb_[:, a, k * 128:(k + 1) * 128], ident_b[:],
                                   [xbk_, "ident_b"], [pbk])
                            CP(xT_[:, :, a * 128:(a + 1) * 128], pb_[:].rearrange("p (k t) -> p k t", k=8), [pbk], [xTk_],
                               eng=ACT if a % 2 == 0 else DVE)

                    if j == 0:
                        transposes(0)
                    xT, xTk = xTs[j % 2], ("xT", j % 2)
                    for fb in range(8):
                        q_ = rot % 2
                        rot += 1
                        pg, pgk = nps()
                        for k in range(8):
                            MM(pg[:, :], wgu[r_][:, k, fb * 128:(fb + 1) * 128], xT[:, k, :], k == 0, k == 7,
                               [("wgu", r_, k), xTk], [pgk])
                        pu, puk = nps()
                        for k in range(8):
                            MM(pu[:, :], wgu[r_][:, k, 1024 + fb * 128:1024 + (fb + 1) * 128], xT[:, k, :], k == 0, k == 7,
                               [("wgu", r_, k), xTk], [puk])
                        TS(g1[q_][:], pg[:, :], bgt[r_][:, fb:fb + 1], 7.0, ALU.add, ALU.min, [pgk, ("bgt", r_)], [("g1", q_)])
                        ACTV(sg[q_][:], g1[q_][:], AF.Sigmoid, [("g1", q_)], [("sg", q_)], scale=1.702)
                        TS(u1[q_][:], pu[:, :], bgt[r_][:, 8 + fb:9 + fb], 7.0, ALU.add, ALU.min, [puk, ("bgt", r_)], [("u1", q_)])
                        TS(u1[q_][:], u1[q_][:], -7.0, 1.0, ALU.max, ALU.add, [("u1", q_)], [("u1", q_)])
                        TT(g1[q_][:], g1[q_][:], sg[q_][:], ALU.mult, [("g1", q_), ("sg", q_)], [("g1", q_)])
                        TT(act[:, fb, :], g1[q_][:], u1[q_][:], ALU.mult, [("g1", q_), ("u1", q_)], ["act"])
                    if j + 1 < NB:
                        transposes(j + 1)
                    for a in range(4):
                        y_, yk = ybt[a % 2], ("ybt", a % 2)
                        for dh in range(2):
                            py, pyk = nps()
                            for fb in range(8):
                                MM(py[:, :], act[:, fb, a * 128:(a + 1) * 128], wdn[r_][:, fb, dh * 512:(dh + 1) * 512],
                                   fb == 0, fb == 7, ["act", ("wdn", r_, fb)], [pyk])
                            TT(y_[:, dh * 512:(dh + 1) * 512], py[:, :], bdt[r_][:, dh * 512:(dh + 1) * 512], ALU.add,
                               [pyk, ("bdt", r_)], [yk])
                        DMA(SP, yb_d[j * BLK + a * 128:j * BLK + (a + 1) * 128, :], y_[:], [yk], ["yb"])
            P.barrier()

            with contextlib.ExitStack() as m5:
                G2 = [sb(f"G2_{i}", [128, 1024], stack=m5) for i in range(2)]
                ybk = [sb(f"ybk{i}", [128, 1024], stack=m5) for i in range(8)]
                ym = sb("ym", [128, 1024], stack=m5)
                sq = sb("sq7", [128, 1024], stack=m5)
                x1r = [sb(f"x1r{i}", [128, 1024], stack=m5) for i in range(2)]
                ot = [sb(f"ot{i}", [128, 1024], stack=m5) for i in range(2)]
                for b in range(nb):
                    load_bc(G2[b], b * 6 + 5, ("G2", b))
                for gt in range(nb * 16):
                    b, lt = gt // 16, gt % 16
                    ys = []
                    for k in range(4):
                        i_ = (gt % 2) * 4 + k
                        c_ = k * 32 + gt
                        P.dma(POOL, lambda e, i_=i_, c_=c_: e.indirect_dma_start(
                            out=ybk[i_][:], out_offset=None, in_=yb_d,
                            in_offset=bass.IndirectOffsetOnAxis(ap=desti[:, c_:c_ + 1], axis=0)),
                            ["yb", "desti"], [("ybk", i_)])
                        ys.append((ybk[i_], ("ybk", i_)))
                    TS(ym[:], ys[0][0][:], wk[:, gt, 0:1], None, ALU.mult, None, [ys[0][1], "wk"], ["ym"])
                    for k in range(1, 4):
                        STT(ym[:], ys[k][0][:], wk[:, gt, k:k + 1], ym[:], ALU.mult, ALU.add, [ys[k][1], "wk", "ym"], ["ym"])
                    ACTV(sq[:], ym[:], AF.Square, ["ym"], ["sq7"])
                    sc, sk = small[:, 20 + lt:21 + lt], ("small", 20 + lt)
                    RED(sc, sq[:], ["sq7"], [sk])
                    rstd_from_ss(sc, sc, 1.0 / 1024, sk)
                    xr, xrk = x1r[gt % 2], ("x1r", gt % 2)
                    DMA(SP, xr[:], x1_d[b, lt * 128:(lt + 1) * 128, :], [("x1d", b, lt)], [xrk])
                    STT(sq[:], ym[:], sc, G2[b][:], ALU.mult, ALU.mult, ["ym", sk, ("G2", b), "sq7"], ["sq7"])
                    o_, ok_ = ot[gt % 2], ("ot", gt % 2)
                    TT(o_[:], sq[:], xr[:], ALU.add, ["sq7", xrk], [ok_])
                    out_toks.append(DMA(SP, out_d[b, lt * 128:(lt + 1) * 128, :], o_[:], [ok_], []))

        if mx is not None:
            mx.close()
        P.final_wait(SP, out_toks)
        P.emit()
    return nc


def _c(a):
    return np.ascontiguousarray(a, dtype=np.float32)


def prep_shared(inp):
    w_in = inp["w_in"][0]
    sh = {}
    sh["w_ada"] = _c(inp["w_ada"][0].reshape(8, 128, 12, 512).transpose(2, 1, 0, 3))
    sh["b_ada"] = _c(inp["b_ada"].reshape(1, 6144))
    sh["nrm"] = _c(np.concatenate([inp["norm_mix_pre"][0], inp["norm_mix_post"][0],
                                   inp["norm_ffn_pre"][0], inp["norm_ffn_post"][0]]).reshape(1, 4096))
    sh["w_in"] = _c(w_in[:, :6656].reshape(8, 128, 52, 128).transpose(2, 1, 0, 3))
    sh["w_mg"] = _c(w_in[:, 6656:].reshape(8, 128, 16).transpose(1, 0, 2))
    sh["b_in_col"] = _c(inp["b_in"][0, :6656].reshape(52, 128).T)
    sh["b_in_row"] = _c(inp["b_in"].reshape(1, 6672))
    sh["conv_col"] = _c(inp["conv_w"][0].reshape(9, 8, 128).transpose(2, 1, 0))
    sh["lb_raw"] = _c(inp["lb_raw"].reshape(1, 2048))
    sh["m_norm"] = _c(inp["m_norm"].reshape(1, 512))
    sh["h_norm"] = _c(inp["h_norm"].reshape(1, 512))
    sh["w_pa"] = _c(inp["w_pa"][0].reshape(4, 128, 1024).transpose(1, 0, 2))
    sh["w_pb"] = _c(inp["w_pb"][0].reshape(4, 128, 1024).transpose(1, 0, 2))
    sh["w_out"] = _c(inp["w_out"][0].reshape(8, 128, 1024).transpose(1, 0, 2))
    sh["w_router"] = _c(inp["w_router"][0].reshape(8, 128, 32).transpose(1, 0, 2))
    sh["b_router"] = _c(inp["b_router"].reshape(1, 32))
    sh["w_gu"] = _c(inp["w_gu"][0].reshape(32, 8, 128, 2048).transpose(0, 2, 1, 3)).reshape(32 * 128 * 8, 2048)
    sh["b_gu_col"] = _c(inp["b_gu"][0].reshape(32, 16, 128).transpose(0, 2, 1)).reshape(32 * 128, 16)
    sh["w_dn"] = _c(inp["w_dn"][0].reshape(32, 8, 128, 1024).transpose(0, 2, 1, 3)).reshape(32 * 128 * 4, 2048)
    sh["b_dn"] = _c(inp["b_dn"][0])
    sh["ident"] = np.eye(128, dtype=np.float32)
    sh["triF"] = np.triu(np.ones((128, 128), np.float32))
    sh["triB"] = np.tril(np.ones((128, 128), np.float32))
    sh["ones"] = np.ones((128, 128), np.float32)
    sh["pidx"] = np.arange(128, dtype=np.float32).reshape(128, 1)
    sh["jidx"] = np.arange(NB, dtype=np.float32).reshape(1, NB)
    sh["triS"] = np.triu(np.ones((128, 128), np.float32), 1)
    return sh


def core_inputs(inp, sh, i):
    m = dict(sh)
    m["x"] = _c(inp["x"][2 * i:2 * i + 2])
    m["ctx"] = _c(inp["ctx"][2 * i:2 * i + 2])
    cv = np.stack([inp["c"][2 * i], inp["c"][2 * i + 1], inp["c_ctx"]])
    m["cvT"] = _c(cv.reshape(3, 8, 128).transpose(2, 0, 1).reshape(128, 24))
    return m


def kernel(**inputs):
    inp = {k: np.asarray(v) for k, v in inputs.items()}
    sh = prep_shared(inp)
    nc = build_nc()
    in_maps = [core_inputs(inp, sh, i) for i in range(N_CORES)]
    res = run_bass_kernel_spmd(nc, in_maps, core_ids=list(range(N_CORES)))
    out = np.concatenate([np.asarray(r["out"], dtype=np.float32) for r in res.results], axis=0)
    return out
```
